# Optimizing a Trainium2 kernel written in Bass

```python
import math
import jax, jax.numpy as jnp
from jax import lax
import numpy as np

D_MODEL = 1024
BATCH = 16
SEQ = 2048
DEPTH = 2

HEAD_DIM = 64
SB_HEADS = 8
DIFF_HEADS = 4
DIFF_SUB = 64
DIFF_VDIM = 2 * DIFF_SUB
DSA_HEADS = 16
DSA_LATENT = 128
DSA_VDIM = 64
IDX_HEADS = 8
IDX_DIM = 64
TOPK_MAX = 256
N_EXPERTS = 32
TOP_K = 4
D_FF = 1024
SWIGLU_LIMIT = 7.0
SWIGLU_ALPHA = 1.702
Q_BLOCK = 128
LN_EPS = 1e-5
RMS_EPS = 1e-5
DEEPNORM_ALPHA = (2 * DEPTH) ** 0.25
DEEPNORM_BETA = (8 * DEPTH) ** -0.25
N_EVEN = (DEPTH + 1) // 2
N_ODD = DEPTH // 2

SB_W = SB_HEADS * HEAD_DIM
DIFF_QK_W = DIFF_HEADS * 2 * DIFF_SUB
DIFF_V_W = DIFF_HEADS * DIFF_VDIM
EVEN_COLS = 3 * SB_W + 2 * DIFF_QK_W + DIFF_V_W
EVEN_SPLITS = (SB_W, 2 * SB_W, 3 * SB_W, 3 * SB_W + DIFF_QK_W, 3 * SB_W + 2 * DIFF_QK_W)
EVEN_MIX_W = SB_W + DIFF_V_W

DSA_Q_W = DSA_HEADS * DSA_LATENT
ODD_SPLITS = (DSA_Q_W, DSA_Q_W + DSA_LATENT, DSA_Q_W + DSA_LATENT + IDX_HEADS * IDX_DIM,
              DSA_Q_W + DSA_LATENT + IDX_HEADS * IDX_DIM + IDX_DIM)
ODD_COLS = DSA_Q_W + DSA_LATENT + IDX_HEADS * IDX_DIM + IDX_DIM + IDX_HEADS
ODD_MIX_W = DSA_HEADS * DSA_VDIM

kernel_name = 'stickbreak_diff_dsa_moe_deepnorm'


def _alibi_slopes(n):
    return 2.0 ** (-8.0 * jnp.arange(1, n + 1, dtype=jnp.float32) / n)


def _layer_norm(x, g, b):
    xf = x.astype(jnp.float32)
    mu = jnp.mean(xf, axis=-1, keepdims=True)
    var = jnp.mean(jnp.square(xf - mu), axis=-1, keepdims=True)
    y = (xf - mu) * lax.rsqrt(var + LN_EPS) * g.astype(jnp.float32) + b.astype(jnp.float32)
    return y.astype(x.dtype)


def _rms_norm(x, w):
    xf = x.astype(jnp.float32)
    y = xf * lax.rsqrt(jnp.mean(xf * xf, axis=-1, keepdims=True) + RMS_EPS) * w.astype(jnp.float32)
    return y.astype(x.dtype)


def _even_mixer(x, w_in, w_out, lam_q1, lam_k1, lam_q2, lam_k2, subln_w, layer):
    f32 = jnp.float32
    bsz, seq_len, _ = x.shape
    proj = x @ w_in
    q_a, k_a, v_a, q_b, k_b, v_b = jnp.split(proj, list(EVEN_SPLITS), axis=-1)
    q_a = q_a.reshape(bsz, seq_len, SB_HEADS, HEAD_DIM)
    k_a = k_a.reshape(bsz, seq_len, SB_HEADS, HEAD_DIM)
    v_a = v_a.reshape(bsz, seq_len, SB_HEADS, HEAD_DIM)
    q_b = q_b.reshape(bsz, seq_len, DIFF_HEADS, 2, DIFF_SUB)
    k_b = k_b.reshape(bsz, seq_len, DIFF_HEADS, 2, DIFF_SUB)
    v_b = v_b.reshape(bsz, seq_len, DIFF_HEADS, DIFF_VDIM)

    lam_init = 0.8 - 0.6 * math.exp(-0.3 * layer)
    lam = (jnp.exp(jnp.sum(lam_q1.astype(f32) * lam_k1.astype(f32)))
           - jnp.exp(jnp.sum(lam_q2.astype(f32) * lam_k2.astype(f32))) + lam_init)
    slopes = _alibi_slopes(DIFF_HEADS)
    sb_scale = HEAD_DIM ** -0.5
    diff_scale = DIFF_SUB ** -0.5
    key_pos = jnp.arange(seq_len)

    def block(i):
        t0 = i * Q_BLOCK
        t = t0 + jnp.arange(Q_BLOCK)
        dist = t[:, None] - key_pos[None, :]
        qa = lax.dynamic_slice_in_dim(q_a, t0, Q_BLOCK, axis=1)
        qb = lax.dynamic_slice_in_dim(q_b, t0, Q_BLOCK, axis=1)

        z = jnp.einsum('bqhd,bshd->bhqs', qa, k_a).astype(f32) * sb_scale
        strict = dist > 0
        log_keep = jnp.where(strict, jax.nn.log_sigmoid(-z), 0.0)
        log_after = lax.cumsum(log_keep, axis=3, reverse=True) - log_keep
        attn_a = jnp.where(strict, jnp.exp(jax.nn.log_sigmoid(z) + log_after), 0.0)
        o_a = jnp.einsum('bhqs,bshd->bqhd', attn_a.astype(v_a.dtype), v_a)

        s = jnp.einsum('bqhcd,bshcd->bhcqs', qb, k_b).astype(f32) * diff_scale
        s = s - slopes[:, None, None, None] * dist.astype(f32)
        s = jnp.where(dist >= 0, s, -jnp.inf)
        p = jax.nn.softmax(s, axis=-1)
        p = p[:, :, 0] - lam * p[:, :, 1]
        o_b = jnp.einsum('bhqs,bshe->bqhe', p.astype(v_b.dtype), v_b)
        o_b = _rms_norm(o_b, subln_w) * (1.0 - lam_init)

        return jnp.concatenate([o_a.reshape(bsz, Q_BLOCK, SB_W),
                                o_b.reshape(bsz, Q_BLOCK, DIFF_V_W)], axis=-1)

    out = lax.map(block, jnp.arange(seq_len // Q_BLOCK))
    out = jnp.moveaxis(out, 0, 1).reshape(bsz, seq_len, EVEN_MIX_W)
    return out @ w_out


def _odd_mixer(x, w_in, kv_norm_w, w_uv, w_out):
    f32 = jnp.float32
    bsz, seq_len, _ = x.shape
    k_sel = min(TOPK_MAX, seq_len // 4)
    proj = x @ w_in
    q, c_kv, q_idx, k_idx, w_idx = jnp.split(proj, list(ODD_SPLITS), axis=-1)
    q = q.reshape(bsz, seq_len, DSA_HEADS, DSA_LATENT)
    c_kv = _rms_norm(c_kv, kv_norm_w)
    q_idx = q_idx.reshape(bsz, seq_len, IDX_HEADS, IDX_DIM)
    w_idx = w_idx * (IDX_HEADS ** -0.5)
    slopes = _alibi_slopes(DSA_HEADS)
    idx_scale = IDX_DIM ** -0.5
    attn_scale = DSA_LATENT ** -0.5
    key_pos = jnp.arange(seq_len)
    batch_ix = jnp.arange(bsz)[:, None, None]

    def block(i):
        t0 = i * Q_BLOCK
        t = t0 + jnp.arange(Q_BLOCK)
        qi = lax.dynamic_slice_in_dim(q_idx, t0, Q_BLOCK, axis=1)
        wi = lax.dynamic_slice_in_dim(w_idx, t0, Q_BLOCK, axis=1)
        qm = lax.dynamic_slice_in_dim(q, t0, Q_BLOCK, axis=1)
        rel = jax.nn.relu(jnp.einsum('bqhd,bsd->bqhs', qi, k_idx).astype(f32) * idx_scale)
        score = jnp.einsum('bqhs,bqh->bqs', rel, wi.astype(f32))
        score = jnp.where(key_pos[None, None, :] <= t[None, :, None], score, -jnp.inf)
        _, idx = lax.top_k(score, k_sel)
        c_sel = c_kv[batch_ix, idx]
        valid = idx <= t[None, :, None]
        dist = (t[None, :, None] - idx).astype(f32)
        s = jnp.einsum('bqhc,bqkc->bqhk', qm, c_sel).astype(f32) * attn_scale
        s = s - slopes[None, None, :, None] * dist[:, :, None, :]
        s = jnp.where(valid[:, :, None, :], s, -jnp.inf)
        p = jax.nn.softmax(s, axis=-1)
        o = jnp.einsum('bqhk,bqkc->bqhc', p.astype(c_sel.dtype), c_sel)
        o = jnp.einsum('bqhc,hcd->bqhd', o, w_uv)
        return o.reshape(bsz, Q_BLOCK, ODD_MIX_W)

    out = lax.map(block, jnp.arange(seq_len // Q_BLOCK))
    out = jnp.moveaxis(out, 0, 1).reshape(bsz, seq_len, ODD_MIX_W)
    return out @ w_out


def _moe(x, router_w, router_b, w_gu, b_gu, w_down, b_down):
    bsz, seq_len, d = x.shape
    xt = x.reshape(-1, d)
    logits = (xt @ router_w + router_b).astype(jnp.float32)
    top_val, top_idx = lax.top_k(logits, TOP_K)
    gates = jax.nn.softmax(top_val, axis=-1)
    combine = jnp.sum(jax.nn.one_hot(top_idx, N_EXPERTS, dtype=jnp.float32) * gates[..., None], axis=1)

    def expert_step(acc, params):
        wgu, bgu, wd, bd, cw = params
        hgu = xt @ wgu + bgu
        gate = jnp.minimum(hgu[:, :D_FF], SWIGLU_LIMIT)
        up = jnp.clip(hgu[:, D_FF:], -SWIGLU_LIMIT, SWIGLU_LIMIT)
        act = gate * jax.nn.sigmoid(SWIGLU_ALPHA * gate) * (up + 1.0)
        y = act @ wd + bd
        return acc + cw[:, None] * y.astype(jnp.float32), None

    acc0 = jnp.zeros(xt.shape, jnp.float32)
    acc, _ = lax.scan(expert_step, acc0, (w_gu, b_gu, w_down, b_down, combine.T))
    return acc.astype(x.dtype).reshape(bsz, seq_len, d)


def setup_inputs(seed: int = 0) -> dict:
    key = jax.random.key(seed)
    ks = jax.random.split(key, 24)
    f32 = jnp.float32
    beta = DEEPNORM_BETA
    nrm = lambda k, shape: jax.random.normal(k, shape, f32)
    even_col_scale = jnp.concatenate([
        jnp.ones((2 * SB_W,), f32), jnp.full((SB_W,), beta, f32),
        jnp.ones((2 * DIFF_QK_W,), f32), jnp.full((DIFF_V_W,), beta, f32)])
    return {
        'x': nrm(ks[0], (BATCH, SEQ, D_MODEL)),
        'ev_w_in': nrm(ks[1], (N_EVEN, D_MODEL, EVEN_COLS)) * (D_MODEL ** -0.5) * even_col_scale,
        'ev_w_out': nrm(ks[2], (N_EVEN, EVEN_MIX_W, D_MODEL)) * (EVEN_MIX_W ** -0.5) * beta,
        'ev_lambda_q1': nrm(ks[3], (N_EVEN, DIFF_SUB)) * 0.1,
        'ev_lambda_k1': nrm(ks[4], (N_EVEN, DIFF_SUB)) * 0.1,
        'ev_lambda_q2': nrm(ks[5], (N_EVEN, DIFF_SUB)) * 0.1,
        'ev_lambda_k2': nrm(ks[6], (N_EVEN, DIFF_SUB)) * 0.1,
        'ev_subln_w': 1.0 + 0.02 * nrm(ks[7], (N_EVEN, DIFF_VDIM)),
        'od_w_in': nrm(ks[8], (N_ODD, D_MODEL, ODD_COLS)) * (D_MODEL ** -0.5),
        'od_kv_norm_w': 1.0 + 0.02 * nrm(ks[9], (N_ODD, DSA_LATENT)),
        'od_w_uv': nrm(ks[10], (N_ODD, DSA_HEADS, DSA_LATENT, DSA_VDIM)) * (DSA_LATENT ** -0.5) * beta,
        'od_w_out': nrm(ks[11], (N_ODD, ODD_MIX_W, D_MODEL)) * (ODD_MIX_W ** -0.5) * beta,
        'ln_mix_g': 1.0 + 0.02 * nrm(ks[12], (DEPTH, D_MODEL)),
        'ln_mix_b': 0.02 * nrm(ks[13], (DEPTH, D_MODEL)),
        'router_w': nrm(ks[14], (DEPTH, D_MODEL, N_EXPERTS)) * (D_MODEL ** -0.5),
        'router_b': 0.01 * nrm(ks[15], (DEPTH, N_EXPERTS)),
        'exp_w_gu': nrm(ks[16], (DEPTH, N_EXPERTS, D_MODEL, 2 * D_FF)) * (D_MODEL ** -0.5) * beta,
        'exp_b_gu': 0.02 * nrm(ks[17], (DEPTH, N_EXPERTS, 2 * D_FF)),
        'exp_w_down': nrm(ks[18], (DEPTH, N_EXPERTS, D_FF, D_MODEL)) * (D_FF ** -0.5) * beta,
        'exp_b_down': 0.02 * nrm(ks[19], (DEPTH, N_EXPERTS, D_MODEL)),
        'ln_ffn_g': 1.0 + 0.02 * nrm(ks[20], (DEPTH, D_MODEL)),
        'ln_ffn_b': 0.02 * nrm(ks[21], (DEPTH, D_MODEL)),
    }


def reference(x, ev_w_in, ev_w_out, ev_lambda_q1, ev_lambda_k1, ev_lambda_q2, ev_lambda_k2,
              ev_subln_w, od_w_in, od_kv_norm_w, od_w_uv, od_w_out, ln_mix_g, ln_mix_b,
              router_w, router_b, exp_w_gu, exp_b_gu, exp_w_down, exp_b_down,
              ln_ffn_g, ln_ffn_b):
    h = x
    for layer in range(DEPTH):
        j = layer // 2
        if layer % 2 == 0:
            mix = _even_mixer(h, ev_w_in[j], ev_w_out[j], ev_lambda_q1[j], ev_lambda_k1[j],
                              ev_lambda_q2[j], ev_lambda_k2[j], ev_subln_w[j], layer)
        else:
            mix = _odd_mixer(h, od_w_in[j], od_kv_norm_w[j], od_w_uv[j], od_w_out[j])
        h = _layer_norm(DEEPNORM_ALPHA * h + mix, ln_mix_g[layer], ln_mix_b[layer])
        ffn = _moe(h, router_w[layer], router_b[layer], exp_w_gu[layer], exp_b_gu[layer],
                   exp_w_down[layer], exp_b_down[layer])
        h = _layer_norm(DEEPNORM_ALPHA * h + ffn, ln_ffn_g[layer], ln_ffn_b[layer])
    return h
```

```python
import math
from concourse.bass_utils import run_bass_kernel_spmd
from contextlib import ExitStack
import numpy as np
import concourse.bass as bass
import concourse.mybir as mybir

F32 = mybir.dt.float32
BF16 = mybir.dt.bfloat16
I32 = mybir.dt.int32
ALU = mybir.AluOpType
AF = mybir.ActivationFunctionType
AX = mybir.AxisListType

ENGS = ("sync", "scalar", "vector", "gpsimd", "tensor")


class Prog:
    def __init__(self, nc, stack):
        self.nc = nc
        self.stack = stack
        self.sem_stack = stack
        self.streams = {e: [] for e in ENGS}
        self.esem = {e: stack.enter_context(nc.semaphore("es_" + e)) for e in ENGS}
        self.etick = {e: 0 for e in ENGS}
        self.known = {e: {} for e in ENGS}
        self.res_w = {}
        self.res_r = {}
        self.dsem = {}
        self.semobj = {}
        for e in ENGS:
            self.semobj[id(self.esem[e])] = self.esem[e]
        self.n_ops = 0

    def sb(self, name, shape, dt=F32):
        return self.stack.enter_context(self.nc.sbuf_tensor(name, list(shape), dt))

    def ps(self, name, shape, dt=F32):
        return self.stack.enter_context(self.nc.psum_tensor(name, list(shape), dt))

    def _dma_sem(self, key):
        if key not in self.dsem:
            s = self.sem_stack.enter_context(self.nc.semaphore("ds_%d" % len(self.dsem)))
            self.dsem[key] = [s, 0]
            self.semobj[id(s)] = s
        return self.dsem[key]

    def _deps(self, eng, reads, writes):
        deps = {}

        def add(ev):
            if ev is None:
                return
            s, v = ev
            if deps.get(s, 0) < v:
                deps[s] = v

        for k in reads:
            add(self.res_w.get(k))
        for k in writes:
            add(self.res_w.get(k))
            for s, v in self.res_r.get(k, {}).items():
                add((s, v))
        waits = []
        kn = self.known[eng]
        for s, v in deps.items():
            if kn.get(s, 0) < v:
                kn[s] = v
                waits.append((s, v))
        return waits

    def _commit(self, ev, reads, writes):
        s, v = ev
        for k in reads:
            d = self.res_r.setdefault(k, {})
            if d.get(s, 0) < v:
                d[s] = v
        for k in writes:
            self.res_w[k] = ev
            self.res_r[k] = {}

    def op(self, eng, fn, reads=(), writes=()):
        waits = self._deps(eng, reads, writes)
        self.etick[eng] += 1
        ev = (id(self.esem[eng]), self.etick[eng])
        self.streams[eng].append((waits, fn, ev, 1))
        self._commit(ev, reads, writes)
        self.n_ops += 1

    def dma(self, eng, out, in_, reads=(), writes=(), slot=None, **kw):
        if slot is None:
            k0 = writes[0] if writes else reads[0]
            slot = ("_slot",) + tuple(k0[1:]) if isinstance(k0, tuple) and len(k0) > 1 else ("_slot", k0)
        ds = self._dma_sem(slot)
        waits = self._deps(eng, reads, writes)
        ds[1] += 16
        ev = (id(ds[0]), ds[1])
        fn = lambda e, out=out, in_=in_, kw=kw: e.dma_start(out=out, in_=in_, **kw)
        self.streams[eng].append((waits, fn, ev, 16))
        self._commit(ev, reads, writes)
        self.n_ops += 1

    def barrier(self):
        evs = [(id(self.esem[e]), self.etick[e]) for e in ENGS if self.etick[e] > 0]
        evs += [(id(s), c) for s, c in self.dsem.values() if c > 0]
        for e in ENGS:
            kn = self.known[e]
            waits = []
            for s, v in evs:
                if s == id(self.esem[e]) and False:
                    continue
                if kn.get(s, 0) < v:
                    kn[s] = v
                    waits.append((s, v))
            if waits:
                self.streams[e].append((waits, None, None, 0))

    def final_wait(self, eng="sync"):
        evs = [(id(s), c) for s, c in self.dsem.values() if c > 0]
        evs += [(id(self.esem[e]), self.etick[e]) for e in ENGS if self.etick[e] > 0 and e != eng]
        kn = self.known[eng]
        waits = [(s, v) for s, v in evs if kn.get(s, 0) < v]
        self.streams[eng].append((waits, None, None, 0))

    def emit(self):
        nc = self.nc
        with nc.Block() as block:
            def runner(name):
                def run(e):
                    for waits, fn, ev, inc in self.streams[name]:
                        for s, v in waits:
                            e.wait_ge(self.semobj[s], v)
                        if fn is not None:
                            inst = fn(e)
                            inst.then_inc(self.semobj[ev[0]], inc)
                return run
            block.sync(runner("sync"))
            block.scalar(runner("scalar"))
            block.vector(runner("vector"))
            block.gpsimd(runner("gpsimd"))
            block.tensor(runner("tensor"))


DEPTH = 2
ALPHA = (2 * DEPTH) ** 0.25
LN_EPS = 1e-5
SW_LIMIT = 7.0
SW_ALPHA = 1.702


def layer_norm_tile(P, ph, src_ap, src_keys, dst_ap, dst_key, g_bc, b_bc, st, idx):
    s1, s2, mean, msq, var, rstd, junk = (st[k] for k in ("s1", "s2", "mean", "msq", "var", "rstd", "junk"))
    k = lambda n: (ph, "ln_" + n)
    P.op("vector", lambda e: e.reduce_sum(s1[:, 0:1], src_ap, axis=AX.X), reads=list(src_keys), writes=[k("s1")])
    P.op("vector", lambda e: e.memset(s2[:, 0:1], 0.0), writes=[k("s2")])
    P.op("scalar", lambda e: e.activation(junk[:], src_ap, AF.Square, accum_out=s2[:, 0:1]),
         reads=list(src_keys) + [k("s2")], writes=[k("s2"), k("junk")])
    P.op("vector", lambda e: e.tensor_scalar(mean[:, 0:1], s1[:, 0:1], 1.0 / 1024, None, ALU.mult),
         reads=[k("s1")], writes=[k("mean")])
    P.op("vector", lambda e: e.tensor_tensor(msq[:, 0:1], mean[:, 0:1], mean[:, 0:1], ALU.mult),
         reads=[k("mean")], writes=[k("msq")])
    P.op("vector", lambda e: e.scalar_tensor_tensor(var[:, 0:1], s2[:, 0:1], 1.0 / 1024, msq[:, 0:1], ALU.mult, ALU.subtract),
         reads=[k("s2"), k("msq")], writes=[k("var")])
    P.op("vector", lambda e: e.tensor_scalar(var[:, 0:1], var[:, 0:1], LN_EPS, None, ALU.add),
         reads=[k("var")], writes=[k("var")])
    P.op("scalar", lambda e: e.sqrt(var[:, 0:1], var[:, 0:1]), reads=[k("var")], writes=[k("var")])
    P.op("vector", lambda e: e.reciprocal(rstd[:, 0:1], var[:, 0:1]), reads=[k("var")], writes=[k("rstd")])
    P.op("vector", lambda e: e.tensor_scalar(dst_ap, src_ap, mean[:, 0:1], rstd[:, 0:1], ALU.subtract, ALU.mult),
         reads=list(src_keys) + [k("mean"), k("rstd")], writes=[dst_key])
    P.op("gpsimd", lambda e: e.tensor_tensor(dst_ap, dst_ap, g_bc[:], ALU.mult), reads=[dst_key, (ph, "g_bc")], writes=[dst_key])
    P.op("gpsimd", lambda e: e.tensor_tensor(dst_ap, dst_ap, b_bc[:], ALU.add), reads=[dst_key, (ph, "b_bc")], writes=[dst_key])


def moe_phase(P, ph, C, lay, h_in, h_out, NT, E, stage=99):
    nc = P.nc
    D = C["dram"]
    ident = C["ident"]
    NBLK = NT // 1024
    sb = lambda n, s, d=F32: P.sb(ph + n, s, d)
    K = lambda *a: (ph,) + a

    rw = sb("rw", [128, 8, E]); rb_bc = sb("rb", [128, E])
    bguT = sb("bguT", [128, 16, E]); bd = sb("bd", [E, 1024])
    g_bc = sb("g_bc", [128, 1024]); b_bc = sb("b_bc", [128, 1024])
    xt = [sb("xt%d" % i, [128, 1024]) for i in range(2)]
    hT = sb("hT", [128, 8, 1024], BF16)
    hTf = sb("hTf", [128, 8, 128])
    acc = sb("acc", [128, 8, 1024])
    wgu = [sb("wgu%d" % i, [128, 8, 2048], BF16) for i in range(2)]
    wd = [sb("wd%d" % i, [128, 8, 1024], BF16) for i in range(2)]
    actT = [sb("actT%d" % i, [128, 8, 512], BF16) for i in range(2)]
    gc = [sb("gc%d" % i, [128, 512]) for i in range(2)]
    ua = [sb("ua%d" % i, [128, 512]) for i in range(2)]
    sl = [sb("sl%d" % i, [128, 512]) for i in range(2)]
    cw = sb("cw", [128, 8, E]); cwT = sb("cwT", [E, 128])
    lg = sb("lg", [128, E]); ex = sb("ex", [128, E]); em = sb("em", [128, E])
    m8 = sb("m8", [128, 8]); sm = sb("sm", [128, 8])
    st = {n: sb("st_" + n, [128, 1]) for n in ("s1", "s2", "mean", "msq", "var", "rstd")}
    st["junk"] = sb("junk", [128, 1024], BF16)

    pA = [P.ps(ph + "pA%d" % i, [128, 512]) for i in range(2)]
    pB = [P.ps(ph + "pB%d" % i, [128, 512]) for i in range(2)]
    pY = [P.ps(ph + "pY%d" % i, [128, 512]) for i in range(2)]
    pT = [P.ps(ph + "pT%d" % i, [128, 4, 128]) for i in range(2)]

    import os
    SK = os.environ.get("SKIP", "")
    if "rw" not in SK:
        P.dma("sync", rw[:], D["router_w"][lay].rearrange("(k p) e -> p k e", p=128), writes=[K("rw")])
    if "rb" not in SK:
        P.dma("sync", rb_bc[:], D["router_b"][lay].partition_broadcast(128), writes=[K("rb")])
    bkeys = [K("acc", 0, 0), K("acc", 0, 1), K("acc", 1, 0), K("acc", 1, 1)]
    P.dma("sync", acc[0:E, 0:2, :], D["exp_b_gu"][lay].rearrange("e (a f) -> e a f", a=2), writes=bkeys, slot="bguraw")
    if "bd" not in SK:
        P.dma("sync", bd[:], D["exp_b_down"][lay], writes=[K("bd")])
    if "gb" not in SK:
        P.dma("sync", g_bc[:], D["ln_ffn_g"][lay].partition_broadcast(128), writes=[K("g_bc")])
    if "gb" not in SK:
        P.dma("sync", b_bc[:], D["ln_ffn_b"][lay].partition_broadcast(128), writes=[K("b_bc")])
    for j in range(16):
        h = j % 2
        P.op("tensor", lambda e, j=j, h=h: e.transpose(pT[h][:, 0, 0:E], acc[0:E, j // 8, (j % 8) * 128:(j % 8 + 1) * 128], ident[0:E, 0:E]),
             reads=bkeys + ["ident"], writes=[K("pT", h)])
        P.op("vector", lambda e, j=j, h=h: e.tensor_copy(bguT[:, j, :], pT[h][:, 0, 0:E]),
             reads=[K("pT", h)], writes=[K("bguT")])

    if stage == 0:
        return
    def load_w(b, e):
        ws = (b * E + e) % 2
        if "now" in SK:
            return
        for kk in range(8):
            P.dma("gpsimd", wgu[ws][:, kk, :], D["exp_w_gu"][lay, e, kk * 128:(kk + 1) * 128, :], writes=[K("wgu", ws, kk)], slot=("wgu", ws))
        for kk in range(8):
            P.dma("gpsimd", wd[ws][:, kk, :], D["exp_w_down"][lay, e, kk * 128:(kk + 1) * 128, :], writes=[K("wd", ws, kk)], slot=("wd", ws))

    for b in range(NBLK):
        load_w(b, 0)
        for i in range(8 if "noa" not in SK else 0):
            tok0 = b * 1024 + i * 128
            s = i % 2
            P.dma("sync", xt[s][:], h_in[tok0:tok0 + 128, :], writes=[K("xt", s)])
            for h in range(2):
                def tr(e, s=s, h=h):
                    for q in range(4):
                        kk = h * 4 + q
                        r = e.transpose(pT[h][:, q, :], xt[s][:, kk * 128:(kk + 1) * 128], ident[:])
                    return r
                P.op("tensor", tr, reads=[K("xt", s), "ident"], writes=[K("pT", h)])
                P.op(os.environ.get("EVE", "scalar"), lambda e, h=h, i=i: (e.activation(hT[:, 4 * h:4 * h + 4, i * 128:(i + 1) * 128], pT[h][:], AF.Identity) if os.environ.get("EVE", "scalar") == "scalar" else e.tensor_copy(hT[:, 4 * h:4 * h + 4, i * 128:(i + 1) * 128], pT[h][:])),
                     reads=[K("pT", h)], writes=[K("hT", i // 4)])
                P.op("vector", lambda e, h=h: e.tensor_copy(hTf[:, 4 * h:4 * h + 4, :], pT[h][:]),
                     reads=[K("pT", h), K("hT", i // 4)] , writes=[K("hTf", h)])
            CUT = int(os.environ.get("CUT", "99"))
            if CUT < 2:
                continue
            pL = pY[0]
            def rt(e):
                for kk in range(8):
                    r = e.matmul(pL[:, 0:E], hTf[:, kk, :], rw[:, kk, :], start=(kk == 0), stop=(kk == 7))
                return r
            P.op("tensor", rt, reads=[K("hTf", 0), K("hTf", 1), K("rw")], writes=[K("pY", 0)])
            P.op("vector", lambda e: e.tensor_tensor(lg[:], pL[:, 0:E], rb_bc[:], ALU.add), reads=[K("pY", 0), K("rb")], writes=[K("lg")])
            if CUT < 3:
                continue
            P.op("vector", lambda e: e.max(m8[:], lg[:]), reads=[K("lg")], writes=[K("m8")])
            P.op("vector", lambda e: e.tensor_scalar(sm[:, 0:1], m8[:, 0:1], -1.0, None, ALU.mult), reads=[K("m8")], writes=[K("negmx")])
            P.op("scalar", lambda e: e.activation(ex[:], lg[:], AF.Exp, bias=sm[:, 0:1], scale=1.0), reads=[K("lg"), K("negmx")], writes=[K("ex")])
            P.op("vector", lambda e: e.scalar_tensor_tensor(em[:], lg[:], m8[:, 3:4], ex[:], ALU.is_ge, ALU.mult),
                 reads=[K("lg"), K("m8"), K("ex")], writes=[K("em")])
            P.op("vector", lambda e: e.reduce_sum(sm[:, 1:2], em[:], axis=AX.X), reads=[K("em")], writes=[K("Z")])
            P.op("vector", lambda e: e.reciprocal(sm[:, 2:3], sm[:, 1:2]), reads=[K("Z")], writes=[K("rz")])
            P.op("vector", lambda e, i=i: e.tensor_scalar(cw[:, i, :], em[:], sm[:, 2:3], None, ALU.mult), reads=[K("em"), K("rz")], writes=[K("cw", i)])
            if CUT < 4:
                continue
            pC = pY[1]
            P.op("tensor", lambda e, i=i: e.transpose(pC[0:E, 0:128], cw[:, i, :], ident[:]), reads=[K("cw", i), "ident"], writes=[K("pY", 1)])
            P.op("scalar", lambda e: e.copy(cwT[:], pC[0:E, 0:128]), reads=[K("pY", 1)], writes=[K("cwT")])
            if CUT < 5:
                continue
            for n in range(2):
                P.op("tensor", lambda e, n=n: e.matmul(pA[n][:], cwT[:], bd[:, n * 512:(n + 1) * 512], start=True, stop=True),
                     reads=[K("cwT"), K("bd")], writes=[K("pA", n)])
                P.op("vector", lambda e, n=n, s=s, i=i: e.scalar_tensor_tensor(acc[:, i, n * 512:(n + 1) * 512], xt[s][:, n * 512:(n + 1) * 512],
                                                                      ALPHA, pA[n][:], ALU.mult, ALU.add),
                     reads=[K("xt", s), K("pA", n)], writes=[K("acc", i, n)])
        if stage == 1:
            return
        cnt = 0
        for ei in range(E):
            ws = (b * E + ei) % 2
            if ei + 1 < E:
                load_w(b, ei + 1)
            for c in range(2):
                a_s = c % 2
                for i in range(8):
                    p = cnt % 2
                    cnt += 1
                    def mm_g(e, off, ps, i=i, c=c, ws=ws):
                        for kk in range(8):
                            r = e.matmul(ps[:], wgu[ws][:, kk, off + i * 128: off + (i + 1) * 128], hT[:, kk, c * 512:(c + 1) * 512],
                                         start=(kk == 0), stop=(kk == 7))
                        return r
                    hk = [K("hT", 2 * c), K("hT", 2 * c + 1)] if False else [K("hT", c)]
                    P.op("tensor", lambda e, p=p, f=mm_g: f(e, 0, pA[p]), reads=[K("wgu", ws, kk) for kk in range(8)] + hk, writes=[K("pA", p)])
                    P.op("tensor", lambda e, p=p, f=mm_g: f(e, 1024, pB[p]), reads=[K("wgu", ws, kk) for kk in range(8)] + hk, writes=[K("pB", p)])
                    P.op("vector", lambda e, p=p, i=i, ei=ei: e.tensor_scalar(gc[p][:], pA[p][:], bguT[:, i, ei:ei + 1], SW_LIMIT, ALU.add, ALU.min),
                         reads=[K("pA", p), K("bguT")], writes=[K("gc", p)])
                    P.op("vector", lambda e, p=p, i=i, ei=ei: e.tensor_scalar(ua[p][:], pB[p][:], bguT[:, 8 + i, ei:ei + 1], SW_LIMIT, ALU.add, ALU.min),
                         reads=[K("pB", p), K("bguT")], writes=[K("ua", p)])
                    P.op("gpsimd", lambda e, p=p: e.tensor_scalar(ua[p][:], ua[p][:], -SW_LIMIT, 1.0, ALU.max, ALU.add),
                         reads=[K("ua", p)], writes=[K("ua", p)])
                    P.op("scalar", lambda e, p=p: e.activation(sl[p][:], gc[p][:], AF.Silu, scale=SW_ALPHA),
                         reads=[K("gc", p)], writes=[K("sl", p)])
                    P.op("vector", lambda e, p=p, i=i, a_s=a_s: e.scalar_tensor_tensor(actT[a_s][:, i, :], sl[p][:], 1.0 / SW_ALPHA, ua[p][:], ALU.mult, ALU.mult),
                         reads=[K("sl", p), K("ua", p)], writes=[K("actT", a_s)])
                for j in range(4):
                    ti = c * 4 + j
                    for n in range(2):
                        q = (j * 2 + n) % 2
                        def mm_d(e, j=j, n=n, q=q, a_s=a_s, ws=ws):
                            for f in range(8):
                                r = e.matmul(pY[q][:], actT[a_s][:, f, j * 128:(j + 1) * 128], wd[ws][:, f, n * 512:(n + 1) * 512],
                                             start=(f == 0), stop=(f == 7))
                            return r
                        P.op("tensor", mm_d, reads=[K("actT", a_s)] + [K("wd", ws, kk) for kk in range(8)], writes=[K("pY", q)])
                        P.op("vector", lambda e, q=q, ti=ti, n=n, ei=ei: e.scalar_tensor_tensor(
                            acc[:, ti, n * 512:(n + 1) * 512], pY[q][:], cw[:, ti, ei:ei + 1], acc[:, ti, n * 512:(n + 1) * 512], ALU.mult, ALU.add),
                             reads=[K("pY", q), K("cw", ti), K("acc", ti, n)], writes=[K("acc", ti, n)])
        if stage == 2:
            return
        for i in range(8):
            tok0 = b * 1024 + i * 128
            s = i % 2
            layer_norm_tile(P, ph, acc[:, i, :], [K("acc", i, 0), K("acc", i, 1)], acc[:, i, :], K("acc", i, 0), g_bc, b_bc, st, i)
            P.dma("sync", h_out[tok0:tok0 + 128, :], acc[:, i, :], reads=[K("acc", i, 0)], writes=[K("hout", b, i)], slot=("yo_st", s))


import math

RMS_EPS = 1e-5
NEGM = -30000.0


def load_w_cols(P, ph, wt, wkey, w_dram, c0, ncols, dst0=0):
    for kk in range(8):
        P.dma("gpsimd", wt[:, kk, dst0:dst0 + ncols], w_dram[kk * 128:(kk + 1) * 128, c0:c0 + ncols],
              writes=[(ph, wkey, kk)], slot=(wkey,))


def wkeys(ph, wkey, dsts=(0,)):
    return [(ph, wkey, kk) for kk in range(8)]


def load_xT(P, ph, C, h_rows, L, xT, xt, ps):
    ident = C["ident"]
    for i in range(L // 128):
        s = i % 2
        P.dma("sync", xt[s][:], h_rows[i * 128:(i + 1) * 128, :], writes=[(ph, "xt", s)])
        for h in range(2):
            pt = ps[h]
            def tr(e, s=s, h=h, pt=pt):
                for q in range(4):
                    kk = h * 4 + q
                    r = e.transpose(pt[:, q * 128:(q + 1) * 128], xt[s][:, kk * 128:(kk + 1) * 128], ident[:])
                return r
            P.op("tensor", tr, reads=[(ph, "xt", s), "ident"], writes=[(ph, "ps", h)])
            P.op("vector", lambda e, h=h, i=i, pt=pt: e.tensor_copy(xT[:, 4 * h:4 * h + 4, i * 128:(i + 1) * 128],
                                                                  pt[:].rearrange("p (a b) -> p a b", a=4)),
                 reads=[(ph, "ps", h)], writes=[(ph, "ps", h), (ph, "xT", i // 4)])


def proj_fm(P, ph, wt, wk, c0, M, xT, L, ps, psi, evac):
    for ch in range(L // 512):
        pi = psi[ch % len(psi)]
        def mm(e, ch=ch, pi=pi):
            for kk in range(8):
                r = e.matmul(ps[pi][0:M, :], wt[:, kk, c0:c0 + M], xT[:, kk, ch * 512:(ch + 1) * 512], start=(kk == 0), stop=(kk == 7))
            return r
        P.op("tensor", mm, reads=wk + [(ph, "xT", ch)], writes=[(ph, "ps", pi)])
        evac(ch, ps[pi][0:M, :], (ph, "ps", pi))


def proj_tm(P, ph, wt, wk, c0, N, xT, L, ps, psi, evac):
    for i in range(L // 128):
        pi = psi[i % len(psi)]
        def mm(e, i=i, pi=pi):
            for kk in range(8):
                r = e.matmul(ps[pi][:, 0:N], xT[:, kk, i * 128:(i + 1) * 128], wt[:, kk, c0:c0 + N], start=(kk == 0), stop=(kk == 7))
            return r
        P.op("tensor", mm, reads=wk + [(ph, "xT", i // 4)], writes=[(ph, "ps", pi)])
        evac(i, ps[pi][:, 0:N], (ph, "ps", pi))


def outproj_ln(P, ph, C, mix, L, w_out, h_rows, out_rows, g_vec, b_vec, T, ps):
    ident_b = C["ident_b"]
    wo, mT, xt, g_bc, b_bc, st, yo = T["wo"], T["mT"], T["xt"], T["g_bc"], T["b_bc"], T["st"], T["yo"]
    for half in range(2):
        load_w_cols(P, ph, wo[half], "wb%d" % half, w_out, half * 512, 512)
    P.dma("sync", g_bc, g_vec.partition_broadcast(128), writes=[(ph, "g_bc")] + T["gk"], slot=("g_bc",))
    P.dma("sync", b_bc, b_vec.partition_broadcast(128), writes=[(ph, "b_bc")] + T["bk"], slot=("b_bc",))
    pbf = [ps[6][:].bitcast(BF16), ps[7][:].bitcast(BF16)]
    P.barrier()
    for i in range(L // 128):
        s = i % 2
        P.dma("sync", xt[s][:], h_rows[i * 128:(i + 1) * 128, :], writes=[(ph, "xt", s)])
        def tr(e, i=i, s=s):
            for kk in range(8):
                r = e.transpose(pbf[s][:, kk * 128:(kk + 1) * 128], mix[:, i, kk * 128:(kk + 1) * 128], ident_b[:])
            return r
        P.op("tensor", tr, reads=[(ph, "mix", i), "ident_b"], writes=[(ph, "ps", 6 + s)])
        P.op("vector", lambda e, s=s: e.tensor_copy(mT[s], pbf[s]), reads=[(ph, "ps", 6 + s)], writes=[(ph, "ps", 6 + s), (ph, "mT", s)] + T["mTk"][s])
        for n in range(2):
            pi = 4 + n
            def mm(e, s=s, n=n, pi=pi):
                for kk in range(8):
                    r = e.matmul(ps[pi][:], mT[s][:, kk * 128:(kk + 1) * 128], wo[n][:, kk, :], start=(kk == 0), stop=(kk == 7))
                return r
            P.op("tensor", mm, reads=[(ph, "mT", s)] + T["wok"][n], writes=[(ph, "ps", pi)])
            P.op("vector", lambda e, s=s, n=n, pi=pi: e.scalar_tensor_tensor(yo[s][:, n * 512:(n + 1) * 512], xt[s][:, n * 512:(n + 1) * 512], ALPHA, ps[pi][:], ALU.mult, ALU.add),
                 reads=[(ph, "xt", s), (ph, "ps", pi)], writes=[(ph, "ps", pi), (ph, "yo", s, n)])
        layer_norm_tile(P, ph, yo[s], [(ph, "yo", s, 0), (ph, "yo", s, 1)], yo[s], (ph, "yo", s, 0), g_bc, b_bc, st, i)
        P.dma("sync", out_rows[i * 128:(i + 1) * 128, :], yo[s], reads=[(ph, "yo", s, 0)], writes=[(ph, "hout", i)], slot=("yo_st", s))
        P.res_w[(ph, "yo", s, 1)] = P.res_w[(ph, "yo", s, 0)]
        P.res_r[(ph, "yo", s, 1)] = dict(P.res_r[(ph, "yo", s, 0)])


def even_mixer_seq(P, ph, C, lay_j, h_rows, out_rows, L, lam_init, ln_g, ln_b):
    D = C["dram"]
    ident, ident_b = C["ident"], C["ident_b"]
    NT = L // 128
    NC = L // 512
    sb = lambda n, s, d=F32: P.sb(ph + n, s, d)
    K = lambda *a: (ph,) + a
    w_in = D["ev_w_in"][lay_j]
    ps = [P.ps(ph + "ps%d" % i, [128, 512]) for i in range(8)]

    xt = [sb("xt%d" % i, [128, 1024]) for i in range(2)]
    xT = sb("xT", [128, 8, L], BF16)
    wb = [sb("wb%d" % i, [128, 8, 512], BF16) for i in range(2)]
    va = sb("va", [128, NT, 512], BF16)
    vb = sb("vb", [128, NT, 4, 132], BF16)
    mix = sb("mix", [128, NT, 1024], BF16)
    qa = [sb("qa%d" % i, [128, L], BF16) for i in range(4)]
    ka = [sb("ka%d" % i, [128, L], BF16) for i in range(4)]
    qd = [sb("qd%d" % i, [68, L], BF16) for i in range(2)]
    kd = [sb("kd%d" % i, [68, L], BF16) for i in range(2)]
    mstrict = sb("mstrict", [128, 4, 512], BF16)
    mcausal = sb("mcausal", [128, 4, 512], BF16)
    uincl = sb("uincl", [128, 128], BF16)
    ones1 = sb("ones1", [1, 128], BF16)
    efg = sb("efg", [128, 4, 512])
    ef = [efg[:, i, :] for i in range(2)]
    eg = [efg[:, 2 + i, :] for i in range(2)]
    atsp = sb("atsp", [128, 4, 512], BF16)
    at = [atsp[:, i, :] for i in range(2)]
    spt = [atsp[:, 2 + i, :] for i in range(2)]
    suf = sb("suf", [1, 512], BF16)
    lamv = sb("lamv", [128, 4, 64]); lam = sb("lam", [128, 4])
    wsub = sb("wsub", [128, 128])
    o1 = sb("o1", [128, 128]); ob = sb("ob", [128, 128]); sm = sb("sm", [128, 8])
    junk = sb("junk", [128, 1024], BF16)

    P.dma("gpsimd", mstrict[:], D["c_mstrict"], writes=[K("mstrict")])
    P.dma("gpsimd", mcausal[:], D["c_mcausal"], writes=[K("mcausal")])
    P.dma("gpsimd", uincl[:], D["c_uincl"], writes=[K("uincl")])
    P.op("vector", lambda e: e.memset(ones1[:], 1.0), writes=[K("ones1")])
    for i, nm in enumerate(["ev_lambda_q1", "ev_lambda_k1", "ev_lambda_q2", "ev_lambda_k2"]):
        P.dma("sync", lamv[:, i, :], D[nm][lay_j].partition_broadcast(128), writes=[K("lamv", i)])
    P.dma("sync", wsub[:], D["ev_subln_w"][lay_j].partition_broadcast(128), writes=[K("wsub")])
    P.op("vector", lambda e: e.tensor_scalar(wsub[:], wsub[:], 1.0 - lam_init, None, ALU.mult), reads=[K("wsub")], writes=[K("wsub")])
    for j in range(2):
        P.op("vector", lambda e, j=j: e.tensor_tensor(lamv[:, 2 * j, :], lamv[:, 2 * j, :], lamv[:, 2 * j + 1, :], ALU.mult),
             reads=[K("lamv", 2 * j), K("lamv", 2 * j + 1)], writes=[K("lamv", 2 * j)])
        P.op("vector", lambda e, j=j: e.reduce_sum(lam[:, j:j + 1], lamv[:, 2 * j, :], axis=AX.X), reads=[K("lamv", 2 * j)], writes=[K("lam", j)])
        P.op("scalar", lambda e, j=j: e.activation(lam[:, j:j + 1], lam[:, j:j + 1], AF.Exp), reads=[K("lam", j)], writes=[K("lam", j)])
    P.op("vector", lambda e: e.tensor_tensor(lam[:, 2:3], lam[:, 1:2], lam[:, 0:1], ALU.subtract), reads=[K("lam", 0), K("lam", 1)], writes=[K("lam", 2)])
    P.op("vector", lambda e: e.tensor_scalar(lam[:, 3:4], lam[:, 2:3], -lam_init, None, ALU.add), reads=[K("lam", 2)], writes=[K("nlam")])

    load_xT(P, ph, C, h_rows, L, xT, xt, ps)

    wslot = [0]
    def next_w(c0, ncols=512):
        s = wslot[0] % 2
        wslot[0] += 1
        load_w_cols(P, ph, wb[s], "wb%d" % s, w_in, c0, ncols)
        return wb[s], wkeys(ph, "wb%d" % s)

    wt, wk = next_w(1024)
    proj_tm(P, ph, wt, wk, 0, 512, xT, L, ps, [2, 3],
            lambda i, pa, pk: P.op("vector", lambda e: e.tensor_copy(va[:, i, :], pa), reads=[pk], writes=[pk, K("va", i)]))
    wt, wk = next_w(2560)
    P.op("vector", lambda e: e.memset(vb[:], 1.0), writes=[K("vb", i) for i in range(NT)])
    proj_tm(P, ph, wt, wk, 0, 512, xT, L, ps, [2, 3],
            lambda i, pa, pk: P.op("vector", lambda e: e.tensor_copy(vb[:, i, :, 0:128], pa.rearrange("p (h d) -> p h d", h=4)), reads=[pk], writes=[pk, K("vb", i)]))

    wt, wk = next_w(0)
    for j in range(4):
        proj_fm(P, ph, wt, wk, j * 128, 128, xT, L, ps, [2, 3],
                lambda ch, pa, pk, j=j: P.op("scalar", lambda e: e.activation(qa[j][:, ch * 512:(ch + 1) * 512], pa, AF.Identity, scale=0.125),
                                            reads=[pk], writes=[pk, K("qa", j)]))
    wt, wk = next_w(512)
    for j in range(4):
        proj_fm(P, ph, wt, wk, j * 128, 128, xT, L, ps, [2, 3],
                lambda ch, pa, pk, j=j: P.op("vector", lambda e: e.tensor_copy(ka[j][:, ch * 512:(ch + 1) * 512], pa),
                                            reads=[pk], writes=[pk, K("ka", j)]))

    it = 0
    for h in range(8):
        qT = qa[h // 2][(h % 2) * 64:(h % 2) * 64 + 64, :]
        kT = ka[h // 2][(h % 2) * 64:(h % 2) * 64 + 64, :]
        qk = [K("qa", h // 2), K("ka", h // 2)]
        for c in range(NC):
            po = ps[4 + (h * NC + c) % 2]
            pok = K("ps", 4 + (h * NC + c) % 2)
            nk = 4 * c + 4
            for kt in range(nk - 1, -1, -1):
                j = kt - 4 * c
                q0 = max(j, 0) * 128
                b = it % 2
                it += 1
                pz, pzk = ps[b], K("ps", b)
                pg, pgk = ps[2 + b], K("ps", 2 + b)
                first = False
                if kt == nk - 1:
                    P.op("vector", lambda e: e.memset(suf[:], 0.0), writes=[K("suf")])
                P.op("tensor", lambda e, pz=pz, kt=kt, c=c, q0=q0, kT=kT, qT=qT: e.matmul(
                    pz[:, q0:512], kT[:, kt * 128:(kt + 1) * 128], qT[:, c * 512 + q0:(c + 1) * 512], start=True, stop=True),
                     reads=qk, writes=[pzk])
                P.op("scalar", lambda e, b=b, pz=pz, q0=q0: e.activation(ef[b][:, q0:512], pz[:, q0:512], AF.Exp), reads=[pzk], writes=[pzk, K("ef", b)])
                P.op("scalar", lambda e, b=b, q0=q0: e.activation(spt[b][:, q0:512], ef[b][:, q0:512], AF.Ln, bias=1.0), reads=[K("ef", b)], writes=[K("sp", b)])
                if j >= 0:
                    P.op("gpsimd", lambda e, b=b, j=j, q0=q0: e.tensor_tensor(spt[b][:, q0:512], spt[b][:, q0:512], mstrict[:, j, q0:512], ALU.mult),
                         reads=[K("sp", b), K("mstrict")], writes=[K("sp", b)])
                def mg(e, b=b, pg=pg, q0=q0, first=first):
                    r = e.matmul(pg[:, q0:512], uincl[:], spt[b][:, q0:512], start=True, stop=first)
                    if not first:
                        r = e.matmul(pg[:, q0:512], ones1[:], suf[:, q0:512], start=False, stop=True)
                    return r
                P.op("tensor", mg, reads=[K("sp", b), K("uincl"), K("ones1"), K("suf")], writes=[pgk])
                P.op("scalar", lambda e, b=b, pg=pg, q0=q0: e.activation(eg[b][:, q0:512], pg[:, q0:512], AF.Exp, scale=-1.0), reads=[pgk], writes=[pgk, K("eg", b)])
                if kt > 0:
                    P.op("vector", lambda e, pg=pg, q0=q0: e.tensor_copy(suf[:, q0:512], pg[0:1, q0:512]), reads=[pgk], writes=[pgk, K("suf")])
                P.op("vector", lambda e, b=b, q0=q0: e.tensor_tensor(at[b][:, q0:512], ef[b][:, q0:512], eg[b][:, q0:512], ALU.mult),
                     reads=[K("ef", b), K("eg", b)], writes=[K("at", b)])
                if j >= 0:
                    P.op("gpsimd", lambda e, b=b, j=j, q0=q0: e.tensor_tensor(at[b][:, q0:512], at[b][:, q0:512], mstrict[:, j, q0:512], ALU.mult),
                         reads=[K("at", b), K("mstrict")], writes=[K("at", b)])
                def pv(e, b=b, kt=kt, c=c, j=j, h=h, po=po):
                    r = None
                    for qs in range(3, max(j, 0) - 1, -1):
                        qt = 4 * c + qs
                        r = e.matmul(po[:, qs * 64:(qs + 1) * 64], at[b][:, qs * 128:(qs + 1) * 128], va[:, kt, h * 64:(h + 1) * 64],
                                     start=(kt == 4 * c + 3 and qs == 3), stop=(kt == 0 and qs == 0))
                    return r
                P.op("tensor", pv, reads=[K("at", b), K("va", kt)], writes=[pok])
            P.op("vector", lambda e, po=po, c=c, h=h: e.tensor_copy(mix[:, 4 * c:4 * c + 4, h * 64:(h + 1) * 64], po[:, 0:256].rearrange("p (a d) -> p a d", a=4)),
                 reads=[pok], writes=[pok] + [K("mix", 4 * c + qs) for qs in range(4)])

    for h in range(4):
        for m in range(2):
            wt, wk = next_w(1536 + (h * 2 + m) * 64, 64)
            proj_fm(P, ph, wt, wk, 0, 64, xT, L, ps, [2, 3],
                    lambda ch, pa, pk, m=m: P.op("scalar", lambda e: e.activation(qd[m][0:64, ch * 512:(ch + 1) * 512], pa, AF.Identity, scale=0.125),
                                                reads=[pk], writes=[pk, K("qd", m)]))
            wt, wk = next_w(2048 + (h * 2 + m) * 64, 64)
            proj_fm(P, ph, wt, wk, 0, 64, xT, L, ps, [2, 3],
                    lambda ch, pa, pk, m=m: P.op("vector", lambda e: e.tensor_copy(kd[m][0:64, ch * 512:(ch + 1) * 512], pa),
                                                reads=[pk], writes=[pk, K("kd", m)]))
            P.dma("gpsimd", qd[m][64:68, :], D["c_qaug4"][h][:, 0:L], writes=[K("qd", m)], slot=("qaug", m))
            P.dma("gpsimd", kd[m][64:68, :], D["c_kaug"][:, 0:L], writes=[K("kd", m)], slot=("kaug", m))
        for c in range(NC):
            nk = 4 * c + 4
            for m in range(2):
                pos = [ps[4 + 2 * m], ps[5 + 2 * m]]
                for kt in range(nk - 1, -1, -1):
                    j = kt - 4 * c
                    q0 = max(j, 0) * 128
                    b = it % 2
                    it += 1
                    pz, pzk = ps[b], K("ps", b)
                    P.op("tensor", lambda e, pz=pz, kt=kt, c=c, q0=q0, m=m: e.matmul(
                        pz[:, q0:512], kd[m][:, kt * 128:(kt + 1) * 128], qd[m][:, c * 512 + q0:(c + 1) * 512], start=True, stop=True),
                         reads=[K("qd", m), K("kd", m)], writes=[pzk])
                    P.op("scalar", lambda e, b=b, pz=pz, q0=q0: e.activation(at[b][:, q0:512], pz[:, q0:512], AF.Exp), reads=[pzk], writes=[pzk, K("at", b)])
                    if j >= 0:
                        P.op("gpsimd", lambda e, b=b, j=j, q0=q0: e.tensor_tensor(at[b][:, q0:512], at[b][:, q0:512], mcausal[:, j, q0:512], ALU.mult),
                             reads=[K("at", b), K("mcausal")], writes=[K("at", b)])
                    def pv(e, b=b, kt=kt, c=c, j=j, h=h, pos=pos):
                        r = None
                        for qs in range(3, max(j, 0) - 1, -1):
                            qt = 4 * c + qs
                            r = e.matmul(pos[qs // 2][:, (qs % 2) * 256:(qs % 2) * 256 + 129], at[b][:, qs * 128:(qs + 1) * 128], vb[:, kt, h, 0:129],
                                         start=(kt == qt and qs % 2 == 1), stop=(kt == 0 and qs % 2 == 0))
                        return r
                    P.op("tensor", pv, reads=[K("at", b), K("vb", kt)], writes=[K("ps", 4 + 2 * m), K("ps", 5 + 2 * m)])
            for qs in range(4):
                qt = 4 * c + qs
                p1 = ps[4 + qs // 2][:, (qs % 2) * 256:(qs % 2) * 256 + 129]
                p2 = ps[6 + qs // 2][:, (qs % 2) * 256:(qs % 2) * 256 + 129]
                k1, k2 = K("ps", 4 + qs // 2), K("ps", 6 + qs // 2)
                P.op("vector", lambda e, p1=p1: e.reciprocal(sm[:, 0:1], p1[:, 128:129]), reads=[k1], writes=[k1, K("sm0")])
                P.op("vector", lambda e, p2=p2: e.reciprocal(sm[:, 1:2], p2[:, 128:129]), reads=[k2], writes=[k2, K("sm1")])
                P.op("vector", lambda e: e.tensor_tensor(sm[:, 1:2], sm[:, 1:2], lam[:, 3:4], ALU.mult), reads=[K("sm1"), K("nlam")], writes=[K("sm1")])
                P.op("vector", lambda e, p1=p1: e.tensor_scalar(o1[:], p1[:, 0:128], sm[:, 0:1], None, ALU.mult), reads=[k1, K("sm0")], writes=[k1, K("o1")])
                P.op("vector", lambda e, p2=p2: e.scalar_tensor_tensor(ob[:], p2[:, 0:128], sm[:, 1:2], o1[:], ALU.mult, ALU.add),
                     reads=[k2, K("sm1"), K("o1")], writes=[k2, K("ob")])
                P.op("vector", lambda e: e.memset(sm[:, 2:3], 0.0), writes=[K("sm2")])
                P.op("scalar", lambda e: e.activation(junk[:, 0:128], ob[:], AF.Square, accum_out=sm[:, 2:3]), reads=[K("ob"), K("sm2")], writes=[K("sm2"), K("junk")])
                P.op("vector", lambda e: e.tensor_scalar(sm[:, 2:3], sm[:, 2:3], 1.0 / 128, RMS_EPS, ALU.mult, ALU.add), reads=[K("sm2")], writes=[K("sm2")])
                P.op("scalar", lambda e: e.sqrt(sm[:, 2:3], sm[:, 2:3]), reads=[K("sm2")], writes=[K("sm2")])
                P.op("vector", lambda e: e.reciprocal(sm[:, 3:4], sm[:, 2:3]), reads=[K("sm2")], writes=[K("sm3")])
                P.op("vector", lambda e, qt=qt, h=h: e.scalar_tensor_tensor(mix[:, qt, 512 + h * 128:512 + (h + 1) * 128], ob[:], sm[:, 3:4], wsub[:], ALU.mult, ALU.mult),
                     reads=[K("ob"), K("sm3"), K("wsub")], writes=[K("mix", qt)])

    if "dbg" in D:
        P.dma("gpsimd", D["dbg"].rearrange("(t p) c -> p t c", p=128), mix[:], reads=[K("mix", i) for i in range(NT)], writes=[K("dbg")])
    xTf = xT[:].rearrange("p a b -> p (a b)").bitcast(F32)
    T = dict(wo=wb, mT=[atsp[:, 0:2, :].rearrange("p a b -> p (a b)"), atsp[:, 2:4, :].rearrange("p a b -> p (a b)")],
             mTk=[[K("at", 0), K("at", 1)], [K("sp", 0), K("sp", 1)]],
             xt=xt, g_bc=efg[:, 0:2, :].rearrange("p a b -> p (a b)"), b_bc=efg[:, 2:4, :].rearrange("p a b -> p (a b)"),
             gk=[K("ef", 0), K("ef", 1)], bk=[K("eg", 0), K("eg", 1)],
             st={n: sb("st_" + n, [128, 1]) for n in ("s1", "s2", "mean", "msq", "var", "rstd")},
             yo=[xTf[:, 0:1024], xTf[:, 1024:2048]], wok=[wkeys(ph, "wb0"), wkeys(ph, "wb1")])
    T["st"]["junk"] = junk
    outproj_ln(P, ph, C, mix, L, D["ev_w_out"][lay_j], h_rows, out_rows, ln_g, ln_b, T, ps)


def odd_mixer_seq(P, ph, C, lay_j, h_rows, out_rows, L, ln_g, ln_b):
    D = C["dram"]
    ident, ident_b = C["ident"], C["ident_b"]
    NT = L // 128
    NC = L // 512
    KSEL = min(256, L // 4)
    sb = lambda n, s, d=F32: P.sb(ph + n, s, d)
    K = lambda *a: (ph,) + a
    w_in = D["od_w_in"][lay_j]
    ps = [P.ps(ph + "ps%d" % i, [128, 512]) for i in range(8)]
    pbf = ps[6][:].bitcast(BF16)

    xt = [sb("xt%d" % i, [128, 1024]) for i in range(2)]
    xT = sb("xT", [128, 8, L], BF16)
    wb = [sb("wb%d" % i, [128, 8, 512], BF16) for i in range(2)]
    mix = sb("mix", [128, NT, 1024], BF16)
    qTc = sb("qTc", [128, 16, 512], BF16)
    ckvT = sb("ckvT", [128, L], BF16)
    ckva = sb("ckva", [128, NT, 132], BF16)
    qiT = [sb("qiT%d" % i, [128, L], BF16) for i in range(4)]
    kiT2 = sb("kiT2", [128, L], BF16)
    widx = sb("widx", [128, NT, 8])
    score = sb("score", [128, L]); wkt = sb("wkt", [128, L])
    MB = [sb("MB%d" % i, [128, L], BF16) for i in range(4)]
    rl = [sb("rl%d" % i, [128, 512]) for i in range(2)]
    efg = sb("efg", [128, 4, 512])
    atsp = sb("atsp", [128, 4, 512], BF16)
    at = [atsp[:, i, :] for i in range(2)]
    kaug = sb("kaug", [9, L], BF16)
    qaugc = sb("qaugc", [9, 16, 512], BF16)
    wuv = sb("wuv", [128, 16, 64], BF16)
    kvw = sb("kvw", [128, 128])
    oh = sb("oh", [128, 128], BF16); ohT = sb("ohT", [128, 128], BF16)
    m8 = sb("m8", [128, 8]); sm = sb("sm", [128, 8])
    junk = sb("junk", [128, 1024], BF16)

    P.dma("gpsimd", kaug[:], D["c_kaug9"][:, 0:L], writes=[K("kaug")])
    P.dma("gpsimd", wuv[:], D["od_w_uv"][lay_j].rearrange("h c d -> c h d"), writes=[K("wuv")])
    P.dma("sync", kvw[:], D["od_kv_norm_w"][lay_j].partition_broadcast(128), writes=[K("kvw")])
    P.op("vector", lambda e: e.memset(ckva[:], 1.0), writes=[K("ckva", i) for i in range(NT)])

    load_xT(P, ph, C, h_rows, L, xT, xt, ps)
    wslot = [0]
    def next_w(c0, ncols=512, dst0=0, new=True):
        if new:
            wslot[0] += 1
        s = wslot[0] % 2
        load_w_cols(P, ph, wb[s], "wb%d" % s, w_in, c0, ncols, dst0=dst0)
        return wb[s], wkeys(ph, "wb%d" % s, (dst0,))

    wt, wk = next_w(2048, 128)
    def ev_ckv(i, pa, pk):
        P.op("vector", lambda e: e.memset(sm[:, 0:1], 0.0), writes=[K("sm0")])
        P.op("scalar", lambda e: e.activation(junk[:, 0:128], pa, AF.Square, accum_out=sm[:, 0:1]), reads=[pk, K("sm0")], writes=[pk, K("sm0"), K("junk")])
        P.op("vector", lambda e: e.tensor_scalar(sm[:, 0:1], sm[:, 0:1], 1.0 / 128, RMS_EPS, ALU.mult, ALU.add), reads=[K("sm0")], writes=[K("sm0")])
        P.op("scalar", lambda e: e.sqrt(sm[:, 0:1], sm[:, 0:1]), reads=[K("sm0")], writes=[K("sm0")])
        P.op("vector", lambda e: e.reciprocal(sm[:, 1:2], sm[:, 0:1]), reads=[K("sm0")], writes=[K("sm1")])
        P.op("vector", lambda e: e.scalar_tensor_tensor(ckva[:, i, 0:128], pa, sm[:, 1:2], kvw[:], ALU.mult, ALU.mult),
             reads=[pk, K("sm1"), K("kvw")], writes=[pk, K("ckva", i)])
        P.op("tensor", lambda e: e.transpose(pbf[:, 0:128], ckva[:, i, 0:128], ident_b[:]), reads=[K("ckva", i), "ident_b"], writes=[K("ps", 6)])
        P.op("vector", lambda e: e.tensor_copy(ckvT[:, i * 128:(i + 1) * 128], pbf[:, 0:128]), reads=[K("ps", 6)], writes=[K("ps", 6), K("ckvT")])
    proj_tm(P, ph, wt, wk, 0, 128, xT, L, ps, [2, 3], ev_ckv)
    wt, wk = next_w(2176, 512)
    for j in range(4):
        proj_fm(P, ph, wt, wk, j * 128, 128, xT, L, ps, [2, 3],
                lambda ch, pa, pk, j=j: P.op("scalar", lambda e: e.activation(qiT[j][:, ch * 512:(ch + 1) * 512], pa, AF.Identity, scale=0.125),
                                            reads=[pk], writes=[pk, K("qiT")]))
    wt, wk0 = next_w(2688, 64, dst0=0)
    _, wk1 = next_w(2688, 64, dst0=64, new=False)
    _, wk2 = next_w(2752, 8, dst0=128, new=False)
    proj_fm(P, ph, wt, wk0 + wk1, 0, 128, xT, L, ps, [2, 3],
            lambda ch, pa, pk: P.op("vector", lambda e: e.tensor_copy(kiT2[:, ch * 512:(ch + 1) * 512], pa), reads=[pk], writes=[pk, K("kiT2")]))
    proj_tm(P, ph, wt, wk2, 128, 8, xT, L, ps, [2, 3],
            lambda i, pa, pk: P.op("vector", lambda e: e.tensor_scalar(widx[:, i, :], pa, 8 ** -0.5, None, ALU.mult), reads=[pk], writes=[pk, K("widx")]))

    it = 0
    for c in range(NC):
        for g in range(4):
            wt, wk = next_w(g * 512, 512)
            for hh in range(4):
                h = g * 4 + hh
                pi = 2 + h % 2
                def mm(e, hh=hh, pi=pi, wt=wt, c=c):
                    for kk in range(8):
                        r = e.matmul(ps[pi][:], wt[:, kk, hh * 128:(hh + 1) * 128], xT[:, kk, c * 512:(c + 1) * 512], start=(kk == 0), stop=(kk == 7))
                    return r
                P.op("tensor", mm, reads=wk + [K("xT", c)], writes=[K("ps", pi)])
                P.op("scalar", lambda e, h=h, pi=pi: e.activation(qTc[:, h, :], ps[pi][:], AF.Identity, scale=128 ** -0.5),
                     reads=[K("ps", pi)], writes=[K("ps", pi), K("qTc", h)])
        P.dma("gpsimd", qaugc[:], D["c_qaug16"][:, :, c * 512:(c + 1) * 512].rearrange("h r l -> r h l"), writes=[K("qaugc")])
        for qs in range(4):
            qt = 4 * c + qs
            n_s = (qt + 1) * 128
            for sc in range((n_s + 511) // 512):
                w = min(512, n_s - sc * 512)
                for ih in range(8):
                    b = it % 2
                    it += 1
                    pi = 2 + b
                    P.op("tensor", lambda e, pi=pi, ih=ih, qt=qt, sc=sc, w=w: e.matmul(
                        ps[pi][:, 0:w], qiT[ih // 2][(ih % 2) * 64:(ih % 2) * 64 + 64, qt * 128:(qt + 1) * 128],
                        kiT2[(ih % 2) * 64:(ih % 2) * 64 + 64, sc * 512:sc * 512 + w], start=True, stop=True),
                         reads=[K("qiT"), K("kiT2")], writes=[K("ps", pi)])
                    P.op("scalar", lambda e, pi=pi, b=b, w=w: e.activation(rl[b][:, 0:w], ps[pi][:, 0:w], AF.Relu), reads=[K("ps", pi)], writes=[K("ps", pi), K("rl", b)])
                    if ih == 0:
                        P.op("vector", lambda e, b=b, w=w, sc=sc, qt=qt, ih=ih: e.tensor_scalar(score[:, sc * 512:sc * 512 + w], rl[b][:, 0:w], widx[:, qt, ih:ih + 1], None, ALU.mult),
                             reads=[K("rl", b), K("widx")], writes=[K("score")])
                    else:
                        P.op("vector", lambda e, b=b, w=w, sc=sc, qt=qt, ih=ih: e.scalar_tensor_tensor(score[:, sc * 512:sc * 512 + w], rl[b][:, 0:w], widx[:, qt, ih:ih + 1],
                                                                                                  score[:, sc * 512:sc * 512 + w], ALU.mult, ALU.add),
                             reads=[K("rl", b), K("widx"), K("score")], writes=[K("score")])
            P.op("gpsimd", lambda e, qt=qt: e.affine_select(score[:, qt * 128:(qt + 1) * 128], score[:, qt * 128:(qt + 1) * 128], [[-1, 128]], ALU.is_ge, -1e30,
                                                         base=0, channel_multiplier=1), reads=[K("score")], writes=[K("score")])
            if qt * 128 >= KSEL:
                R = KSEL // 8
                for r in range(R):
                    src = score if r == 0 else wkt
                    P.op("vector", lambda e, src=src, n_s=n_s: e.max(m8[:], src[:, 0:n_s]), reads=[K("score"), K("wkt")], writes=[K("m8")])
                    if r < R - 1:
                        P.op("vector", lambda e, src=src, n_s=n_s: e.match_replace(wkt[:, 0:n_s], m8[:], src[:, 0:n_s], -1e30),
                             reads=[K("score"), K("wkt"), K("m8")], writes=[K("wkt")])
                P.op("vector", lambda e, qs=qs, n_s=n_s: e.tensor_scalar(MB[qs][:, 0:n_s], score[:, 0:n_s], m8[:, 7:8], NEGM, ALU.is_lt, ALU.mult),
                     reads=[K("score"), K("m8")], writes=[K("MB", qs)])
            else:
                P.op("vector", lambda e, qs=qs, n_s=n_s: e.tensor_scalar(MB[qs][:, 0:n_s], score[:, 0:n_s], -1e29, NEGM, ALU.is_lt, ALU.mult),
                     reads=[K("score")], writes=[K("MB", qs)])
        nk = 4 * c + 4
        for h in range(16):
            pos = [ps[4], ps[5]]
            for kt in range(nk - 1, -1, -1):
                j = kt - 4 * c
                jm = max(j, 0)
                q0 = jm * 128
                b = it % 2
                it += 1
                pz, pzk = ps[b], K("ps", b)
                def sc_mm(e, pz=pz, kt=kt, q0=q0, jm=jm, h=h):
                    e.matmul(pz[:, q0:512], ckvT[:, kt * 128:(kt + 1) * 128], qTc[:, h, q0:512], start=True, stop=False)
                    r = e.matmul(pz[:, q0:512], kaug[:, kt * 128:(kt + 1) * 128], qaugc[:, h, q0:512], start=False, stop=False)
                    for qs in range(jm, 4):
                        r = e.matmul(pz[:, qs * 128:(qs + 1) * 128], MB[qs][:, kt * 128:(kt + 1) * 128], ident_b[:], start=False, stop=(qs == 3))
                    return r
                P.op("tensor", sc_mm, reads=[K("ckvT"), K("qTc", h), K("kaug"), K("qaugc"), "ident_b"] + [K("MB", q) for q in range(jm, 4)], writes=[pzk])
                P.op("scalar", lambda e, b=b, pz=pz, q0=q0: e.activation(at[b][:, q0:512], pz[:, q0:512], AF.Exp), reads=[pzk], writes=[pzk, K("at", b)])
                def pv(e, b=b, kt=kt, c=c, jm=jm):
                    r = None
                    for qs in range(3, jm - 1, -1):
                        qt = 4 * c + qs
                        r = e.matmul(pos[qs // 2][:, (qs % 2) * 256:(qs % 2) * 256 + 129], at[b][:, qs * 128:(qs + 1) * 128], ckva[:, kt, 0:129],
                                     start=(kt == qt and qs % 2 == 1), stop=(kt == 0 and qs % 2 == 0))
                    return r
                P.op("tensor", pv, reads=[K("at", b), K("ckva", kt)], writes=[K("ps", 4), K("ps", 5)])
            for qs in range(4):
                qt = 4 * c + qs
                p1 = ps[4 + qs // 2][:, (qs % 2) * 256:(qs % 2) * 256 + 129]
                k1 = K("ps", 4 + qs // 2)
                P.op("vector", lambda e, p1=p1: e.reciprocal(sm[:, 2:3], p1[:, 128:129]), reads=[k1], writes=[k1, K("sm2")])
                P.op("vector", lambda e, p1=p1: e.tensor_scalar(oh[:], p1[:, 0:128], sm[:, 2:3], None, ALU.mult), reads=[k1, K("sm2")], writes=[k1, K("oh")])
                P.op("tensor", lambda e: e.transpose(pbf[:, 0:128], oh[:], ident_b[:]), reads=[K("oh"), "ident_b"], writes=[K("ps", 6)])
                P.op("vector", lambda e: e.tensor_copy(ohT[:], pbf[:, 0:128]), reads=[K("ps", 6)], writes=[K("ps", 6), K("ohT")])
                P.op("tensor", lambda e, h=h: e.matmul(ps[7][:, 0:64], ohT[:], wuv[:, h, :], start=True, stop=True), reads=[K("ohT"), K("wuv")], writes=[K("ps", 7)])
                P.op("scalar", lambda e, qt=qt, h=h: e.activation(mix[:, qt, h * 64:(h + 1) * 64], ps[7][:, 0:64], AF.Identity),
                     reads=[K("ps", 7)], writes=[K("ps", 7), K("mix", qt)])

    if "dbg" in D:
        P.dma("gpsimd", D["dbg"].rearrange("(t p) c -> p t c", p=128), mix[:], reads=[K("mix", i) for i in range(NT)], writes=[K("dbg")])
    xTf = xT[:].rearrange("p a b -> p (a b)").bitcast(F32)
    T = dict(wo=wb, mT=[atsp[:, 0:2, :].rearrange("p a b -> p (a b)"), atsp[:, 2:4, :].rearrange("p a b -> p (a b)")],
             mTk=[[K("at", 0), K("at", 1)], [K("sp", 0), K("sp", 1)]],
             xt=xt, g_bc=efg[:, 0:2, :].rearrange("p a b -> p (a b)"), b_bc=efg[:, 2:4, :].rearrange("p a b -> p (a b)"),
             gk=[K("ef", 0), K("ef", 1)], bk=[K("eg", 0), K("eg", 1)],
             st={n: sb("st_" + n, [128, 1]) for n in ("s1", "s2", "mean", "msq", "var", "rstd")},
             yo=[xTf[:, 0:1024], xTf[:, 1024:2048]], wok=[wkeys(ph, "wb0"), wkeys(ph, "wb1")])
    T["st"]["junk"] = junk
    outproj_ln(P, ph, C, mix, L, D["od_w_out"][lay_j], h_rows, out_rows, ln_g, ln_b, T, ps)


def make_consts(L=2048):
    s = np.arange(128)[:, None]; q = np.arange(512)[None, :]
    mstrict = np.stack([((q - j * 128) > s) for j in range(4)], axis=1).astype(np.float32)
    mcausal = np.stack([((q - j * 128) >= s) for j in range(4)], axis=1).astype(np.float32)
    jj = np.arange(128)[:, None]; ss = np.arange(128)[None, :]
    uincl = (jj >= ss).astype(np.float32)
    t = np.arange(L)
    hi = (t // 256) * 256; lo = t % 256
    def aug(slopes):
        qa = np.stack([np.stack([np.full(L, c), np.full(L, c), -c * hi, -c * lo]) for c in slopes]).astype(np.float32)
        return qa
    sl4 = 2.0 ** (-8.0 * np.arange(1, 5) / 4)
    sl16 = 2.0 ** (-8.0 * np.arange(1, 17) / 16)
    kaug = np.stack([hi, lo, np.ones(L), np.ones(L)]).astype(np.float32)
    import ml_dtypes
    def bsplit(v):
        v = np.asarray(v, dtype=np.float64); outp = []
        for _ in range(3):
            p = v.astype(ml_dtypes.bfloat16).astype(np.float64); outp.append(p); v = v - p
        return outp
    q9 = []
    for c in sl16:
        c1, c2, c3 = bsplit(np.full(L, c)); v1, v2, v3 = bsplit(c * t.astype(np.float64))
        q9.append(np.stack([c1, c1, c2, c2, c3, c3, -v1, -v2, -v3]))
    qaug9 = np.stack(q9).astype(np.float32)
    kaug9 = np.stack([hi, lo, hi, lo, hi, lo, np.ones(L), np.ones(L), np.ones(L)]).astype(np.float32)
    return dict(c_mstrict=mstrict, c_mcausal=mcausal, c_uincl=uincl, c_qaug4=aug(sl4), c_qaug16=qaug9, c_kaug9=kaug9, c_kaug=kaug,
                c_ident=np.eye(128, dtype=np.float32))


NCORES = 8
SEQ = 2048
NTOK = 2 * SEQ
N_EXPERTS = 32
_W_NAMES = ["ev_w_in", "ev_w_out", "ev_lambda_q1", "ev_lambda_k1", "ev_lambda_q2", "ev_lambda_k2", "ev_subln_w",
            "od_w_in", "od_kv_norm_w", "od_w_uv", "od_w_out", "ln_mix_g", "ln_mix_b", "router_w", "router_b",
            "exp_w_gu", "exp_b_gu", "exp_w_down", "exp_b_down", "ln_ffn_g", "ln_ffn_b"]


def build_program(shapes, cshapes):
    nc = bass.Bass("TRN2", target_bir_lowering=False)
    D = {}
    for n in _W_NAMES:
        D[n] = nc.dram_tensor(n, list(shapes[n]), F32, kind="ExternalInput").ap()
    for n, s in cshapes.items():
        D[n] = nc.dram_tensor(n, list(s), F32, kind="ExternalInput").ap()
    x = nc.dram_tensor("x", [NTOK, 1024], F32, kind="ExternalInput").ap()
    out = nc.dram_tensor("out", [NTOK, 1024], F32, kind="ExternalOutput").ap()
    h1 = nc.dram_tensor("h1", [NTOK, 1024], F32, kind="Internal").ap()
    h2 = nc.dram_tensor("h2", [NTOK, 1024], F32, kind="Internal").ap()
    h3 = nc.dram_tensor("h3", [NTOK, 1024], F32, kind="Internal").ap()
    with ExitStack() as st:
        P = Prog(nc, st)
        ident = P.sb("ident", [128, 128]); ident_b = P.sb("ident_b", [128, 128], BF16)
        P.dma("sync", ident[:], D["c_ident"], writes=["ident"])
        P.dma("gpsimd", ident_b[:], D["c_ident"], writes=["ident_b"])
        C = {"dram": D, "ident": ident, "ident_b": ident_b}
        lam_init0 = 0.8 - 0.6 * math.exp(-0.3 * 0)

        def phase(fn):
            with ExitStack() as ts:
                P.stack = ts
                fn()
                P.barrier()
            P.stack = st

        for sq in range(2):
            phase(lambda sq=sq: even_mixer_seq(P, "e%d" % sq, C, 0, x[sq * SEQ:(sq + 1) * SEQ, :], h1[sq * SEQ:(sq + 1) * SEQ, :], SEQ,
                                                lam_init0, D["ln_mix_g"][0], D["ln_mix_b"][0]))
        phase(lambda: moe_phase(P, "m0", C, 0, h1, h2, NTOK, N_EXPERTS))
        for sq in range(2):
            phase(lambda sq=sq: odd_mixer_seq(P, "o%d" % sq, C, 0, h2[sq * SEQ:(sq + 1) * SEQ, :], h3[sq * SEQ:(sq + 1) * SEQ, :], SEQ,
                                               D["ln_mix_g"][1], D["ln_mix_b"][1]))
        phase(lambda: moe_phase(P, "m1", C, 1, h3, out, NTOK, N_EXPERTS))
        P.final_wait()
        P.emit()
    return nc


def kernel(**inputs):
    consts = make_consts(SEQ)
    x = np.ascontiguousarray(np.asarray(inputs["x"], dtype=np.float32)).reshape(NCORES, NTOK, 1024)
    ws = {n: np.ascontiguousarray(np.asarray(inputs[n], dtype=np.float32)) for n in _W_NAMES}
    nc = build_program({n: ws[n].shape for n in _W_NAMES}, {n: v.shape for n, v in consts.items()})
    in_maps = []
    for c in range(NCORES):
        m = dict(ws)
        m.update(consts)
        m["x"] = x[c]
        in_maps.append(m)
    res = run_bass_kernel_spmd(nc, in_maps, core_ids=list(range(NCORES)))
    outs = [np.asarray(r["out"], dtype=np.float32) for r in res.results]
    return np.stack(outs, axis=0).reshape(16, SEQ, 1024)
```

```python
import math
from concourse.bass_utils import run_bass_kernel_spmd
from contextlib import ExitStack
import numpy as np
import concourse.bass as bass
import concourse.mybir as mybir

F32 = mybir.dt.float32
BF16 = mybir.dt.bfloat16
I32 = mybir.dt.int32
ALU = mybir.AluOpType
AF = mybir.ActivationFunctionType
AX = mybir.AxisListType

ENGS = ("sync", "scalar", "vector", "gpsimd", "tensor")


class Prog:
    def __init__(self, nc, stack):
        self.nc = nc
        self.stack = stack
        self.sem_stack = stack
        self.streams = {e: [] for e in ENGS}
        self.esem = {e: stack.enter_context(nc.semaphore("es_" + e)) for e in ENGS}
        self.etick = {e: 0 for e in ENGS}
        self.known = {e: {} for e in ENGS}
        self.res_w = {}
        self.res_r = {}
        self.dsem = {}
        self.semobj = {}
        for e in ENGS:
            self.semobj[id(self.esem[e])] = self.esem[e]
        self.n_ops = 0

    def sb(self, name, shape, dt=F32):
        return self.stack.enter_context(self.nc.sbuf_tensor(name, list(shape), dt))

    def ps(self, name, shape, dt=F32):
        return self.stack.enter_context(self.nc.psum_tensor(name, list(shape), dt))

    def _dma_sem(self, key):
        if key not in self.dsem:
            s = self.sem_stack.enter_context(self.nc.semaphore("ds_%d" % len(self.dsem)))
            self.dsem[key] = [s, 0]
            self.semobj[id(s)] = s
        return self.dsem[key]

    def _deps(self, eng, reads, writes):
        deps = {}

        def add(ev):
            if ev is None:
                return
            s, v = ev
            if deps.get(s, 0) < v:
                deps[s] = v

        for k in reads:
            add(self.res_w.get(k))
        for k in writes:
            add(self.res_w.get(k))
            for s, v in self.res_r.get(k, {}).items():
                add((s, v))
        waits = []
        kn = self.known[eng]
        for s, v in deps.items():
            if kn.get(s, 0) < v:
                kn[s] = v
                waits.append((s, v))
        return waits

    def _commit(self, ev, reads, writes):
        s, v = ev
        for k in reads:
            d = self.res_r.setdefault(k, {})
            if d.get(s, 0) < v:
                d[s] = v
        for k in writes:
            self.res_w[k] = ev
            self.res_r[k] = {}

    def op(self, eng, fn, reads=(), writes=()):
        waits = self._deps(eng, reads, writes)
        self.etick[eng] += 1
        ev = (id(self.esem[eng]), self.etick[eng])
        self.streams[eng].append((waits, fn, ev, 1))
        self._commit(ev, reads, writes)
        self.n_ops += 1

    def dma(self, eng, out, in_, reads=(), writes=(), slot=None, **kw):
        if slot is None:
            k0 = writes[0] if writes else reads[0]
            slot = ("_slot",) + tuple(k0[1:]) if isinstance(k0, tuple) and len(k0) > 1 else ("_slot", k0)
        ds = self._dma_sem(slot)
        waits = self._deps(eng, reads, writes)
        ds[1] += 16
        ev = (id(ds[0]), ds[1])
        fn = lambda e, out=out, in_=in_, kw=kw: e.dma_start(out=out, in_=in_, **kw)
        self.streams[eng].append((waits, fn, ev, 16))
        self._commit(ev, reads, writes)
        self.n_ops += 1

    def barrier(self):
        evs = [(id(self.esem[e]), self.etick[e]) for e in ENGS if self.etick[e] > 0]
        evs += [(id(s), c) for s, c in self.dsem.values() if c > 0]
        for e in ENGS:
            kn = self.known[e]
            waits = []
            for s, v in evs:
                if s == id(self.esem[e]) and False:
                    continue
                if kn.get(s, 0) < v:
                    kn[s] = v
                    waits.append((s, v))
            if waits:
                self.streams[e].append((waits, None, None, 0))

    def final_wait(self, eng="sync"):
        evs = [(id(s), c) for s, c in self.dsem.values() if c > 0]
        evs += [(id(self.esem[e]), self.etick[e]) for e in ENGS if self.etick[e] > 0 and e != eng]
        kn = self.known[eng]
        waits = [(s, v) for s, v in evs if kn.get(s, 0) < v]
        self.streams[eng].append((waits, None, None, 0))

    def emit(self):
        nc = self.nc
        with nc.Block() as block:
            def runner(name):
                def run(e):
                    for waits, fn, ev, inc in self.streams[name]:
                        for s, v in waits:
                            e.wait_ge(self.semobj[s], v)
                        if fn is not None:
                            inst = fn(e)
                            inst.then_inc(self.semobj[ev[0]], inc)
                return run
            block.sync(runner("sync"))
            block.scalar(runner("scalar"))
            block.vector(runner("vector"))
            block.gpsimd(runner("gpsimd"))
            block.tensor(runner("tensor"))


DEPTH = 2
ALPHA = (2 * DEPTH) ** 0.25
LN_EPS = 1e-5
SW_LIMIT = 7.0
SW_ALPHA = 1.702


def layer_norm_tile(P, ph, src_ap, src_keys, dst_ap, dst_key, g_bc, b_bc, st, idx):
    s1, s2, mean, msq, var, rstd, junk = (st[k] for k in ("s1", "s2", "mean", "msq", "var", "rstd", "junk"))
    k = lambda n: (ph, "ln_" + n)
    P.op("vector", lambda e: e.reduce_sum(s1[:, 0:1], src_ap, axis=AX.X), reads=list(src_keys), writes=[k("s1")])
    P.op("vector", lambda e: e.memset(s2[:, 0:1], 0.0), writes=[k("s2")])
    P.op("scalar", lambda e: e.activation(junk[:], src_ap, AF.Square, accum_out=s2[:, 0:1]),
         reads=list(src_keys) + [k("s2")], writes=[k("s2"), k("junk")])
    P.op("vector", lambda e: e.tensor_scalar(mean[:, 0:1], s1[:, 0:1], 1.0 / 1024, None, ALU.mult),
         reads=[k("s1")], writes=[k("mean")])
    P.op("vector", lambda e: e.tensor_tensor(msq[:, 0:1], mean[:, 0:1], mean[:, 0:1], ALU.mult),
         reads=[k("mean")], writes=[k("msq")])
    P.op("vector", lambda e: e.scalar_tensor_tensor(var[:, 0:1], s2[:, 0:1], 1.0 / 1024, msq[:, 0:1], ALU.mult, ALU.subtract),
         reads=[k("s2"), k("msq")], writes=[k("var")])
    P.op("vector", lambda e: e.tensor_scalar(var[:, 0:1], var[:, 0:1], LN_EPS, None, ALU.add),
         reads=[k("var")], writes=[k("var")])
    P.op("scalar", lambda e: e.sqrt(var[:, 0:1], var[:, 0:1]), reads=[k("var")], writes=[k("var")])
    P.op("vector", lambda e: e.reciprocal(rstd[:, 0:1], var[:, 0:1]), reads=[k("var")], writes=[k("rstd")])
    P.op("vector", lambda e: e.tensor_scalar(dst_ap, src_ap, mean[:, 0:1], rstd[:, 0:1], ALU.subtract, ALU.mult),
         reads=list(src_keys) + [k("mean"), k("rstd")], writes=[dst_key])
    P.op("vector", lambda e: e.tensor_tensor(dst_ap, dst_ap, g_bc[:], ALU.mult), reads=[dst_key, (ph, "g_bc")], writes=[dst_key])
    P.op("vector", lambda e: e.tensor_tensor(dst_ap, dst_ap, b_bc[:], ALU.add), reads=[dst_key, (ph, "b_bc")], writes=[dst_key])


def moe_phase(P, ph, C, lay, h_in, h_out, NT, E, stage=99):
    nc = P.nc
    D = C["dram"]
    ident = C["ident"]
    NBLK = NT // 1024
    sb = lambda n, s, d=F32: P.sb(ph + n, s, d)
    K = lambda *a: (ph,) + a

    rw = sb("rw", [128, 8, E]); rb_bc = sb("rb", [128, E])
    bguT = sb("bguT", [128, 16, E]); bd = sb("bd", [E, 1024])
    g_bc = sb("g_bc", [128, 1024]); b_bc = sb("b_bc", [128, 1024])
    xt = [sb("xt%d" % i, [128, 1024]) for i in range(2)]
    hT = sb("hT", [128, 8, 1024], BF16)
    hTf = sb("hTf", [128, 8, 128])
    acc = sb("acc", [128, 8, 1024])
    wgu = [sb("wgu%d" % i, [128, 8, 2048], BF16) for i in range(2)]
    wd = [sb("wd%d" % i, [128, 8, 1024], BF16) for i in range(2)]
    actT = [sb("actT%d" % i, [128, 8, 512], BF16) for i in range(2)]
    gc = [sb("gc%d" % i, [128, 512]) for i in range(2)]
    ua = [sb("ua%d" % i, [128, 512]) for i in range(2)]
    sl = [sb("sl%d" % i, [128, 512]) for i in range(2)]
    cw = sb("cw", [128, 8, E]); cwT = sb("cwT", [E, 128]); cws = sb("cws", [128, 8, E])
    lg = sb("lg", [128, E]); ex = sb("ex", [128, E]); em = sb("em", [128, E])
    m8 = sb("m8", [128, 8]); sm = sb("sm", [128, 8])
    st = {n: sb("st_" + n, [128, 1]) for n in ("s1", "s2", "mean", "msq", "var", "rstd")}
    st["junk"] = sb("junk", [128, 1024], BF16)

    pA = [P.ps(ph + "pA%d" % i, [128, 512]) for i in range(2)]
    pB = [P.ps(ph + "pB%d" % i, [128, 512]) for i in range(2)]
    pY = [P.ps(ph + "pY%d" % i, [128, 512]) for i in range(2)]
    pT = [P.ps(ph + "pT%d" % i, [128, 4, 128]) for i in range(2)]

    import os
    SK = os.environ.get("SKIP", "")
    if "rw" not in SK:
        P.dma("sync", rw[:], D["router_w"][lay].rearrange("(k p) e -> p k e", p=128), writes=[K("rw")])
    if "rb" not in SK:
        P.dma("sync", rb_bc[:], D["router_b"][lay].partition_broadcast(128), writes=[K("rb")])
    bkeys = [K("acc", 0, 0), K("acc", 0, 1), K("acc", 1, 0), K("acc", 1, 1)]
    P.dma("sync", acc[0:E, 0:2, :], D["exp_b_gu"][lay].rearrange("e (a f) -> e a f", a=2), writes=bkeys, slot="bguraw")
    if "bd" not in SK:
        P.dma("sync", bd[:], D["exp_b_down"][lay], writes=[K("bd")])
    if "gb" not in SK:
        P.dma("sync", g_bc[:], D["ln_ffn_g"][lay].partition_broadcast(128), writes=[K("g_bc")])
    if "gb" not in SK:
        P.dma("sync", b_bc[:], D["ln_ffn_b"][lay].partition_broadcast(128), writes=[K("b_bc")])
    for j in range(16):
        h = j % 2
        P.op("tensor", lambda e, j=j, h=h: e.transpose(pT[h][:, 0, 0:E], acc[0:E, j // 8, (j % 8) * 128:(j % 8 + 1) * 128], ident[0:E, 0:E]),
             reads=bkeys + ["ident"], writes=[K("pT", h)])
        P.op("vector", lambda e, j=j, h=h: e.tensor_copy(bguT[:, j, :], pT[h][:, 0, 0:E]),
             reads=[K("pT", h)], writes=[K("bguT")])

    if stage == 0:
        return
    def load_w(b, e, which="both"):
        ws = (b * E + e) % 2
        if "now" in SK:
            return
        if which in ("both", "gu"):
            for kk in range(8):
                P.dma("gpsimd", wgu[ws][:, kk, :], D["exp_w_gu"][lay, e, kk * 128:(kk + 1) * 128, :], writes=[K("wgu", ws, kk)], slot=("wgu", ws))
        if which in ("both", "d"):
            for kk in range(8):
                P.dma("gpsimd", wd[ws][:, kk, :], D["exp_w_down"][lay, e, kk * 128:(kk + 1) * 128, :], writes=[K("wd", ws, kk)], slot=("wd", ws))

    for b in range(NBLK):
        load_w(b, 0)
        for i in range(8 if "noa" not in SK else 0):
            tok0 = b * 1024 + i * 128
            s = i % 2
            P.dma("sync", xt[s][:], h_in[tok0:tok0 + 128, :], writes=[K("xt", s)])
            for h in range(2):
                def tr(e, s=s, h=h):
                    for q in range(4):
                        kk = h * 4 + q
                        r = e.transpose(pT[h][:, q, :], xt[s][:, kk * 128:(kk + 1) * 128], ident[:])
                    return r
                P.op("tensor", tr, reads=[K("xt", s), "ident"], writes=[K("pT", h)])
                P.op(os.environ.get("EVE", "scalar"), lambda e, h=h, i=i: (e.activation(hT[:, 4 * h:4 * h + 4, i * 128:(i + 1) * 128], pT[h][:], AF.Identity) if os.environ.get("EVE", "scalar") == "scalar" else e.tensor_copy(hT[:, 4 * h:4 * h + 4, i * 128:(i + 1) * 128], pT[h][:])),
                     reads=[K("pT", h)], writes=[K("hT", i // 4)])
                P.op("vector", lambda e, h=h: e.tensor_copy(hTf[:, 4 * h:4 * h + 4, :], pT[h][:]),
                     reads=[K("pT", h), K("hT", i // 4)] , writes=[K("hTf", h)])
            CUT = int(os.environ.get("CUT", "99"))
            if CUT < 2:
                continue
            pL = pY[0]
            def rt(e):
                for kk in range(8):
                    r = e.matmul(pL[:, 0:E], hTf[:, kk, :], rw[:, kk, :], start=(kk == 0), stop=(kk == 7))
                return r
            P.op("tensor", rt, reads=[K("hTf", 0), K("hTf", 1), K("rw")], writes=[K("pY", 0)])
            P.op("vector", lambda e: e.tensor_tensor(lg[:], pL[:, 0:E], rb_bc[:], ALU.add), reads=[K("pY", 0), K("rb")], writes=[K("lg")])
            if CUT < 3:
                continue
            P.op("vector", lambda e: e.max(m8[:], lg[:]), reads=[K("lg")], writes=[K("m8")])
            P.op("vector", lambda e: e.tensor_scalar(sm[:, 0:1], m8[:, 0:1], -1.0, None, ALU.mult), reads=[K("m8")], writes=[K("negmx")])
            P.op("scalar", lambda e: e.activation(ex[:], lg[:], AF.Exp, bias=sm[:, 0:1], scale=1.0), reads=[K("lg"), K("negmx")], writes=[K("ex")])
            P.op("vector", lambda e: e.scalar_tensor_tensor(em[:], lg[:], m8[:, 3:4], ex[:], ALU.is_ge, ALU.mult),
                 reads=[K("lg"), K("m8"), K("ex")], writes=[K("em")])
            P.op("vector", lambda e: e.reduce_sum(sm[:, 1:2], em[:], axis=AX.X), reads=[K("em")], writes=[K("Z")])
            P.op("vector", lambda e: e.reciprocal(sm[:, 2:3], sm[:, 1:2]), reads=[K("Z")], writes=[K("rz")])
            P.op("vector", lambda e, i=i: e.tensor_scalar(cw[:, i, :], em[:], sm[:, 2:3], None, ALU.mult), reads=[K("em"), K("rz")], writes=[K("cw", i)])
            P.op("vector", lambda e, i=i: e.tensor_scalar(cws[:, i, :], cw[:, i, :], 1.0 / SW_ALPHA, None, ALU.mult), reads=[K("cw", i)], writes=[K("cws", i)])
            if CUT < 4:
                continue
            pC = pY[1]
            P.op("tensor", lambda e, i=i: e.transpose(pC[0:E, 0:128], cw[:, i, :], ident[:]), reads=[K("cw", i), "ident"], writes=[K("pY", 1)])
            P.op("scalar", lambda e: e.copy(cwT[:], pC[0:E, 0:128]), reads=[K("pY", 1)], writes=[K("cwT")])
            if CUT < 5:
                continue
            for n in range(2):
                P.op("tensor", lambda e, n=n: e.matmul(pA[n][:], cwT[:], bd[:, n * 512:(n + 1) * 512], start=True, stop=True),
                     reads=[K("cwT"), K("bd")], writes=[K("pA", n)])
                P.op("vector", lambda e, n=n, s=s, i=i: e.scalar_tensor_tensor(acc[:, i, n * 512:(n + 1) * 512], xt[s][:, n * 512:(n + 1) * 512],
                                                                      ALPHA, pA[n][:], ALU.mult, ALU.add),
                     reads=[K("xt", s), K("pA", n)], writes=[K("acc", i, n)])
        if stage == 1:
            return
        cntb = [0]
        pend = [None]

        def flush_act():
            if pend[0] is not None:
                p, i, a_s = pend[0]
                P.op("vector", lambda e, p=p, i=i, a_s=a_s: e.scalar_tensor_tensor(actT[a_s][:, i, :], ua[p][:], 1.0 - SW_LIMIT, sl[p][:], ALU.add, ALU.mult),
                     reads=[K("sl", p), K("ua", p)], writes=[K("actT", a_s)])
                pend[0] = None

        def gu_unit(ei, c, i):
            ws = (b * E + ei) % 2
            a_s = c % 2
            p = cntb[0] % 2
            cntb[0] += 1
            def mm_g(e, off, ps, i=i, c=c, ws=ws):
                for kk in range(8):
                    r = e.matmul(ps[:], wgu[ws][:, kk, off + i * 128: off + (i + 1) * 128], hT[:, kk, c * 512:(c + 1) * 512],
                                 start=(kk == 0), stop=(kk == 7))
                return r
            hk = [K("hT", c)]
            P.op("tensor", lambda e, p=p, f=mm_g: f(e, 0, pA[p]), reads=[K("wgu", ws, kk) for kk in range(8)] + hk, writes=[K("pA", p)])
            P.op("tensor", lambda e, p=p, f=mm_g: f(e, 1024, pB[p]), reads=[K("wgu", ws, kk) for kk in range(8)] + hk, writes=[K("pB", p)])
            P.op("vector", lambda e, p=p, i=i, ei=ei: e.tensor_scalar(gc[p][:], pA[p][:], bguT[:, i, ei:ei + 1], SW_LIMIT, ALU.add, ALU.min),
                 reads=[K("pA", p), K("bguT")], writes=[K("gc", p)])
            P.op("vector", lambda e, p=p, i=i, ei=ei: e.tensor_scalar(ua[p][:], pB[p][:], bguT[:, 8 + i, ei:ei + 1], SW_LIMIT, ALU.add, ALU.min),
                 reads=[K("pB", p), K("bguT")], writes=[K("ua", p)])
            P.op("scalar", lambda e, p=p: e.activation(sl[p][:], gc[p][:], AF.Silu, scale=SW_ALPHA),
                 reads=[K("gc", p)], writes=[K("sl", p)])
            P.op("scalar", lambda e, p=p: e.activation(ua[p][:], ua[p][:], AF.Relu, bias=SW_LIMIT),
                 reads=[K("ua", p)], writes=[K("ua", p)])
            flush_act()
            pend[0] = (p, i, a_s)

        def down_unit(ei, c, idx):
            ws = (b * E + ei) % 2
            a_s = c % 2
            j, n = idx // 2, idx % 2
            ti = c * 4 + j
            q = idx % 2
            def mm_d(e, j=j, n=n, q=q, a_s=a_s, ws=ws):
                for f in range(8):
                    r = e.matmul(pY[q][:], actT[a_s][:, f, j * 128:(j + 1) * 128], wd[ws][:, f, n * 512:(n + 1) * 512],
                                 start=(f == 0), stop=(f == 7))
                return r
            P.op("tensor", mm_d, reads=[K("actT", a_s)] + [K("wd", ws, kk) for kk in range(8)], writes=[K("pY", q)])
            P.op("vector", lambda e, q=q, ti=ti, n=n, ei=ei: e.scalar_tensor_tensor(
                acc[:, ti, n * 512:(n + 1) * 512], pY[q][:], cws[:, ti, ei:ei + 1], acc[:, ti, n * 512:(n + 1) * 512], ALU.mult, ALU.add),
                 reads=[K("pY", q), K("cws", ti), K("acc", ti, n)], writes=[K("acc", ti, n)])

        seq = [(ei, c) for ei in range(E) for c in range(2)]
        for k in range(len(seq) + 1):
            if k < len(seq):
                ei, c = seq[k]
                if c == 0 and ei + 1 < E:
                    load_w(b, ei + 1, "gu")
                if c == 1 and ei + 1 < E:
                    load_w(b, ei + 1, "d")
            for idx in range(8):
                if k < len(seq):
                    gu_unit(seq[k][0], seq[k][1], idx)
                else:
                    flush_act()
                if k >= 1:
                    if idx == 0:
                        pass
                    down_unit(seq[k - 1][0], seq[k - 1][1], idx)
            flush_act()
        if stage == 2:
            return
        for i in range(8):
            tok0 = b * 1024 + i * 128
            s = i % 2
            layer_norm_tile(P, ph, acc[:, i, :], [K("acc", i, 0), K("acc", i, 1)], acc[:, i, :], K("acc", i, 0), g_bc, b_bc, st, i)
            P.dma("sync", h_out[tok0:tok0 + 128, :], acc[:, i, :], reads=[K("acc", i, 0)], writes=[K("hout", b, i)], slot=("yo_st", s))


import math

RMS_EPS = 1e-5
NEGM = -30000.0


def load_w_cols(P, ph, wt, wkey, w_dram, c0, ncols, dst0=0):
    for kk in range(8):
        P.dma("gpsimd", wt[:, kk, dst0:dst0 + ncols], w_dram[kk * 128:(kk + 1) * 128, c0:c0 + ncols],
              writes=[(ph, wkey, kk)], slot=(wkey,))


def wkeys(ph, wkey, dsts=(0,)):
    return [(ph, wkey, kk) for kk in range(8)]


def load_xT(P, ph, C, h_rows, L, xT, xt, ps):
    ident = C["ident"]
    for i in range(L // 128):
        s = i % 2
        P.dma("sync", xt[s][:], h_rows[i * 128:(i + 1) * 128, :], writes=[(ph, "xt", s)])
        for h in range(2):
            pt = ps[h]
            def tr(e, s=s, h=h, pt=pt):
                for q in range(4):
                    kk = h * 4 + q
                    r = e.transpose(pt[:, q * 128:(q + 1) * 128], xt[s][:, kk * 128:(kk + 1) * 128], ident[:])
                return r
            P.op("tensor", tr, reads=[(ph, "xt", s), "ident"], writes=[(ph, "ps", h)])
            P.op("vector", lambda e, h=h, i=i, pt=pt: e.tensor_copy(xT[:, 4 * h:4 * h + 4, i * 128:(i + 1) * 128],
                                                                  pt[:].rearrange("p (a b) -> p a b", a=4)),
                 reads=[(ph, "ps", h)], writes=[(ph, "ps", h), (ph, "xT", i // 4)])


def proj_fm(P, ph, wt, wk, c0, M, xT, L, ps, psi, evac):
    for ch in range(L // 512):
        pi = psi[ch % len(psi)]
        def mm(e, ch=ch, pi=pi):
            for kk in range(8):
                r = e.matmul(ps[pi][0:M, :], wt[:, kk, c0:c0 + M], xT[:, kk, ch * 512:(ch + 1) * 512], start=(kk == 0), stop=(kk == 7))
            return r
        P.op("tensor", mm, reads=wk + [(ph, "xT", ch)], writes=[(ph, "ps", pi)])
        evac(ch, ps[pi][0:M, :], (ph, "ps", pi))


def proj_tm(P, ph, wt, wk, c0, N, xT, L, ps, psi, evac):
    for i in range(L // 128):
        pi = psi[i % len(psi)]
        def mm(e, i=i, pi=pi):
            for kk in range(8):
                r = e.matmul(ps[pi][:, 0:N], xT[:, kk, i * 128:(i + 1) * 128], wt[:, kk, c0:c0 + N], start=(kk == 0), stop=(kk == 7))
            return r
        P.op("tensor", mm, reads=wk + [(ph, "xT", i // 4)], writes=[(ph, "ps", pi)])
        evac(i, ps[pi][:, 0:N], (ph, "ps", pi))


def outproj_ln(P, ph, C, mix, L, w_out, h_rows, out_rows, g_vec, b_vec, T, ps):
    ident_b = C["ident_b"]
    wo, mT, xt, g_bc, b_bc, st, yo = T["wo"], T["mT"], T["xt"], T["g_bc"], T["b_bc"], T["st"], T["yo"]
    for half in range(2):
        load_w_cols(P, ph, wo[half], "wb%d" % half, w_out, half * 512, 512)
    P.dma("sync", g_bc, g_vec.partition_broadcast(128), writes=[(ph, "g_bc")] + T["gk"], slot=("g_bc",))
    P.dma("sync", b_bc, b_vec.partition_broadcast(128), writes=[(ph, "b_bc")] + T["bk"], slot=("b_bc",))
    pbf = [ps[6][:].bitcast(BF16), ps[7][:].bitcast(BF16)]
    P.barrier()
    for i in range(L // 128):
        s = i % 2
        P.dma("sync", xt[s][:], h_rows[i * 128:(i + 1) * 128, :], writes=[(ph, "xt", s)])
        def tr(e, i=i, s=s):
            for kk in range(8):
                r = e.transpose(pbf[s][:, kk * 128:(kk + 1) * 128], mix[:, i, kk * 128:(kk + 1) * 128], ident_b[:])
            return r
        P.op("tensor", tr, reads=[(ph, "mix", i), "ident_b"], writes=[(ph, "ps", 6 + s)])
        P.op("vector", lambda e, s=s: e.tensor_copy(mT[s], pbf[s]), reads=[(ph, "ps", 6 + s)], writes=[(ph, "ps", 6 + s), (ph, "mT", s)] + T["mTk"][s])
        for n in range(2):
            pi = 4 + n
            def mm(e, s=s, n=n, pi=pi):
                for kk in range(8):
                    r = e.matmul(ps[pi][:], mT[s][:, kk * 128:(kk + 1) * 128], wo[n][:, kk, :], start=(kk == 0), stop=(kk == 7))
                return r
            P.op("tensor", mm, reads=[(ph, "mT", s)] + T["wok"][n], writes=[(ph, "ps", pi)])
            P.op("vector", lambda e, s=s, n=n, pi=pi: e.scalar_tensor_tensor(yo[s][:, n * 512:(n + 1) * 512], xt[s][:, n * 512:(n + 1) * 512], ALPHA, ps[pi][:], ALU.mult, ALU.add),
                 reads=[(ph, "xt", s), (ph, "ps", pi)], writes=[(ph, "ps", pi), (ph, "yo", s, n)])
        layer_norm_tile(P, ph, yo[s], [(ph, "yo", s, 0), (ph, "yo", s, 1)], yo[s], (ph, "yo", s, 0), g_bc, b_bc, st, i)
        P.dma("sync", out_rows[i * 128:(i + 1) * 128, :], yo[s], reads=[(ph, "yo", s, 0)], writes=[(ph, "hout", i)], slot=("yo_st", s))
        P.res_w[(ph, "yo", s, 1)] = P.res_w[(ph, "yo", s, 0)]
        P.res_r[(ph, "yo", s, 1)] = dict(P.res_r[(ph, "yo", s, 0)])


def even_mixer_seq(P, ph, C, lay_j, h_rows, out_rows, L, lam_init, ln_g, ln_b):
    D = C["dram"]
    ident, ident_b = C["ident"], C["ident_b"]
    NT = L // 128
    NC = L // 512
    sb = lambda n, s, d=F32: P.sb(ph + n, s, d)
    K = lambda *a: (ph,) + a
    w_in = D["ev_w_in"][lay_j]
    ps = [P.ps(ph + "ps%d" % i, [128, 512]) for i in range(8)]

    xt = [sb("xt%d" % i, [128, 1024]) for i in range(2)]
    xT = sb("xT", [128, 8, L], BF16)
    wb = [sb("wb%d" % i, [128, 8, 512], BF16) for i in range(2)]
    va = sb("va", [128, NT, 512], BF16)
    vb = sb("vb", [128, NT, 4, 132], BF16)
    mix = sb("mix", [128, NT, 1024], BF16)
    qa = [sb("qa%d" % i, [128, L], BF16) for i in range(4)]
    ka = [sb("ka%d" % i, [128, L], BF16) for i in range(4)]
    qd = [sb("qd%d" % i, [68, L], BF16) for i in range(2)]
    kd = [sb("kd%d" % i, [68, L], BF16) for i in range(2)]
    mstrict = sb("mstrict", [128, 4, 512], BF16)
    mcausal = sb("mcausal", [128, 4, 512], BF16)
    uincl = sb("uincl", [128, 128], BF16)
    ones1 = sb("ones1", [1, 128], BF16)
    efg = sb("efg", [128, 4, 512])
    ef = [efg[:, i, :] for i in range(2)]
    eg = [efg[:, 2 + i, :] for i in range(2)]
    atsp = sb("atsp", [128, 4, 512], BF16)
    at = [atsp[:, i, :] for i in range(2)]
    spt = [atsp[:, 2 + i, :] for i in range(2)]
    suf = sb("suf", [1, 512], BF16)
    lamv = sb("lamv", [128, 4, 64]); lam = sb("lam", [128, 4])
    wsub = sb("wsub", [128, 128])
    o1 = sb("o1", [128, 128]); ob = sb("ob", [128, 128]); sm = sb("sm", [128, 8])
    junk = sb("junk", [128, 1024], BF16)

    P.dma("gpsimd", mstrict[:], D["c_mstrict"], writes=[K("mstrict")])
    P.dma("gpsimd", mcausal[:], D["c_mcausal"], writes=[K("mcausal")])
    P.dma("gpsimd", uincl[:], D["c_uincl"], writes=[K("uincl")])
    P.op("vector", lambda e: e.memset(ones1[:], 1.0), writes=[K("ones1")])
    for i, nm in enumerate(["ev_lambda_q1", "ev_lambda_k1", "ev_lambda_q2", "ev_lambda_k2"]):
        P.dma("sync", lamv[:, i, :], D[nm][lay_j].partition_broadcast(128), writes=[K("lamv", i)])
    P.dma("sync", wsub[:], D["ev_subln_w"][lay_j].partition_broadcast(128), writes=[K("wsub")])
    P.op("vector", lambda e: e.tensor_scalar(wsub[:], wsub[:], 1.0 - lam_init, None, ALU.mult), reads=[K("wsub")], writes=[K("wsub")])
    for j in range(2):
        P.op("vector", lambda e, j=j: e.tensor_tensor(lamv[:, 2 * j, :], lamv[:, 2 * j, :], lamv[:, 2 * j + 1, :], ALU.mult),
             reads=[K("lamv", 2 * j), K("lamv", 2 * j + 1)], writes=[K("lamv", 2 * j)])
        P.op("vector", lambda e, j=j: e.reduce_sum(lam[:, j:j + 1], lamv[:, 2 * j, :], axis=AX.X), reads=[K("lamv", 2 * j)], writes=[K("lam", j)])
        P.op("scalar", lambda e, j=j: e.activation(lam[:, j:j + 1], lam[:, j:j + 1], AF.Exp), reads=[K("lam", j)], writes=[K("lam", j)])
    P.op("vector", lambda e: e.tensor_tensor(lam[:, 2:3], lam[:, 1:2], lam[:, 0:1], ALU.subtract), reads=[K("lam", 0), K("lam", 1)], writes=[K("lam", 2)])
    P.op("vector", lambda e: e.tensor_scalar(lam[:, 3:4], lam[:, 2:3], -lam_init, None, ALU.add), reads=[K("lam", 2)], writes=[K("nlam")])

    load_xT(P, ph, C, h_rows, L, xT, xt, ps)

    wslot = [0]
    def next_w(c0, ncols=512):
        s = wslot[0] % 2
        wslot[0] += 1
        load_w_cols(P, ph, wb[s], "wb%d" % s, w_in, c0, ncols)
        return wb[s], wkeys(ph, "wb%d" % s)

    wt, wk = next_w(1024)
    proj_tm(P, ph, wt, wk, 0, 512, xT, L, ps, [2, 3],
            lambda i, pa, pk: P.op("vector", lambda e: e.tensor_copy(va[:, i, :], pa), reads=[pk], writes=[pk, K("va", i)]))
    wt, wk = next_w(2560)
    P.op("vector", lambda e: e.memset(vb[:], 1.0), writes=[K("vb", i) for i in range(NT)])
    proj_tm(P, ph, wt, wk, 0, 512, xT, L, ps, [2, 3],
            lambda i, pa, pk: P.op("vector", lambda e: e.tensor_copy(vb[:, i, :, 0:128], pa.rearrange("p (h d) -> p h d", h=4)), reads=[pk], writes=[pk, K("vb", i)]))

    wt, wk = next_w(0)
    for j in range(4):
        proj_fm(P, ph, wt, wk, j * 128, 128, xT, L, ps, [2, 3],
                lambda ch, pa, pk, j=j: P.op("scalar", lambda e: e.activation(qa[j][:, ch * 512:(ch + 1) * 512], pa, AF.Identity, scale=0.125),
                                            reads=[pk], writes=[pk, K("qa", j)]))
    wt, wk = next_w(512)
    for j in range(4):
        proj_fm(P, ph, wt, wk, j * 128, 128, xT, L, ps, [2, 3],
                lambda ch, pa, pk, j=j: P.op("vector", lambda e: e.tensor_copy(ka[j][:, ch * 512:(ch + 1) * 512], pa),
                                            reads=[pk], writes=[pk, K("ka", j)]))

    it = 0
    for h in range(8):
        qT = qa[h // 2][(h % 2) * 64:(h % 2) * 64 + 64, :]
        kT = ka[h // 2][(h % 2) * 64:(h % 2) * 64 + 64, :]
        qk = [K("qa", h // 2), K("ka", h // 2)]
        for c in range(NC):
            po = ps[4 + (h * NC + c) % 2]
            pok = K("ps", 4 + (h * NC + c) % 2)
            nk = 4 * c + 4
            for kt in range(nk - 1, -1, -1):
                j = kt - 4 * c
                q0 = max(j, 0) * 128
                b = it % 2
                it += 1
                pz, pzk = ps[b], K("ps", b)
                pg, pgk = ps[2 + b], K("ps", 2 + b)
                first = False
                if kt == nk - 1:
                    P.op("vector", lambda e: e.memset(suf[:], 0.0), writes=[K("suf")])
                P.op("tensor", lambda e, pz=pz, kt=kt, c=c, q0=q0, kT=kT, qT=qT: e.matmul(
                    pz[:, q0:512], kT[:, kt * 128:(kt + 1) * 128], qT[:, c * 512 + q0:(c + 1) * 512], start=True, stop=True),
                     reads=qk, writes=[pzk])
                P.op("scalar", lambda e, b=b, pz=pz, q0=q0: e.activation(ef[b][:, q0:512], pz[:, q0:512], AF.Exp), reads=[pzk], writes=[pzk, K("ef", b)])
                P.op("scalar", lambda e, b=b, q0=q0: e.activation(spt[b][:, q0:512], ef[b][:, q0:512], AF.Ln, bias=1.0), reads=[K("ef", b)], writes=[K("sp", b)])
                if j >= 0:
                    P.op("vector", lambda e, b=b, j=j, q0=q0: e.tensor_tensor(spt[b][:, q0:512], spt[b][:, q0:512], mstrict[:, j, q0:512], ALU.mult),
                         reads=[K("sp", b), K("mstrict")], writes=[K("sp", b)])
                def mg(e, b=b, pg=pg, q0=q0, first=first):
                    r = e.matmul(pg[:, q0:512], uincl[:], spt[b][:, q0:512], start=True, stop=first)
                    if not first:
                        r = e.matmul(pg[:, q0:512], ones1[:], suf[:, q0:512], start=False, stop=True)
                    return r
                P.op("tensor", mg, reads=[K("sp", b), K("uincl"), K("ones1"), K("suf")], writes=[pgk])
                P.op("scalar", lambda e, b=b, pg=pg, q0=q0: e.activation(eg[b][:, q0:512], pg[:, q0:512], AF.Exp, scale=-1.0), reads=[pgk], writes=[pgk, K("eg", b)])
                if kt > 0:
                    P.op("vector", lambda e, pg=pg, q0=q0: e.tensor_copy(suf[:, q0:512], pg[0:1, q0:512]), reads=[pgk], writes=[pgk, K("suf")])
                P.op("vector", lambda e, b=b, q0=q0: e.tensor_tensor(at[b][:, q0:512], ef[b][:, q0:512], eg[b][:, q0:512], ALU.mult),
                     reads=[K("ef", b), K("eg", b)], writes=[K("at", b)])
                if j >= 0:
                    P.op("vector", lambda e, b=b, j=j, q0=q0: e.tensor_tensor(at[b][:, q0:512], at[b][:, q0:512], mstrict[:, j, q0:512], ALU.mult),
                         reads=[K("at", b), K("mstrict")], writes=[K("at", b)])
                def pv(e, b=b, kt=kt, c=c, j=j, h=h, po=po):
                    r = None
                    for qs in range(3, max(j, 0) - 1, -1):
                        qt = 4 * c + qs
                        r = e.matmul(po[:, qs * 64:(qs + 1) * 64], at[b][:, qs * 128:(qs + 1) * 128], va[:, kt, h * 64:(h + 1) * 64],
                                     start=(kt == 4 * c + 3 and qs == 3), stop=(kt == 0 and qs == 0))
                    return r
                P.op("tensor", pv, reads=[K("at", b), K("va", kt)], writes=[pok])
            P.op("vector", lambda e, po=po, c=c, h=h: e.tensor_copy(mix[:, 4 * c:4 * c + 4, h * 64:(h + 1) * 64], po[:, 0:256].rearrange("p (a d) -> p a d", a=4)),
                 reads=[pok], writes=[pok] + [K("mix", 4 * c + qs) for qs in range(4)])

    for h in range(4):
        for m in range(2):
            wt, wk = next_w(1536 + (h * 2 + m) * 64, 64)
            proj_fm(P, ph, wt, wk, 0, 64, xT, L, ps, [2, 3],
                    lambda ch, pa, pk, m=m: P.op("scalar", lambda e: e.activation(qd[m][0:64, ch * 512:(ch + 1) * 512], pa, AF.Identity, scale=0.125),
                                                reads=[pk], writes=[pk, K("qd", m)]))
            wt, wk = next_w(2048 + (h * 2 + m) * 64, 64)
            proj_fm(P, ph, wt, wk, 0, 64, xT, L, ps, [2, 3],
                    lambda ch, pa, pk, m=m: P.op("vector", lambda e: e.tensor_copy(kd[m][0:64, ch * 512:(ch + 1) * 512], pa),
                                                reads=[pk], writes=[pk, K("kd", m)]))
            P.dma("gpsimd", qd[m][64:68, :], D["c_qaug4"][h][:, 0:L], writes=[K("qd", m)], slot=("qaug", m))
            P.dma("gpsimd", kd[m][64:68, :], D["c_kaug"][:, 0:L], writes=[K("kd", m)], slot=("kaug", m))
        for c in range(NC):
            nk = 4 * c + 4
            for m in range(2):
                pos = [ps[4 + 2 * m], ps[5 + 2 * m]]
                for kt in range(nk - 1, -1, -1):
                    j = kt - 4 * c
                    q0 = max(j, 0) * 128
                    b = it % 2
                    it += 1
                    pz, pzk = ps[b], K("ps", b)
                    P.op("tensor", lambda e, pz=pz, kt=kt, c=c, q0=q0, m=m: e.matmul(
                        pz[:, q0:512], kd[m][:, kt * 128:(kt + 1) * 128], qd[m][:, c * 512 + q0:(c + 1) * 512], start=True, stop=True),
                         reads=[K("qd", m), K("kd", m)], writes=[pzk])
                    P.op("scalar", lambda e, b=b, pz=pz, q0=q0: e.activation(at[b][:, q0:512], pz[:, q0:512], AF.Exp), reads=[pzk], writes=[pzk, K("at", b)])
                    if j >= 0:
                        P.op("vector", lambda e, b=b, j=j, q0=q0: e.tensor_tensor(at[b][:, q0:512], at[b][:, q0:512], mcausal[:, j, q0:512], ALU.mult),
                             reads=[K("at", b), K("mcausal")], writes=[K("at", b)])
                    def pv(e, b=b, kt=kt, c=c, j=j, h=h, pos=pos):
                        r = None
                        for qs in range(3, max(j, 0) - 1, -1):
                            qt = 4 * c + qs
                            r = e.matmul(pos[qs // 2][:, (qs % 2) * 256:(qs % 2) * 256 + 129], at[b][:, qs * 128:(qs + 1) * 128], vb[:, kt, h, 0:129],
                                         start=(kt == qt and qs % 2 == 1), stop=(kt == 0 and qs % 2 == 0))
                        return r
                    P.op("tensor", pv, reads=[K("at", b), K("vb", kt)], writes=[K("ps", 4 + 2 * m), K("ps", 5 + 2 * m)])
            for qs in range(4):
                qt = 4 * c + qs
                p1 = ps[4 + qs // 2][:, (qs % 2) * 256:(qs % 2) * 256 + 129]
                p2 = ps[6 + qs // 2][:, (qs % 2) * 256:(qs % 2) * 256 + 129]
                k1, k2 = K("ps", 4 + qs // 2), K("ps", 6 + qs // 2)
                P.op("vector", lambda e, p1=p1: e.reciprocal(sm[:, 0:1], p1[:, 128:129]), reads=[k1], writes=[k1, K("sm0")])
                P.op("vector", lambda e, p2=p2: e.reciprocal(sm[:, 1:2], p2[:, 128:129]), reads=[k2], writes=[k2, K("sm1")])
                P.op("vector", lambda e: e.tensor_tensor(sm[:, 1:2], sm[:, 1:2], lam[:, 3:4], ALU.mult), reads=[K("sm1"), K("nlam")], writes=[K("sm1")])
                P.op("vector", lambda e, p1=p1: e.tensor_scalar(o1[:], p1[:, 0:128], sm[:, 0:1], None, ALU.mult), reads=[k1, K("sm0")], writes=[k1, K("o1")])
                P.op("vector", lambda e, p2=p2: e.scalar_tensor_tensor(ob[:], p2[:, 0:128], sm[:, 1:2], o1[:], ALU.mult, ALU.add),
                     reads=[k2, K("sm1"), K("o1")], writes=[k2, K("ob")])
                P.op("vector", lambda e: e.memset(sm[:, 2:3], 0.0), writes=[K("sm2")])
                P.op("scalar", lambda e: e.activation(junk[:, 0:128], ob[:], AF.Square, accum_out=sm[:, 2:3]), reads=[K("ob"), K("sm2")], writes=[K("sm2"), K("junk")])
                P.op("vector", lambda e: e.tensor_scalar(sm[:, 2:3], sm[:, 2:3], 1.0 / 128, RMS_EPS, ALU.mult, ALU.add), reads=[K("sm2")], writes=[K("sm2")])
                P.op("scalar", lambda e: e.sqrt(sm[:, 2:3], sm[:, 2:3]), reads=[K("sm2")], writes=[K("sm2")])
                P.op("vector", lambda e: e.reciprocal(sm[:, 3:4], sm[:, 2:3]), reads=[K("sm2")], writes=[K("sm3")])
                P.op("vector", lambda e, qt=qt, h=h: e.scalar_tensor_tensor(mix[:, qt, 512 + h * 128:512 + (h + 1) * 128], ob[:], sm[:, 3:4], wsub[:], ALU.mult, ALU.mult),
                     reads=[K("ob"), K("sm3"), K("wsub")], writes=[K("mix", qt)])

    if "dbg" in D:
        P.dma("gpsimd", D["dbg"].rearrange("(t p) c -> p t c", p=128), mix[:], reads=[K("mix", i) for i in range(NT)], writes=[K("dbg")])
    xTf = xT[:].rearrange("p a b -> p (a b)").bitcast(F32)
    T = dict(wo=wb, mT=[atsp[:, 0:2, :].rearrange("p a b -> p (a b)"), atsp[:, 2:4, :].rearrange("p a b -> p (a b)")],
             mTk=[[K("at", 0), K("at", 1)], [K("sp", 0), K("sp", 1)]],
             xt=xt, g_bc=efg[:, 0:2, :].rearrange("p a b -> p (a b)"), b_bc=efg[:, 2:4, :].rearrange("p a b -> p (a b)"),
             gk=[K("ef", 0), K("ef", 1)], bk=[K("eg", 0), K("eg", 1)],
             st={n: sb("st_" + n, [128, 1]) for n in ("s1", "s2", "mean", "msq", "var", "rstd")},
             yo=[xTf[:, 0:1024], xTf[:, 1024:2048]], wok=[wkeys(ph, "wb0"), wkeys(ph, "wb1")])
    T["st"]["junk"] = junk
    outproj_ln(P, ph, C, mix, L, D["ev_w_out"][lay_j], h_rows, out_rows, ln_g, ln_b, T, ps)


def odd_mixer_seq(P, ph, C, lay_j, h_rows, out_rows, L, ln_g, ln_b):
    D = C["dram"]
    ident, ident_b = C["ident"], C["ident_b"]
    NT = L // 128
    NC = L // 512
    KSEL = min(256, L // 4)
    sb = lambda n, s, d=F32: P.sb(ph + n, s, d)
    K = lambda *a: (ph,) + a
    w_in = D["od_w_in"][lay_j]
    ps = [P.ps(ph + "ps%d" % i, [128, 512]) for i in range(8)]
    pbf = ps[6][:].bitcast(BF16)

    xt = [sb("xt%d" % i, [128, 1024]) for i in range(2)]
    xT = sb("xT", [128, 8, L], BF16)
    wb = [sb("wb%d" % i, [128, 8, 512], BF16) for i in range(2)]
    mix = sb("mix", [128, NT, 1024], BF16)
    qTc = sb("qTc", [128, 16, 512], BF16)
    ckvT = sb("ckvT", [128, L], BF16)
    ckva = sb("ckva", [128, NT, 132], BF16)
    qiT = [sb("qiT%d" % i, [128, L], BF16) for i in range(4)]
    kiT2 = sb("kiT2", [128, L], BF16)
    widx = sb("widx", [128, NT, 8])
    score = sb("score", [128, L]); wkt = sb("wkt", [128, L])
    MB = [sb("MB%d" % i, [128, L], BF16) for i in range(4)]
    rl = [sb("rl%d" % i, [128, 512]) for i in range(2)]
    efg = sb("efg", [128, 4, 512])
    atsp = sb("atsp", [128, 4, 512], BF16)
    at = [atsp[:, i, :] for i in range(2)]
    kaug = sb("kaug", [9, L], BF16)
    qaugc = sb("qaugc", [9, 16, 512], BF16)
    wuv = sb("wuv", [128, 16, 64], BF16)
    kvw = sb("kvw", [128, 128])
    oh = sb("oh", [128, 128], BF16); ohT = sb("ohT", [128, 128], BF16)
    m8 = sb("m8", [128, 8]); sm = sb("sm", [128, 8])
    junk = sb("junk", [128, 1024], BF16)

    P.dma("gpsimd", kaug[:], D["c_kaug9"][:, 0:L], writes=[K("kaug")])
    P.dma("gpsimd", wuv[:], D["od_w_uv"][lay_j].rearrange("h c d -> c h d"), writes=[K("wuv")])
    P.dma("sync", kvw[:], D["od_kv_norm_w"][lay_j].partition_broadcast(128), writes=[K("kvw")])
    P.op("vector", lambda e: e.memset(ckva[:], 1.0), writes=[K("ckva", i) for i in range(NT)])

    load_xT(P, ph, C, h_rows, L, xT, xt, ps)
    wslot = [0]
    def next_w(c0, ncols=512, dst0=0, new=True):
        if new:
            wslot[0] += 1
        s = wslot[0] % 2
        load_w_cols(P, ph, wb[s], "wb%d" % s, w_in, c0, ncols, dst0=dst0)
        return wb[s], wkeys(ph, "wb%d" % s, (dst0,))

    wt, wk = next_w(2048, 128)
    def ev_ckv(i, pa, pk):
        P.op("vector", lambda e: e.memset(sm[:, 0:1], 0.0), writes=[K("sm0")])
        P.op("scalar", lambda e: e.activation(junk[:, 0:128], pa, AF.Square, accum_out=sm[:, 0:1]), reads=[pk, K("sm0")], writes=[pk, K("sm0"), K("junk")])
        P.op("vector", lambda e: e.tensor_scalar(sm[:, 0:1], sm[:, 0:1], 1.0 / 128, RMS_EPS, ALU.mult, ALU.add), reads=[K("sm0")], writes=[K("sm0")])
        P.op("scalar", lambda e: e.sqrt(sm[:, 0:1], sm[:, 0:1]), reads=[K("sm0")], writes=[K("sm0")])
        P.op("vector", lambda e: e.reciprocal(sm[:, 1:2], sm[:, 0:1]), reads=[K("sm0")], writes=[K("sm1")])
        P.op("vector", lambda e: e.scalar_tensor_tensor(ckva[:, i, 0:128], pa, sm[:, 1:2], kvw[:], ALU.mult, ALU.mult),
             reads=[pk, K("sm1"), K("kvw")], writes=[pk, K("ckva", i)])
        P.op("tensor", lambda e: e.transpose(pbf[:, 0:128], ckva[:, i, 0:128], ident_b[:]), reads=[K("ckva", i), "ident_b"], writes=[K("ps", 6)])
        P.op("vector", lambda e: e.tensor_copy(ckvT[:, i * 128:(i + 1) * 128], pbf[:, 0:128]), reads=[K("ps", 6)], writes=[K("ps", 6), K("ckvT")])
    proj_tm(P, ph, wt, wk, 0, 128, xT, L, ps, [2, 3], ev_ckv)
    wt, wk = next_w(2176, 512)
    for j in range(4):
        proj_fm(P, ph, wt, wk, j * 128, 128, xT, L, ps, [2, 3],
                lambda ch, pa, pk, j=j: P.op("scalar", lambda e: e.activation(qiT[j][:, ch * 512:(ch + 1) * 512], pa, AF.Identity, scale=0.125),
                                            reads=[pk], writes=[pk, K("qiT")]))
    wt, wk0 = next_w(2688, 64, dst0=0)
    _, wk1 = next_w(2688, 64, dst0=64, new=False)
    _, wk2 = next_w(2752, 8, dst0=128, new=False)
    proj_fm(P, ph, wt, wk0 + wk1, 0, 128, xT, L, ps, [2, 3],
            lambda ch, pa, pk: P.op("vector", lambda e: e.tensor_copy(kiT2[:, ch * 512:(ch + 1) * 512], pa), reads=[pk], writes=[pk, K("kiT2")]))
    proj_tm(P, ph, wt, wk2, 128, 8, xT, L, ps, [2, 3],
            lambda i, pa, pk: P.op("vector", lambda e: e.tensor_scalar(widx[:, i, :], pa, 8 ** -0.5, None, ALU.mult), reads=[pk], writes=[pk, K("widx")]))

    if "dbg2" in D and C.get("dumpc", 0) == -1:
        P.dma("gpsimd", D["dbg2"][:, 0:L], kiT2[:, 0:L], reads=[K("kiT2")], writes=[K("dbg2")])
        P.dma("gpsimd", D["dbg3"][:, 0:L], qiT[0][:, 0:L], reads=[K("qiT")], writes=[K("dbg3")])
        P.dma("gpsimd", D["dbg"][0:128, 0:NT * 8], widx[:].rearrange("p a b -> p (a b)"), reads=[K("widx")], writes=[K("dbgw")])
    def dump_w(stage):
        if "dbg2" in D and C.get("dumpat", None) == stage:
            P.dma("gpsimd", D["dbg"][0:128, 0:NT * 8], widx[:].rearrange("p a b -> p (a b)"), reads=[K("widx")], writes=[K("dbgw")])
    it = 0
    for c in range(NC):
        for g in range(4):
            if c == 0:
                dump_w(10 + g)
            wt, wk = next_w(g * 512, 512)
            if c == 0:
                dump_w(20 + g)
            for hh in range(4):
                h = g * 4 + hh
                pi = 2 + h % 2
                def mm(e, hh=hh, pi=pi, wt=wt, c=c):
                    for kk in range(8):
                        r = e.matmul(ps[pi][:], wt[:, kk, hh * 128:(hh + 1) * 128], xT[:, kk, c * 512:(c + 1) * 512], start=(kk == 0), stop=(kk == 7))
                    return r
                P.op("tensor", mm, reads=wk + [K("xT", c)], writes=[K("ps", pi)])
                P.op("scalar", lambda e, h=h, pi=pi: e.activation(qTc[:, h, :], ps[pi][:], AF.Identity, scale=128 ** -0.5),
                     reads=[K("ps", pi)], writes=[K("ps", pi), K("qTc", h)])
        if c == 0:
            dump_w(1)
        P.dma("gpsimd", qaugc[:], D["c_qaug16"][:, :, c * 512:(c + 1) * 512].rearrange("h r l -> r h l"), writes=[K("qaugc")])
        if c == 0:
            P.op("vector", lambda e: e.engine_nop() if False else e.memset(sm[:, 7:8], 0.0), reads=[K("qaugc")], writes=[K("sm7")])
            if C.get("dumpat", None) == 2:
                P.dma("gpsimd", D["dbg"][0:128, 0:NT * 8], widx[:].rearrange("p a b -> p (a b)"), reads=[K("widx"), K("sm7")], writes=[K("dbgw")])
        for qs in range(4):
            qt = 4 * c + qs
            n_s = (qt + 1) * 128
            for sc in range((n_s + 511) // 512):
                w = min(512, n_s - sc * 512)
                for ih in range(8):
                    b = it % 2
                    it += 1
                    pi = 2 + b
                    P.op("tensor", lambda e, pi=pi, ih=ih, qt=qt, sc=sc, w=w: e.matmul(
                        ps[pi][:, 0:w], qiT[ih // 2][(ih % 2) * 64:(ih % 2) * 64 + 64, qt * 128:(qt + 1) * 128],
                        kiT2[(ih % 2) * 64:(ih % 2) * 64 + 64, sc * 512:sc * 512 + w], start=True, stop=True),
                         reads=[K("qiT"), K("kiT2")], writes=[K("ps", pi)])
                    P.op("scalar", lambda e, pi=pi, b=b, w=w: e.activation(rl[b][:, 0:w], ps[pi][:, 0:w], AF.Relu), reads=[K("ps", pi)], writes=[K("ps", pi), K("rl", b)])
                    if ih == 0:
                        P.op("vector", lambda e, b=b, w=w, sc=sc, qt=qt, ih=ih: e.tensor_scalar(score[:, sc * 512:sc * 512 + w], rl[b][:, 0:w], widx[:, qt, ih:ih + 1], None, ALU.mult),
                             reads=[K("rl", b), K("widx")], writes=[K("score")])
                    else:
                        P.op("vector", lambda e, b=b, w=w, sc=sc, qt=qt, ih=ih: e.scalar_tensor_tensor(score[:, sc * 512:sc * 512 + w], rl[b][:, 0:w], widx[:, qt, ih:ih + 1],
                                                                                                  score[:, sc * 512:sc * 512 + w], ALU.mult, ALU.add),
                             reads=[K("rl", b), K("widx"), K("score")], writes=[K("score")])
            P.op("gpsimd", lambda e, qt=qt: e.affine_select(score[:, qt * 128:(qt + 1) * 128], score[:, qt * 128:(qt + 1) * 128], [[-1, 128]], ALU.is_ge, -1e30,
                                                         base=0, channel_multiplier=1), reads=[K("score")], writes=[K("score")])
            if c == 0 and qs == 0:
                dump_w(3)
            if c == 0 and qs == 2:
                dump_w(4)
            if qt * 128 >= KSEL:
                R = KSEL // 8
                for r in range(R):
                    src = score if r == 0 else wkt
                    P.op("vector", lambda e, src=src, n_s=n_s: e.max(m8[:], src[:, 0:n_s]), reads=[K("score"), K("wkt")], writes=[K("m8")])
                    if r < R - 1:
                        P.op("vector", lambda e, src=src, n_s=n_s: e.match_replace(wkt[:, 0:n_s], m8[:], src[:, 0:n_s], -1e30),
                             reads=[K("score"), K("wkt"), K("m8")], writes=[K("wkt")])
                P.op("vector", lambda e, qs=qs, n_s=n_s: e.tensor_scalar(MB[qs][:, 0:n_s], score[:, 0:n_s], m8[:, 7:8], NEGM, ALU.is_lt, ALU.mult),
                     reads=[K("score"), K("m8")], writes=[K("MB", qs)])
            else:
                P.op("vector", lambda e, qs=qs, n_s=n_s: e.tensor_scalar(MB[qs][:, 0:n_s], score[:, 0:n_s], -1e29, NEGM, ALU.is_lt, ALU.mult),
                     reads=[K("score")], writes=[K("MB", qs)])
        if "dbg2" in D and C.get("dumpc", 0) == -2 and c == 1:
            P.dma("gpsimd", D["dbg2"][:, 0:L], kiT2[:, 0:L], reads=[K("kiT2")], writes=[K("dbg2")])
            P.dma("gpsimd", D["dbg3"][:, 0:L], qiT[0][:, 0:L], reads=[K("qiT")], writes=[K("dbg3")])
            P.dma("gpsimd", D["dbg"][0:128, 0:NT * 8], widx[:].rearrange("p a b -> p (a b)"), reads=[K("widx")], writes=[K("dbgw")])
        if "dbg2" in D and c == C.get("dumpc", 0):
            P.dma("gpsimd", D["dbg2"][:, 0:L], MB[3][:, 0:L], reads=[K("MB", 3)], writes=[K("dbg2")])
            P.dma("sync", D["dbg3"][:, 0:L], score[:, 0:L], reads=[K("score")], writes=[K("dbg3")])
        if c == 0:
            dump_w(5)
        nk = 4 * c + 4
        for h in range(16):
            pos = [ps[4], ps[5]]
            for kt in range(nk - 1, -1, -1):
                j = kt - 4 * c
                jm = max(j, 0)
                q0 = jm * 128
                b = it % 2
                it += 1
                pz, pzk = ps[b], K("ps", b)
                def sc_mm(e, pz=pz, kt=kt, q0=q0, jm=jm, h=h):
                    e.matmul(pz[:, q0:512], ckvT[:, kt * 128:(kt + 1) * 128], qTc[:, h, q0:512], start=True, stop=False)
                    r = e.matmul(pz[:, q0:512], kaug[:, kt * 128:(kt + 1) * 128], qaugc[:, h, q0:512], start=False, stop=False)
                    for qs in range(jm, 4):
                        r = e.matmul(pz[:, qs * 128:(qs + 1) * 128], MB[qs][:, kt * 128:(kt + 1) * 128], ident_b[:], start=False, stop=(qs == 3))
                    return r
                P.op("tensor", sc_mm, reads=[K("ckvT"), K("qTc", h), K("kaug"), K("qaugc"), "ident_b"] + [K("MB", q) for q in range(jm, 4)], writes=[pzk])
                P.op("scalar", lambda e, b=b, pz=pz, q0=q0: e.activation(at[b][:, q0:512], pz[:, q0:512], AF.Exp), reads=[pzk], writes=[pzk, K("at", b)])
                def pv(e, b=b, kt=kt, c=c, jm=jm):
                    r = None
                    for qs in range(3, jm - 1, -1):
                        qt = 4 * c + qs
                        r = e.matmul(pos[qs // 2][:, (qs % 2) * 256:(qs % 2) * 256 + 129], at[b][:, qs * 128:(qs + 1) * 128], ckva[:, kt, 0:129],
                                     start=(kt == qt and qs % 2 == 1), stop=(kt == 0 and qs % 2 == 0))
                    return r
                P.op("tensor", pv, reads=[K("at", b), K("ckva", kt)], writes=[K("ps", 4), K("ps", 5)])
            for qs in range(4):
                qt = 4 * c + qs
                p1 = ps[4 + qs // 2][:, (qs % 2) * 256:(qs % 2) * 256 + 129]
                k1 = K("ps", 4 + qs // 2)
                P.op("vector", lambda e, p1=p1: e.reciprocal(sm[:, 2:3], p1[:, 128:129]), reads=[k1], writes=[k1, K("sm2")])
                P.op("vector", lambda e, p1=p1: e.tensor_scalar(oh[:], p1[:, 0:128], sm[:, 2:3], None, ALU.mult), reads=[k1, K("sm2")], writes=[k1, K("oh")])
                P.op("tensor", lambda e: e.transpose(pbf[:, 0:128], oh[:], ident_b[:]), reads=[K("oh"), "ident_b"], writes=[K("ps", 6)])
                P.op("vector", lambda e: e.tensor_copy(ohT[:], pbf[:, 0:128]), reads=[K("ps", 6)], writes=[K("ps", 6), K("ohT")])
                P.op("tensor", lambda e, h=h: e.matmul(ps[7][:, 0:64], ohT[:], wuv[:, h, :], start=True, stop=True), reads=[K("ohT"), K("wuv")], writes=[K("ps", 7)])
                P.op("scalar", lambda e, qt=qt, h=h: e.activation(mix[:, qt, h * 64:(h + 1) * 64], ps[7][:, 0:64], AF.Identity),
                     reads=[K("ps", 7)], writes=[K("ps", 7), K("mix", qt)])

    if "dbg" in D and C.get("dumpc", 0) >= 0 and C.get("dumpat", None) is None:
        P.dma("gpsimd", D["dbg"].rearrange("(t p) c -> p t c", p=128), mix[:], reads=[K("mix", i) for i in range(NT)], writes=[K("dbg")])
    xTf = xT[:].rearrange("p a b -> p (a b)").bitcast(F32)
    T = dict(wo=wb, mT=[atsp[:, 0:2, :].rearrange("p a b -> p (a b)"), atsp[:, 2:4, :].rearrange("p a b -> p (a b)")],
             mTk=[[K("at", 0), K("at", 1)], [K("sp", 0), K("sp", 1)]],
             xt=xt, g_bc=efg[:, 0:2, :].rearrange("p a b -> p (a b)"), b_bc=efg[:, 2:4, :].rearrange("p a b -> p (a b)"),
             gk=[K("ef", 0), K("ef", 1)], bk=[K("eg", 0), K("eg", 1)],
             st={n: sb("st_" + n, [128, 1]) for n in ("s1", "s2", "mean", "msq", "var", "rstd")},
             yo=[xTf[:, 0:1024], xTf[:, 1024:2048]], wok=[wkeys(ph, "wb0"), wkeys(ph, "wb1")])
    T["st"]["junk"] = junk
    outproj_ln(P, ph, C, mix, L, D["od_w_out"][lay_j], h_rows, out_rows, ln_g, ln_b, T, ps)


def make_consts(L=2048):
    s = np.arange(128)[:, None]; q = np.arange(512)[None, :]
    mstrict = np.stack([((q - j * 128) > s) for j in range(4)], axis=1).astype(np.float32)
    mcausal = np.stack([((q - j * 128) >= s) for j in range(4)], axis=1).astype(np.float32)
    jj = np.arange(128)[:, None]; ss = np.arange(128)[None, :]
    uincl = (jj >= ss).astype(np.float32)
    t = np.arange(L)
    hi = (t // 256) * 256; lo = t % 256
    def aug(slopes):
        qa = np.stack([np.stack([np.full(L, c), np.full(L, c), -c * hi, -c * lo]) for c in slopes]).astype(np.float32)
        return qa
    sl4 = 2.0 ** (-8.0 * np.arange(1, 5) / 4)
    sl16 = 2.0 ** (-8.0 * np.arange(1, 17) / 16)
    kaug = np.stack([hi, lo, np.ones(L), np.ones(L)]).astype(np.float32)
    import ml_dtypes
    def bsplit(v):
        v = np.asarray(v, dtype=np.float64); outp = []
        for _ in range(3):
            p = v.astype(ml_dtypes.bfloat16).astype(np.float64); outp.append(p); v = v - p
        return outp
    q9 = []
    for c in sl16:
        c1, c2, c3 = bsplit(np.full(L, c)); v1, v2, v3 = bsplit(c * t.astype(np.float64))
        q9.append(np.stack([c1, c1, c2, c2, c3, c3, -v1, -v2, -v3]))
    qaug9 = np.stack(q9).astype(np.float32)
    kaug9 = np.stack([hi, lo, hi, lo, hi, lo, np.ones(L), np.ones(L), np.ones(L)]).astype(np.float32)
    return dict(c_mstrict=mstrict, c_mcausal=mcausal, c_uincl=uincl, c_qaug4=aug(sl4), c_qaug16=qaug9, c_kaug9=kaug9, c_kaug=kaug,
                c_ident=np.eye(128, dtype=np.float32))


NCORES = 8
SEQ = 2048
NTOK = 2 * SEQ
N_EXPERTS = 32
_W_NAMES = ["ev_w_in", "ev_w_out", "ev_lambda_q1", "ev_lambda_k1", "ev_lambda_q2", "ev_lambda_k2", "ev_subln_w",
            "od_w_in", "od_kv_norm_w", "od_w_uv", "od_w_out", "ln_mix_g", "ln_mix_b", "router_w", "router_b",
            "exp_w_gu", "exp_b_gu", "exp_w_down", "exp_b_down", "ln_ffn_g", "ln_ffn_b"]


def build_program(shapes, cshapes):
    nc = bass.Bass("TRN2", target_bir_lowering=False)
    D = {}
    for n in _W_NAMES:
        D[n] = nc.dram_tensor(n, list(shapes[n]), F32, kind="ExternalInput").ap()
    for n, s in cshapes.items():
        D[n] = nc.dram_tensor(n, list(s), F32, kind="ExternalInput").ap()
    x = nc.dram_tensor("x", [NTOK, 1024], F32, kind="ExternalInput").ap()
    out = nc.dram_tensor("out", [NTOK, 1024], F32, kind="ExternalOutput").ap()
    h1 = nc.dram_tensor("h1", [NTOK, 1024], F32, kind="Internal").ap()
    h2 = nc.dram_tensor("h2", [NTOK, 1024], F32, kind="Internal").ap()
    h3 = nc.dram_tensor("h3", [NTOK, 1024], F32, kind="Internal").ap()
    with ExitStack() as st:
        P = Prog(nc, st)
        ident = P.sb("ident", [128, 128]); ident_b = P.sb("ident_b", [128, 128], BF16)
        P.dma("sync", ident[:], D["c_ident"], writes=["ident"])
        P.dma("gpsimd", ident_b[:], D["c_ident"], writes=["ident_b"])
        C = {"dram": D, "ident": ident, "ident_b": ident_b}
        lam_init0 = 0.8 - 0.6 * math.exp(-0.3 * 0)

        def phase(fn):
            with ExitStack() as ts:
                P.stack = ts
                fn()
                P.barrier()
            P.stack = st

        for sq in range(2):
            phase(lambda sq=sq: even_mixer_seq(P, "e%d" % sq, C, 0, x[sq * SEQ:(sq + 1) * SEQ, :], h1[sq * SEQ:(sq + 1) * SEQ, :], SEQ,
                                                lam_init0, D["ln_mix_g"][0], D["ln_mix_b"][0]))
        phase(lambda: moe_phase(P, "m0", C, 0, h1, h2, NTOK, N_EXPERTS))
        for sq in range(2):
            phase(lambda sq=sq: odd_mixer_seq(P, "o%d" % sq, C, 0, h2[sq * SEQ:(sq + 1) * SEQ, :], h3[sq * SEQ:(sq + 1) * SEQ, :], SEQ,
                                               D["ln_mix_g"][1], D["ln_mix_b"][1]))
        phase(lambda: moe_phase(P, "m1", C, 1, h3, out, NTOK, N_EXPERTS))
        P.final_wait()
        P.emit()
    return nc


def kernel(**inputs):
    consts = make_consts(SEQ)
    x = np.ascontiguousarray(np.asarray(inputs["x"], dtype=np.float32)).reshape(NCORES, NTOK, 1024)
    ws = {n: np.ascontiguousarray(np.asarray(inputs[n], dtype=np.float32)) for n in _W_NAMES}
    nc = build_program({n: ws[n].shape for n in _W_NAMES}, {n: v.shape for n, v in consts.items()})
    in_maps = []
    for c in range(NCORES):
        m = dict(ws)
        m.update(consts)
        m["x"] = x[c]
        in_maps.append(m)
    res = run_bass_kernel_spmd(nc, in_maps, core_ids=list(range(NCORES)))
    outs = [np.asarray(r["out"], dtype=np.float32) for r in res.results]
    return np.stack(outs, axis=0).reshape(16, SEQ, 1024)
```

```python
import math
from concourse.bass_utils import run_bass_kernel_spmd
from contextlib import ExitStack
import numpy as np
import concourse.bass as bass
import concourse.mybir as mybir

F32 = mybir.dt.float32
BF16 = mybir.dt.bfloat16
I32 = mybir.dt.int32
ALU = mybir.AluOpType
AF = mybir.ActivationFunctionType
AX = mybir.AxisListType

ENGS = ("sync", "scalar", "vector", "gpsimd", "tensor")


class Prog:
    def __init__(self, nc, stack):
        self.nc = nc
        self.stack = stack
        self.sem_stack = stack
        self.streams = {e: [] for e in ENGS}
        self.esem = {e: stack.enter_context(nc.semaphore("es_" + e)) for e in ENGS}
        self.etick = {e: 0 for e in ENGS}
        self.known = {e: {} for e in ENGS}
        self.res_w = {}
        self.res_r = {}
        self.dsem = {}
        self.semobj = {}
        for e in ENGS:
            self.semobj[id(self.esem[e])] = self.esem[e]
        self.n_ops = 0

    def sb(self, name, shape, dt=F32):
        return self.stack.enter_context(self.nc.sbuf_tensor(name, list(shape), dt))

    def ps(self, name, shape, dt=F32):
        return self.stack.enter_context(self.nc.psum_tensor(name, list(shape), dt))

    def _dma_sem(self, key):
        if key not in self.dsem:
            s = self.sem_stack.enter_context(self.nc.semaphore("ds_%d" % len(self.dsem)))
            self.dsem[key] = [s, 0]
            self.semobj[id(s)] = s
        return self.dsem[key]

    def _deps(self, eng, reads, writes):
        deps = {}

        def add(ev):
            if ev is None:
                return
            s, v = ev
            if deps.get(s, 0) < v:
                deps[s] = v

        for k in reads:
            add(self.res_w.get(k))
        for k in writes:
            add(self.res_w.get(k))
            for s, v in self.res_r.get(k, {}).items():
                add((s, v))
        waits = []
        kn = self.known[eng]
        for s, v in deps.items():
            if kn.get(s, 0) < v:
                kn[s] = v
                waits.append((s, v))
        return waits

    def _commit(self, ev, reads, writes):
        s, v = ev
        for k in reads:
            d = self.res_r.setdefault(k, {})
            if d.get(s, 0) < v:
                d[s] = v
        for k in writes:
            self.res_w[k] = ev
            self.res_r[k] = {}

    def op(self, eng, fn, reads=(), writes=()):
        waits = self._deps(eng, reads, writes)
        self.etick[eng] += 1
        ev = (id(self.esem[eng]), self.etick[eng])
        self.streams[eng].append((waits, fn, ev, 1))
        self._commit(ev, reads, writes)
        self.n_ops += 1

    def dma(self, eng, out, in_, reads=(), writes=(), slot=None, **kw):
        if slot is None:
            k0 = writes[0] if writes else reads[0]
            slot = ("_slot",) + tuple(k0[1:]) if isinstance(k0, tuple) and len(k0) > 1 else ("_slot", k0)
        ds = self._dma_sem(slot)
        waits = self._deps(eng, reads, writes)
        ds[1] += 16
        ev = (id(ds[0]), ds[1])
        fn = lambda e, out=out, in_=in_, kw=kw: e.dma_start(out=out, in_=in_, **kw)
        self.streams[eng].append((waits, fn, ev, 16))
        self._commit(ev, reads, writes)
        self.n_ops += 1

    def barrier(self):
        evs = [(id(self.esem[e]), self.etick[e]) for e in ENGS if self.etick[e] > 0]
        evs += [(id(s), c) for s, c in self.dsem.values() if c > 0]
        for e in ENGS:
            kn = self.known[e]
            waits = []
            for s, v in evs:
                if s == id(self.esem[e]) and False:
                    continue
                if kn.get(s, 0) < v:
                    kn[s] = v
                    waits.append((s, v))
            if waits:
                self.streams[e].append((waits, None, None, 0))

    def final_wait(self, eng="sync"):
        evs = [(id(s), c) for s, c in self.dsem.values() if c > 0]
        evs += [(id(self.esem[e]), self.etick[e]) for e in ENGS if self.etick[e] > 0 and e != eng]
        kn = self.known[eng]
        waits = [(s, v) for s, v in evs if kn.get(s, 0) < v]
        self.streams[eng].append((waits, None, None, 0))

    def emit(self):
        nc = self.nc
        with nc.Block() as block:
            def runner(name):
                def run(e):
                    for waits, fn, ev, inc in self.streams[name]:
                        for s, v in waits:
                            e.wait_ge(self.semobj[s], v)
                        if fn is not None:
                            inst = fn(e)
                            inst.then_inc(self.semobj[ev[0]], inc)
                return run
            block.sync(runner("sync"))
            block.scalar(runner("scalar"))
            block.vector(runner("vector"))
            block.gpsimd(runner("gpsimd"))
            block.tensor(runner("tensor"))


DEPTH = 2
ALPHA = (2 * DEPTH) ** 0.25
LN_EPS = 1e-5
SW_LIMIT = 7.0
SW_ALPHA = 1.702


def layer_norm_tile(P, ph, src_ap, src_keys, dst_ap, dst_key, g_bc, b_bc, st, idx):
    s1, s2, mean, msq, var, rstd, junk = (st[k] for k in ("s1", "s2", "mean", "msq", "var", "rstd", "junk"))
    k = lambda n: (ph, "ln_" + n)
    P.op("vector", lambda e: e.reduce_sum(s1[:, 0:1], src_ap, axis=AX.X), reads=list(src_keys), writes=[k("s1")])
    P.op("vector", lambda e: e.memset(s2[:, 0:1], 0.0), writes=[k("s2")])
    P.op("scalar", lambda e: e.activation(junk[:], src_ap, AF.Square, accum_out=s2[:, 0:1]),
         reads=list(src_keys) + [k("s2")], writes=[k("s2"), k("junk")])
    P.op("vector", lambda e: e.tensor_scalar(mean[:, 0:1], s1[:, 0:1], 1.0 / 1024, None, ALU.mult),
         reads=[k("s1")], writes=[k("mean")])
    P.op("vector", lambda e: e.tensor_tensor(msq[:, 0:1], mean[:, 0:1], mean[:, 0:1], ALU.mult),
         reads=[k("mean")], writes=[k("msq")])
    P.op("vector", lambda e: e.scalar_tensor_tensor(var[:, 0:1], s2[:, 0:1], 1.0 / 1024, msq[:, 0:1], ALU.mult, ALU.subtract),
         reads=[k("s2"), k("msq")], writes=[k("var")])
    P.op("vector", lambda e: e.tensor_scalar(var[:, 0:1], var[:, 0:1], LN_EPS, None, ALU.add),
         reads=[k("var")], writes=[k("var")])
    P.op("scalar", lambda e: e.sqrt(var[:, 0:1], var[:, 0:1]), reads=[k("var")], writes=[k("var")])
    P.op("vector", lambda e: e.reciprocal(rstd[:, 0:1], var[:, 0:1]), reads=[k("var")], writes=[k("rstd")])
    P.op("vector", lambda e: e.tensor_scalar(dst_ap, src_ap, mean[:, 0:1], rstd[:, 0:1], ALU.subtract, ALU.mult),
         reads=list(src_keys) + [k("mean"), k("rstd")], writes=[dst_key])
    P.op("vector", lambda e: e.tensor_tensor(dst_ap, dst_ap, g_bc[:], ALU.mult), reads=[dst_key, (ph, "g_bc")], writes=[dst_key])
    P.op("vector", lambda e: e.tensor_tensor(dst_ap, dst_ap, b_bc[:], ALU.add), reads=[dst_key, (ph, "b_bc")], writes=[dst_key])


def moe_phase(P, ph, C, lay, h_in, h_out, NT, E, stage=99):
    nc = P.nc
    D = C["dram"]
    ident = C["ident"]
    NBLK = NT // 1024
    sb = lambda n, s, d=F32: P.sb(ph + n, s, d)
    K = lambda *a: (ph,) + a

    rw = sb("rw", [128, 8, E]); rb_bc = sb("rb", [128, E])
    bguT = sb("bguT", [128, 16, E]); bd = sb("bd", [E, 1024])
    g_bc = sb("g_bc", [128, 1024]); b_bc = sb("b_bc", [128, 1024])
    xt = [sb("xt%d" % i, [128, 1024]) for i in range(2)]
    hT = sb("hT", [128, 8, 1024], BF16)
    hTf = sb("hTf", [128, 8, 128])
    acc = sb("acc", [128, 8, 1024])
    wgu = [sb("wgu%d" % i, [128, 8, 2048], BF16) for i in range(2)]
    wd = [sb("wd%d" % i, [128, 8, 1024], BF16) for i in range(2)]
    actT = [sb("actT%d" % i, [128, 8, 512], BF16) for i in range(2)]
    gc = [sb("gc%d" % i, [128, 512]) for i in range(2)]
    ua = [sb("ua%d" % i, [128, 512]) for i in range(2)]
    sl = [sb("sl%d" % i, [128, 512]) for i in range(2)]
    cw = sb("cw", [128, 8, E]); cwT = sb("cwT", [E, 128]); cws = sb("cws", [128, 8, E])
    lg = sb("lg", [128, E]); ex = sb("ex", [128, E]); em = sb("em", [128, E])
    m8 = sb("m8", [128, 8]); sm = sb("sm", [128, 8])
    st = {n: sb("st_" + n, [128, 1]) for n in ("s1", "s2", "mean", "msq", "var", "rstd")}
    st["junk"] = sb("junk", [128, 1024], BF16)

    pA = [P.ps(ph + "pA%d" % i, [128, 512]) for i in range(2)]
    pB = [P.ps(ph + "pB%d" % i, [128, 512]) for i in range(2)]
    pY = [P.ps(ph + "pY%d" % i, [128, 512]) for i in range(2)]
    pT = [P.ps(ph + "pT%d" % i, [128, 4, 128]) for i in range(2)]

    import os
    SK = os.environ.get("SKIP", "")
    if "rw" not in SK:
        P.dma("sync", rw[:], D["router_w"][lay].rearrange("(k p) e -> p k e", p=128), writes=[K("rw")])
    if "rb" not in SK:
        P.dma("sync", rb_bc[:], D["router_b"][lay].partition_broadcast(128), writes=[K("rb")])
    bkeys = [K("acc", 0, 0), K("acc", 0, 1), K("acc", 1, 0), K("acc", 1, 1)]
    P.dma("sync", acc[0:E, 0:2, :], D["exp_b_gu"][lay].rearrange("e (a f) -> e a f", a=2), writes=bkeys, slot="bguraw")
    if "bd" not in SK:
        P.dma("sync", bd[:], D["exp_b_down"][lay], writes=[K("bd")])
    if "gb" not in SK:
        P.dma("sync", g_bc[:], D["ln_ffn_g"][lay].partition_broadcast(128), writes=[K("g_bc")])
    if "gb" not in SK:
        P.dma("sync", b_bc[:], D["ln_ffn_b"][lay].partition_broadcast(128), writes=[K("b_bc")])
    for j in range(16):
        h = j % 2
        P.op("tensor", lambda e, j=j, h=h: e.transpose(pT[h][:, 0, 0:E], acc[0:E, j // 8, (j % 8) * 128:(j % 8 + 1) * 128], ident[0:E, 0:E]),
             reads=bkeys + ["ident"], writes=[K("pT", h)])
        P.op("vector", lambda e, j=j, h=h: e.tensor_copy(bguT[:, j, :], pT[h][:, 0, 0:E]),
             reads=[K("pT", h)], writes=[K("bguT")])

    if stage == 0:
        return
    def load_w(b, e, which="both"):
        ws = (b * E + e) % 2
        if "now" in SK:
            return
        if which in ("both", "gu"):
            for kk in range(8):
                P.dma("gpsimd", wgu[ws][:, kk, :], D["exp_w_gu"][lay, e, kk * 128:(kk + 1) * 128, :], writes=[K("wgu", ws, kk)], slot=("wgu", ws))
        if which in ("both", "d"):
            for kk in range(8):
                P.dma("gpsimd", wd[ws][:, kk, :], D["exp_w_down"][lay, e, kk * 128:(kk + 1) * 128, :], writes=[K("wd", ws, kk)], slot=("wd", ws))

    for b in range(NBLK):
        load_w(b, 0)
        for i in range(8 if "noa" not in SK else 0):
            tok0 = b * 1024 + i * 128
            s = i % 2
            P.dma("sync", xt[s][:], h_in[tok0:tok0 + 128, :], writes=[K("xt", s)])
            for h in range(2):
                def tr(e, s=s, h=h):
                    for q in range(4):
                        kk = h * 4 + q
                        r = e.transpose(pT[h][:, q, :], xt[s][:, kk * 128:(kk + 1) * 128], ident[:])
                    return r
                P.op("tensor", tr, reads=[K("xt", s), "ident"], writes=[K("pT", h)])
                P.op(os.environ.get("EVE", "scalar"), lambda e, h=h, i=i: (e.activation(hT[:, 4 * h:4 * h + 4, i * 128:(i + 1) * 128], pT[h][:], AF.Identity) if os.environ.get("EVE", "scalar") == "scalar" else e.tensor_copy(hT[:, 4 * h:4 * h + 4, i * 128:(i + 1) * 128], pT[h][:])),
                     reads=[K("pT", h)], writes=[K("hT", i // 4)])
                P.op("vector", lambda e, h=h: e.tensor_copy(hTf[:, 4 * h:4 * h + 4, :], pT[h][:]),
                     reads=[K("pT", h), K("hT", i // 4)] , writes=[K("hTf", h)])
            CUT = int(os.environ.get("CUT", "99"))
            if CUT < 2:
                continue
            pL = pY[0]
            def rt(e):
                for kk in range(8):
                    r = e.matmul(pL[:, 0:E], hTf[:, kk, :], rw[:, kk, :], start=(kk == 0), stop=(kk == 7))
                return r
            P.op("tensor", rt, reads=[K("hTf", 0), K("hTf", 1), K("rw")], writes=[K("pY", 0)])
            P.op("vector", lambda e: e.tensor_tensor(lg[:], pL[:, 0:E], rb_bc[:], ALU.add), reads=[K("pY", 0), K("rb")], writes=[K("lg")])
            if CUT < 3:
                continue
            P.op("vector", lambda e: e.max(m8[:], lg[:]), reads=[K("lg")], writes=[K("m8")])
            P.op("vector", lambda e: e.tensor_scalar(sm[:, 0:1], m8[:, 0:1], -1.0, None, ALU.mult), reads=[K("m8")], writes=[K("negmx")])
            P.op("scalar", lambda e: e.activation(ex[:], lg[:], AF.Exp, bias=sm[:, 0:1], scale=1.0), reads=[K("lg"), K("negmx")], writes=[K("ex")])
            P.op("vector", lambda e: e.scalar_tensor_tensor(em[:], lg[:], m8[:, 3:4], ex[:], ALU.is_ge, ALU.mult),
                 reads=[K("lg"), K("m8"), K("ex")], writes=[K("em")])
            P.op("vector", lambda e: e.reduce_sum(sm[:, 1:2], em[:], axis=AX.X), reads=[K("em")], writes=[K("Z")])
            P.op("vector", lambda e: e.reciprocal(sm[:, 2:3], sm[:, 1:2]), reads=[K("Z")], writes=[K("rz")])
            P.op("vector", lambda e, i=i: e.tensor_scalar(cw[:, i, :], em[:], sm[:, 2:3], None, ALU.mult), reads=[K("em"), K("rz")], writes=[K("cw", i)])
            P.op("vector", lambda e, i=i: e.tensor_scalar(cws[:, i, :], cw[:, i, :], 1.0 / SW_ALPHA, None, ALU.mult), reads=[K("cw", i)], writes=[K("cws", i)])
            if CUT < 4:
                continue
            pC = pY[1]
            P.op("tensor", lambda e, i=i: e.transpose(pC[0:E, 0:128], cw[:, i, :], ident[:]), reads=[K("cw", i), "ident"], writes=[K("pY", 1)])
            P.op("scalar", lambda e: e.copy(cwT[:], pC[0:E, 0:128]), reads=[K("pY", 1)], writes=[K("cwT")])
            if CUT < 5:
                continue
            for n in range(2):
                P.op("tensor", lambda e, n=n: e.matmul(pA[n][:], cwT[:], bd[:, n * 512:(n + 1) * 512], start=True, stop=True),
                     reads=[K("cwT"), K("bd")], writes=[K("pA", n)])
                P.op("vector", lambda e, n=n, s=s, i=i: e.scalar_tensor_tensor(acc[:, i, n * 512:(n + 1) * 512], xt[s][:, n * 512:(n + 1) * 512],
                                                                      ALPHA, pA[n][:], ALU.mult, ALU.add),
                     reads=[K("xt", s), K("pA", n)], writes=[K("acc", i, n)])
        if stage == 1:
            return
        cntb = [0]
        pend = [None]

        def flush_act():
            if pend[0] is not None:
                p, i, a_s = pend[0]
                P.op("vector", lambda e, p=p, i=i, a_s=a_s: e.scalar_tensor_tensor(actT[a_s][:, i, :], ua[p][:], 1.0 - SW_LIMIT, sl[p][:], ALU.add, ALU.mult),
                     reads=[K("sl", p), K("ua", p)], writes=[K("actT", a_s)])
                pend[0] = None

        def gu_unit(ei, c, i):
            ws = (b * E + ei) % 2
            a_s = c % 2
            p = cntb[0] % 2
            cntb[0] += 1
            def mm_g(e, off, ps, i=i, c=c, ws=ws):
                for kk in range(8):
                    r = e.matmul(ps[:], wgu[ws][:, kk, off + i * 128: off + (i + 1) * 128], hT[:, kk, c * 512:(c + 1) * 512],
                                 start=(kk == 0), stop=(kk == 7))
                return r
            hk = [K("hT", c)]
            P.op("tensor", lambda e, p=p, f=mm_g: f(e, 0, pA[p]), reads=[K("wgu", ws, kk) for kk in range(8)] + hk, writes=[K("pA", p)])
            P.op("tensor", lambda e, p=p, f=mm_g: f(e, 1024, pB[p]), reads=[K("wgu", ws, kk) for kk in range(8)] + hk, writes=[K("pB", p)])
            P.op("vector", lambda e, p=p, i=i, ei=ei: e.tensor_scalar(gc[p][:], pA[p][:], bguT[:, i, ei:ei + 1], SW_LIMIT, ALU.add, ALU.min),
                 reads=[K("pA", p), K("bguT")], writes=[K("gc", p)])
            P.op("vector", lambda e, p=p, i=i, ei=ei: e.tensor_scalar(ua[p][:], pB[p][:], bguT[:, 8 + i, ei:ei + 1], SW_LIMIT, ALU.add, ALU.min),
                 reads=[K("pB", p), K("bguT")], writes=[K("ua", p)])
            P.op("scalar", lambda e, p=p: e.activation(sl[p][:], gc[p][:], AF.Silu, scale=SW_ALPHA),
                 reads=[K("gc", p)], writes=[K("sl", p)])
            P.op("scalar", lambda e, p=p: e.activation(ua[p][:], ua[p][:], AF.Relu, bias=SW_LIMIT),
                 reads=[K("ua", p)], writes=[K("ua", p)])
            flush_act()
            pend[0] = (p, i, a_s)

        def down_unit(ei, c, idx):
            ws = (b * E + ei) % 2
            a_s = c % 2
            j, n = idx // 2, idx % 2
            ti = c * 4 + j
            q = idx % 2
            def mm_d(e, j=j, n=n, q=q, a_s=a_s, ws=ws):
                for f in range(8):
                    r = e.matmul(pY[q][:], actT[a_s][:, f, j * 128:(j + 1) * 128], wd[ws][:, f, n * 512:(n + 1) * 512],
                                 start=(f == 0), stop=(f == 7))
                return r
            P.op("tensor", mm_d, reads=[K("actT", a_s)] + [K("wd", ws, kk) for kk in range(8)], writes=[K("pY", q)])
            P.op("vector", lambda e, q=q, ti=ti, n=n, ei=ei: e.scalar_tensor_tensor(
                acc[:, ti, n * 512:(n + 1) * 512], pY[q][:], cws[:, ti, ei:ei + 1], acc[:, ti, n * 512:(n + 1) * 512], ALU.mult, ALU.add),
                 reads=[K("pY", q), K("cws", ti), K("acc", ti, n)], writes=[K("acc", ti, n)])

        seq = [(ei, c) for ei in range(E) for c in range(2)]
        for k in range(len(seq) + 1):
            if k < len(seq):
                ei, c = seq[k]
                if c == 0 and ei + 1 < E:
                    load_w(b, ei + 1, "gu")
                if c == 1 and ei + 1 < E:
                    load_w(b, ei + 1, "d")
            for idx in range(8):
                if k < len(seq):
                    gu_unit(seq[k][0], seq[k][1], idx)
                else:
                    flush_act()
                if k >= 1:
                    if idx == 0:
                        pass
                    down_unit(seq[k - 1][0], seq[k - 1][1], idx)
            flush_act()
        if stage == 2:
            return
        for i in range(8):
            tok0 = b * 1024 + i * 128
            s = i % 2
            layer_norm_tile(P, ph, acc[:, i, :], [K("acc", i, 0), K("acc", i, 1)], acc[:, i, :], K("acc", i, 0), g_bc, b_bc, st, i)
            P.dma("sync", h_out[tok0:tok0 + 128, :], acc[:, i, :], reads=[K("acc", i, 0)], writes=[K("hout", b, i)], slot=("yo_st", s))


import math

RMS_EPS = 1e-5
NEGM = -30000.0


def load_w_cols(P, ph, wt, wkey, w_dram, c0, ncols, dst0=0):
    for kk in range(8):
        P.dma("gpsimd", wt[:, kk, dst0:dst0 + ncols], w_dram[kk * 128:(kk + 1) * 128, c0:c0 + ncols],
              writes=[(ph, wkey, kk)], slot=(wkey,))


def wkeys(ph, wkey, dsts=(0,)):
    return [(ph, wkey, kk) for kk in range(8)]


def load_xT(P, ph, C, h_rows, L, xT, xt, ps):
    ident = C["ident"]
    for i in range(L // 128):
        s = i % 2
        P.dma("sync", xt[s][:], h_rows[i * 128:(i + 1) * 128, :], writes=[(ph, "xt", s)])
        for h in range(2):
            pt = ps[h]
            def tr(e, s=s, h=h, pt=pt):
                for q in range(4):
                    kk = h * 4 + q
                    r = e.transpose(pt[:, q * 128:(q + 1) * 128], xt[s][:, kk * 128:(kk + 1) * 128], ident[:])
                return r
            P.op("tensor", tr, reads=[(ph, "xt", s), "ident"], writes=[(ph, "ps", h)])
            P.op("vector", lambda e, h=h, i=i, pt=pt: e.tensor_copy(xT[:, 4 * h:4 * h + 4, i * 128:(i + 1) * 128],
                                                                  pt[:].rearrange("p (a b) -> p a b", a=4)),
                 reads=[(ph, "ps", h)], writes=[(ph, "ps", h), (ph, "xT", i // 4)])


def proj_fm(P, ph, wt, wk, c0, M, xT, L, ps, psi, evac):
    for ch in range(L // 512):
        pi = psi[ch % len(psi)]
        def mm(e, ch=ch, pi=pi):
            for kk in range(8):
                r = e.matmul(ps[pi][0:M, :], wt[:, kk, c0:c0 + M], xT[:, kk, ch * 512:(ch + 1) * 512], start=(kk == 0), stop=(kk == 7))
            return r
        P.op("tensor", mm, reads=wk + [(ph, "xT", ch)], writes=[(ph, "ps", pi)])
        evac(ch, ps[pi][0:M, :], (ph, "ps", pi))


def proj_tm(P, ph, wt, wk, c0, N, xT, L, ps, psi, evac):
    for i in range(L // 128):
        pi = psi[i % len(psi)]
        def mm(e, i=i, pi=pi):
            for kk in range(8):
                r = e.matmul(ps[pi][:, 0:N], xT[:, kk, i * 128:(i + 1) * 128], wt[:, kk, c0:c0 + N], start=(kk == 0), stop=(kk == 7))
            return r
        P.op("tensor", mm, reads=wk + [(ph, "xT", i // 4)], writes=[(ph, "ps", pi)])
        evac(i, ps[pi][:, 0:N], (ph, "ps", pi))


def outproj_ln(P, ph, C, mix, L, w_out, h_rows, out_rows, g_vec, b_vec, T, ps):
    ident_b = C["ident_b"]
    wo, mT, xt, g_bc, b_bc, st, yo = T["wo"], T["mT"], T["xt"], T["g_bc"], T["b_bc"], T["st"], T["yo"]
    for half in range(2):
        load_w_cols(P, ph, wo[half], "wb%d" % half, w_out, half * 512, 512)
    P.dma("sync", g_bc, g_vec.partition_broadcast(128), writes=[(ph, "g_bc")] + T["gk"], slot=("g_bc",))
    P.dma("sync", b_bc, b_vec.partition_broadcast(128), writes=[(ph, "b_bc")] + T["bk"], slot=("b_bc",))
    pbf = [ps[6][:].bitcast(BF16), ps[7][:].bitcast(BF16)]
    P.barrier()
    for i in range(L // 128):
        s = i % 2
        P.dma("sync", xt[s][:], h_rows[i * 128:(i + 1) * 128, :], writes=[(ph, "xt", s)])
        def tr(e, i=i, s=s):
            for kk in range(8):
                r = e.transpose(pbf[s][:, kk * 128:(kk + 1) * 128], mix[:, i, kk * 128:(kk + 1) * 128], ident_b[:])
            return r
        P.op("tensor", tr, reads=[(ph, "mix", i), "ident_b"], writes=[(ph, "ps", 6 + s)])
        P.op("vector", lambda e, s=s: e.tensor_copy(mT[s], pbf[s]), reads=[(ph, "ps", 6 + s)], writes=[(ph, "ps", 6 + s), (ph, "mT", s)] + T["mTk"][s])
        for n in range(2):
            pi = 4 + n
            def mm(e, s=s, n=n, pi=pi):
                for kk in range(8):
                    r = e.matmul(ps[pi][:], mT[s][:, kk * 128:(kk + 1) * 128], wo[n][:, kk, :], start=(kk == 0), stop=(kk == 7))
                return r
            P.op("tensor", mm, reads=[(ph, "mT", s)] + T["wok"][n], writes=[(ph, "ps", pi)])
            P.op("vector", lambda e, s=s, n=n, pi=pi: e.scalar_tensor_tensor(yo[s][:, n * 512:(n + 1) * 512], xt[s][:, n * 512:(n + 1) * 512], ALPHA, ps[pi][:], ALU.mult, ALU.add),
                 reads=[(ph, "xt", s), (ph, "ps", pi)], writes=[(ph, "ps", pi), (ph, "yo", s, n)])
        layer_norm_tile(P, ph, yo[s], [(ph, "yo", s, 0), (ph, "yo", s, 1)], yo[s], (ph, "yo", s, 0), g_bc, b_bc, st, i)
        P.dma("sync", out_rows[i * 128:(i + 1) * 128, :], yo[s], reads=[(ph, "yo", s, 0)], writes=[(ph, "hout", i)], slot=("yo_st", s))
        P.res_w[(ph, "yo", s, 1)] = P.res_w[(ph, "yo", s, 0)]
        P.res_r[(ph, "yo", s, 1)] = dict(P.res_r[(ph, "yo", s, 0)])


def even_mixer_seq(P, ph, C, lay_j, h_rows, out_rows, L, lam_init, ln_g, ln_b):
    D = C["dram"]
    ident, ident_b = C["ident"], C["ident_b"]
    NT = L // 128
    NC = L // 512
    sb = lambda n, s, d=F32: P.sb(ph + n, s, d)
    K = lambda *a: (ph,) + a
    w_in = D["ev_w_in"][lay_j]
    ps = [P.ps(ph + "ps%d" % i, [128, 512]) for i in range(8)]

    xt = [sb("xt%d" % i, [128, 1024]) for i in range(2)]
    xT = sb("xT", [128, 8, L], BF16)
    wb = [sb("wb%d" % i, [128, 8, 512], BF16) for i in range(2)]
    va = sb("va", [128, NT, 512], BF16)
    vb = sb("vb", [128, NT, 4, 132], BF16)
    mix = sb("mix", [128, NT, 1024], BF16)
    qa = [sb("qa%d" % i, [128, L], BF16) for i in range(4)]
    ka = [sb("ka%d" % i, [128, L], BF16) for i in range(4)]
    qd = [sb("qd%d" % i, [68, L], BF16) for i in range(2)]
    kd = [sb("kd%d" % i, [68, L], BF16) for i in range(2)]
    mstrict = sb("mstrict", [128, 4, 512], BF16)
    mcausal = sb("mcausal", [128, 4, 512], BF16)
    uincl = sb("uincl", [128, 128], BF16)
    ones1 = sb("ones1", [1, 128], BF16)
    efg = sb("efg", [128, 4, 512])
    ef = [efg[:, i, :] for i in range(2)]
    eg = [efg[:, 2 + i, :] for i in range(2)]
    atsp = sb("atsp", [128, 4, 512], BF16)
    at = [atsp[:, i, :] for i in range(2)]
    spt = [atsp[:, 2 + i, :] for i in range(2)]
    suf = sb("suf", [1, 512], BF16)
    lamv = sb("lamv", [128, 4, 64]); lam = sb("lam", [128, 4])
    wsub = sb("wsub", [128, 128])
    o1 = sb("o1", [128, 128]); ob = sb("ob", [128, 128]); sm = sb("sm", [128, 8])
    junk = sb("junk", [128, 1024], BF16)

    P.dma("gpsimd", mstrict[:], D["c_mstrict"], writes=[K("mstrict")])
    P.dma("gpsimd", mcausal[:], D["c_mcausal"], writes=[K("mcausal")])
    P.dma("gpsimd", uincl[:], D["c_uincl"], writes=[K("uincl")])
    P.op("vector", lambda e: e.memset(ones1[:], 1.0), writes=[K("ones1")])
    for i, nm in enumerate(["ev_lambda_q1", "ev_lambda_k1", "ev_lambda_q2", "ev_lambda_k2"]):
        P.dma("sync", lamv[:, i, :], D[nm][lay_j].partition_broadcast(128), writes=[K("lamv", i)])
    P.dma("sync", wsub[:], D["ev_subln_w"][lay_j].partition_broadcast(128), writes=[K("wsub")])
    P.op("vector", lambda e: e.tensor_scalar(wsub[:], wsub[:], 1.0 - lam_init, None, ALU.mult), reads=[K("wsub")], writes=[K("wsub")])
    for j in range(2):
        P.op("vector", lambda e, j=j: e.tensor_tensor(lamv[:, 2 * j, :], lamv[:, 2 * j, :], lamv[:, 2 * j + 1, :], ALU.mult),
             reads=[K("lamv", 2 * j), K("lamv", 2 * j + 1)], writes=[K("lamv", 2 * j)])
        P.op("vector", lambda e, j=j: e.reduce_sum(lam[:, j:j + 1], lamv[:, 2 * j, :], axis=AX.X), reads=[K("lamv", 2 * j)], writes=[K("lam", j)])
        P.op("scalar", lambda e, j=j: e.activation(lam[:, j:j + 1], lam[:, j:j + 1], AF.Exp), reads=[K("lam", j)], writes=[K("lam", j)])
    P.op("vector", lambda e: e.tensor_tensor(lam[:, 2:3], lam[:, 1:2], lam[:, 0:1], ALU.subtract), reads=[K("lam", 0), K("lam", 1)], writes=[K("lam", 2)])
    P.op("vector", lambda e: e.tensor_scalar(lam[:, 3:4], lam[:, 2:3], -lam_init, None, ALU.add), reads=[K("lam", 2)], writes=[K("nlam")])

    load_xT(P, ph, C, h_rows, L, xT, xt, ps)

    wslot = [0]
    def next_w(c0, ncols=512):
        s = wslot[0] % 2
        wslot[0] += 1
        load_w_cols(P, ph, wb[s], "wb%d" % s, w_in, c0, ncols)
        return wb[s], wkeys(ph, "wb%d" % s)

    wt, wk = next_w(1024)
    proj_tm(P, ph, wt, wk, 0, 512, xT, L, ps, [2, 3],
            lambda i, pa, pk: P.op("vector", lambda e: e.tensor_copy(va[:, i, :], pa), reads=[pk], writes=[pk, K("va", i)]))
    wt, wk = next_w(2560)
    P.op("vector", lambda e: e.memset(vb[:], 1.0), writes=[K("vb", i) for i in range(NT)])
    proj_tm(P, ph, wt, wk, 0, 512, xT, L, ps, [2, 3],
            lambda i, pa, pk: P.op("vector", lambda e: e.tensor_copy(vb[:, i, :, 0:128], pa.rearrange("p (h d) -> p h d", h=4)), reads=[pk], writes=[pk, K("vb", i)]))

    wt, wk = next_w(0)
    for j in range(4):
        proj_fm(P, ph, wt, wk, j * 128, 128, xT, L, ps, [2, 3],
                lambda ch, pa, pk, j=j: P.op("scalar", lambda e: e.activation(qa[j][:, ch * 512:(ch + 1) * 512], pa, AF.Identity, scale=0.125),
                                            reads=[pk], writes=[pk, K("qa", j)]))
    wt, wk = next_w(512)
    for j in range(4):
        proj_fm(P, ph, wt, wk, j * 128, 128, xT, L, ps, [2, 3],
                lambda ch, pa, pk, j=j: P.op("vector", lambda e: e.tensor_copy(ka[j][:, ch * 512:(ch + 1) * 512], pa),
                                            reads=[pk], writes=[pk, K("ka", j)]))

    it = 0
    suf2 = [suf, sb("suf1", [1, 512], BF16)]

    def sb_head(h):
        suf = suf2[h % 2]
        sufk = K("suf", h % 2)
        qT = qa[h // 2][(h % 2) * 64:(h % 2) * 64 + 64, :]
        kT = ka[h // 2][(h % 2) * 64:(h % 2) * 64 + 64, :]
        qk = [K("qa", h // 2), K("ka", h // 2)]
        for c in range(NC):
            po = ps[4 + (h % 2) * 2 + c % 2]
            pok = K("ps", 4 + (h % 2) * 2 + c % 2)
            nk = 4 * c + 4
            for kt in range(nk - 1, -1, -1):
                j = kt - 4 * c
                q0 = max(j, 0) * 128
                b = h % 2
                pz, pzk = ps[b], K("ps", b)
                pg, pgk = ps[2 + b], K("ps", 2 + b)
                first = False
                if kt == nk - 1:
                    P.op("vector", lambda e: e.memset(suf[:], 0.0), writes=[sufk])
                P.op("tensor", lambda e, pz=pz, kt=kt, c=c, q0=q0, kT=kT, qT=qT: e.matmul(
                    pz[:, q0:512], kT[:, kt * 128:(kt + 1) * 128], qT[:, c * 512 + q0:(c + 1) * 512], start=True, stop=True),
                     reads=qk, writes=[pzk])
                P.op("scalar", lambda e, b=b, pz=pz, q0=q0: e.activation(ef[b][:, q0:512], pz[:, q0:512], AF.Exp), reads=[pzk], writes=[pzk, K("ef", b)])
                P.op("scalar", lambda e, b=b, q0=q0: e.activation(spt[b][:, q0:512], ef[b][:, q0:512], AF.Ln, bias=1.0), reads=[K("ef", b)], writes=[K("sp", b)])
                if j >= 0:
                    P.op("vector", lambda e, b=b, j=j, q0=q0: e.tensor_tensor(spt[b][:, q0:512], spt[b][:, q0:512], mstrict[:, j, q0:512], ALU.mult),
                         reads=[K("sp", b), K("mstrict")], writes=[K("sp", b)])
                yield
                def mg(e, b=b, pg=pg, q0=q0, first=first):
                    r = e.matmul(pg[:, q0:512], uincl[:], spt[b][:, q0:512], start=True, stop=first)
                    if not first:
                        r = e.matmul(pg[:, q0:512], ones1[:], suf[:, q0:512], start=False, stop=True)
                    return r
                P.op("tensor", mg, reads=[K("sp", b), K("uincl"), K("ones1"), sufk], writes=[pgk])
                P.op("scalar", lambda e, b=b, pg=pg, q0=q0: e.activation(eg[b][:, q0:512], pg[:, q0:512], AF.Exp, scale=-1.0), reads=[pgk], writes=[pgk, K("eg", b)])
                if kt > 0:
                    P.op("vector", lambda e, pg=pg, q0=q0: e.tensor_copy(suf[:, q0:512], pg[0:1, q0:512]), reads=[pgk], writes=[pgk, sufk])
                P.op("vector", lambda e, b=b, q0=q0: e.tensor_tensor(at[b][:, q0:512], ef[b][:, q0:512], eg[b][:, q0:512], ALU.mult),
                     reads=[K("ef", b), K("eg", b)], writes=[K("at", b)])
                if j >= 0:
                    P.op("vector", lambda e, b=b, j=j, q0=q0: e.tensor_tensor(at[b][:, q0:512], at[b][:, q0:512], mstrict[:, j, q0:512], ALU.mult),
                         reads=[K("at", b), K("mstrict")], writes=[K("at", b)])
                yield
                def pv(e, b=b, kt=kt, c=c, j=j, h=h, po=po):
                    r = None
                    for qs in range(3, max(j, 0) - 1, -1):
                        qt = 4 * c + qs
                        r = e.matmul(po[:, qs * 64:(qs + 1) * 64], at[b][:, qs * 128:(qs + 1) * 128], va[:, kt, h * 64:(h + 1) * 64],
                                     start=(kt == 4 * c + 3 and qs == 3), stop=(kt == 0 and qs == 0))
                    return r
                P.op("tensor", pv, reads=[K("at", b), K("va", kt)], writes=[pok])
                yield
            P.op("vector", lambda e, po=po, c=c, h=h: e.tensor_copy(mix[:, 4 * c:4 * c + 4, h * 64:(h + 1) * 64], po[:, 0:256].rearrange("p (a d) -> p a d", a=4)),
                 reads=[pok], writes=[pok] + [K("mix", 4 * c + qs) for qs in range(4)])

    for h0 in range(0, 8, 2):
        gens = [sb_head(h0), sb_head(h0 + 1)]
        alive = [True, True]
        while any(alive):
            for gi in range(2):
                if alive[gi]:
                    try:
                        next(gens[gi])
                    except StopIteration:
                        alive[gi] = False

    for h in range(4):
        for m in range(2):
            wt, wk = next_w(1536 + (h * 2 + m) * 64, 64)
            proj_fm(P, ph, wt, wk, 0, 64, xT, L, ps, [2, 3],
                    lambda ch, pa, pk, m=m: P.op("scalar", lambda e: e.activation(qd[m][0:64, ch * 512:(ch + 1) * 512], pa, AF.Identity, scale=0.125),
                                                reads=[pk], writes=[pk, K("qd", m)]))
            wt, wk = next_w(2048 + (h * 2 + m) * 64, 64)
            proj_fm(P, ph, wt, wk, 0, 64, xT, L, ps, [2, 3],
                    lambda ch, pa, pk, m=m: P.op("vector", lambda e: e.tensor_copy(kd[m][0:64, ch * 512:(ch + 1) * 512], pa),
                                                reads=[pk], writes=[pk, K("kd", m)]))
            P.dma("gpsimd", qd[m][64:68, :], D["c_qaug4"][h][:, 0:L], writes=[K("qd", m)], slot=("qaug", m))
            P.dma("gpsimd", kd[m][64:68, :], D["c_kaug"][:, 0:L], writes=[K("kd", m)], slot=("kaug", m))
        for c in range(NC):
            nk = 4 * c + 4
            for m in range(2):
                pos = [ps[4 + 2 * m], ps[5 + 2 * m]]
                for kt in range(nk - 1, -1, -1):
                    j = kt - 4 * c
                    q0 = max(j, 0) * 128
                    b = it % 2
                    it += 1
                    pz, pzk = ps[b], K("ps", b)
                    P.op("tensor", lambda e, pz=pz, kt=kt, c=c, q0=q0, m=m: e.matmul(
                        pz[:, q0:512], kd[m][:, kt * 128:(kt + 1) * 128], qd[m][:, c * 512 + q0:(c + 1) * 512], start=True, stop=True),
                         reads=[K("qd", m), K("kd", m)], writes=[pzk])
                    P.op("scalar", lambda e, b=b, pz=pz, q0=q0: e.activation(at[b][:, q0:512], pz[:, q0:512], AF.Exp), reads=[pzk], writes=[pzk, K("at", b)])
                    if j >= 0:
                        P.op("vector", lambda e, b=b, j=j, q0=q0: e.tensor_tensor(at[b][:, q0:512], at[b][:, q0:512], mcausal[:, j, q0:512], ALU.mult),
                             reads=[K("at", b), K("mcausal")], writes=[K("at", b)])
                    def pv(e, b=b, kt=kt, c=c, j=j, h=h, pos=pos):
                        r = None
                        for qs in range(3, max(j, 0) - 1, -1):
                            qt = 4 * c + qs
                            r = e.matmul(pos[qs // 2][:, (qs % 2) * 256:(qs % 2) * 256 + 129], at[b][:, qs * 128:(qs + 1) * 128], vb[:, kt, h, 0:129],
                                         start=(kt == qt and qs % 2 == 1), stop=(kt == 0 and qs % 2 == 0))
                        return r
                    P.op("tensor", pv, reads=[K("at", b), K("vb", kt)], writes=[K("ps", 4 + 2 * m), K("ps", 5 + 2 * m)])
            for qs in range(4):
                qt = 4 * c + qs
                p1 = ps[4 + qs // 2][:, (qs % 2) * 256:(qs % 2) * 256 + 129]
                p2 = ps[6 + qs // 2][:, (qs % 2) * 256:(qs % 2) * 256 + 129]
                k1, k2 = K("ps", 4 + qs // 2), K("ps", 6 + qs // 2)
                P.op("vector", lambda e, p1=p1: e.reciprocal(sm[:, 0:1], p1[:, 128:129]), reads=[k1], writes=[k1, K("sm0")])
                P.op("vector", lambda e, p2=p2: e.reciprocal(sm[:, 1:2], p2[:, 128:129]), reads=[k2], writes=[k2, K("sm1")])
                P.op("vector", lambda e: e.tensor_tensor(sm[:, 1:2], sm[:, 1:2], lam[:, 3:4], ALU.mult), reads=[K("sm1"), K("nlam")], writes=[K("sm1")])
                P.op("vector", lambda e, p1=p1: e.tensor_scalar(o1[:], p1[:, 0:128], sm[:, 0:1], None, ALU.mult), reads=[k1, K("sm0")], writes=[k1, K("o1")])
                P.op("vector", lambda e, p2=p2: e.scalar_tensor_tensor(ob[:], p2[:, 0:128], sm[:, 1:2], o1[:], ALU.mult, ALU.add),
                     reads=[k2, K("sm1"), K("o1")], writes=[k2, K("ob")])
                P.op("vector", lambda e: e.memset(sm[:, 2:3], 0.0), writes=[K("sm2")])
                P.op("scalar", lambda e: e.activation(junk[:, 0:128], ob[:], AF.Square, accum_out=sm[:, 2:3]), reads=[K("ob"), K("sm2")], writes=[K("sm2"), K("junk")])
                P.op("vector", lambda e: e.tensor_scalar(sm[:, 2:3], sm[:, 2:3], 1.0 / 128, RMS_EPS, ALU.mult, ALU.add), reads=[K("sm2")], writes=[K("sm2")])
                P.op("scalar", lambda e: e.sqrt(sm[:, 2:3], sm[:, 2:3]), reads=[K("sm2")], writes=[K("sm2")])
                P.op("vector", lambda e: e.reciprocal(sm[:, 3:4], sm[:, 2:3]), reads=[K("sm2")], writes=[K("sm3")])
                P.op("vector", lambda e, qt=qt, h=h: e.scalar_tensor_tensor(mix[:, qt, 512 + h * 128:512 + (h + 1) * 128], ob[:], sm[:, 3:4], wsub[:], ALU.mult, ALU.mult),
                     reads=[K("ob"), K("sm3"), K("wsub")], writes=[K("mix", qt)])

    if "dbg" in D:
        P.dma("gpsimd", D["dbg"].rearrange("(t p) c -> p t c", p=128), mix[:], reads=[K("mix", i) for i in range(NT)], writes=[K("dbg")])
    xTf = xT[:].rearrange("p a b -> p (a b)").bitcast(F32)
    T = dict(wo=wb, mT=[atsp[:, 0:2, :].rearrange("p a b -> p (a b)"), atsp[:, 2:4, :].rearrange("p a b -> p (a b)")],
             mTk=[[K("at", 0), K("at", 1)], [K("sp", 0), K("sp", 1)]],
             xt=xt, g_bc=efg[:, 0:2, :].rearrange("p a b -> p (a b)"), b_bc=efg[:, 2:4, :].rearrange("p a b -> p (a b)"),
             gk=[K("ef", 0), K("ef", 1)], bk=[K("eg", 0), K("eg", 1)],
             st={n: sb("st_" + n, [128, 1]) for n in ("s1", "s2", "mean", "msq", "var", "rstd")},
             yo=[xTf[:, 0:1024], xTf[:, 1024:2048]], wok=[wkeys(ph, "wb0"), wkeys(ph, "wb1")])
    T["st"]["junk"] = junk
    outproj_ln(P, ph, C, mix, L, D["ev_w_out"][lay_j], h_rows, out_rows, ln_g, ln_b, T, ps)


def odd_mixer_seq(P, ph, C, lay_j, h_rows, out_rows, L, ln_g, ln_b):
    D = C["dram"]
    ident, ident_b = C["ident"], C["ident_b"]
    NT = L // 128
    NC = L // 512
    KSEL = min(256, L // 4)
    sb = lambda n, s, d=F32: P.sb(ph + n, s, d)
    K = lambda *a: (ph,) + a
    w_in = D["od_w_in"][lay_j]
    ps = [P.ps(ph + "ps%d" % i, [128, 512]) for i in range(8)]
    pbf = ps[6][:].bitcast(BF16)

    xt = [sb("xt%d" % i, [128, 1024]) for i in range(2)]
    xT = sb("xT", [128, 8, L], BF16)
    wb = [sb("wb%d" % i, [128, 8, 512], BF16) for i in range(2)]
    mix = sb("mix", [128, NT, 1024], BF16)
    qTc = sb("qTc", [128, 16, 512], BF16)
    ckvT = sb("ckvT", [128, L], BF16)
    ckva = sb("ckva", [128, NT, 132], BF16)
    qiT = [sb("qiT%d" % i, [128, L], BF16) for i in range(4)]
    kiT2 = sb("kiT2", [128, L], BF16)
    widx = sb("widx", [128, NT, 8])
    score = sb("score", [128, L]); wkt = sb("wkt", [128, L])
    MB = [sb("MB%d" % i, [128, L], BF16) for i in range(4)]
    rl = [sb("rl%d" % i, [128, 512]) for i in range(2)]
    efg = sb("efg", [128, 4, 512])
    atsp = sb("atsp", [128, 4, 512], BF16)
    at = [atsp[:, i, :] for i in range(2)]
    kaug = sb("kaug", [9, L], BF16)
    qaugc = sb("qaugc", [9, 16, 512], BF16)
    wuv = sb("wuv", [128, 16, 64], BF16)
    kvw = sb("kvw", [128, 128])
    oh = sb("oh", [128, 128], BF16); ohT = sb("ohT", [128, 128], BF16)
    m8 = sb("m8", [128, 8]); sm = sb("sm", [128, 8])
    junk = sb("junk", [128, 1024], BF16)

    P.dma("gpsimd", kaug[:], D["c_kaug9"][:, 0:L], writes=[K("kaug")])
    P.dma("gpsimd", wuv[:], D["od_w_uv"][lay_j].rearrange("h c d -> c h d"), writes=[K("wuv")])
    P.dma("sync", kvw[:], D["od_kv_norm_w"][lay_j].partition_broadcast(128), writes=[K("kvw")])
    P.op("vector", lambda e: e.memset(ckva[:], 1.0), writes=[K("ckva", i) for i in range(NT)])

    load_xT(P, ph, C, h_rows, L, xT, xt, ps)
    wslot = [0]
    def next_w(c0, ncols=512, dst0=0, new=True):
        if new:
            wslot[0] += 1
        s = wslot[0] % 2
        load_w_cols(P, ph, wb[s], "wb%d" % s, w_in, c0, ncols, dst0=dst0)
        return wb[s], wkeys(ph, "wb%d" % s, (dst0,))

    wt, wk = next_w(2048, 128)
    def ev_ckv(i, pa, pk):
        P.op("vector", lambda e: e.memset(sm[:, 0:1], 0.0), writes=[K("sm0")])
        P.op("scalar", lambda e: e.activation(junk[:, 0:128], pa, AF.Square, accum_out=sm[:, 0:1]), reads=[pk, K("sm0")], writes=[pk, K("sm0"), K("junk")])
        P.op("vector", lambda e: e.tensor_scalar(sm[:, 0:1], sm[:, 0:1], 1.0 / 128, RMS_EPS, ALU.mult, ALU.add), reads=[K("sm0")], writes=[K("sm0")])
        P.op("scalar", lambda e: e.sqrt(sm[:, 0:1], sm[:, 0:1]), reads=[K("sm0")], writes=[K("sm0")])
        P.op("vector", lambda e: e.reciprocal(sm[:, 1:2], sm[:, 0:1]), reads=[K("sm0")], writes=[K("sm1")])
        P.op("vector", lambda e: e.scalar_tensor_tensor(ckva[:, i, 0:128], pa, sm[:, 1:2], kvw[:], ALU.mult, ALU.mult),
             reads=[pk, K("sm1"), K("kvw")], writes=[pk, K("ckva", i)])
        P.op("tensor", lambda e: e.transpose(pbf[:, 0:128], ckva[:, i, 0:128], ident_b[:]), reads=[K("ckva", i), "ident_b"], writes=[K("ps", 6)])
        P.op("vector", lambda e: e.tensor_copy(ckvT[:, i * 128:(i + 1) * 128], pbf[:, 0:128]), reads=[K("ps", 6)], writes=[K("ps", 6), K("ckvT")])
    proj_tm(P, ph, wt, wk, 0, 128, xT, L, ps, [2, 3], ev_ckv)
    wt, wk = next_w(2176, 512)
    for j in range(4):
        proj_fm(P, ph, wt, wk, j * 128, 128, xT, L, ps, [2, 3],
                lambda ch, pa, pk, j=j: P.op("scalar", lambda e: e.activation(qiT[j][:, ch * 512:(ch + 1) * 512], pa, AF.Identity, scale=0.125),
                                            reads=[pk], writes=[pk, K("qiT")]))
    wt, wk0 = next_w(2688, 64, dst0=0)
    _, wk1 = next_w(2688, 64, dst0=64, new=False)
    _, wk2 = next_w(2752, 8, dst0=128, new=False)
    proj_fm(P, ph, wt, wk0 + wk1, 0, 128, xT, L, ps, [2, 3],
            lambda ch, pa, pk: P.op("vector", lambda e: e.tensor_copy(kiT2[:, ch * 512:(ch + 1) * 512], pa), reads=[pk], writes=[pk, K("kiT2")]))
    proj_tm(P, ph, wt, wk2, 128, 8, xT, L, ps, [2, 3],
            lambda i, pa, pk: P.op("vector", lambda e: e.tensor_scalar(widx[:, i, :], pa, 8 ** -0.5, None, ALU.mult), reads=[pk], writes=[pk, K("widx")]))

    if "dbg2" in D and C.get("dumpc", 0) == -1:
        P.dma("gpsimd", D["dbg2"][:, 0:L], kiT2[:, 0:L], reads=[K("kiT2")], writes=[K("dbg2")])
        P.dma("gpsimd", D["dbg3"][:, 0:L], qiT[0][:, 0:L], reads=[K("qiT")], writes=[K("dbg3")])
        P.dma("gpsimd", D["dbg"][0:128, 0:NT * 8], widx[:].rearrange("p a b -> p (a b)"), reads=[K("widx")], writes=[K("dbgw")])
    def dump_w(stage):
        if "dbg2" in D and C.get("dumpat", None) == stage:
            P.dma("gpsimd", D["dbg"][0:128, 0:NT * 8], widx[:].rearrange("p a b -> p (a b)"), reads=[K("widx")], writes=[K("dbgw")])
    it = 0
    for c in range(NC):
        for g in range(4):
            if c == 0:
                dump_w(10 + g)
            wt, wk = next_w(g * 512, 512)
            if c == 0:
                dump_w(20 + g)
            for hh in range(4):
                h = g * 4 + hh
                pi = 2 + h % 2
                def mm(e, hh=hh, pi=pi, wt=wt, c=c):
                    for kk in range(8):
                        r = e.matmul(ps[pi][:], wt[:, kk, hh * 128:(hh + 1) * 128], xT[:, kk, c * 512:(c + 1) * 512], start=(kk == 0), stop=(kk == 7))
                    return r
                P.op("tensor", mm, reads=wk + [K("xT", c)], writes=[K("ps", pi)])
                P.op("scalar", lambda e, h=h, pi=pi: e.activation(qTc[:, h, :], ps[pi][:], AF.Identity, scale=128 ** -0.5),
                     reads=[K("ps", pi)], writes=[K("ps", pi), K("qTc", h)])
        if c == 0:
            dump_w(1)
        P.dma("gpsimd", qaugc[:], D["c_qaug16"][:, :, c * 512:(c + 1) * 512].rearrange("h r l -> r h l"), writes=[K("qaugc")])
        if c == 0:
            P.op("vector", lambda e: e.engine_nop() if False else e.memset(sm[:, 7:8], 0.0), reads=[K("qaugc")], writes=[K("sm7")])
            if C.get("dumpat", None) == 2:
                P.dma("gpsimd", D["dbg"][0:128, 0:NT * 8], widx[:].rearrange("p a b -> p (a b)"), reads=[K("widx"), K("sm7")], writes=[K("dbgw")])
        for qs in range(4):
            qt = 4 * c + qs
            n_s = (qt + 1) * 128
            for sc in range((n_s + 511) // 512):
                w = min(512, n_s - sc * 512)
                for ih in range(8):
                    b = it % 2
                    it += 1
                    pi = 2 + b
                    P.op("tensor", lambda e, pi=pi, ih=ih, qt=qt, sc=sc, w=w: e.matmul(
                        ps[pi][:, 0:w], qiT[ih // 2][(ih % 2) * 64:(ih % 2) * 64 + 64, qt * 128:(qt + 1) * 128],
                        kiT2[(ih % 2) * 64:(ih % 2) * 64 + 64, sc * 512:sc * 512 + w], start=True, stop=True),
                         reads=[K("qiT"), K("kiT2")], writes=[K("ps", pi)])
                    P.op("scalar", lambda e, pi=pi, b=b, w=w: e.activation(rl[b][:, 0:w], ps[pi][:, 0:w], AF.Relu), reads=[K("ps", pi)], writes=[K("ps", pi), K("rl", b)])
                    if ih == 0:
                        P.op("vector", lambda e, b=b, w=w, sc=sc, qt=qt, ih=ih: e.tensor_scalar(score[:, sc * 512:sc * 512 + w], rl[b][:, 0:w], widx[:, qt, ih:ih + 1], None, ALU.mult),
                             reads=[K("rl", b), K("widx")], writes=[K("score")])
                    else:
                        P.op("vector", lambda e, b=b, w=w, sc=sc, qt=qt, ih=ih: e.scalar_tensor_tensor(score[:, sc * 512:sc * 512 + w], rl[b][:, 0:w], widx[:, qt, ih:ih + 1],
                                                                                                  score[:, sc * 512:sc * 512 + w], ALU.mult, ALU.add),
                             reads=[K("rl", b), K("widx"), K("score")], writes=[K("score")])
            P.op("gpsimd", lambda e, qt=qt: e.affine_select(score[:, qt * 128:(qt + 1) * 128], score[:, qt * 128:(qt + 1) * 128], [[-1, 128]], ALU.is_ge, -1e30,
                                                         base=0, channel_multiplier=1), reads=[K("score")], writes=[K("score")])
            if c == 0 and qs == 0:
                dump_w(3)
            if c == 0 and qs == 2:
                dump_w(4)
            if qt * 128 >= KSEL:
                R = KSEL // 8
                for r in range(R):
                    src = score if r == 0 else wkt
                    P.op("vector", lambda e, src=src, n_s=n_s: e.max(m8[:], src[:, 0:n_s]), reads=[K("score"), K("wkt")], writes=[K("m8")])
                    if r < R - 1:
                        P.op("vector", lambda e, src=src, n_s=n_s: e.match_replace(wkt[:, 0:n_s], m8[:], src[:, 0:n_s], -1e30),
                             reads=[K("score"), K("wkt"), K("m8")], writes=[K("wkt")])
                P.op("vector", lambda e, qs=qs, n_s=n_s: e.tensor_scalar(MB[qs][:, 0:n_s], score[:, 0:n_s], m8[:, 7:8], NEGM, ALU.is_lt, ALU.mult),
                     reads=[K("score"), K("m8")], writes=[K("MB", qs)])
            else:
                P.op("vector", lambda e, qs=qs, n_s=n_s: e.tensor_scalar(MB[qs][:, 0:n_s], score[:, 0:n_s], -1e29, NEGM, ALU.is_lt, ALU.mult),
                     reads=[K("score")], writes=[K("MB", qs)])
        if "dbg2" in D and C.get("dumpc", 0) == -2 and c == 1:
            P.dma("gpsimd", D["dbg2"][:, 0:L], kiT2[:, 0:L], reads=[K("kiT2")], writes=[K("dbg2")])
            P.dma("gpsimd", D["dbg3"][:, 0:L], qiT[0][:, 0:L], reads=[K("qiT")], writes=[K("dbg3")])
            P.dma("gpsimd", D["dbg"][0:128, 0:NT * 8], widx[:].rearrange("p a b -> p (a b)"), reads=[K("widx")], writes=[K("dbgw")])
        if "dbg2" in D and c == C.get("dumpc", 0):
            P.dma("gpsimd", D["dbg2"][:, 0:L], MB[3][:, 0:L], reads=[K("MB", 3)], writes=[K("dbg2")])
            P.dma("sync", D["dbg3"][:, 0:L], score[:, 0:L], reads=[K("score")], writes=[K("dbg3")])
        if c == 0:
            dump_w(5)
        nk = 4 * c + 4
        for h in range(16):
            pos = [ps[4], ps[5]]
            for kt in range(nk - 1, -1, -1):
                j = kt - 4 * c
                jm = max(j, 0)
                q0 = jm * 128
                b = it % 2
                it += 1
                pz, pzk = ps[b], K("ps", b)
                def sc_mm(e, pz=pz, kt=kt, q0=q0, jm=jm, h=h):
                    e.matmul(pz[:, q0:512], ckvT[:, kt * 128:(kt + 1) * 128], qTc[:, h, q0:512], start=True, stop=False)
                    r = e.matmul(pz[:, q0:512], kaug[:, kt * 128:(kt + 1) * 128], qaugc[:, h, q0:512], start=False, stop=False)
                    for qs in range(jm, 4):
                        r = e.matmul(pz[:, qs * 128:(qs + 1) * 128], MB[qs][:, kt * 128:(kt + 1) * 128], ident_b[:], start=False, stop=(qs == 3))
                    return r
                P.op("tensor", sc_mm, reads=[K("ckvT"), K("qTc", h), K("kaug"), K("qaugc"), "ident_b"] + [K("MB", q) for q in range(jm, 4)], writes=[pzk])
                P.op("scalar", lambda e, b=b, pz=pz, q0=q0: e.activation(at[b][:, q0:512], pz[:, q0:512], AF.Exp), reads=[pzk], writes=[pzk, K("at", b)])
                def pv(e, b=b, kt=kt, c=c, jm=jm):
                    r = None
                    for qs in range(3, jm - 1, -1):
                        qt = 4 * c + qs
                        r = e.matmul(pos[qs // 2][:, (qs % 2) * 256:(qs % 2) * 256 + 129], at[b][:, qs * 128:(qs + 1) * 128], ckva[:, kt, 0:129],
                                     start=(kt == qt and qs % 2 == 1), stop=(kt == 0 and qs % 2 == 0))
                    return r
                P.op("tensor", pv, reads=[K("at", b), K("ckva", kt)], writes=[K("ps", 4), K("ps", 5)])
            for qs in range(4):
                qt = 4 * c + qs
                p1 = ps[4 + qs // 2][:, (qs % 2) * 256:(qs % 2) * 256 + 129]
                k1 = K("ps", 4 + qs // 2)
                P.op("vector", lambda e, p1=p1: e.reciprocal(sm[:, 2:3], p1[:, 128:129]), reads=[k1], writes=[k1, K("sm2")])
                P.op("vector", lambda e, p1=p1: e.tensor_scalar(oh[:], p1[:, 0:128], sm[:, 2:3], None, ALU.mult), reads=[k1, K("sm2")], writes=[k1, K("oh")])
                P.op("tensor", lambda e: e.transpose(pbf[:, 0:128], oh[:], ident_b[:]), reads=[K("oh"), "ident_b"], writes=[K("ps", 6)])
                P.op("vector", lambda e: e.tensor_copy(ohT[:], pbf[:, 0:128]), reads=[K("ps", 6)], writes=[K("ps", 6), K("ohT")])
                P.op("tensor", lambda e, h=h: e.matmul(ps[7][:, 0:64], ohT[:], wuv[:, h, :], start=True, stop=True), reads=[K("ohT"), K("wuv")], writes=[K("ps", 7)])
                P.op("scalar", lambda e, qt=qt, h=h: e.activation(mix[:, qt, h * 64:(h + 1) * 64], ps[7][:, 0:64], AF.Identity),
                     reads=[K("ps", 7)], writes=[K("ps", 7), K("mix", qt)])

    if "dbg" in D and C.get("dumpc", 0) >= 0 and C.get("dumpat", None) is None:
        P.dma("gpsimd", D["dbg"].rearrange("(t p) c -> p t c", p=128), mix[:], reads=[K("mix", i) for i in range(NT)], writes=[K("dbg")])
    xTf = xT[:].rearrange("p a b -> p (a b)").bitcast(F32)
    T = dict(wo=wb, mT=[atsp[:, 0:2, :].rearrange("p a b -> p (a b)"), atsp[:, 2:4, :].rearrange("p a b -> p (a b)")],
             mTk=[[K("at", 0), K("at", 1)], [K("sp", 0), K("sp", 1)]],
             xt=xt, g_bc=efg[:, 0:2, :].rearrange("p a b -> p (a b)"), b_bc=efg[:, 2:4, :].rearrange("p a b -> p (a b)"),
             gk=[K("ef", 0), K("ef", 1)], bk=[K("eg", 0), K("eg", 1)],
             st={n: sb("st_" + n, [128, 1]) for n in ("s1", "s2", "mean", "msq", "var", "rstd")},
             yo=[xTf[:, 0:1024], xTf[:, 1024:2048]], wok=[wkeys(ph, "wb0"), wkeys(ph, "wb1")])
    T["st"]["junk"] = junk
    outproj_ln(P, ph, C, mix, L, D["od_w_out"][lay_j], h_rows, out_rows, ln_g, ln_b, T, ps)


def make_consts(L=2048):
    s = np.arange(128)[:, None]; q = np.arange(512)[None, :]
    mstrict = np.stack([((q - j * 128) > s) for j in range(4)], axis=1).astype(np.float32)
    mcausal = np.stack([((q - j * 128) >= s) for j in range(4)], axis=1).astype(np.float32)
    jj = np.arange(128)[:, None]; ss = np.arange(128)[None, :]
    uincl = (jj >= ss).astype(np.float32)
    t = np.arange(L)
    hi = (t // 256) * 256; lo = t % 256
    def aug(slopes):
        qa = np.stack([np.stack([np.full(L, c), np.full(L, c), -c * hi, -c * lo]) for c in slopes]).astype(np.float32)
        return qa
    sl4 = 2.0 ** (-8.0 * np.arange(1, 5) / 4)
    sl16 = 2.0 ** (-8.0 * np.arange(1, 17) / 16)
    kaug = np.stack([hi, lo, np.ones(L), np.ones(L)]).astype(np.float32)
    import ml_dtypes
    def bsplit(v):
        v = np.asarray(v, dtype=np.float64); outp = []
        for _ in range(3):
            p = v.astype(ml_dtypes.bfloat16).astype(np.float64); outp.append(p); v = v - p
        return outp
    q9 = []
    for c in sl16:
        c1, c2, c3 = bsplit(np.full(L, c)); v1, v2, v3 = bsplit(c * t.astype(np.float64))
        q9.append(np.stack([c1, c1, c2, c2, c3, c3, -v1, -v2, -v3]))
    qaug9 = np.stack(q9).astype(np.float32)
    kaug9 = np.stack([hi, lo, hi, lo, hi, lo, np.ones(L), np.ones(L), np.ones(L)]).astype(np.float32)
    return dict(c_mstrict=mstrict, c_mcausal=mcausal, c_uincl=uincl, c_qaug4=aug(sl4), c_qaug16=qaug9, c_kaug9=kaug9, c_kaug=kaug,
                c_ident=np.eye(128, dtype=np.float32))


NCORES = 8
SEQ = 2048
NTOK = 2 * SEQ
N_EXPERTS = 32
_W_NAMES = ["ev_w_in", "ev_w_out", "ev_lambda_q1", "ev_lambda_k1", "ev_lambda_q2", "ev_lambda_k2", "ev_subln_w",
            "od_w_in", "od_kv_norm_w", "od_w_uv", "od_w_out", "ln_mix_g", "ln_mix_b", "router_w", "router_b",
            "exp_w_gu", "exp_b_gu", "exp_w_down", "exp_b_down", "ln_ffn_g", "ln_ffn_b"]


def build_program(shapes, cshapes):
    nc = bass.Bass("TRN2", target_bir_lowering=False)
    D = {}
    for n in _W_NAMES:
        D[n] = nc.dram_tensor(n, list(shapes[n]), F32, kind="ExternalInput").ap()
    for n, s in cshapes.items():
        D[n] = nc.dram_tensor(n, list(s), F32, kind="ExternalInput").ap()
    x = nc.dram_tensor("x", [NTOK, 1024], F32, kind="ExternalInput").ap()
    out = nc.dram_tensor("out", [NTOK, 1024], F32, kind="ExternalOutput").ap()
    h1 = nc.dram_tensor("h1", [NTOK, 1024], F32, kind="Internal").ap()
    h2 = nc.dram_tensor("h2", [NTOK, 1024], F32, kind="Internal").ap()
    h3 = nc.dram_tensor("h3", [NTOK, 1024], F32, kind="Internal").ap()
    with ExitStack() as st:
        P = Prog(nc, st)
        ident = P.sb("ident", [128, 128]); ident_b = P.sb("ident_b", [128, 128], BF16)
        P.dma("sync", ident[:], D["c_ident"], writes=["ident"])
        P.dma("gpsimd", ident_b[:], D["c_ident"], writes=["ident_b"])
        C = {"dram": D, "ident": ident, "ident_b": ident_b}
        lam_init0 = 0.8 - 0.6 * math.exp(-0.3 * 0)

        def phase(fn):
            with ExitStack() as ts:
                P.stack = ts
                fn()
                P.barrier()
            P.stack = st

        for sq in range(2):
            phase(lambda sq=sq: even_mixer_seq(P, "e%d" % sq, C, 0, x[sq * SEQ:(sq + 1) * SEQ, :], h1[sq * SEQ:(sq + 1) * SEQ, :], SEQ,
                                                lam_init0, D["ln_mix_g"][0], D["ln_mix_b"][0]))
        phase(lambda: moe_phase(P, "m0", C, 0, h1, h2, NTOK, N_EXPERTS))
        for sq in range(2):
            phase(lambda sq=sq: odd_mixer_seq(P, "o%d" % sq, C, 0, h2[sq * SEQ:(sq + 1) * SEQ, :], h3[sq * SEQ:(sq + 1) * SEQ, :], SEQ,
                                               D["ln_mix_g"][1], D["ln_mix_b"][1]))
        phase(lambda: moe_phase(P, "m1", C, 1, h3, out, NTOK, N_EXPERTS))
        P.final_wait()
        P.emit()
    return nc


def kernel(**inputs):
    consts = make_consts(SEQ)
    x = np.ascontiguousarray(np.asarray(inputs["x"], dtype=np.float32)).reshape(NCORES, NTOK, 1024)
    ws = {n: np.ascontiguousarray(np.asarray(inputs[n], dtype=np.float32)) for n in _W_NAMES}
    nc = build_program({n: ws[n].shape for n in _W_NAMES}, {n: v.shape for n, v in consts.items()})
    in_maps = []
    for c in range(NCORES):
        m = dict(ws)
        m.update(consts)
        m["x"] = x[c]
        in_maps.append(m)
    res = run_bass_kernel_spmd(nc, in_maps, core_ids=list(range(NCORES)))
    outs = [np.asarray(r["out"], dtype=np.float32) for r in res.results]
    return np.stack(outs, axis=0).reshape(16, SEQ, 1024)
```

```python
import math
from concourse.bass_utils import run_bass_kernel_spmd
from contextlib import ExitStack
import numpy as np
import concourse.bass as bass
import concourse.mybir as mybir

F32 = mybir.dt.float32
BF16 = mybir.dt.bfloat16
I32 = mybir.dt.int32
ALU = mybir.AluOpType
AF = mybir.ActivationFunctionType
AX = mybir.AxisListType

ENGS = ("sync", "scalar", "vector", "gpsimd", "tensor")


class Prog:
    def __init__(self, nc, stack):
        self.nc = nc
        self.stack = stack
        self.sem_stack = stack
        self.streams = {e: [] for e in ENGS}
        self.esem = {e: stack.enter_context(nc.semaphore("es_" + e)) for e in ENGS}
        self.etick = {e: 0 for e in ENGS}
        self.known = {e: {} for e in ENGS}
        self.res_w = {}
        self.res_r = {}
        self.dsem = {}
        self.semobj = {}
        for e in ENGS:
            self.semobj[id(self.esem[e])] = self.esem[e]
        self.n_ops = 0

    def sb(self, name, shape, dt=F32):
        return self.stack.enter_context(self.nc.sbuf_tensor(name, list(shape), dt))

    def ps(self, name, shape, dt=F32):
        return self.stack.enter_context(self.nc.psum_tensor(name, list(shape), dt))

    def _dma_sem(self, key):
        if key not in self.dsem:
            s = self.sem_stack.enter_context(self.nc.semaphore("ds_%d" % len(self.dsem)))
            self.dsem[key] = [s, 0]
            self.semobj[id(s)] = s
        return self.dsem[key]

    def _deps(self, eng, reads, writes):
        deps = {}

        def add(ev):
            if ev is None:
                return
            s, v = ev
            if deps.get(s, 0) < v:
                deps[s] = v

        for k in reads:
            add(self.res_w.get(k))
        for k in writes:
            add(self.res_w.get(k))
            for s, v in self.res_r.get(k, {}).items():
                add((s, v))
        waits = []
        kn = self.known[eng]
        for s, v in deps.items():
            if kn.get(s, 0) < v:
                kn[s] = v
                waits.append((s, v))
        return waits

    def _commit(self, ev, reads, writes):
        s, v = ev
        for k in reads:
            d = self.res_r.setdefault(k, {})
            if d.get(s, 0) < v:
                d[s] = v
        for k in writes:
            self.res_w[k] = ev
            self.res_r[k] = {}

    def op(self, eng, fn, reads=(), writes=()):
        waits = self._deps(eng, reads, writes)
        self.etick[eng] += 1
        ev = (id(self.esem[eng]), self.etick[eng])
        self.streams[eng].append((waits, fn, ev, 1))
        self._commit(ev, reads, writes)
        self.n_ops += 1

    def dma(self, eng, out, in_, reads=(), writes=(), slot=None, **kw):
        if slot is None:
            k0 = writes[0] if writes else reads[0]
            slot = ("_slot",) + tuple(k0[1:]) if isinstance(k0, tuple) and len(k0) > 1 else ("_slot", k0)
        ds = self._dma_sem(slot)
        waits = self._deps(eng, reads, writes)
        ds[1] += 16
        ev = (id(ds[0]), ds[1])
        fn = lambda e, out=out, in_=in_, kw=kw: e.dma_start(out=out, in_=in_, **kw)
        self.streams[eng].append((waits, fn, ev, 16))
        self._commit(ev, reads, writes)
        self.n_ops += 1

    def barrier(self):
        evs = [(id(self.esem[e]), self.etick[e]) for e in ENGS if self.etick[e] > 0]
        evs += [(id(s), c) for s, c in self.dsem.values() if c > 0]
        for e in ENGS:
            kn = self.known[e]
            waits = []
            for s, v in evs:
                if s == id(self.esem[e]) and False:
                    continue
                if kn.get(s, 0) < v:
                    kn[s] = v
                    waits.append((s, v))
            if waits:
                self.streams[e].append((waits, None, None, 0))

    def final_wait(self, eng="sync"):
        evs = [(id(s), c) for s, c in self.dsem.values() if c > 0]
        evs += [(id(self.esem[e]), self.etick[e]) for e in ENGS if self.etick[e] > 0 and e != eng]
        kn = self.known[eng]
        waits = [(s, v) for s, v in evs if kn.get(s, 0) < v]
        self.streams[eng].append((waits, None, None, 0))

    def emit(self):
        nc = self.nc
        with nc.Block() as block:
            def runner(name):
                def run(e):
                    for waits, fn, ev, inc in self.streams[name]:
                        for s, v in waits:
                            e.wait_ge(self.semobj[s], v)
                        if fn is not None:
                            inst = fn(e)
                            inst.then_inc(self.semobj[ev[0]], inc)
                return run
            block.sync(runner("sync"))
            block.scalar(runner("scalar"))
            block.vector(runner("vector"))
            block.gpsimd(runner("gpsimd"))
            block.tensor(runner("tensor"))


DEPTH = 2
ALPHA = (2 * DEPTH) ** 0.25
LN_EPS = 1e-5
SW_LIMIT = 7.0
SW_ALPHA = 1.702


def layer_norm_tile(P, ph, src_ap, src_keys, dst_ap, dst_key, g_bc, b_bc, st, idx):
    s1, s2, mean, msq, var, rstd, junk = (st[k] for k in ("s1", "s2", "mean", "msq", "var", "rstd", "junk"))
    k = lambda n: (ph, "ln_" + n)
    P.op("vector", lambda e: e.reduce_sum(s1[:, 0:1], src_ap, axis=AX.X), reads=list(src_keys), writes=[k("s1")])
    P.op("vector", lambda e: e.memset(s2[:, 0:1], 0.0), writes=[k("s2")])
    P.op("scalar", lambda e: e.activation(junk[:], src_ap, AF.Square, accum_out=s2[:, 0:1]),
         reads=list(src_keys) + [k("s2")], writes=[k("s2"), k("junk")])
    P.op("vector", lambda e: e.tensor_scalar(mean[:, 0:1], s1[:, 0:1], 1.0 / 1024, None, ALU.mult),
         reads=[k("s1")], writes=[k("mean")])
    P.op("vector", lambda e: e.tensor_tensor(msq[:, 0:1], mean[:, 0:1], mean[:, 0:1], ALU.mult),
         reads=[k("mean")], writes=[k("msq")])
    P.op("vector", lambda e: e.scalar_tensor_tensor(var[:, 0:1], s2[:, 0:1], 1.0 / 1024, msq[:, 0:1], ALU.mult, ALU.subtract),
         reads=[k("s2"), k("msq")], writes=[k("var")])
    P.op("vector", lambda e: e.tensor_scalar(var[:, 0:1], var[:, 0:1], LN_EPS, None, ALU.add),
         reads=[k("var")], writes=[k("var")])
    P.op("scalar", lambda e: e.sqrt(var[:, 0:1], var[:, 0:1]), reads=[k("var")], writes=[k("var")])
    P.op("vector", lambda e: e.reciprocal(rstd[:, 0:1], var[:, 0:1]), reads=[k("var")], writes=[k("rstd")])
    P.op("vector", lambda e: e.tensor_scalar(dst_ap, src_ap, mean[:, 0:1], rstd[:, 0:1], ALU.subtract, ALU.mult),
         reads=list(src_keys) + [k("mean"), k("rstd")], writes=[dst_key])
    P.op("vector", lambda e: e.tensor_tensor(dst_ap, dst_ap, g_bc[:], ALU.mult), reads=[dst_key, (ph, "g_bc")], writes=[dst_key])
    P.op("vector", lambda e: e.tensor_tensor(dst_ap, dst_ap, b_bc[:], ALU.add), reads=[dst_key, (ph, "b_bc")], writes=[dst_key])


def moe_phase(P, ph, C, lay, h_in, h_out, NT, E, stage=99):
    nc = P.nc
    D = C["dram"]
    ident = C["ident"]
    NBLK = NT // 1024
    sb = lambda n, s, d=F32: P.sb(ph + n, s, d)
    K = lambda *a: (ph,) + a

    rw = sb("rw", [128, 8, E]); rb_bc = sb("rb", [128, E])
    bguT = sb("bguT", [128, 16, E]); bd = sb("bd", [E, 1024])
    g_bc = sb("g_bc", [128, 1024]); b_bc = sb("b_bc", [128, 1024])
    xt = [sb("xt%d" % i, [128, 1024]) for i in range(2)]
    hT = sb("hT", [128, 8, 1024], BF16)
    hTf = sb("hTf", [128, 8, 128])
    acc = sb("acc", [128, 8, 1024])
    wgu = [sb("wgu%d" % i, [128, 8, 2048], BF16) for i in range(2)]
    wd = [sb("wd%d" % i, [128, 8, 1024], BF16) for i in range(2)]
    actT = [sb("actT%d" % i, [128, 8, 512], BF16) for i in range(2)]
    gc = [sb("gc%d" % i, [128, 512]) for i in range(2)]
    ua = [sb("ua%d" % i, [128, 512]) for i in range(2)]
    sl = [sb("sl%d" % i, [128, 512]) for i in range(2)]
    cw = sb("cw", [128, 8, E]); cwT = sb("cwT", [E, 128]); cws = sb("cws", [128, 8, E])
    lg = sb("lg", [128, E]); ex = sb("ex", [128, E]); em = sb("em", [128, E])
    m8 = sb("m8", [128, 8]); sm = sb("sm", [128, 8])
    st = {n: sb("st_" + n, [128, 1]) for n in ("s1", "s2", "mean", "msq", "var", "rstd")}
    st["junk"] = sb("junk", [128, 1024], BF16)

    pA = [P.ps(ph + "pA%d" % i, [128, 512]) for i in range(2)]
    pB = [P.ps(ph + "pB%d" % i, [128, 512]) for i in range(2)]
    pY = [P.ps(ph + "pY%d" % i, [128, 512]) for i in range(2)]
    pT = [P.ps(ph + "pT%d" % i, [128, 4, 128]) for i in range(2)]

    import os
    SK = os.environ.get("SKIP", "")
    if "rw" not in SK:
        P.dma("sync", rw[:], D["router_w"][lay].rearrange("(k p) e -> p k e", p=128), writes=[K("rw")])
    if "rb" not in SK:
        P.dma("sync", rb_bc[:], D["router_b"][lay].partition_broadcast(128), writes=[K("rb")])
    bkeys = [K("acc", 0, 0), K("acc", 0, 1), K("acc", 1, 0), K("acc", 1, 1)]
    P.dma("sync", acc[0:E, 0:2, :], D["exp_b_gu"][lay].rearrange("e (a f) -> e a f", a=2), writes=bkeys, slot="bguraw")
    if "bd" not in SK:
        P.dma("sync", bd[:], D["exp_b_down"][lay], writes=[K("bd")])
    if "gb" not in SK:
        P.dma("sync", g_bc[:], D["ln_ffn_g"][lay].partition_broadcast(128), writes=[K("g_bc")])
    if "gb" not in SK:
        P.dma("sync", b_bc[:], D["ln_ffn_b"][lay].partition_broadcast(128), writes=[K("b_bc")])
    for j in range(16):
        h = j % 2
        P.op("tensor", lambda e, j=j, h=h: e.transpose(pT[h][:, 0, 0:E], acc[0:E, j // 8, (j % 8) * 128:(j % 8 + 1) * 128], ident[0:E, 0:E]),
             reads=bkeys + ["ident"], writes=[K("pT", h)])
        P.op("vector", lambda e, j=j, h=h: e.tensor_copy(bguT[:, j, :], pT[h][:, 0, 0:E]),
             reads=[K("pT", h)], writes=[K("bguT")])

    if stage == 0:
        return
    def load_w(b, e, which="both"):
        ws = (b * E + e) % 2
        if "now" in SK:
            return
        if which in ("both", "gu"):
            for kk in range(8):
                P.dma("gpsimd", wgu[ws][:, kk, :], D["exp_w_gu"][lay, e, kk * 128:(kk + 1) * 128, :], writes=[K("wgu", ws, kk)], slot=("wgu", ws))
        if which in ("both", "d"):
            for kk in range(8):
                P.dma("gpsimd", wd[ws][:, kk, :], D["exp_w_down"][lay, e, kk * 128:(kk + 1) * 128, :], writes=[K("wd", ws, kk)], slot=("wd", ws))

    for b in range(NBLK):
        load_w(b, 0)
        for i in range(8 if "noa" not in SK else 0):
            tok0 = b * 1024 + i * 128
            s = i % 2
            P.dma("sync", xt[s][:], h_in[tok0:tok0 + 128, :], writes=[K("xt", s)])
            for h in range(2):
                def tr(e, s=s, h=h):
                    for q in range(4):
                        kk = h * 4 + q
                        r = e.transpose(pT[h][:, q, :], xt[s][:, kk * 128:(kk + 1) * 128], ident[:])
                    return r
                P.op("tensor", tr, reads=[K("xt", s), "ident"], writes=[K("pT", h)])
                P.op(os.environ.get("EVE", "scalar"), lambda e, h=h, i=i: (e.activation(hT[:, 4 * h:4 * h + 4, i * 128:(i + 1) * 128], pT[h][:], AF.Identity) if os.environ.get("EVE", "scalar") == "scalar" else e.tensor_copy(hT[:, 4 * h:4 * h + 4, i * 128:(i + 1) * 128], pT[h][:])),
                     reads=[K("pT", h)], writes=[K("hT", i // 4)])
                P.op("vector", lambda e, h=h: e.tensor_copy(hTf[:, 4 * h:4 * h + 4, :], pT[h][:]),
                     reads=[K("pT", h), K("hT", i // 4)] , writes=[K("hTf", h)])
            CUT = int(os.environ.get("CUT", "99"))
            if CUT < 2:
                continue
            pL = pY[0]
            def rt(e):
                for kk in range(8):
                    r = e.matmul(pL[:, 0:E], hTf[:, kk, :], rw[:, kk, :], start=(kk == 0), stop=(kk == 7))
                return r
            P.op("tensor", rt, reads=[K("hTf", 0), K("hTf", 1), K("rw")], writes=[K("pY", 0)])
            P.op("vector", lambda e: e.tensor_tensor(lg[:], pL[:, 0:E], rb_bc[:], ALU.add), reads=[K("pY", 0), K("rb")], writes=[K("lg")])
            if CUT < 3:
                continue
            P.op("vector", lambda e: e.max(m8[:], lg[:]), reads=[K("lg")], writes=[K("m8")])
            P.op("vector", lambda e: e.tensor_scalar(sm[:, 0:1], m8[:, 0:1], -1.0, None, ALU.mult), reads=[K("m8")], writes=[K("negmx")])
            P.op("scalar", lambda e: e.activation(ex[:], lg[:], AF.Exp, bias=sm[:, 0:1], scale=1.0), reads=[K("lg"), K("negmx")], writes=[K("ex")])
            P.op("vector", lambda e: e.scalar_tensor_tensor(em[:], lg[:], m8[:, 3:4], ex[:], ALU.is_ge, ALU.mult),
                 reads=[K("lg"), K("m8"), K("ex")], writes=[K("em")])
            P.op("vector", lambda e: e.reduce_sum(sm[:, 1:2], em[:], axis=AX.X), reads=[K("em")], writes=[K("Z")])
            P.op("vector", lambda e: e.reciprocal(sm[:, 2:3], sm[:, 1:2]), reads=[K("Z")], writes=[K("rz")])
            P.op("vector", lambda e, i=i: e.tensor_scalar(cw[:, i, :], em[:], sm[:, 2:3], None, ALU.mult), reads=[K("em"), K("rz")], writes=[K("cw", i)])
            P.op("vector", lambda e, i=i: e.tensor_scalar(cws[:, i, :], cw[:, i, :], 1.0 / SW_ALPHA, None, ALU.mult), reads=[K("cw", i)], writes=[K("cws", i)])
            if CUT < 4:
                continue
            pC = pY[1]
            P.op("tensor", lambda e, i=i: e.transpose(pC[0:E, 0:128], cw[:, i, :], ident[:]), reads=[K("cw", i), "ident"], writes=[K("pY", 1)])
            P.op("scalar", lambda e: e.copy(cwT[:], pC[0:E, 0:128]), reads=[K("pY", 1)], writes=[K("cwT")])
            if CUT < 5:
                continue
            for n in range(2):
                P.op("tensor", lambda e, n=n: e.matmul(pA[n][:], cwT[:], bd[:, n * 512:(n + 1) * 512], start=True, stop=True),
                     reads=[K("cwT"), K("bd")], writes=[K("pA", n)])
                P.op("vector", lambda e, n=n, s=s, i=i: e.scalar_tensor_tensor(acc[:, i, n * 512:(n + 1) * 512], xt[s][:, n * 512:(n + 1) * 512],
                                                                      ALPHA, pA[n][:], ALU.mult, ALU.add),
                     reads=[K("xt", s), K("pA", n)], writes=[K("acc", i, n)])
        if stage == 1:
            return
        cntb = [0]
        pend = [None]

        def flush_act():
            if pend[0] is not None:
                p, i, a_s = pend[0]
                P.op("vector", lambda e, p=p, i=i, a_s=a_s: e.scalar_tensor_tensor(actT[a_s][:, i, :], ua[p][:], 1.0 - SW_LIMIT, sl[p][:], ALU.add, ALU.mult),
                     reads=[K("sl", p), K("ua", p)], writes=[K("actT", a_s)])
                pend[0] = None

        def gu_unit(ei, c, i):
            ws = (b * E + ei) % 2
            a_s = c % 2
            p = cntb[0] % 2
            cntb[0] += 1
            def mm_g(e, off, ps, i=i, c=c, ws=ws):
                for kk in range(8):
                    r = e.matmul(ps[:], wgu[ws][:, kk, off + i * 128: off + (i + 1) * 128], hT[:, kk, c * 512:(c + 1) * 512],
                                 start=(kk == 0), stop=(kk == 7))
                return r
            hk = [K("hT", c)]
            P.op("tensor", lambda e, p=p, f=mm_g: f(e, 0, pA[p]), reads=[K("wgu", ws, kk) for kk in range(8)] + hk, writes=[K("pA", p)])
            P.op("tensor", lambda e, p=p, f=mm_g: f(e, 1024, pB[p]), reads=[K("wgu", ws, kk) for kk in range(8)] + hk, writes=[K("pB", p)])
            P.op("vector", lambda e, p=p, i=i, ei=ei: e.tensor_scalar(gc[p][:], pA[p][:], bguT[:, i, ei:ei + 1], SW_LIMIT, ALU.add, ALU.min),
                 reads=[K("pA", p), K("bguT")], writes=[K("gc", p)])
            P.op("vector", lambda e, p=p, i=i, ei=ei: e.tensor_scalar(ua[p][:], pB[p][:], bguT[:, 8 + i, ei:ei + 1], SW_LIMIT, ALU.add, ALU.min),
                 reads=[K("pB", p), K("bguT")], writes=[K("ua", p)])
            P.op("scalar", lambda e, p=p: e.activation(sl[p][:], gc[p][:], AF.Silu, scale=SW_ALPHA),
                 reads=[K("gc", p)], writes=[K("sl", p)])
            P.op("scalar", lambda e, p=p: e.activation(ua[p][:], ua[p][:], AF.Relu, bias=SW_LIMIT),
                 reads=[K("ua", p)], writes=[K("ua", p)])
            flush_act()
            pend[0] = (p, i, a_s)

        def down_unit(ei, c, idx):
            ws = (b * E + ei) % 2
            a_s = c % 2
            j, n = idx // 2, idx % 2
            ti = c * 4 + j
            q = idx % 2
            def mm_d(e, j=j, n=n, q=q, a_s=a_s, ws=ws):
                for f in range(8):
                    r = e.matmul(pY[q][:], actT[a_s][:, f, j * 128:(j + 1) * 128], wd[ws][:, f, n * 512:(n + 1) * 512],
                                 start=(f == 0), stop=(f == 7))
                return r
            P.op("tensor", mm_d, reads=[K("actT", a_s)] + [K("wd", ws, kk) for kk in range(8)], writes=[K("pY", q)])
            P.op("vector", lambda e, q=q, ti=ti, n=n, ei=ei: e.scalar_tensor_tensor(
                acc[:, ti, n * 512:(n + 1) * 512], pY[q][:], cws[:, ti, ei:ei + 1], acc[:, ti, n * 512:(n + 1) * 512], ALU.mult, ALU.add),
                 reads=[K("pY", q), K("cws", ti), K("acc", ti, n)], writes=[K("acc", ti, n)])

        seq = [(ei, c) for ei in range(E) for c in range(2)]
        for k in range(len(seq) + 1):
            if k < len(seq):
                ei, c = seq[k]
                if c == 0 and ei + 1 < E:
                    load_w(b, ei + 1, "gu")
                if c == 1 and ei + 1 < E:
                    load_w(b, ei + 1, "d")
            for idx in range(8):
                if k < len(seq):
                    gu_unit(seq[k][0], seq[k][1], idx)
                else:
                    flush_act()
                if k >= 1:
                    if idx == 0:
                        pass
                    down_unit(seq[k - 1][0], seq[k - 1][1], idx)
            flush_act()
        if stage == 2:
            return
        for i in range(8):
            tok0 = b * 1024 + i * 128
            s = i % 2
            layer_norm_tile(P, ph, acc[:, i, :], [K("acc", i, 0), K("acc", i, 1)], acc[:, i, :], K("acc", i, 0), g_bc, b_bc, st, i)
            P.dma("sync", h_out[tok0:tok0 + 128, :], acc[:, i, :], reads=[K("acc", i, 0)], writes=[K("hout", b, i)], slot=("yo_st", s))


import math

RMS_EPS = 1e-5
NEGM = -30000.0


def load_w_cols(P, ph, wt, wkey, w_dram, c0, ncols, dst0=0):
    for kk in range(8):
        P.dma("gpsimd", wt[:, kk, dst0:dst0 + ncols], w_dram[kk * 128:(kk + 1) * 128, c0:c0 + ncols],
              writes=[(ph, wkey, kk)], slot=(wkey,))


def wkeys(ph, wkey, dsts=(0,)):
    return [(ph, wkey, kk) for kk in range(8)]


def load_xT(P, ph, C, h_rows, L, xT, xt, ps):
    ident = C["ident"]
    for i in range(L // 128):
        s = i % 2
        P.dma("sync", xt[s][:], h_rows[i * 128:(i + 1) * 128, :], writes=[(ph, "xt", s)])
        for h in range(2):
            pt = ps[h]
            def tr(e, s=s, h=h, pt=pt):
                for q in range(4):
                    kk = h * 4 + q
                    r = e.transpose(pt[:, q * 128:(q + 1) * 128], xt[s][:, kk * 128:(kk + 1) * 128], ident[:])
                return r
            P.op("tensor", tr, reads=[(ph, "xt", s), "ident"], writes=[(ph, "ps", h)])
            P.op("vector", lambda e, h=h, i=i, pt=pt: e.tensor_copy(xT[:, 4 * h:4 * h + 4, i * 128:(i + 1) * 128],
                                                                  pt[:].rearrange("p (a b) -> p a b", a=4)),
                 reads=[(ph, "ps", h)], writes=[(ph, "ps", h), (ph, "xT", i // 4)])


def proj_fm(P, ph, wt, wk, c0, M, xT, L, ps, psi, evac):
    for ch in range(L // 512):
        pi = psi[ch % len(psi)]
        def mm(e, ch=ch, pi=pi):
            for kk in range(8):
                r = e.matmul(ps[pi][0:M, :], wt[:, kk, c0:c0 + M], xT[:, kk, ch * 512:(ch + 1) * 512], start=(kk == 0), stop=(kk == 7))
            return r
        P.op("tensor", mm, reads=wk + [(ph, "xT", ch)], writes=[(ph, "ps", pi)])
        evac(ch, ps[pi][0:M, :], (ph, "ps", pi))


def proj_tm(P, ph, wt, wk, c0, N, xT, L, ps, psi, evac):
    for i in range(L // 128):
        pi = psi[i % len(psi)]
        def mm(e, i=i, pi=pi):
            for kk in range(8):
                r = e.matmul(ps[pi][:, 0:N], xT[:, kk, i * 128:(i + 1) * 128], wt[:, kk, c0:c0 + N], start=(kk == 0), stop=(kk == 7))
            return r
        P.op("tensor", mm, reads=wk + [(ph, "xT", i // 4)], writes=[(ph, "ps", pi)])
        evac(i, ps[pi][:, 0:N], (ph, "ps", pi))


def outproj_ln(P, ph, C, mix, L, w_out, h_rows, out_rows, g_vec, b_vec, T, ps):
    ident_b = C["ident_b"]
    wo, mT, xt, g_bc, b_bc, st, yo = T["wo"], T["mT"], T["xt"], T["g_bc"], T["b_bc"], T["st"], T["yo"]
    for half in range(2):
        load_w_cols(P, ph, wo[half], "wb%d" % half, w_out, half * 512, 512)
    P.dma("sync", g_bc, g_vec.partition_broadcast(128), writes=[(ph, "g_bc")] + T["gk"], slot=("g_bc",))
    P.dma("sync", b_bc, b_vec.partition_broadcast(128), writes=[(ph, "b_bc")] + T["bk"], slot=("b_bc",))
    pbf = [ps[6][:].bitcast(BF16), ps[7][:].bitcast(BF16)]
    P.barrier()
    for i in range(L // 128):
        s = i % 2
        P.dma("sync", xt[s][:], h_rows[i * 128:(i + 1) * 128, :], writes=[(ph, "xt", s)])
        def tr(e, i=i, s=s):
            for kk in range(8):
                r = e.transpose(pbf[s][:, kk * 128:(kk + 1) * 128], mix[:, i, kk * 128:(kk + 1) * 128], ident_b[:])
            return r
        P.op("tensor", tr, reads=[(ph, "mix", i), "ident_b"], writes=[(ph, "ps", 6 + s)])
        P.op("vector", lambda e, s=s: e.tensor_copy(mT[s], pbf[s]), reads=[(ph, "ps", 6 + s)], writes=[(ph, "ps", 6 + s), (ph, "mT", s)] + T["mTk"][s])
        for n in range(2):
            pi = 4 + n
            def mm(e, s=s, n=n, pi=pi):
                for kk in range(8):
                    r = e.matmul(ps[pi][:], mT[s][:, kk * 128:(kk + 1) * 128], wo[n][:, kk, :], start=(kk == 0), stop=(kk == 7))
                return r
            P.op("tensor", mm, reads=[(ph, "mT", s)] + T["wok"][n], writes=[(ph, "ps", pi)])
            P.op("vector", lambda e, s=s, n=n, pi=pi: e.scalar_tensor_tensor(yo[s][:, n * 512:(n + 1) * 512], xt[s][:, n * 512:(n + 1) * 512], ALPHA, ps[pi][:], ALU.mult, ALU.add),
                 reads=[(ph, "xt", s), (ph, "ps", pi)], writes=[(ph, "ps", pi), (ph, "yo", s, n)])
        layer_norm_tile(P, ph, yo[s], [(ph, "yo", s, 0), (ph, "yo", s, 1)], yo[s], (ph, "yo", s, 0), g_bc, b_bc, st, i)
        P.dma("sync", out_rows[i * 128:(i + 1) * 128, :], yo[s], reads=[(ph, "yo", s, 0)], writes=[(ph, "hout", i)], slot=("yo_st", s))
        P.res_w[(ph, "yo", s, 1)] = P.res_w[(ph, "yo", s, 0)]
        P.res_r[(ph, "yo", s, 1)] = dict(P.res_r[(ph, "yo", s, 0)])


def even_mixer_seq(P, ph, C, lay_j, h_rows, out_rows, L, lam_init, ln_g, ln_b):
    D = C["dram"]
    ident, ident_b = C["ident"], C["ident_b"]
    NT = L // 128
    NC = L // 512
    sb = lambda n, s, d=F32: P.sb(ph + n, s, d)
    K = lambda *a: (ph,) + a
    w_in = D["ev_w_in"][lay_j]
    ps = [P.ps(ph + "ps%d" % i, [128, 512]) for i in range(8)]

    xt = [sb("xt%d" % i, [128, 1024]) for i in range(2)]
    xT = sb("xT", [128, 8, L], BF16)
    wb = [sb("wb%d" % i, [128, 8, 512], BF16) for i in range(2)]
    va = sb("va", [128, NT, 512], BF16)
    vb = sb("vb", [128, NT, 4, 132], BF16)
    mix = sb("mix", [128, NT, 1024], BF16)
    qa = [sb("qa%d" % i, [128, L], BF16) for i in range(4)]
    ka = [sb("ka%d" % i, [128, L], BF16) for i in range(4)]
    qd = [sb("qd%d" % i, [68, L], BF16) for i in range(2)]
    kd = [sb("kd%d" % i, [68, L], BF16) for i in range(2)]
    mstrict = sb("mstrict", [128, 4, 512], BF16)
    mcausal = sb("mcausal", [128, 4, 512], BF16)
    uincl = sb("uincl", [128, 128], BF16)
    ones1 = sb("ones1", [1, 128], BF16)
    efg = sb("efg", [128, 4, 512])
    ef = [efg[:, i, :] for i in range(2)]
    eg = [efg[:, 2 + i, :] for i in range(2)]
    atsp = sb("atsp", [128, 4, 512], BF16)
    at = [atsp[:, i, :] for i in range(2)]
    spt = [atsp[:, 2 + i, :] for i in range(2)]
    suf = sb("suf", [1, 512], BF16)
    lamv = sb("lamv", [128, 4, 64]); lam = sb("lam", [128, 4])
    wsub = sb("wsub", [128, 128])
    o14 = sb("o14", [128, 4, 128]); ob4 = sb("ob4", [128, 4, 128]); sm4 = sb("sm4", [128, 4, 8])
    junk = sb("junk", [128, 1024], BF16)

    P.dma("gpsimd", mstrict[:], D["c_mstrict"], writes=[K("mstrict")])
    P.dma("gpsimd", mcausal[:], D["c_mcausal"], writes=[K("mcausal")])
    P.dma("gpsimd", uincl[:], D["c_uincl"], writes=[K("uincl")])
    P.op("vector", lambda e: e.memset(ones1[:], 1.0), writes=[K("ones1")])
    for i, nm in enumerate(["ev_lambda_q1", "ev_lambda_k1", "ev_lambda_q2", "ev_lambda_k2"]):
        P.dma("sync", lamv[:, i, :], D[nm][lay_j].partition_broadcast(128), writes=[K("lamv", i)])
    P.dma("sync", wsub[:], D["ev_subln_w"][lay_j].partition_broadcast(128), writes=[K("wsub")])
    P.op("vector", lambda e: e.tensor_scalar(wsub[:], wsub[:], 1.0 - lam_init, None, ALU.mult), reads=[K("wsub")], writes=[K("wsub")])
    for j in range(2):
        P.op("vector", lambda e, j=j: e.tensor_tensor(lamv[:, 2 * j, :], lamv[:, 2 * j, :], lamv[:, 2 * j + 1, :], ALU.mult),
             reads=[K("lamv", 2 * j), K("lamv", 2 * j + 1)], writes=[K("lamv", 2 * j)])
        P.op("vector", lambda e, j=j: e.reduce_sum(lam[:, j:j + 1], lamv[:, 2 * j, :], axis=AX.X), reads=[K("lamv", 2 * j)], writes=[K("lam", j)])
        P.op("scalar", lambda e, j=j: e.activation(lam[:, j:j + 1], lam[:, j:j + 1], AF.Exp), reads=[K("lam", j)], writes=[K("lam", j)])
    P.op("vector", lambda e: e.tensor_tensor(lam[:, 2:3], lam[:, 1:2], lam[:, 0:1], ALU.subtract), reads=[K("lam", 0), K("lam", 1)], writes=[K("lam", 2)])
    P.op("vector", lambda e: e.tensor_scalar(lam[:, 3:4], lam[:, 2:3], -lam_init, None, ALU.add), reads=[K("lam", 2)], writes=[K("nlam")])

    load_xT(P, ph, C, h_rows, L, xT, xt, ps)

    wslot = [0]
    def next_w(c0, ncols=512):
        s = wslot[0] % 2
        wslot[0] += 1
        load_w_cols(P, ph, wb[s], "wb%d" % s, w_in, c0, ncols)
        return wb[s], wkeys(ph, "wb%d" % s)

    wt, wk = next_w(1024)
    proj_tm(P, ph, wt, wk, 0, 512, xT, L, ps, [2, 3],
            lambda i, pa, pk: P.op("vector", lambda e: e.tensor_copy(va[:, i, :], pa), reads=[pk], writes=[pk, K("va", i)]))
    wt, wk = next_w(2560)
    P.op("vector", lambda e: e.memset(vb[:], 1.0), writes=[K("vb", i) for i in range(NT)])
    proj_tm(P, ph, wt, wk, 0, 512, xT, L, ps, [2, 3],
            lambda i, pa, pk: P.op("vector", lambda e: e.tensor_copy(vb[:, i, :, 0:128], pa.rearrange("p (h d) -> p h d", h=4)), reads=[pk], writes=[pk, K("vb", i)]))

    wt, wk = next_w(0)
    for j in range(4):
        proj_fm(P, ph, wt, wk, j * 128, 128, xT, L, ps, [2, 3],
                lambda ch, pa, pk, j=j: P.op("scalar", lambda e: e.activation(qa[j][:, ch * 512:(ch + 1) * 512], pa, AF.Identity, scale=0.125),
                                            reads=[pk], writes=[pk, K("qa", j)]))
    wt, wk = next_w(512)
    for j in range(4):
        proj_fm(P, ph, wt, wk, j * 128, 128, xT, L, ps, [2, 3],
                lambda ch, pa, pk, j=j: P.op("vector", lambda e: e.tensor_copy(ka[j][:, ch * 512:(ch + 1) * 512], pa),
                                            reads=[pk], writes=[pk, K("ka", j)]))

    it = 0
    suf2 = [suf, sb("suf1", [1, 512], BF16)]

    def sb_head(h):
        suf = suf2[h % 2]
        sufk = K("suf", h % 2)
        qT = qa[h // 2][(h % 2) * 64:(h % 2) * 64 + 64, :]
        kT = ka[h // 2][(h % 2) * 64:(h % 2) * 64 + 64, :]
        qk = [K("qa", h // 2), K("ka", h // 2)]
        for c in range(NC):
            po = ps[4 + (h % 2) * 2 + c % 2]
            pok = K("ps", 4 + (h % 2) * 2 + c % 2)
            nk = 4 * c + 4
            for kt in range(nk - 1, -1, -1):
                j = kt - 4 * c
                q0 = max(j, 0) * 128
                b = h % 2
                pz, pzk = ps[b], K("ps", b)
                pg, pgk = ps[2 + b], K("ps", 2 + b)
                first = False
                if kt == nk - 1:
                    P.op("vector", lambda e: e.memset(suf[:], 0.0), writes=[sufk])
                P.op("tensor", lambda e, pz=pz, kt=kt, c=c, q0=q0, kT=kT, qT=qT: e.matmul(
                    pz[:, q0:512], kT[:, kt * 128:(kt + 1) * 128], qT[:, c * 512 + q0:(c + 1) * 512], start=True, stop=True),
                     reads=qk, writes=[pzk])
                P.op("scalar", lambda e, b=b, pz=pz, q0=q0: e.activation(ef[b][:, q0:512], pz[:, q0:512], AF.Exp), reads=[pzk], writes=[pzk, K("ef", b)])
                P.op("scalar", lambda e, b=b, q0=q0: e.activation(spt[b][:, q0:512], ef[b][:, q0:512], AF.Ln, bias=1.0), reads=[K("ef", b)], writes=[K("sp", b)])
                if j >= 0:
                    P.op("vector", lambda e, b=b, j=j, q0=q0: e.tensor_tensor(spt[b][:, q0:512], spt[b][:, q0:512], mstrict[:, j, q0:512], ALU.mult),
                         reads=[K("sp", b), K("mstrict")], writes=[K("sp", b)])
                yield
                def mg(e, b=b, pg=pg, q0=q0, first=first):
                    r = e.matmul(pg[:, q0:512], uincl[:], spt[b][:, q0:512], start=True, stop=first)
                    if not first:
                        r = e.matmul(pg[:, q0:512], ones1[:], suf[:, q0:512], start=False, stop=True)
                    return r
                P.op("tensor", mg, reads=[K("sp", b), K("uincl"), K("ones1"), sufk], writes=[pgk])
                P.op("scalar", lambda e, b=b, pg=pg, q0=q0: e.activation(eg[b][:, q0:512], pg[:, q0:512], AF.Exp, scale=-1.0), reads=[pgk], writes=[pgk, K("eg", b)])
                if kt > 0:
                    P.op("vector", lambda e, pg=pg, q0=q0: e.tensor_copy(suf[:, q0:512], pg[0:1, q0:512]), reads=[pgk], writes=[pgk, sufk])
                P.op("vector", lambda e, b=b, q0=q0: e.tensor_tensor(at[b][:, q0:512], ef[b][:, q0:512], eg[b][:, q0:512], ALU.mult),
                     reads=[K("ef", b), K("eg", b)], writes=[K("at", b)])
                if j >= 0:
                    P.op("vector", lambda e, b=b, j=j, q0=q0: e.tensor_tensor(at[b][:, q0:512], at[b][:, q0:512], mstrict[:, j, q0:512], ALU.mult),
                         reads=[K("at", b), K("mstrict")], writes=[K("at", b)])
                yield
                def pv(e, b=b, kt=kt, c=c, j=j, h=h, po=po):
                    r = None
                    for qs in range(3, max(j, 0) - 1, -1):
                        qt = 4 * c + qs
                        r = e.matmul(po[:, qs * 64:(qs + 1) * 64], at[b][:, qs * 128:(qs + 1) * 128], va[:, kt, h * 64:(h + 1) * 64],
                                     start=(kt == 4 * c + 3 and qs == 3), stop=(kt == 0 and qs == 0))
                    return r
                P.op("tensor", pv, reads=[K("at", b), K("va", kt)], writes=[pok])
                yield
            P.op("vector", lambda e, po=po, c=c, h=h: e.tensor_copy(mix[:, 4 * c:4 * c + 4, h * 64:(h + 1) * 64], po[:, 0:256].rearrange("p (a d) -> p a d", a=4)),
                 reads=[pok], writes=[pok] + [K("mix", 4 * c + qs) for qs in range(4)])

    for h0 in range(0, 8, 2):
        gens = [sb_head(h0), sb_head(h0 + 1)]
        alive = [True, True]
        while any(alive):
            for gi in range(2):
                if alive[gi]:
                    try:
                        next(gens[gi])
                    except StopIteration:
                        alive[gi] = False

    for h in range(4):
        for m in range(2):
            wt, wk = next_w(1536 + (h * 2 + m) * 64, 64)
            proj_fm(P, ph, wt, wk, 0, 64, xT, L, ps, [2, 3],
                    lambda ch, pa, pk, m=m: P.op("scalar", lambda e: e.activation(qd[m][0:64, ch * 512:(ch + 1) * 512], pa, AF.Identity, scale=0.125),
                                                reads=[pk], writes=[pk, K("qd", m)]))
            wt, wk = next_w(2048 + (h * 2 + m) * 64, 64)
            proj_fm(P, ph, wt, wk, 0, 64, xT, L, ps, [2, 3],
                    lambda ch, pa, pk, m=m: P.op("vector", lambda e: e.tensor_copy(kd[m][0:64, ch * 512:(ch + 1) * 512], pa),
                                                reads=[pk], writes=[pk, K("kd", m)]))
            P.dma("gpsimd", qd[m][64:68, :], D["c_qaug4"][h][:, 0:L], writes=[K("qd", m)], slot=("qaug", m))
            P.dma("gpsimd", kd[m][64:68, :], D["c_kaug"][:, 0:L], writes=[K("kd", m)], slot=("kaug", m))
        for c in range(NC):
            nk = 4 * c + 4
            tiles = [(m, kt) for m in range(2) for kt in range(nk - 1, -1, -1)]
            bsl = []
            for _ in tiles:
                bsl.append(it % 2)
                it += 1

            def emit_s(n, c=c):
                m, kt = tiles[n]
                b = bsl[n]
                q0 = max(kt - 4 * c, 0) * 128
                pz, pzk = ps[b], K("ps", b)
                P.op("tensor", lambda e, pz=pz, kt=kt, c=c, q0=q0, m=m: e.matmul(
                    pz[:, q0:512], kd[m][:, kt * 128:(kt + 1) * 128], qd[m][:, c * 512 + q0:(c + 1) * 512], start=True, stop=True),
                     reads=[K("qd", m), K("kd", m)], writes=[pzk])

            def emit_rest(n, c=c, h=h):
                m, kt = tiles[n]
                b = bsl[n]
                j = kt - 4 * c
                q0 = max(j, 0) * 128
                pz, pzk = ps[b], K("ps", b)
                pos = [ps[4 + 2 * m], ps[5 + 2 * m]]
                P.op("scalar", lambda e, b=b, pz=pz, q0=q0: e.activation(at[b][:, q0:512], pz[:, q0:512], AF.Exp), reads=[pzk], writes=[pzk, K("at", b)])
                if j >= 0:
                    P.op("vector", lambda e, b=b, j=j, q0=q0: e.tensor_tensor(at[b][:, q0:512], at[b][:, q0:512], mcausal[:, j, q0:512], ALU.mult),
                         reads=[K("at", b), K("mcausal")], writes=[K("at", b)])
                def pv(e, b=b, kt=kt, c=c, j=j, h=h, pos=pos):
                    r = None
                    for qs in range(3, max(j, 0) - 1, -1):
                        qt = 4 * c + qs
                        r = e.matmul(pos[qs // 2][:, (qs % 2) * 256:(qs % 2) * 256 + 129], at[b][:, qs * 128:(qs + 1) * 128], vb[:, kt, h, 0:129],
                                     start=(kt == qt and qs % 2 == 1), stop=(kt == 0 and qs % 2 == 0))
                    return r
                P.op("tensor", pv, reads=[K("at", b), K("vb", kt)], writes=[K("ps", 4 + 2 * m), K("ps", 5 + 2 * m)])

            emit_s(0)
            for n in range(len(tiles)):
                if n + 1 < len(tiles):
                    emit_s(n + 1)
                emit_rest(n)
            def P1(qs): return ps[4 + qs // 2][:, (qs % 2) * 256:(qs % 2) * 256 + 129]
            def P2(qs): return ps[6 + qs // 2][:, (qs % 2) * 256:(qs % 2) * 256 + 129]
            def K1(qs): return K("ps", 4 + qs // 2)
            def K2(qs): return K("ps", 6 + qs // 2)
            for qs in range(4):
                P.op("vector", lambda e, qs=qs: e.reciprocal(sm4[:, qs, 0:1], P1(qs)[:, 128:129]), reads=[K1(qs)], writes=[K1(qs), K("sm0", qs)])
            for qs in range(4):
                P.op("vector", lambda e, qs=qs: e.reciprocal(sm4[:, qs, 1:2], P2(qs)[:, 128:129]), reads=[K2(qs)], writes=[K2(qs), K("sm1", qs)])
            for qs in range(4):
                P.op("vector", lambda e, qs=qs: e.tensor_tensor(sm4[:, qs, 1:2], sm4[:, qs, 1:2], lam[:, 3:4], ALU.mult), reads=[K("sm1", qs), K("nlam")], writes=[K("sm1", qs)])
            for qs in range(4):
                P.op("vector", lambda e, qs=qs: e.tensor_scalar(o14[:, qs, :], P1(qs)[:, 0:128], sm4[:, qs, 0:1], None, ALU.mult), reads=[K1(qs), K("sm0", qs)], writes=[K1(qs), K("o1", qs)])
            for qs in range(4):
                P.op("vector", lambda e, qs=qs: e.scalar_tensor_tensor(ob4[:, qs, :], P2(qs)[:, 0:128], sm4[:, qs, 1:2], o14[:, qs, :], ALU.mult, ALU.add),
                     reads=[K2(qs), K("sm1", qs), K("o1", qs)], writes=[K2(qs), K("ob", qs)])
            for qs in range(4):
                P.op("vector", lambda e, qs=qs: e.memset(sm4[:, qs, 2:3], 0.0), writes=[K("sm2", qs)])
            for qs in range(4):
                P.op("scalar", lambda e, qs=qs: e.activation(junk[:, qs * 128:(qs + 1) * 128], ob4[:, qs, :], AF.Square, accum_out=sm4[:, qs, 2:3]),
                     reads=[K("ob", qs), K("sm2", qs)], writes=[K("sm2", qs), K("junk", qs)])
            for qs in range(4):
                P.op("vector", lambda e, qs=qs: e.tensor_scalar(sm4[:, qs, 2:3], sm4[:, qs, 2:3], 1.0 / 128, RMS_EPS, ALU.mult, ALU.add), reads=[K("sm2", qs)], writes=[K("sm2", qs)])
            for qs in range(4):
                P.op("scalar", lambda e, qs=qs: e.sqrt(sm4[:, qs, 2:3], sm4[:, qs, 2:3]), reads=[K("sm2", qs)], writes=[K("sm2", qs)])
            for qs in range(4):
                P.op("vector", lambda e, qs=qs: e.reciprocal(sm4[:, qs, 3:4], sm4[:, qs, 2:3]), reads=[K("sm2", qs)], writes=[K("sm3", qs)])
            for qs in range(4):
                qt = 4 * c + qs
                P.op("vector", lambda e, qs=qs, qt=qt, h=h: e.scalar_tensor_tensor(mix[:, qt, 512 + h * 128:512 + (h + 1) * 128], ob4[:, qs, :], sm4[:, qs, 3:4], wsub[:], ALU.mult, ALU.mult),
                     reads=[K("ob", qs), K("sm3", qs), K("wsub")], writes=[K("mix", qt)])

    if "dbg" in D:
        P.dma("gpsimd", D["dbg"].rearrange("(t p) c -> p t c", p=128), mix[:], reads=[K("mix", i) for i in range(NT)], writes=[K("dbg")])
    xTf = xT[:].rearrange("p a b -> p (a b)").bitcast(F32)
    T = dict(wo=wb, mT=[atsp[:, 0:2, :].rearrange("p a b -> p (a b)"), atsp[:, 2:4, :].rearrange("p a b -> p (a b)")],
             mTk=[[K("at", 0), K("at", 1)], [K("sp", 0), K("sp", 1)]],
             xt=xt, g_bc=efg[:, 0:2, :].rearrange("p a b -> p (a b)"), b_bc=efg[:, 2:4, :].rearrange("p a b -> p (a b)"),
             gk=[K("ef", 0), K("ef", 1)], bk=[K("eg", 0), K("eg", 1)],
             st={n: sb("st_" + n, [128, 1]) for n in ("s1", "s2", "mean", "msq", "var", "rstd")},
             yo=[xTf[:, 0:1024], xTf[:, 1024:2048]], wok=[wkeys(ph, "wb0"), wkeys(ph, "wb1")])
    T["st"]["junk"] = junk
    outproj_ln(P, ph, C, mix, L, D["ev_w_out"][lay_j], h_rows, out_rows, ln_g, ln_b, T, ps)


def odd_mixer_seq(P, ph, C, lay_j, h_rows, out_rows, L, ln_g, ln_b):
    D = C["dram"]
    ident, ident_b = C["ident"], C["ident_b"]
    NT = L // 128
    NC = L // 512
    KSEL = min(256, L // 4)
    sb = lambda n, s, d=F32: P.sb(ph + n, s, d)
    K = lambda *a: (ph,) + a
    w_in = D["od_w_in"][lay_j]
    ps = [P.ps(ph + "ps%d" % i, [128, 512]) for i in range(8)]
    pbf = ps[6][:].bitcast(BF16)

    xt = [sb("xt%d" % i, [128, 1024]) for i in range(2)]
    xT = sb("xT", [128, 8, L], BF16)
    wb = [sb("wb%d" % i, [128, 8, 512], BF16) for i in range(2)]
    mix = sb("mix", [128, NT, 1024], BF16)
    qTc = sb("qTc", [128, 16, 512], BF16)
    ckvT = sb("ckvT", [128, L], BF16)
    ckva = sb("ckva", [128, NT, 132], BF16)
    qiT = [sb("qiT%d" % i, [128, L], BF16) for i in range(4)]
    kiT2 = sb("kiT2", [128, L], BF16)
    widx = sb("widx", [128, NT, 8])
    score = sb("score", [128, L]); wkt = sb("wkt", [128, L])
    MB = [sb("MB%d" % i, [128, L], BF16) for i in range(4)]
    rl = [sb("rl%d" % i, [128, 512]) for i in range(2)]
    atsp = sb("atsp", [128, 4, 512], BF16)
    at = [atsp[:, i, :] for i in range(2)]
    kaug = sb("kaug", [9, L], BF16)
    qaugc = sb("qaugc", [9, 16, 512], BF16)
    wuv = sb("wuv", [128, 16, 64], BF16)
    kvw = sb("kvw", [128, 128])
    oh4 = sb("oh4", [128, 4, 128], BF16); ohT4 = sb("ohT4", [128, 512], BF16); sm4 = sb("sm4", [128, 4])
    m8 = sb("m8", [128, 8]); sm = sb("sm", [128, 8])
    junk = sb("junk", [128, 1024], BF16)

    P.dma("gpsimd", kaug[:], D["c_kaug9"][:, 0:L], writes=[K("kaug")])
    P.dma("gpsimd", wuv[:], D["od_w_uv"][lay_j].rearrange("h c d -> c h d"), writes=[K("wuv")])
    P.dma("sync", kvw[:], D["od_kv_norm_w"][lay_j].partition_broadcast(128), writes=[K("kvw")])
    P.op("vector", lambda e: e.memset(ckva[:], 1.0), writes=[K("ckva", i) for i in range(NT)])

    load_xT(P, ph, C, h_rows, L, xT, xt, ps)
    wslot = [0]
    def next_w(c0, ncols=512, dst0=0, new=True):
        if new:
            wslot[0] += 1
        s = wslot[0] % 2
        load_w_cols(P, ph, wb[s], "wb%d" % s, w_in, c0, ncols, dst0=dst0)
        return wb[s], wkeys(ph, "wb%d" % s, (dst0,))

    wt, wk = next_w(2048, 128)
    def ev_ckv(i, pa, pk):
        P.op("vector", lambda e: e.memset(sm[:, 0:1], 0.0), writes=[K("sm0")])
        P.op("scalar", lambda e: e.activation(junk[:, 0:128], pa, AF.Square, accum_out=sm[:, 0:1]), reads=[pk, K("sm0")], writes=[pk, K("sm0"), K("junk")])
        P.op("vector", lambda e: e.tensor_scalar(sm[:, 0:1], sm[:, 0:1], 1.0 / 128, RMS_EPS, ALU.mult, ALU.add), reads=[K("sm0")], writes=[K("sm0")])
        P.op("scalar", lambda e: e.sqrt(sm[:, 0:1], sm[:, 0:1]), reads=[K("sm0")], writes=[K("sm0")])
        P.op("vector", lambda e: e.reciprocal(sm[:, 1:2], sm[:, 0:1]), reads=[K("sm0")], writes=[K("sm1")])
        P.op("vector", lambda e: e.scalar_tensor_tensor(ckva[:, i, 0:128], pa, sm[:, 1:2], kvw[:], ALU.mult, ALU.mult),
             reads=[pk, K("sm1"), K("kvw")], writes=[pk, K("ckva", i)])
        P.op("tensor", lambda e: e.transpose(pbf[:, 0:128], ckva[:, i, 0:128], ident_b[:]), reads=[K("ckva", i), "ident_b"], writes=[K("ps", 6)])
        P.op("vector", lambda e: e.tensor_copy(ckvT[:, i * 128:(i + 1) * 128], pbf[:, 0:128]), reads=[K("ps", 6)], writes=[K("ps", 6), K("ckvT")])
    proj_tm(P, ph, wt, wk, 0, 128, xT, L, ps, [2, 3], ev_ckv)
    wt, wk = next_w(2176, 512)
    for j in range(4):
        proj_fm(P, ph, wt, wk, j * 128, 128, xT, L, ps, [2, 3],
                lambda ch, pa, pk, j=j: P.op("scalar", lambda e: e.activation(qiT[j][:, ch * 512:(ch + 1) * 512], pa, AF.Identity, scale=0.125),
                                            reads=[pk], writes=[pk, K("qiT")]))
    wt, wk0 = next_w(2688, 64, dst0=0)
    _, wk1 = next_w(2688, 64, dst0=64, new=False)
    _, wk2 = next_w(2752, 8, dst0=128, new=False)
    proj_fm(P, ph, wt, wk0 + wk1, 0, 128, xT, L, ps, [2, 3],
            lambda ch, pa, pk: P.op("vector", lambda e: e.tensor_copy(kiT2[:, ch * 512:(ch + 1) * 512], pa), reads=[pk], writes=[pk, K("kiT2")]))
    proj_tm(P, ph, wt, wk2, 128, 8, xT, L, ps, [2, 3],
            lambda i, pa, pk: P.op("vector", lambda e: e.tensor_scalar(widx[:, i, :], pa, 8 ** -0.5, None, ALU.mult), reads=[pk], writes=[pk, K("widx")]))

    if "dbg2" in D and C.get("dumpc", 0) == -1:
        P.dma("gpsimd", D["dbg2"][:, 0:L], kiT2[:, 0:L], reads=[K("kiT2")], writes=[K("dbg2")])
        P.dma("gpsimd", D["dbg3"][:, 0:L], qiT[0][:, 0:L], reads=[K("qiT")], writes=[K("dbg3")])
        P.dma("gpsimd", D["dbg"][0:128, 0:NT * 8], widx[:].rearrange("p a b -> p (a b)"), reads=[K("widx")], writes=[K("dbgw")])
    def dump_w(stage):
        if "dbg2" in D and C.get("dumpat", None) == stage:
            P.dma("gpsimd", D["dbg"][0:128, 0:NT * 8], widx[:].rearrange("p a b -> p (a b)"), reads=[K("widx")], writes=[K("dbgw")])
    it = 0
    for c in range(NC):
        for g in range(4):
            if c == 0:
                dump_w(10 + g)
            wt, wk = next_w(g * 512, 512)
            if c == 0:
                dump_w(20 + g)
            for hh in range(4):
                h = g * 4 + hh
                pi = 2 + h % 2
                def mm(e, hh=hh, pi=pi, wt=wt, c=c):
                    for kk in range(8):
                        r = e.matmul(ps[pi][:], wt[:, kk, hh * 128:(hh + 1) * 128], xT[:, kk, c * 512:(c + 1) * 512], start=(kk == 0), stop=(kk == 7))
                    return r
                P.op("tensor", mm, reads=wk + [K("xT", c)], writes=[K("ps", pi)])
                P.op("scalar", lambda e, h=h, pi=pi: e.activation(qTc[:, h, :], ps[pi][:], AF.Identity, scale=128 ** -0.5),
                     reads=[K("ps", pi)], writes=[K("ps", pi), K("qTc", h)])
        if c == 0:
            dump_w(1)
        P.dma("gpsimd", qaugc[:], D["c_qaug16"][:, :, c * 512:(c + 1) * 512].rearrange("h r l -> r h l"), writes=[K("qaugc")])
        if c == 0:
            P.op("vector", lambda e: e.engine_nop() if False else e.memset(sm[:, 7:8], 0.0), reads=[K("qaugc")], writes=[K("sm7")])
            if C.get("dumpat", None) == 2:
                P.dma("gpsimd", D["dbg"][0:128, 0:NT * 8], widx[:].rearrange("p a b -> p (a b)"), reads=[K("widx"), K("sm7")], writes=[K("dbgw")])
        for qs in range(4):
            qt = 4 * c + qs
            n_s = (qt + 1) * 128
            for sc in range((n_s + 511) // 512):
                w = min(512, n_s - sc * 512)
                for ih in range(8):
                    b = it % 2
                    it += 1
                    pi = 2 + b
                    P.op("tensor", lambda e, pi=pi, ih=ih, qt=qt, sc=sc, w=w: e.matmul(
                        ps[pi][:, 0:w], qiT[ih // 2][(ih % 2) * 64:(ih % 2) * 64 + 64, qt * 128:(qt + 1) * 128],
                        kiT2[(ih % 2) * 64:(ih % 2) * 64 + 64, sc * 512:sc * 512 + w], start=True, stop=True),
                         reads=[K("qiT"), K("kiT2")], writes=[K("ps", pi)])
                    P.op("scalar", lambda e, pi=pi, b=b, w=w: e.activation(rl[b][:, 0:w], ps[pi][:, 0:w], AF.Relu), reads=[K("ps", pi)], writes=[K("ps", pi), K("rl", b)])
                    if ih == 0:
                        P.op("vector", lambda e, b=b, w=w, sc=sc, qt=qt, ih=ih: e.tensor_scalar(score[:, sc * 512:sc * 512 + w], rl[b][:, 0:w], widx[:, qt, ih:ih + 1], None, ALU.mult),
                             reads=[K("rl", b), K("widx")], writes=[K("score")])
                    else:
                        P.op("vector", lambda e, b=b, w=w, sc=sc, qt=qt, ih=ih: e.scalar_tensor_tensor(score[:, sc * 512:sc * 512 + w], rl[b][:, 0:w], widx[:, qt, ih:ih + 1],
                                                                                                  score[:, sc * 512:sc * 512 + w], ALU.mult, ALU.add),
                             reads=[K("rl", b), K("widx"), K("score")], writes=[K("score")])
            P.op("gpsimd", lambda e, qt=qt: e.affine_select(score[:, qt * 128:(qt + 1) * 128], score[:, qt * 128:(qt + 1) * 128], [[-1, 128]], ALU.is_ge, -1e30,
                                                         base=0, channel_multiplier=1), reads=[K("score")], writes=[K("score")])
            if c == 0 and qs == 0:
                dump_w(3)
            if c == 0 and qs == 2:
                dump_w(4)
            if qt * 128 >= KSEL:
                R = KSEL // 8
                for r in range(R):
                    src = score if r == 0 else wkt
                    P.op("vector", lambda e, src=src, n_s=n_s: e.max(m8[:], src[:, 0:n_s]), reads=[K("score"), K("wkt")], writes=[K("m8")])
                    if r < R - 1:
                        P.op("vector", lambda e, src=src, n_s=n_s: e.match_replace(wkt[:, 0:n_s], m8[:], src[:, 0:n_s], -1e30),
                             reads=[K("score"), K("wkt"), K("m8")], writes=[K("wkt")])
                P.op("vector", lambda e, qs=qs, n_s=n_s: e.tensor_scalar(MB[qs][:, 0:n_s], score[:, 0:n_s], m8[:, 7:8], NEGM, ALU.is_lt, ALU.mult),
                     reads=[K("score"), K("m8")], writes=[K("MB", qs)])
            else:
                P.op("vector", lambda e, qs=qs, n_s=n_s: e.tensor_scalar(MB[qs][:, 0:n_s], score[:, 0:n_s], -1e29, NEGM, ALU.is_lt, ALU.mult),
                     reads=[K("score")], writes=[K("MB", qs)])
        if "dbg2" in D and C.get("dumpc", 0) == -2 and c == 1:
            P.dma("gpsimd", D["dbg2"][:, 0:L], kiT2[:, 0:L], reads=[K("kiT2")], writes=[K("dbg2")])
            P.dma("gpsimd", D["dbg3"][:, 0:L], qiT[0][:, 0:L], reads=[K("qiT")], writes=[K("dbg3")])
            P.dma("gpsimd", D["dbg"][0:128, 0:NT * 8], widx[:].rearrange("p a b -> p (a b)"), reads=[K("widx")], writes=[K("dbgw")])
        if "dbg2" in D and c == C.get("dumpc", 0):
            P.dma("gpsimd", D["dbg2"][:, 0:L], MB[3][:, 0:L], reads=[K("MB", 3)], writes=[K("dbg2")])
            P.dma("sync", D["dbg3"][:, 0:L], score[:, 0:L], reads=[K("score")], writes=[K("dbg3")])
        if c == 0:
            dump_w(5)
        nk = 4 * c + 4
        tiles = [(h, kt) for h in range(16) for kt in range(nk - 1, -1, -1)]
        bsl = []
        for _ in tiles:
            bsl.append(it % 2)
            it += 1
        POS = [[ps[4], ps[5]], [ps[2], ps[3]]]
        POSK = [[K("ps", 4), K("ps", 5)], [K("ps", 2), K("ps", 3)]]

        def emit_s(n, c=c):
            h, kt = tiles[n]
            b = bsl[n]
            jm = max(kt - 4 * c, 0)
            q0 = jm * 128
            pz, pzk = ps[b], K("ps", b)
            def sc_mm(e, pz=pz, kt=kt, q0=q0, jm=jm, h=h):
                e.matmul(pz[:, q0:512], ckvT[:, kt * 128:(kt + 1) * 128], qTc[:, h, q0:512], start=True, stop=False)
                r = e.matmul(pz[:, q0:512], kaug[:, kt * 128:(kt + 1) * 128], qaugc[:, h, q0:512], start=False, stop=False)
                for qs in range(jm, 4):
                    r = e.matmul(pz[:, qs * 128:(qs + 1) * 128], MB[qs][:, kt * 128:(kt + 1) * 128], ident_b[:], start=False, stop=(qs == 3))
                return r
            P.op("tensor", sc_mm, reads=[K("ckvT"), K("qTc", h), K("kaug"), K("qaugc"), "ident_b"] + [K("MB", q) for q in range(jm, 4)], writes=[pzk])

        def emit_rest(n, c=c):
            h, kt = tiles[n]
            b = bsl[n]
            jm = max(kt - 4 * c, 0)
            q0 = jm * 128
            pz, pzk = ps[b], K("ps", b)
            pos, posk = POS[h % 2], POSK[h % 2]
            P.op("scalar", lambda e, b=b, pz=pz, q0=q0: e.activation(at[b][:, q0:512], pz[:, q0:512], AF.Exp), reads=[pzk], writes=[pzk, K("at", b)])
            def pv(e, b=b, kt=kt, c=c, jm=jm, pos=pos):
                r = None
                for qs in range(3, jm - 1, -1):
                    qt = 4 * c + qs
                    r = e.matmul(pos[qs // 2][:, (qs % 2) * 256:(qs % 2) * 256 + 129], at[b][:, qs * 128:(qs + 1) * 128], ckva[:, kt, 0:129],
                                 start=(kt == qt and qs % 2 == 1), stop=(kt == 0 and qs % 2 == 0))
                return r
            P.op("tensor", pv, reads=[K("at", b), K("ckva", kt)], writes=list(posk))

        def ep1(h):
            pos, posk = POS[h % 2], POSK[h % 2]
            for qs in range(4):
                p1 = pos[qs // 2][:, (qs % 2) * 256:(qs % 2) * 256 + 129]
                P.op("vector", lambda e, p1=p1, qs=qs: e.reciprocal(sm4[:, qs:qs + 1], p1[:, 128:129]), reads=[posk[qs // 2]], writes=[posk[qs // 2], K("rz", qs)])
            for qs in range(4):
                p1 = pos[qs // 2][:, (qs % 2) * 256:(qs % 2) * 256 + 129]
                P.op("vector", lambda e, p1=p1, qs=qs: e.tensor_scalar(oh4[:, qs, :], p1[:, 0:128], sm4[:, qs:qs + 1], None, ALU.mult),
                     reads=[posk[qs // 2], K("rz", qs)], writes=[posk[qs // 2], K("oh4")])

        def ep2(h):
            def tr(e):
                for qs in range(4):
                    r = e.transpose(pbf[:, qs * 128:(qs + 1) * 128], oh4[:, qs, :], ident_b[:])
                return r
            P.op("tensor", tr, reads=[K("oh4"), "ident_b"], writes=[K("ps", 6)])
            P.op("vector", lambda e: e.tensor_copy(ohT4[:], pbf[:, 0:512]), reads=[K("ps", 6)], writes=[K("ps", 6), K("ohT4")])

        def ep3(h, c=c):
            def mm(e, h=h):
                for qs in range(4):
                    r = e.matmul(ps[7][:, qs * 64:(qs + 1) * 64], ohT4[:, qs * 128:(qs + 1) * 128], wuv[:, h, :], start=(qs == 0), stop=(qs == 3))
                return r
            P.op("tensor", mm, reads=[K("ohT4"), K("wuv")], writes=[K("ps", 7)])
            P.op("scalar", lambda e, h=h, c=c: e.activation(mix[:, 4 * c:4 * c + 4, h * 64:(h + 1) * 64], ps[7][:, 0:256].rearrange("p (a d) -> p a d", a=4), AF.Identity),
                 reads=[K("ps", 7)], writes=[K("ps", 7)] + [K("mix", 4 * c + qs) for qs in range(4)])

        deferred = []
        emit_s(0)
        for n in range(len(tiles)):
            if n + 1 < len(tiles):
                emit_s(n + 1)
            emit_rest(n)
            h, kt = tiles[n]
            if kt == 0:
                ep1(h)
                deferred.append((n + 2, ep2, h))
                deferred.append((n + 4, ep3, h))
            keep = []
            for due, fn, hh in deferred:
                if due <= n:
                    fn(hh)
                else:
                    keep.append((due, fn, hh))
            deferred = keep
        for due, fn, hh in sorted(deferred, key=lambda t: t[0]):
            fn(hh)

    if "dbg" in D and C.get("dumpc", 0) >= 0 and C.get("dumpat", None) is None:
        P.dma("gpsimd", D["dbg"].rearrange("(t p) c -> p t c", p=128), mix[:], reads=[K("mix", i) for i in range(NT)], writes=[K("dbg")])
    xTf = xT[:].rearrange("p a b -> p (a b)").bitcast(F32)
    T = dict(wo=wb, mT=[atsp[:, 0:2, :].rearrange("p a b -> p (a b)"), atsp[:, 2:4, :].rearrange("p a b -> p (a b)")],
             mTk=[[K("at", 0), K("at", 1)], [K("sp", 0), K("sp", 1)]],
             xt=xt, g_bc=score[:, 0:1024], b_bc=wkt[:, 0:1024],
             gk=[K("score")], bk=[K("wkt")],
             st={n: sb("st_" + n, [128, 1]) for n in ("s1", "s2", "mean", "msq", "var", "rstd")},
             yo=[xTf[:, 0:1024], xTf[:, 1024:2048]], wok=[wkeys(ph, "wb0"), wkeys(ph, "wb1")])
    T["st"]["junk"] = junk
    outproj_ln(P, ph, C, mix, L, D["od_w_out"][lay_j], h_rows, out_rows, ln_g, ln_b, T, ps)


def make_consts(L=2048):
    s = np.arange(128)[:, None]; q = np.arange(512)[None, :]
    mstrict = np.stack([((q - j * 128) > s) for j in range(4)], axis=1).astype(np.float32)
    mcausal = np.stack([((q - j * 128) >= s) for j in range(4)], axis=1).astype(np.float32)
    jj = np.arange(128)[:, None]; ss = np.arange(128)[None, :]
    uincl = (jj >= ss).astype(np.float32)
    t = np.arange(L)
    hi = (t // 256) * 256; lo = t % 256
    def aug(slopes):
        qa = np.stack([np.stack([np.full(L, c), np.full(L, c), -c * hi, -c * lo]) for c in slopes]).astype(np.float32)
        return qa
    sl4 = 2.0 ** (-8.0 * np.arange(1, 5) / 4)
    sl16 = 2.0 ** (-8.0 * np.arange(1, 17) / 16)
    kaug = np.stack([hi, lo, np.ones(L), np.ones(L)]).astype(np.float32)
    import ml_dtypes
    def bsplit(v):
        v = np.asarray(v, dtype=np.float64); outp = []
        for _ in range(3):
            p = v.astype(ml_dtypes.bfloat16).astype(np.float64); outp.append(p); v = v - p
        return outp
    q9 = []
    for c in sl16:
        c1, c2, c3 = bsplit(np.full(L, c)); v1, v2, v3 = bsplit(c * t.astype(np.float64))
        q9.append(np.stack([c1, c1, c2, c2, c3, c3, -v1, -v2, -v3]))
    qaug9 = np.stack(q9).astype(np.float32)
    kaug9 = np.stack([hi, lo, hi, lo, hi, lo, np.ones(L), np.ones(L), np.ones(L)]).astype(np.float32)
    return dict(c_mstrict=mstrict, c_mcausal=mcausal, c_uincl=uincl, c_qaug4=aug(sl4), c_qaug16=qaug9, c_kaug9=kaug9, c_kaug=kaug,
                c_ident=np.eye(128, dtype=np.float32))


NCORES = 8
SEQ = 2048
NTOK = 2 * SEQ
N_EXPERTS = 32
_W_NAMES = ["ev_w_in", "ev_w_out", "ev_lambda_q1", "ev_lambda_k1", "ev_lambda_q2", "ev_lambda_k2", "ev_subln_w",
            "od_w_in", "od_kv_norm_w", "od_w_uv", "od_w_out", "ln_mix_g", "ln_mix_b", "router_w", "router_b",
            "exp_w_gu", "exp_b_gu", "exp_w_down", "exp_b_down", "ln_ffn_g", "ln_ffn_b"]


def build_program(shapes, cshapes):
    nc = bass.Bass("TRN2", target_bir_lowering=False)
    D = {}
    for n in _W_NAMES:
        D[n] = nc.dram_tensor(n, list(shapes[n]), F32, kind="ExternalInput").ap()
    for n, s in cshapes.items():
        D[n] = nc.dram_tensor(n, list(s), F32, kind="ExternalInput").ap()
    x = nc.dram_tensor("x", [NTOK, 1024], F32, kind="ExternalInput").ap()
    out = nc.dram_tensor("out", [NTOK, 1024], F32, kind="ExternalOutput").ap()
    h1 = nc.dram_tensor("h1", [NTOK, 1024], F32, kind="Internal").ap()
    h2 = nc.dram_tensor("h2", [NTOK, 1024], F32, kind="Internal").ap()
    h3 = nc.dram_tensor("h3", [NTOK, 1024], F32, kind="Internal").ap()
    with ExitStack() as st:
        P = Prog(nc, st)
        ident = P.sb("ident", [128, 128]); ident_b = P.sb("ident_b", [128, 128], BF16)
        P.dma("sync", ident[:], D["c_ident"], writes=["ident"])
        P.dma("gpsimd", ident_b[:], D["c_ident"], writes=["ident_b"])
        C = {"dram": D, "ident": ident, "ident_b": ident_b}
        lam_init0 = 0.8 - 0.6 * math.exp(-0.3 * 0)

        def phase(fn):
            with ExitStack() as ts:
                P.stack = ts
                fn()
                P.barrier()
            P.stack = st

        for sq in range(2):
            phase(lambda sq=sq: even_mixer_seq(P, "e%d" % sq, C, 0, x[sq * SEQ:(sq + 1) * SEQ, :], h1[sq * SEQ:(sq + 1) * SEQ, :], SEQ,
                                                lam_init0, D["ln_mix_g"][0], D["ln_mix_b"][0]))
        phase(lambda: moe_phase(P, "m0", C, 0, h1, h2, NTOK, N_EXPERTS))
        for sq in range(2):
            phase(lambda sq=sq: odd_mixer_seq(P, "o%d" % sq, C, 0, h2[sq * SEQ:(sq + 1) * SEQ, :], h3[sq * SEQ:(sq + 1) * SEQ, :], SEQ,
                                               D["ln_mix_g"][1], D["ln_mix_b"][1]))
        phase(lambda: moe_phase(P, "m1", C, 1, h3, out, NTOK, N_EXPERTS))
        P.final_wait()
        P.emit()
    return nc


def kernel(**inputs):
    consts = make_consts(SEQ)
    x = np.ascontiguousarray(np.asarray(inputs["x"], dtype=np.float32)).reshape(NCORES, NTOK, 1024)
    ws = {n: np.ascontiguousarray(np.asarray(inputs[n], dtype=np.float32)) for n in _W_NAMES}
    nc = build_program({n: ws[n].shape for n in _W_NAMES}, {n: v.shape for n, v in consts.items()})
    in_maps = []
    for c in range(NCORES):
        m = dict(ws)
        m.update(consts)
        m["x"] = x[c]
        in_maps.append(m)
    res = run_bass_kernel_spmd(nc, in_maps, core_ids=list(range(NCORES)))
    outs = [np.asarray(r["out"], dtype=np.float32) for r in res.results]
    return np.stack(outs, axis=0).reshape(16, SEQ, 1024)
```

```python
import math
from concourse.bass_utils import run_bass_kernel_spmd
from contextlib import ExitStack
import numpy as np
import concourse.bass as bass
import concourse.mybir as mybir

F32 = mybir.dt.float32
BF16 = mybir.dt.bfloat16
I32 = mybir.dt.int32
ALU = mybir.AluOpType
AF = mybir.ActivationFunctionType
AX = mybir.AxisListType

ENGS = ("sync", "scalar", "vector", "gpsimd", "tensor")


class Prog:
    def __init__(self, nc, stack):
        self.nc = nc
        self.stack = stack
        self.sem_stack = stack
        self.streams = {e: [] for e in ENGS}
        self.esem = {e: stack.enter_context(nc.semaphore("es_" + e)) for e in ENGS}
        self.etick = {e: 0 for e in ENGS}
        self.known = {e: {} for e in ENGS}
        self.res_w = {}
        self.res_r = {}
        self.dsem = {}
        self.semobj = {}
        for e in ENGS:
            self.semobj[id(self.esem[e])] = self.esem[e]
        self.n_ops = 0

    def sb(self, name, shape, dt=F32):
        return self.stack.enter_context(self.nc.sbuf_tensor(name, list(shape), dt))

    def ps(self, name, shape, dt=F32):
        return self.stack.enter_context(self.nc.psum_tensor(name, list(shape), dt))

    def _dma_sem(self, key):
        if key not in self.dsem:
            s = self.sem_stack.enter_context(self.nc.semaphore("ds_%d" % len(self.dsem)))
            self.dsem[key] = [s, 0]
            self.semobj[id(s)] = s
        return self.dsem[key]

    def _deps(self, eng, reads, writes):
        deps = {}

        def add(ev):
            if ev is None:
                return
            s, v = ev
            if deps.get(s, 0) < v:
                deps[s] = v

        for k in reads:
            add(self.res_w.get(k))
        for k in writes:
            add(self.res_w.get(k))
            for s, v in self.res_r.get(k, {}).items():
                add((s, v))
        waits = []
        kn = self.known[eng]
        for s, v in deps.items():
            if kn.get(s, 0) < v:
                kn[s] = v
                waits.append((s, v))
        return waits

    def _commit(self, ev, reads, writes):
        s, v = ev
        for k in reads:
            d = self.res_r.setdefault(k, {})
            if d.get(s, 0) < v:
                d[s] = v
        for k in writes:
            self.res_w[k] = ev
            self.res_r[k] = {}

    def op(self, eng, fn, reads=(), writes=()):
        waits = self._deps(eng, reads, writes)
        self.etick[eng] += 1
        ev = (id(self.esem[eng]), self.etick[eng])
        self.streams[eng].append((waits, fn, ev, 1))
        self._commit(ev, reads, writes)
        self.n_ops += 1

    def dma(self, eng, out, in_, reads=(), writes=(), slot=None, **kw):
        if slot is None:
            k0 = writes[0] if writes else reads[0]
            slot = ("_slot",) + tuple(k0[1:]) if isinstance(k0, tuple) and len(k0) > 1 else ("_slot", k0)
        ds = self._dma_sem(slot)
        waits = self._deps(eng, reads, writes)
        ds[1] += 16
        ev = (id(ds[0]), ds[1])
        fn = lambda e, out=out, in_=in_, kw=kw: e.dma_start(out=out, in_=in_, **kw)
        self.streams[eng].append((waits, fn, ev, 16))
        self._commit(ev, reads, writes)
        self.n_ops += 1

    def barrier(self):
        evs = [(id(self.esem[e]), self.etick[e]) for e in ENGS if self.etick[e] > 0]
        evs += [(id(s), c) for s, c in self.dsem.values() if c > 0]
        for e in ENGS:
            kn = self.known[e]
            waits = []
            for s, v in evs:
                if s == id(self.esem[e]) and False:
                    continue
                if kn.get(s, 0) < v:
                    kn[s] = v
                    waits.append((s, v))
            if waits:
                self.streams[e].append((waits, None, None, 0))

    def final_wait(self, eng="sync"):
        evs = [(id(s), c) for s, c in self.dsem.values() if c > 0]
        evs += [(id(self.esem[e]), self.etick[e]) for e in ENGS if self.etick[e] > 0 and e != eng]
        kn = self.known[eng]
        waits = [(s, v) for s, v in evs if kn.get(s, 0) < v]
        self.streams[eng].append((waits, None, None, 0))

    def emit(self):
        nc = self.nc
        with nc.Block() as block:
            def runner(name):
                def run(e):
                    for waits, fn, ev, inc in self.streams[name]:
                        for s, v in waits:
                            e.wait_ge(self.semobj[s], v)
                        if fn is not None:
                            inst = fn(e)
                            inst.then_inc(self.semobj[ev[0]], inc)
                return run
            block.sync(runner("sync"))
            block.scalar(runner("scalar"))
            block.vector(runner("vector"))
            block.gpsimd(runner("gpsimd"))
            block.tensor(runner("tensor"))


DEPTH = 2
ALPHA = (2 * DEPTH) ** 0.25
LN_EPS = 1e-5
SW_LIMIT = 7.0
SW_ALPHA = 1.702


def layer_norm_tile(P, ph, src_ap, src_keys, dst_ap, dst_key, g_bc, b_bc, st, idx):
    s1, s2, mean, msq, var, rstd, junk = (st[k] for k in ("s1", "s2", "mean", "msq", "var", "rstd", "junk"))
    k = lambda n: (ph, "ln_" + n)
    P.op("vector", lambda e: e.reduce_sum(s1[:, 0:1], src_ap, axis=AX.X), reads=list(src_keys), writes=[k("s1")])
    P.op("vector", lambda e: e.memset(s2[:, 0:1], 0.0), writes=[k("s2")])
    P.op("scalar", lambda e: e.activation(junk[:], src_ap, AF.Square, accum_out=s2[:, 0:1]),
         reads=list(src_keys) + [k("s2")], writes=[k("s2"), k("junk")])
    P.op("vector", lambda e: e.tensor_scalar(mean[:, 0:1], s1[:, 0:1], 1.0 / 1024, None, ALU.mult),
         reads=[k("s1")], writes=[k("mean")])
    P.op("vector", lambda e: e.tensor_tensor(msq[:, 0:1], mean[:, 0:1], mean[:, 0:1], ALU.mult),
         reads=[k("mean")], writes=[k("msq")])
    P.op("vector", lambda e: e.scalar_tensor_tensor(var[:, 0:1], s2[:, 0:1], 1.0 / 1024, msq[:, 0:1], ALU.mult, ALU.subtract),
         reads=[k("s2"), k("msq")], writes=[k("var")])
    P.op("vector", lambda e: e.tensor_scalar(var[:, 0:1], var[:, 0:1], LN_EPS, None, ALU.add),
         reads=[k("var")], writes=[k("var")])
    P.op("scalar", lambda e: e.sqrt(var[:, 0:1], var[:, 0:1]), reads=[k("var")], writes=[k("var")])
    P.op("vector", lambda e: e.reciprocal(rstd[:, 0:1], var[:, 0:1]), reads=[k("var")], writes=[k("rstd")])
    P.op("vector", lambda e: e.tensor_scalar(dst_ap, src_ap, mean[:, 0:1], rstd[:, 0:1], ALU.subtract, ALU.mult),
         reads=list(src_keys) + [k("mean"), k("rstd")], writes=[dst_key])
    P.op("vector", lambda e: e.tensor_tensor(dst_ap, dst_ap, g_bc[:], ALU.mult), reads=[dst_key, (ph, "g_bc")], writes=[dst_key])
    P.op("vector", lambda e: e.tensor_tensor(dst_ap, dst_ap, b_bc[:], ALU.add), reads=[dst_key, (ph, "b_bc")], writes=[dst_key])


def moe_phase(P, ph, C, lay, h_in, h_out, NT, E, stage=99):
    nc = P.nc
    D = C["dram"]
    ident = C["ident"]
    NBLK = NT // 1024
    sb = lambda n, s, d=F32: P.sb(ph + n, s, d)
    K = lambda *a: (ph,) + a

    rw = sb("rw", [128, 8, E]); rb_bc = sb("rb", [128, E])
    bguT = sb("bguT", [128, 16, E]); bd = sb("bd", [E, 1024])
    g_bc = sb("g_bc", [128, 1024]); b_bc = sb("b_bc", [128, 1024])
    xt = [sb("xt%d" % i, [128, 1024]) for i in range(2)]
    hT = sb("hT", [128, 8, 1024], BF16)
    hTf = sb("hTf", [128, 8, 128])
    acc = sb("acc", [128, 8, 1024])
    wgu = [sb("wgu%d" % i, [128, 8, 2048], BF16) for i in range(2)]
    wd = [sb("wd%d" % i, [128, 8, 1024], BF16) for i in range(2)]
    actT = [sb("actT%d" % i, [128, 8, 512], BF16) for i in range(2)]
    gc = [sb("gc%d" % i, [128, 512]) for i in range(2)]
    ua = [sb("ua%d" % i, [128, 512]) for i in range(2)]
    sl = [sb("sl%d" % i, [128, 512]) for i in range(2)]
    cw = sb("cw", [128, 8, E]); cwT = sb("cwT", [E, 128]); cws = sb("cws", [128, 8, E])
    lg = sb("lg", [128, E]); ex = sb("ex", [128, E]); em = sb("em", [128, E])
    m8 = sb("m8", [128, 8]); sm = sb("sm", [128, 8])
    st = {n: sb("st_" + n, [128, 1]) for n in ("s1", "s2", "mean", "msq", "var", "rstd")}
    st["junk"] = sb("junk", [128, 1024], BF16)

    pA = [P.ps(ph + "pA%d" % i, [128, 512]) for i in range(2)]
    pB = [P.ps(ph + "pB%d" % i, [128, 512]) for i in range(2)]
    pY = [P.ps(ph + "pY%d" % i, [128, 512]) for i in range(2)]
    pT = [P.ps(ph + "pT%d" % i, [128, 4, 128]) for i in range(2)]

    import os
    SK = os.environ.get("SKIP", "")
    if "rw" not in SK:
        P.dma("sync", rw[:], D["router_w"][lay].rearrange("(k p) e -> p k e", p=128), writes=[K("rw")])
    if "rb" not in SK:
        P.dma("sync", rb_bc[:], D["router_b"][lay].partition_broadcast(128), writes=[K("rb")])
    bkeys = [K("acc", 0, 0), K("acc", 0, 1), K("acc", 1, 0), K("acc", 1, 1)]
    P.dma("sync", acc[0:E, 0:2, :], D["exp_b_gu"][lay].rearrange("e (a f) -> e a f", a=2), writes=bkeys, slot="bguraw")
    if "bd" not in SK:
        P.dma("sync", bd[:], D["exp_b_down"][lay], writes=[K("bd")])
    if "gb" not in SK:
        P.dma("sync", g_bc[:], D["ln_ffn_g"][lay].partition_broadcast(128), writes=[K("g_bc")])
    if "gb" not in SK:
        P.dma("sync", b_bc[:], D["ln_ffn_b"][lay].partition_broadcast(128), writes=[K("b_bc")])
    for j in range(16):
        h = j % 2
        P.op("tensor", lambda e, j=j, h=h: e.transpose(pT[h][:, 0, 0:E], acc[0:E, j // 8, (j % 8) * 128:(j % 8 + 1) * 128], ident[0:E, 0:E]),
             reads=bkeys + ["ident"], writes=[K("pT", h)])
        P.op("vector", lambda e, j=j, h=h: e.tensor_copy(bguT[:, j, :], pT[h][:, 0, 0:E]),
             reads=[K("pT", h)], writes=[K("bguT")])

    if stage == 0:
        return
    def load_w(b, e, which="both"):
        ws = (b * E + e) % 2
        if "now" in SK:
            return
        if which in ("both", "gu"):
            for kk in range(8):
                P.dma("gpsimd", wgu[ws][:, kk, :], D["exp_w_gu"][lay, e, kk * 128:(kk + 1) * 128, :], writes=[K("wgu", ws, kk)], slot=("wgu", ws))
        if which in ("both", "d"):
            for kk in range(8):
                P.dma("gpsimd", wd[ws][:, kk, :], D["exp_w_down"][lay, e, kk * 128:(kk + 1) * 128, :], writes=[K("wd", ws, kk)], slot=("wd", ws))

    for b in range(NBLK):
        load_w(b, 0)
        for i in range(8 if "noa" not in SK else 0):
            tok0 = b * 1024 + i * 128
            s = i % 2
            P.dma("sync", xt[s][:], h_in[tok0:tok0 + 128, :], writes=[K("xt", s)])
            for h in range(2):
                def tr(e, s=s, h=h):
                    for q in range(4):
                        kk = h * 4 + q
                        r = e.transpose(pT[h][:, q, :], xt[s][:, kk * 128:(kk + 1) * 128], ident[:])
                    return r
                P.op("tensor", tr, reads=[K("xt", s), "ident"], writes=[K("pT", h)])
                P.op(os.environ.get("EVE", "scalar"), lambda e, h=h, i=i: (e.activation(hT[:, 4 * h:4 * h + 4, i * 128:(i + 1) * 128], pT[h][:], AF.Identity) if os.environ.get("EVE", "scalar") == "scalar" else e.tensor_copy(hT[:, 4 * h:4 * h + 4, i * 128:(i + 1) * 128], pT[h][:])),
                     reads=[K("pT", h)], writes=[K("hT", i // 4)])
                P.op("vector", lambda e, h=h: e.tensor_copy(hTf[:, 4 * h:4 * h + 4, :], pT[h][:]),
                     reads=[K("pT", h), K("hT", i // 4)] , writes=[K("hTf", h)])
            CUT = int(os.environ.get("CUT", "99"))
            if CUT < 2:
                continue
            pL = pY[0]
            def rt(e):
                for kk in range(8):
                    r = e.matmul(pL[:, 0:E], hTf[:, kk, :], rw[:, kk, :], start=(kk == 0), stop=(kk == 7))
                return r
            P.op("tensor", rt, reads=[K("hTf", 0), K("hTf", 1), K("rw")], writes=[K("pY", 0)])
            P.op("vector", lambda e: e.tensor_tensor(lg[:], pL[:, 0:E], rb_bc[:], ALU.add), reads=[K("pY", 0), K("rb")], writes=[K("lg")])
            if CUT < 3:
                continue
            P.op("vector", lambda e: e.max(m8[:], lg[:]), reads=[K("lg")], writes=[K("m8")])
            P.op("vector", lambda e: e.tensor_scalar(sm[:, 0:1], m8[:, 0:1], -1.0, None, ALU.mult), reads=[K("m8")], writes=[K("negmx")])
            P.op("scalar", lambda e: e.activation(ex[:], lg[:], AF.Exp, bias=sm[:, 0:1], scale=1.0), reads=[K("lg"), K("negmx")], writes=[K("ex")])
            P.op("vector", lambda e: e.scalar_tensor_tensor(em[:], lg[:], m8[:, 3:4], ex[:], ALU.is_ge, ALU.mult),
                 reads=[K("lg"), K("m8"), K("ex")], writes=[K("em")])
            P.op("vector", lambda e: e.reduce_sum(sm[:, 1:2], em[:], axis=AX.X), reads=[K("em")], writes=[K("Z")])
            P.op("vector", lambda e: e.reciprocal(sm[:, 2:3], sm[:, 1:2]), reads=[K("Z")], writes=[K("rz")])
            P.op("vector", lambda e, i=i: e.tensor_scalar(cw[:, i, :], em[:], sm[:, 2:3], None, ALU.mult), reads=[K("em"), K("rz")], writes=[K("cw", i)])
            P.op("vector", lambda e, i=i: e.tensor_scalar(cws[:, i, :], cw[:, i, :], 1.0 / SW_ALPHA, None, ALU.mult), reads=[K("cw", i)], writes=[K("cws", i)])
            if CUT < 4:
                continue
            pC = pY[1]
            P.op("tensor", lambda e, i=i: e.transpose(pC[0:E, 0:128], cw[:, i, :], ident[:]), reads=[K("cw", i), "ident"], writes=[K("pY", 1)])
            P.op("scalar", lambda e: e.copy(cwT[:], pC[0:E, 0:128]), reads=[K("pY", 1)], writes=[K("cwT")])
            if CUT < 5:
                continue
            for n in range(2):
                P.op("tensor", lambda e, n=n: e.matmul(pA[n][:], cwT[:], bd[:, n * 512:(n + 1) * 512], start=True, stop=True),
                     reads=[K("cwT"), K("bd")], writes=[K("pA", n)])
                P.op("vector", lambda e, n=n, s=s, i=i: e.scalar_tensor_tensor(acc[:, i, n * 512:(n + 1) * 512], xt[s][:, n * 512:(n + 1) * 512],
                                                                      ALPHA, pA[n][:], ALU.mult, ALU.add),
                     reads=[K("xt", s), K("pA", n)], writes=[K("acc", i, n)])
        if stage == 1:
            return
        cntb = [0]
        pend = [None]

        def flush_act():
            if pend[0] is not None:
                p, i, a_s = pend[0]
                P.op("vector", lambda e, p=p, i=i, a_s=a_s: e.scalar_tensor_tensor(actT[a_s][:, i, :], ua[p][:], 1.0 - SW_LIMIT, sl[p][:], ALU.add, ALU.mult),
                     reads=[K("sl", p), K("ua", p)], writes=[K("actT", a_s)])
                pend[0] = None

        def gu_unit(ei, c, i):
            ws = (b * E + ei) % 2
            a_s = c % 2
            p = cntb[0] % 2
            cntb[0] += 1
            def mm_g(e, off, ps, i=i, c=c, ws=ws):
                for kk in range(8):
                    r = e.matmul(ps[:], wgu[ws][:, kk, off + i * 128: off + (i + 1) * 128], hT[:, kk, c * 512:(c + 1) * 512],
                                 start=(kk == 0), stop=(kk == 7))
                return r
            hk = [K("hT", c)]
            P.op("tensor", lambda e, p=p, f=mm_g: f(e, 0, pA[p]), reads=[K("wgu", ws, kk) for kk in range(8)] + hk, writes=[K("pA", p)])
            P.op("tensor", lambda e, p=p, f=mm_g: f(e, 1024, pB[p]), reads=[K("wgu", ws, kk) for kk in range(8)] + hk, writes=[K("pB", p)])
            P.op("vector", lambda e, p=p, i=i, ei=ei: e.tensor_scalar(gc[p][:], pA[p][:], bguT[:, i, ei:ei + 1], SW_LIMIT, ALU.add, ALU.min),
                 reads=[K("pA", p), K("bguT")], writes=[K("gc", p)])
            P.op("vector", lambda e, p=p, i=i, ei=ei: e.tensor_scalar(ua[p][:], pB[p][:], bguT[:, 8 + i, ei:ei + 1], SW_LIMIT, ALU.add, ALU.min),
                 reads=[K("pB", p), K("bguT")], writes=[K("ua", p)])
            P.op("scalar", lambda e, p=p: e.activation(sl[p][:], gc[p][:], AF.Silu, scale=SW_ALPHA),
                 reads=[K("gc", p)], writes=[K("sl", p)])
            P.op("scalar", lambda e, p=p: e.activation(ua[p][:], ua[p][:], AF.Relu, bias=SW_LIMIT),
                 reads=[K("ua", p)], writes=[K("ua", p)])
            flush_act()
            pend[0] = (p, i, a_s)

        def down_unit(ei, c, idx):
            ws = (b * E + ei) % 2
            a_s = c % 2
            j, n = idx // 2, idx % 2
            ti = c * 4 + j
            q = idx % 2
            def mm_d(e, j=j, n=n, q=q, a_s=a_s, ws=ws):
                for f in range(8):
                    r = e.matmul(pY[q][:], actT[a_s][:, f, j * 128:(j + 1) * 128], wd[ws][:, f, n * 512:(n + 1) * 512],
                                 start=(f == 0), stop=(f == 7))
                return r
            P.op("tensor", mm_d, reads=[K("actT", a_s)] + [K("wd", ws, kk) for kk in range(8)], writes=[K("pY", q)])
            P.op("vector", lambda e, q=q, ti=ti, n=n, ei=ei: e.scalar_tensor_tensor(
                acc[:, ti, n * 512:(n + 1) * 512], pY[q][:], cws[:, ti, ei:ei + 1], acc[:, ti, n * 512:(n + 1) * 512], ALU.mult, ALU.add),
                 reads=[K("pY", q), K("cws", ti), K("acc", ti, n)], writes=[K("acc", ti, n)])

        seq = [(ei, c) for ei in range(E) for c in range(2)]
        for k in range(len(seq) + 1):
            if k < len(seq):
                ei, c = seq[k]
                if c == 0 and ei + 1 < E:
                    load_w(b, ei + 1, "gu")
                if c == 1 and ei + 1 < E:
                    load_w(b, ei + 1, "d")
            for idx in range(8):
                if k < len(seq):
                    gu_unit(seq[k][0], seq[k][1], idx)
                else:
                    flush_act()
                if k >= 1:
                    if idx == 0:
                        pass
                    down_unit(seq[k - 1][0], seq[k - 1][1], idx)
            flush_act()
        if stage == 2:
            return
        for i in range(8):
            tok0 = b * 1024 + i * 128
            s = i % 2
            layer_norm_tile(P, ph, acc[:, i, :], [K("acc", i, 0), K("acc", i, 1)], acc[:, i, :], K("acc", i, 0), g_bc, b_bc, st, i)
            P.dma("sync", h_out[tok0:tok0 + 128, :], acc[:, i, :], reads=[K("acc", i, 0)], writes=[K("hout", b, i)], slot=("yo_st", s))


import math

RMS_EPS = 1e-5
NEGM = -30000.0


def load_w_cols(P, ph, wt, wkey, w_dram, c0, ncols, dst0=0):
    for kk in range(8):
        P.dma("gpsimd", wt[:, kk, dst0:dst0 + ncols], w_dram[kk * 128:(kk + 1) * 128, c0:c0 + ncols],
              writes=[(ph, wkey, kk)], slot=(wkey,))


def wkeys(ph, wkey, dsts=(0,)):
    return [(ph, wkey, kk) for kk in range(8)]


def load_xT(P, ph, C, h_rows, L, xT, xt, ps):
    ident = C["ident"]
    for i in range(L // 128):
        s = i % 2
        P.dma("sync", xt[s][:], h_rows[i * 128:(i + 1) * 128, :], writes=[(ph, "xt", s)])
        for h in range(2):
            pt = ps[h]
            def tr(e, s=s, h=h, pt=pt):
                for q in range(4):
                    kk = h * 4 + q
                    r = e.transpose(pt[:, q * 128:(q + 1) * 128], xt[s][:, kk * 128:(kk + 1) * 128], ident[:])
                return r
            P.op("tensor", tr, reads=[(ph, "xt", s), "ident"], writes=[(ph, "ps", h)])
            P.op("vector", lambda e, h=h, i=i, pt=pt: e.tensor_copy(xT[:, 4 * h:4 * h + 4, i * 128:(i + 1) * 128],
                                                                  pt[:].rearrange("p (a b) -> p a b", a=4)),
                 reads=[(ph, "ps", h)], writes=[(ph, "ps", h), (ph, "xT", i // 4)])


def proj_fm(P, ph, wt, wk, c0, M, xT, L, ps, psi, evac):
    for ch in range(L // 512):
        pi = psi[ch % len(psi)]
        def mm(e, ch=ch, pi=pi):
            for kk in range(8):
                r = e.matmul(ps[pi][0:M, :], wt[:, kk, c0:c0 + M], xT[:, kk, ch * 512:(ch + 1) * 512], start=(kk == 0), stop=(kk == 7))
            return r
        P.op("tensor", mm, reads=wk + [(ph, "xT", ch)], writes=[(ph, "ps", pi)])
        evac(ch, ps[pi][0:M, :], (ph, "ps", pi))


def proj_tm(P, ph, wt, wk, c0, N, xT, L, ps, psi, evac):
    for i in range(L // 128):
        pi = psi[i % len(psi)]
        def mm(e, i=i, pi=pi):
            for kk in range(8):
                r = e.matmul(ps[pi][:, 0:N], xT[:, kk, i * 128:(i + 1) * 128], wt[:, kk, c0:c0 + N], start=(kk == 0), stop=(kk == 7))
            return r
        P.op("tensor", mm, reads=wk + [(ph, "xT", i // 4)], writes=[(ph, "ps", pi)])
        evac(i, ps[pi][:, 0:N], (ph, "ps", pi))


def outproj_ln(P, ph, C, mix, L, w_out, h_rows, out_rows, g_vec, b_vec, T, ps):
    ident_b = C["ident_b"]
    wo, mT, xt, g_bc, b_bc, st, yo = T["wo"], T["mT"], T["xt"], T["g_bc"], T["b_bc"], T["st"], T["yo"]
    for half in range(2):
        load_w_cols(P, ph, wo[half], "wb%d" % half, w_out, half * 512, 512)
    P.dma("sync", g_bc, g_vec.partition_broadcast(128), writes=[(ph, "g_bc")] + T["gk"], slot=("g_bc",))
    P.dma("sync", b_bc, b_vec.partition_broadcast(128), writes=[(ph, "b_bc")] + T["bk"], slot=("b_bc",))
    pbf = [ps[6][:].bitcast(BF16), ps[7][:].bitcast(BF16)]
    P.barrier()
    for i in range(L // 128):
        s = i % 2
        P.dma("sync", xt[s][:], h_rows[i * 128:(i + 1) * 128, :], writes=[(ph, "xt", s)])
        def tr(e, i=i, s=s):
            for kk in range(8):
                r = e.transpose(pbf[s][:, kk * 128:(kk + 1) * 128], mix[:, i, kk * 128:(kk + 1) * 128], ident_b[:])
            return r
        P.op("tensor", tr, reads=[(ph, "mix", i), "ident_b"], writes=[(ph, "ps", 6 + s)])
        P.op("vector", lambda e, s=s: e.tensor_copy(mT[s], pbf[s]), reads=[(ph, "ps", 6 + s)], writes=[(ph, "ps", 6 + s), (ph, "mT", s)] + T["mTk"][s])
        for n in range(2):
            pi = 4 + n
            def mm(e, s=s, n=n, pi=pi):
                for kk in range(8):
                    r = e.matmul(ps[pi][:], mT[s][:, kk * 128:(kk + 1) * 128], wo[n][:, kk, :], start=(kk == 0), stop=(kk == 7))
                return r
            P.op("tensor", mm, reads=[(ph, "mT", s)] + T["wok"][n], writes=[(ph, "ps", pi)])
            P.op("vector", lambda e, s=s, n=n, pi=pi: e.scalar_tensor_tensor(yo[s][:, n * 512:(n + 1) * 512], xt[s][:, n * 512:(n + 1) * 512], ALPHA, ps[pi][:], ALU.mult, ALU.add),
                 reads=[(ph, "xt", s), (ph, "ps", pi)], writes=[(ph, "ps", pi), (ph, "yo", s, n)])
        layer_norm_tile(P, ph, yo[s], [(ph, "yo", s, 0), (ph, "yo", s, 1)], yo[s], (ph, "yo", s, 0), g_bc, b_bc, st, i)
        P.dma("sync", out_rows[i * 128:(i + 1) * 128, :], yo[s], reads=[(ph, "yo", s, 0)], writes=[(ph, "hout", i)], slot=("yo_st", s))
        P.res_w[(ph, "yo", s, 1)] = P.res_w[(ph, "yo", s, 0)]
        P.res_r[(ph, "yo", s, 1)] = dict(P.res_r[(ph, "yo", s, 0)])


def even_mixer_seq(P, ph, C, lay_j, h_rows, out_rows, L, lam_init, ln_g, ln_b):
    D = C["dram"]
    ident, ident_b = C["ident"], C["ident_b"]
    NT = L // 128
    NC = L // 512
    sb = lambda n, s, d=F32: P.sb(ph + n, s, d)
    K = lambda *a: (ph,) + a
    w_in = D["ev_w_in"][lay_j]
    ps = [P.ps(ph + "ps%d" % i, [128, 512]) for i in range(8)]

    xt = [sb("xt%d" % i, [128, 1024]) for i in range(2)]
    xT = sb("xT", [128, 8, L], BF16)
    wb = [sb("wb%d" % i, [128, 8, 512], BF16) for i in range(2)]
    va = sb("va", [128, NT, 512], BF16)
    vb = sb("vb", [128, NT, 4, 132], BF16)
    mix = sb("mix", [128, NT, 1024], BF16)
    qa = [sb("qa%d" % i, [128, L], BF16) for i in range(4)]
    ka = [sb("ka%d" % i, [128, L], BF16) for i in range(4)]
    qd = [sb("qd%d" % i, [68, L], BF16) for i in range(2)]
    kd = [sb("kd%d" % i, [68, L], BF16) for i in range(2)]
    mstrict = sb("mstrict", [128, 4, 512], BF16)
    mcausal = sb("mcausal", [128, 4, 512], BF16)
    uincl = sb("uincl", [128, 128], BF16)
    ones1 = sb("ones1", [1, 128], BF16)
    efg = sb("efg", [128, 4, 512])
    ef = [efg[:, i, :] for i in range(2)]
    eg = [efg[:, 2 + i, :] for i in range(2)]
    atsp = sb("atsp", [128, 4, 512], BF16)
    at = [atsp[:, i, :] for i in range(2)]
    spt = [atsp[:, 2 + i, :] for i in range(2)]
    suf = sb("suf", [1, 512], BF16)
    lamv = sb("lamv", [128, 4, 64]); lam = sb("lam", [128, 4])
    wsub = sb("wsub", [128, 128])
    o14 = sb("o14", [128, 4, 128]); ob4 = sb("ob4", [128, 4, 128]); sm4 = sb("sm4", [128, 4, 8])
    junk = sb("junk", [128, 1024], BF16)

    P.dma("gpsimd", mstrict[:], D["c_mstrict"], writes=[K("mstrict")])
    P.dma("gpsimd", mcausal[:], D["c_mcausal"], writes=[K("mcausal")])
    P.dma("gpsimd", uincl[:], D["c_uincl"], writes=[K("uincl")])
    P.op("vector", lambda e: e.memset(ones1[:], 1.0), writes=[K("ones1")])
    for i, nm in enumerate(["ev_lambda_q1", "ev_lambda_k1", "ev_lambda_q2", "ev_lambda_k2"]):
        P.dma("sync", lamv[:, i, :], D[nm][lay_j].partition_broadcast(128), writes=[K("lamv", i)])
    P.dma("sync", wsub[:], D["ev_subln_w"][lay_j].partition_broadcast(128), writes=[K("wsub")])
    P.op("vector", lambda e: e.tensor_scalar(wsub[:], wsub[:], 1.0 - lam_init, None, ALU.mult), reads=[K("wsub")], writes=[K("wsub")])
    for j in range(2):
        P.op("vector", lambda e, j=j: e.tensor_tensor(lamv[:, 2 * j, :], lamv[:, 2 * j, :], lamv[:, 2 * j + 1, :], ALU.mult),
             reads=[K("lamv", 2 * j), K("lamv", 2 * j + 1)], writes=[K("lamv", 2 * j)])
        P.op("vector", lambda e, j=j: e.reduce_sum(lam[:, j:j + 1], lamv[:, 2 * j, :], axis=AX.X), reads=[K("lamv", 2 * j)], writes=[K("lam", j)])
        P.op("scalar", lambda e, j=j: e.activation(lam[:, j:j + 1], lam[:, j:j + 1], AF.Exp), reads=[K("lam", j)], writes=[K("lam", j)])
    P.op("vector", lambda e: e.tensor_tensor(lam[:, 2:3], lam[:, 1:2], lam[:, 0:1], ALU.subtract), reads=[K("lam", 0), K("lam", 1)], writes=[K("lam", 2)])
    P.op("vector", lambda e: e.tensor_scalar(lam[:, 3:4], lam[:, 2:3], -lam_init, None, ALU.add), reads=[K("lam", 2)], writes=[K("nlam")])

    load_xT(P, ph, C, h_rows, L, xT, xt, ps)

    wslot = [0]
    def next_w(c0, ncols=512):
        s = wslot[0] % 2
        wslot[0] += 1
        load_w_cols(P, ph, wb[s], "wb%d" % s, w_in, c0, ncols)
        return wb[s], wkeys(ph, "wb%d" % s)

    wt, wk = next_w(1024)
    proj_tm(P, ph, wt, wk, 0, 512, xT, L, ps, [2, 3],
            lambda i, pa, pk: P.op("vector", lambda e: e.tensor_copy(va[:, i, :], pa), reads=[pk], writes=[pk, K("va", i)]))
    wt, wk = next_w(2560)
    P.op("vector", lambda e: e.memset(vb[:], 1.0), writes=[K("vb", i) for i in range(NT)])
    proj_tm(P, ph, wt, wk, 0, 512, xT, L, ps, [2, 3],
            lambda i, pa, pk: P.op("vector", lambda e: e.tensor_copy(vb[:, i, :, 0:128], pa.rearrange("p (h d) -> p h d", h=4)), reads=[pk], writes=[pk, K("vb", i)]))

    wt, wk = next_w(0)
    for j in range(4):
        proj_fm(P, ph, wt, wk, j * 128, 128, xT, L, ps, [2, 3],
                lambda ch, pa, pk, j=j: P.op("scalar", lambda e: e.activation(qa[j][:, ch * 512:(ch + 1) * 512], pa, AF.Identity, scale=0.125),
                                            reads=[pk], writes=[pk, K("qa", j)]))
    wt, wk = next_w(512)
    for j in range(4):
        proj_fm(P, ph, wt, wk, j * 128, 128, xT, L, ps, [2, 3],
                lambda ch, pa, pk, j=j: P.op("vector", lambda e: e.tensor_copy(ka[j][:, ch * 512:(ch + 1) * 512], pa),
                                            reads=[pk], writes=[pk, K("ka", j)]))

    it = 0
    suf2 = [suf, sb("suf1", [1, 512], BF16)]

    def sb_head(h):
        suf = suf2[h % 2]
        sufk = K("suf", h % 2)
        qT = qa[h // 2][(h % 2) * 64:(h % 2) * 64 + 64, :]
        kT = ka[h // 2][(h % 2) * 64:(h % 2) * 64 + 64, :]
        qk = [K("qa", h // 2), K("ka", h // 2)]
        for c in range(NC):
            po = ps[4 + (h % 2) * 2 + c % 2]
            pok = K("ps", 4 + (h % 2) * 2 + c % 2)
            nk = 4 * c + 4
            for kt in range(nk - 1, -1, -1):
                j = kt - 4 * c
                q0 = max(j, 0) * 128
                b = h % 2
                pz, pzk = ps[b], K("ps", b)
                pg, pgk = ps[2 + b], K("ps", 2 + b)
                first = False
                if kt == nk - 1:
                    P.op("vector", lambda e: e.memset(suf[:], 0.0), writes=[sufk])
                P.op("tensor", lambda e, pz=pz, kt=kt, c=c, q0=q0, kT=kT, qT=qT: e.matmul(
                    pz[:, q0:512], kT[:, kt * 128:(kt + 1) * 128], qT[:, c * 512 + q0:(c + 1) * 512], start=True, stop=True),
                     reads=qk, writes=[pzk])
                P.op("scalar", lambda e, b=b, pz=pz, q0=q0: e.activation(ef[b][:, q0:512], pz[:, q0:512], AF.Exp), reads=[pzk], writes=[pzk, K("ef", b)])
                P.op("scalar", lambda e, b=b, q0=q0: e.activation(spt[b][:, q0:512], ef[b][:, q0:512], AF.Ln, bias=1.0), reads=[K("ef", b)], writes=[K("sp", b)])
                if j >= 0:
                    P.op("vector", lambda e, b=b, j=j, q0=q0: e.tensor_tensor(spt[b][:, q0:512], spt[b][:, q0:512], mstrict[:, j, q0:512], ALU.mult),
                         reads=[K("sp", b), K("mstrict")], writes=[K("sp", b)])
                yield
                def mg(e, b=b, pg=pg, q0=q0, first=first):
                    r = e.matmul(pg[:, q0:512], uincl[:], spt[b][:, q0:512], start=True, stop=first)
                    if not first:
                        r = e.matmul(pg[:, q0:512], ones1[:], suf[:, q0:512], start=False, stop=True)
                    return r
                P.op("tensor", mg, reads=[K("sp", b), K("uincl"), K("ones1"), sufk], writes=[pgk])
                P.op("scalar", lambda e, b=b, pg=pg, q0=q0: e.activation(eg[b][:, q0:512], pg[:, q0:512], AF.Exp, scale=-1.0), reads=[pgk], writes=[pgk, K("eg", b)])
                if kt > 0:
                    P.op("vector", lambda e, pg=pg, q0=q0: e.tensor_copy(suf[:, q0:512], pg[0:1, q0:512]), reads=[pgk], writes=[pgk, sufk])
                P.op("vector", lambda e, b=b, q0=q0: e.tensor_tensor(at[b][:, q0:512], ef[b][:, q0:512], eg[b][:, q0:512], ALU.mult),
                     reads=[K("ef", b), K("eg", b)], writes=[K("at", b)])
                if j >= 0:
                    P.op("vector", lambda e, b=b, j=j, q0=q0: e.tensor_tensor(at[b][:, q0:512], at[b][:, q0:512], mstrict[:, j, q0:512], ALU.mult),
                         reads=[K("at", b), K("mstrict")], writes=[K("at", b)])
                yield
                def pv(e, b=b, kt=kt, c=c, j=j, h=h, po=po):
                    r = None
                    for qs in range(3, max(j, 0) - 1, -1):
                        qt = 4 * c + qs
                        r = e.matmul(po[:, qs * 64:(qs + 1) * 64], at[b][:, qs * 128:(qs + 1) * 128], va[:, kt, h * 64:(h + 1) * 64],
                                     start=(kt == 4 * c + 3 and qs == 3), stop=(kt == 0 and qs == 0))
                    return r
                P.op("tensor", pv, reads=[K("at", b), K("va", kt)], writes=[pok])
                yield
            P.op("vector", lambda e, po=po, c=c, h=h: e.tensor_copy(mix[:, 4 * c:4 * c + 4, h * 64:(h + 1) * 64], po[:, 0:256].rearrange("p (a d) -> p a d", a=4)),
                 reads=[pok], writes=[pok] + [K("mix", 4 * c + qs) for qs in range(4)])

    for h0 in range(0, 8, 2):
        gens = [sb_head(h0), sb_head(h0 + 1)]
        alive = [True, True]
        while any(alive):
            for gi in range(2):
                if alive[gi]:
                    try:
                        next(gens[gi])
                    except StopIteration:
                        alive[gi] = False

    for h in range(4):
        for m in range(2):
            wt, wk = next_w(1536 + (h * 2 + m) * 64, 64)
            proj_fm(P, ph, wt, wk, 0, 64, xT, L, ps, [2, 3],
                    lambda ch, pa, pk, m=m: P.op("scalar", lambda e: e.activation(qd[m][0:64, ch * 512:(ch + 1) * 512], pa, AF.Identity, scale=0.125),
                                                reads=[pk], writes=[pk, K("qd", m)]))
            wt, wk = next_w(2048 + (h * 2 + m) * 64, 64)
            proj_fm(P, ph, wt, wk, 0, 64, xT, L, ps, [2, 3],
                    lambda ch, pa, pk, m=m: P.op("vector", lambda e: e.tensor_copy(kd[m][0:64, ch * 512:(ch + 1) * 512], pa),
                                                reads=[pk], writes=[pk, K("kd", m)]))
            P.dma("gpsimd", qd[m][64:68, :], D["c_qaug4"][h][:, 0:L], writes=[K("qd", m)], slot=("qaug", m))
            P.dma("gpsimd", kd[m][64:68, :], D["c_kaug"][:, 0:L], writes=[K("kd", m)], slot=("kaug", m))
        for c in range(NC):
            nk = 4 * c + 4
            tiles = [(m, kt) for m in range(2) for kt in range(nk - 1, -1, -1)]
            bsl = []
            for _ in tiles:
                bsl.append(it % 2)
                it += 1

            def emit_s(n, c=c):
                m, kt = tiles[n]
                b = bsl[n]
                q0 = max(kt - 4 * c, 0) * 128
                pz, pzk = ps[b], K("ps", b)
                P.op("tensor", lambda e, pz=pz, kt=kt, c=c, q0=q0, m=m: e.matmul(
                    pz[:, q0:512], kd[m][:, kt * 128:(kt + 1) * 128], qd[m][:, c * 512 + q0:(c + 1) * 512], start=True, stop=True),
                     reads=[K("qd", m), K("kd", m)], writes=[pzk])

            def emit_rest(n, c=c, h=h):
                m, kt = tiles[n]
                b = bsl[n]
                j = kt - 4 * c
                q0 = max(j, 0) * 128
                pz, pzk = ps[b], K("ps", b)
                pos = [ps[4 + 2 * m], ps[5 + 2 * m]]
                P.op("scalar", lambda e, b=b, pz=pz, q0=q0: e.activation(at[b][:, q0:512], pz[:, q0:512], AF.Exp), reads=[pzk], writes=[pzk, K("at", b)])
                if j >= 0:
                    P.op("vector", lambda e, b=b, j=j, q0=q0: e.tensor_tensor(at[b][:, q0:512], at[b][:, q0:512], mcausal[:, j, q0:512], ALU.mult),
                         reads=[K("at", b), K("mcausal")], writes=[K("at", b)])
                def pv(e, b=b, kt=kt, c=c, j=j, h=h, pos=pos):
                    r = None
                    for qs in range(3, max(j, 0) - 1, -1):
                        qt = 4 * c + qs
                        r = e.matmul(pos[qs // 2][:, (qs % 2) * 256:(qs % 2) * 256 + 129], at[b][:, qs * 128:(qs + 1) * 128], vb[:, kt, h, 0:129],
                                     start=(kt == qt and qs % 2 == 1), stop=(kt == 0 and qs % 2 == 0))
                    return r
                P.op("tensor", pv, reads=[K("at", b), K("vb", kt)], writes=[K("ps", 4 + 2 * m), K("ps", 5 + 2 * m)])

            emit_s(0)
            for n in range(len(tiles)):
                if n + 1 < len(tiles):
                    emit_s(n + 1)
                emit_rest(n)
            def P1(qs): return ps[4 + qs // 2][:, (qs % 2) * 256:(qs % 2) * 256 + 129]
            def P2(qs): return ps[6 + qs // 2][:, (qs % 2) * 256:(qs % 2) * 256 + 129]
            def K1(qs): return K("ps", 4 + qs // 2)
            def K2(qs): return K("ps", 6 + qs // 2)
            for qs in range(4):
                P.op("vector", lambda e, qs=qs: e.reciprocal(sm4[:, qs, 0:1], P1(qs)[:, 128:129]), reads=[K1(qs)], writes=[K1(qs), K("sm0", qs)])
            for qs in range(4):
                P.op("vector", lambda e, qs=qs: e.reciprocal(sm4[:, qs, 1:2], P2(qs)[:, 128:129]), reads=[K2(qs)], writes=[K2(qs), K("sm1", qs)])
            for qs in range(4):
                P.op("vector", lambda e, qs=qs: e.tensor_tensor(sm4[:, qs, 1:2], sm4[:, qs, 1:2], lam[:, 3:4], ALU.mult), reads=[K("sm1", qs), K("nlam")], writes=[K("sm1", qs)])
            for qs in range(4):
                P.op("vector", lambda e, qs=qs: e.tensor_scalar(o14[:, qs, :], P1(qs)[:, 0:128], sm4[:, qs, 0:1], None, ALU.mult), reads=[K1(qs), K("sm0", qs)], writes=[K1(qs), K("o1", qs)])
            for qs in range(4):
                P.op("vector", lambda e, qs=qs: e.scalar_tensor_tensor(ob4[:, qs, :], P2(qs)[:, 0:128], sm4[:, qs, 1:2], o14[:, qs, :], ALU.mult, ALU.add),
                     reads=[K2(qs), K("sm1", qs), K("o1", qs)], writes=[K2(qs), K("ob", qs)])
            for qs in range(4):
                P.op("vector", lambda e, qs=qs: e.memset(sm4[:, qs, 2:3], 0.0), writes=[K("sm2", qs)])
            for qs in range(4):
                P.op("scalar", lambda e, qs=qs: e.activation(junk[:, qs * 128:(qs + 1) * 128], ob4[:, qs, :], AF.Square, accum_out=sm4[:, qs, 2:3]),
                     reads=[K("ob", qs), K("sm2", qs)], writes=[K("sm2", qs), K("junk", qs)])
            for qs in range(4):
                P.op("vector", lambda e, qs=qs: e.tensor_scalar(sm4[:, qs, 2:3], sm4[:, qs, 2:3], 1.0 / 128, RMS_EPS, ALU.mult, ALU.add), reads=[K("sm2", qs)], writes=[K("sm2", qs)])
            for qs in range(4):
                P.op("scalar", lambda e, qs=qs: e.sqrt(sm4[:, qs, 2:3], sm4[:, qs, 2:3]), reads=[K("sm2", qs)], writes=[K("sm2", qs)])
            for qs in range(4):
                P.op("vector", lambda e, qs=qs: e.reciprocal(sm4[:, qs, 3:4], sm4[:, qs, 2:3]), reads=[K("sm2", qs)], writes=[K("sm3", qs)])
            for qs in range(4):
                qt = 4 * c + qs
                P.op("vector", lambda e, qs=qs, qt=qt, h=h: e.scalar_tensor_tensor(mix[:, qt, 512 + h * 128:512 + (h + 1) * 128], ob4[:, qs, :], sm4[:, qs, 3:4], wsub[:], ALU.mult, ALU.mult),
                     reads=[K("ob", qs), K("sm3", qs), K("wsub")], writes=[K("mix", qt)])

    if "dbg" in D:
        P.dma("gpsimd", D["dbg"].rearrange("(t p) c -> p t c", p=128), mix[:], reads=[K("mix", i) for i in range(NT)], writes=[K("dbg")])
    xTf = xT[:].rearrange("p a b -> p (a b)").bitcast(F32)
    T = dict(wo=wb, mT=[atsp[:, 0:2, :].rearrange("p a b -> p (a b)"), atsp[:, 2:4, :].rearrange("p a b -> p (a b)")],
             mTk=[[K("at", 0), K("at", 1)], [K("sp", 0), K("sp", 1)]],
             xt=xt, g_bc=efg[:, 0:2, :].rearrange("p a b -> p (a b)"), b_bc=efg[:, 2:4, :].rearrange("p a b -> p (a b)"),
             gk=[K("ef", 0), K("ef", 1)], bk=[K("eg", 0), K("eg", 1)],
             st={n: sb("st_" + n, [128, 1]) for n in ("s1", "s2", "mean", "msq", "var", "rstd")},
             yo=[xTf[:, 0:1024], xTf[:, 1024:2048]], wok=[wkeys(ph, "wb0"), wkeys(ph, "wb1")])
    T["st"]["junk"] = junk
    outproj_ln(P, ph, C, mix, L, D["ev_w_out"][lay_j], h_rows, out_rows, ln_g, ln_b, T, ps)


def odd_mixer_seq(P, ph, C, lay_j, h_rows, out_rows, L, ln_g, ln_b):
    D = C["dram"]
    ident, ident_b = C["ident"], C["ident_b"]
    NT = L // 128
    NC = L // 512
    KSEL = min(256, L // 4)
    sb = lambda n, s, d=F32: P.sb(ph + n, s, d)
    K = lambda *a: (ph,) + a
    w_in = D["od_w_in"][lay_j]
    ps = [P.ps(ph + "ps%d" % i, [128, 512]) for i in range(8)]
    pbf = ps[6][:].bitcast(BF16)

    xt = [sb("xt%d" % i, [128, 1024]) for i in range(2)]
    xT = sb("xT", [128, 8, L], BF16)
    wb = [sb("wb%d" % i, [128, 8, 512], BF16) for i in range(2)]
    mix = sb("mix", [128, NT, 1024], BF16)
    qTc = sb("qTc", [128, 16, 512], BF16)
    ckvT = sb("ckvT", [128, L], BF16)
    ckva = sb("ckva", [128, NT, 132], BF16)
    qiT = [sb("qiT%d" % i, [128, L], BF16) for i in range(4)]
    kiT2 = sb("kiT2", [128, L], BF16)
    widx = sb("widx", [128, NT, 8])
    score = sb("score", [128, L]); wkt = sb("wkt", [128, L])
    MB = [sb("MB%d" % i, [128, L], BF16) for i in range(4)]
    MB2 = [xt[0][:].bitcast(BF16), xt[1][:].bitcast(BF16)] + [sb("MBx%d" % i, [128, L], BF16) for i in range(2)]
    MBS = [MB, MB2]
    iti = [0]
    rl = [sb("rl%d" % i, [128, 512]) for i in range(2)]
    atsp = sb("atsp", [128, 4, 512], BF16)
    at = [atsp[:, i, :] for i in range(2)]
    kaug = sb("kaug", [9, L], BF16)
    qaugc = sb("qaugc", [9, 16, 512], BF16)
    wuv = sb("wuv", [128, 16, 64], BF16)
    kvw = sb("kvw", [128, 128])
    oh4 = sb("oh4", [128, 4, 128], BF16); ohT4 = sb("ohT4", [128, 512], BF16); sm4 = sb("sm4", [128, 4])
    m8 = sb("m8", [128, 8]); sm = sb("sm", [128, 8])
    junk = oh4[:].rearrange("p a b -> p (a b)")

    P.dma("gpsimd", kaug[:], D["c_kaug9"][:, 0:L], writes=[K("kaug")])
    P.dma("gpsimd", wuv[:], D["od_w_uv"][lay_j].rearrange("h c d -> c h d"), writes=[K("wuv")])
    P.dma("sync", kvw[:], D["od_kv_norm_w"][lay_j].partition_broadcast(128), writes=[K("kvw")])
    P.op("vector", lambda e: e.memset(ckva[:], 1.0), writes=[K("ckva", i) for i in range(NT)])

    load_xT(P, ph, C, h_rows, L, xT, xt, ps)
    wslot = [0]
    def next_w(c0, ncols=512, dst0=0, new=True):
        if new:
            wslot[0] += 1
        s = wslot[0] % 2
        load_w_cols(P, ph, wb[s], "wb%d" % s, w_in, c0, ncols, dst0=dst0)
        return wb[s], wkeys(ph, "wb%d" % s, (dst0,))

    wt, wk = next_w(2048, 128)
    def ev_ckv(i, pa, pk):
        P.op("vector", lambda e: e.memset(sm[:, 0:1], 0.0), writes=[K("sm0")])
        P.op("scalar", lambda e: e.activation(junk[:, 0:128], pa, AF.Square, accum_out=sm[:, 0:1]), reads=[pk, K("sm0")], writes=[pk, K("sm0"), K("junk")])
        P.op("vector", lambda e: e.tensor_scalar(sm[:, 0:1], sm[:, 0:1], 1.0 / 128, RMS_EPS, ALU.mult, ALU.add), reads=[K("sm0")], writes=[K("sm0")])
        P.op("scalar", lambda e: e.sqrt(sm[:, 0:1], sm[:, 0:1]), reads=[K("sm0")], writes=[K("sm0")])
        P.op("vector", lambda e: e.reciprocal(sm[:, 1:2], sm[:, 0:1]), reads=[K("sm0")], writes=[K("sm1")])
        P.op("vector", lambda e: e.scalar_tensor_tensor(ckva[:, i, 0:128], pa, sm[:, 1:2], kvw[:], ALU.mult, ALU.mult),
             reads=[pk, K("sm1"), K("kvw")], writes=[pk, K("ckva", i)])
        P.op("tensor", lambda e: e.transpose(pbf[:, 0:128], ckva[:, i, 0:128], ident_b[:]), reads=[K("ckva", i), "ident_b"], writes=[K("ps", 6)])
        P.op("vector", lambda e: e.tensor_copy(ckvT[:, i * 128:(i + 1) * 128], pbf[:, 0:128]), reads=[K("ps", 6)], writes=[K("ps", 6), K("ckvT")])
    proj_tm(P, ph, wt, wk, 0, 128, xT, L, ps, [2, 3], ev_ckv)
    wt, wk = next_w(2176, 512)
    for j in range(4):
        proj_fm(P, ph, wt, wk, j * 128, 128, xT, L, ps, [2, 3],
                lambda ch, pa, pk, j=j: P.op("scalar", lambda e: e.activation(qiT[j][:, ch * 512:(ch + 1) * 512], pa, AF.Identity, scale=0.125),
                                            reads=[pk], writes=[pk, K("qiT")]))
    wt, wk0 = next_w(2688, 64, dst0=0)
    _, wk1 = next_w(2688, 64, dst0=64, new=False)
    _, wk2 = next_w(2752, 8, dst0=128, new=False)
    proj_fm(P, ph, wt, wk0 + wk1, 0, 128, xT, L, ps, [2, 3],
            lambda ch, pa, pk: P.op("vector", lambda e: e.tensor_copy(kiT2[:, ch * 512:(ch + 1) * 512], pa), reads=[pk], writes=[pk, K("kiT2")]))
    proj_tm(P, ph, wt, wk2, 128, 8, xT, L, ps, [2, 3],
            lambda i, pa, pk: P.op("vector", lambda e: e.tensor_scalar(widx[:, i, :], pa, 8 ** -0.5, None, ALU.mult), reads=[pk], writes=[pk, K("widx")]))

    if "dbg2" in D and C.get("dumpc", 0) == -1:
        P.dma("gpsimd", D["dbg2"][:, 0:L], kiT2[:, 0:L], reads=[K("kiT2")], writes=[K("dbg2")])
        P.dma("gpsimd", D["dbg3"][:, 0:L], qiT[0][:, 0:L], reads=[K("qiT")], writes=[K("dbg3")])
        P.dma("gpsimd", D["dbg"][0:128, 0:NT * 8], widx[:].rearrange("p a b -> p (a b)"), reads=[K("widx")], writes=[K("dbgw")])
    def dump_w(stage):
        if "dbg2" in D and C.get("dumpat", None) == stage:
            P.dma("gpsimd", D["dbg"][0:128, 0:NT * 8], widx[:].rearrange("p a b -> p (a b)"), reads=[K("widx")], writes=[K("dbgw")])
    it = 0
    for c in range(NC):
        for g in range(4):
            if c == 0:
                dump_w(10 + g)
            wt, wk = next_w(g * 512, 512)
            if c == 0:
                dump_w(20 + g)
            for hh in range(4):
                h = g * 4 + hh
                pi = 2 + h % 2
                def mm(e, hh=hh, pi=pi, wt=wt, c=c):
                    for kk in range(8):
                        r = e.matmul(ps[pi][:], wt[:, kk, hh * 128:(hh + 1) * 128], xT[:, kk, c * 512:(c + 1) * 512], start=(kk == 0), stop=(kk == 7))
                    return r
                P.op("tensor", mm, reads=wk + [K("xT", c)], writes=[K("ps", pi)])
                P.op("scalar", lambda e, h=h, pi=pi: e.activation(qTc[:, h, :], ps[pi][:], AF.Identity, scale=128 ** -0.5),
                     reads=[K("ps", pi)], writes=[K("ps", pi), K("qTc", h)])
        if c == 0:
            dump_w(1)
        P.dma("gpsimd", qaugc[:], D["c_qaug16"][:, :, c * 512:(c + 1) * 512].rearrange("h r l -> r h l"), writes=[K("qaugc")])
        if c == 0:
            P.op("vector", lambda e: e.engine_nop() if False else e.memset(sm[:, 7:8], 0.0), reads=[K("qaugc")], writes=[K("sm7")])
            if C.get("dumpat", None) == 2:
                P.dma("gpsimd", D["dbg"][0:128, 0:NT * 8], widx[:].rearrange("p a b -> p (a b)"), reads=[K("widx"), K("sm7")], writes=[K("dbgw")])
        def idx_chunk(c):
            mset = c % 2
            for qs in range(4):
                qt = 4 * c + qs
                n_s = (qt + 1) * 128
                for sc in range((n_s + 511) // 512):
                    w = min(512, n_s - sc * 512)
                    for ih in range(8):
                        b = iti[0] % 2
                        iti[0] += 1
                        pi = 2 + b
                        P.op("tensor", lambda e, pi=pi, ih=ih, qt=qt, sc=sc, w=w: e.matmul(
                            ps[pi][:, 0:w], qiT[ih // 2][(ih % 2) * 64:(ih % 2) * 64 + 64, qt * 128:(qt + 1) * 128],
                            kiT2[(ih % 2) * 64:(ih % 2) * 64 + 64, sc * 512:sc * 512 + w], start=True, stop=True),
                             reads=[K("qiT"), K("kiT2")], writes=[K("ps", pi)])
                        P.op("scalar", lambda e, pi=pi, b=b, w=w: e.activation(rl[b][:, 0:w], ps[pi][:, 0:w], AF.Relu), reads=[K("ps", pi)], writes=[K("ps", pi), K("rl", b)])
                        if ih == 0:
                            P.op("vector", lambda e, b=b, w=w, sc=sc, qt=qt, ih=ih: e.tensor_scalar(score[:, sc * 512:sc * 512 + w], rl[b][:, 0:w], widx[:, qt, ih:ih + 1], None, ALU.mult),
                                 reads=[K("rl", b), K("widx")], writes=[K("score")])
                        else:
                            P.op("vector", lambda e, b=b, w=w, sc=sc, qt=qt, ih=ih: e.scalar_tensor_tensor(score[:, sc * 512:sc * 512 + w], rl[b][:, 0:w], widx[:, qt, ih:ih + 1],
                                                                                                      score[:, sc * 512:sc * 512 + w], ALU.mult, ALU.add),
                                 reads=[K("rl", b), K("widx"), K("score")], writes=[K("score")])
                P.op("gpsimd", lambda e, qt=qt: e.affine_select(score[:, qt * 128:(qt + 1) * 128], score[:, qt * 128:(qt + 1) * 128], [[-1, 128]], ALU.is_ge, -1e30,
                                                             base=0, channel_multiplier=1), reads=[K("score")], writes=[K("score")])
                if qt * 128 >= KSEL:
                    R = KSEL // 8
                    for r in range(R):
                        src = score if r == 0 else wkt
                        P.op("vector", lambda e, src=src, n_s=n_s: e.max(m8[:], src[:, 0:n_s]), reads=[K("score"), K("wkt")], writes=[K("m8")])
                        if r < R - 1:
                            P.op("vector", lambda e, src=src, n_s=n_s: e.match_replace(wkt[:, 0:n_s], m8[:], src[:, 0:n_s], -1e30),
                                 reads=[K("score"), K("wkt"), K("m8")], writes=[K("wkt")])
                    P.op("vector", lambda e, qs=qs, n_s=n_s: e.tensor_scalar(MBS[mset][qs][:, 0:n_s], score[:, 0:n_s], m8[:, 7:8], NEGM, ALU.is_lt, ALU.mult),
                         reads=[K("score"), K("m8")], writes=[K("MB", mset, qs)])
                else:
                    P.op("vector", lambda e, qs=qs, n_s=n_s: e.tensor_scalar(MBS[mset][qs][:, 0:n_s], score[:, 0:n_s], -1e29, NEGM, ALU.is_lt, ALU.mult),
                         reads=[K("score")], writes=[K("MB", mset, qs)])
            if "dbg2" in D and C.get("dumpc", 0) == -2 and c == 1:
                P.dma("gpsimd", D["dbg2"][:, 0:L], kiT2[:, 0:L], reads=[K("kiT2")], writes=[K("dbg2")])
                P.dma("gpsimd", D["dbg3"][:, 0:L], qiT[0][:, 0:L], reads=[K("qiT")], writes=[K("dbg3")])
                P.dma("gpsimd", D["dbg"][0:128, 0:NT * 8], widx[:].rearrange("p a b -> p (a b)"), reads=[K("widx")], writes=[K("dbgw")])

        def record(fn, *a):
            rec = []
            real = P.op
            P.op = lambda *aa, **kk: rec.append((aa, kk))
            try:
                fn(*a)
            finally:
                P.op = real
            return rec

        if c == 0:
            idx_chunk(0)
        nxt = record(idx_chunk, c + 1) if c + 1 < NC else []
        if "dbg2" in D and c == C.get("dumpc", 0):
            P.dma("gpsimd", D["dbg2"][:, 0:L], MB[3][:, 0:L], reads=[K("MB", 3)], writes=[K("dbg2")])
            P.dma("sync", D["dbg3"][:, 0:L], score[:, 0:L], reads=[K("score")], writes=[K("dbg3")])
        if c == 0:
            dump_w(5)
        nk = 4 * c + 4
        tiles = [(h, kt) for h in range(16) for kt in range(nk - 1, -1, -1)]
        bsl = []
        for _ in tiles:
            bsl.append(it % 2)
            it += 1
        POS = [[ps[4], ps[5]], [ps[4], ps[5]]]
        POSK = [[K("ps", 4), K("ps", 5)], [K("ps", 4), K("ps", 5)]]

        def emit_s(n, c=c):
            h, kt = tiles[n]
            b = bsl[n]
            jm = max(kt - 4 * c, 0)
            q0 = jm * 128
            pz, pzk = ps[b], K("ps", b)
            def sc_mm(e, pz=pz, kt=kt, q0=q0, jm=jm, h=h):
                e.matmul(pz[:, q0:512], ckvT[:, kt * 128:(kt + 1) * 128], qTc[:, h, q0:512], start=True, stop=False)
                r = e.matmul(pz[:, q0:512], kaug[:, kt * 128:(kt + 1) * 128], qaugc[:, h, q0:512], start=False, stop=False)
                for qs in range(jm, 4):
                    r = e.matmul(pz[:, qs * 128:(qs + 1) * 128], MBS[c % 2][qs][:, kt * 128:(kt + 1) * 128], ident_b[:], start=False, stop=(qs == 3))
                return r
            P.op("tensor", sc_mm, reads=[K("ckvT"), K("qTc", h), K("kaug"), K("qaugc"), "ident_b"] + [K("MB", c % 2, q) for q in range(jm, 4)], writes=[pzk])

        def emit_rest(n, c=c):
            h, kt = tiles[n]
            b = bsl[n]
            jm = max(kt - 4 * c, 0)
            q0 = jm * 128
            pz, pzk = ps[b], K("ps", b)
            pos, posk = POS[h % 2], POSK[h % 2]
            P.op("scalar", lambda e, b=b, pz=pz, q0=q0: e.activation(at[b][:, q0:512], pz[:, q0:512], AF.Exp), reads=[pzk], writes=[pzk, K("at", b)])
            def pv(e, b=b, kt=kt, c=c, jm=jm, pos=pos):
                r = None
                for qs in range(3, jm - 1, -1):
                    qt = 4 * c + qs
                    r = e.matmul(pos[qs // 2][:, (qs % 2) * 256:(qs % 2) * 256 + 129], at[b][:, qs * 128:(qs + 1) * 128], ckva[:, kt, 0:129],
                                 start=(kt == qt and qs % 2 == 1), stop=(kt == 0 and qs % 2 == 0))
                return r
            P.op("tensor", pv, reads=[K("at", b), K("ckva", kt)], writes=list(posk))

        def ep1(h):
            pos, posk = POS[h % 2], POSK[h % 2]
            for qs in range(4):
                p1 = pos[qs // 2][:, (qs % 2) * 256:(qs % 2) * 256 + 129]
                P.op("vector", lambda e, p1=p1, qs=qs: e.reciprocal(sm4[:, qs:qs + 1], p1[:, 128:129]), reads=[posk[qs // 2]], writes=[posk[qs // 2], K("rz", qs)])
            for qs in range(4):
                p1 = pos[qs // 2][:, (qs % 2) * 256:(qs % 2) * 256 + 129]
                P.op("vector", lambda e, p1=p1, qs=qs: e.tensor_scalar(oh4[:, qs, :], p1[:, 0:128], sm4[:, qs:qs + 1], None, ALU.mult),
                     reads=[posk[qs // 2], K("rz", qs)], writes=[posk[qs // 2], K("oh4")])

        def ep2(h):
            def tr(e):
                for qs in range(4):
                    r = e.transpose(pbf[:, qs * 128:(qs + 1) * 128], oh4[:, qs, :], ident_b[:])
                return r
            P.op("tensor", tr, reads=[K("oh4"), "ident_b"], writes=[K("ps", 6)])
            P.op("vector", lambda e: e.tensor_copy(ohT4[:], pbf[:, 0:512]), reads=[K("ps", 6)], writes=[K("ps", 6), K("ohT4")])

        def ep3(h, c=c):
            def mm(e, h=h):
                for qs in range(4):
                    r = e.matmul(ps[7][:, qs * 64:(qs + 1) * 64], ohT4[:, qs * 128:(qs + 1) * 128], wuv[:, h, :], start=(qs == 0), stop=(qs == 3))
                return r
            P.op("tensor", mm, reads=[K("ohT4"), K("wuv")], writes=[K("ps", 7)])
            P.op("scalar", lambda e, h=h, c=c: e.activation(mix[:, 4 * c:4 * c + 4, h * 64:(h + 1) * 64], ps[7][:, 0:256].rearrange("p (a d) -> p a d", a=4), AF.Identity),
                 reads=[K("ps", 7)], writes=[K("ps", 7)] + [K("mix", 4 * c + qs) for qs in range(4)])

        deferred = []
        emit_s(0)
        for n in range(len(tiles)):
            if n + 1 < len(tiles):
                emit_s(n + 1)
            emit_rest(n)
            h, kt = tiles[n]
            if kt == 0:
                ep1(h)
                deferred.append((n + 2, ep2, h))
                deferred.append((n + 4, ep3, h))
            keep = []
            for due, fn, hh in deferred:
                if due <= n:
                    fn(hh)
                else:
                    keep.append((due, fn, hh))
            deferred = keep
            per = -(-len(nxt) // len(tiles))
            for aa, kk in nxt[n * per:(n + 1) * per]:
                P.op(*aa, **kk)
        for due, fn, hh in sorted(deferred, key=lambda t: t[0]):
            fn(hh)
        for aa, kk in nxt[len(tiles) * per:]:
            P.op(*aa, **kk)

    if "dbg" in D and C.get("dumpc", 0) >= 0 and C.get("dumpat", None) is None:
        P.dma("gpsimd", D["dbg"].rearrange("(t p) c -> p t c", p=128), mix[:], reads=[K("mix", i) for i in range(NT)], writes=[K("dbg")])
    xTf = xT[:].rearrange("p a b -> p (a b)").bitcast(F32)
    T = dict(wo=wb, mT=[atsp[:, 0:2, :].rearrange("p a b -> p (a b)"), atsp[:, 2:4, :].rearrange("p a b -> p (a b)")],
             mTk=[[K("at", 0), K("at", 1)], [K("sp", 0), K("sp", 1)]],
             xt=xt, g_bc=score[:, 0:1024], b_bc=wkt[:, 0:1024],
             gk=[K("score")], bk=[K("wkt")],
             st={n: sb("st_" + n, [128, 1]) for n in ("s1", "s2", "mean", "msq", "var", "rstd")},
             yo=[xTf[:, 0:1024], xTf[:, 1024:2048]], wok=[wkeys(ph, "wb0"), wkeys(ph, "wb1")])
    T["st"]["junk"] = MB[0][:, 0:1024]
    outproj_ln(P, ph, C, mix, L, D["od_w_out"][lay_j], h_rows, out_rows, ln_g, ln_b, T, ps)


def make_consts(L=2048):
    s = np.arange(128)[:, None]; q = np.arange(512)[None, :]
    mstrict = np.stack([((q - j * 128) > s) for j in range(4)], axis=1).astype(np.float32)
    mcausal = np.stack([((q - j * 128) >= s) for j in range(4)], axis=1).astype(np.float32)
    jj = np.arange(128)[:, None]; ss = np.arange(128)[None, :]
    uincl = (jj >= ss).astype(np.float32)
    t = np.arange(L)
    hi = (t // 256) * 256; lo = t % 256
    def aug(slopes):
        qa = np.stack([np.stack([np.full(L, c), np.full(L, c), -c * hi, -c * lo]) for c in slopes]).astype(np.float32)
        return qa
    sl4 = 2.0 ** (-8.0 * np.arange(1, 5) / 4)
    sl16 = 2.0 ** (-8.0 * np.arange(1, 17) / 16)
    kaug = np.stack([hi, lo, np.ones(L), np.ones(L)]).astype(np.float32)
    import ml_dtypes
    def bsplit(v):
        v = np.asarray(v, dtype=np.float64); outp = []
        for _ in range(3):
            p = v.astype(ml_dtypes.bfloat16).astype(np.float64); outp.append(p); v = v - p
        return outp
    q9 = []
    for c in sl16:
        c1, c2, c3 = bsplit(np.full(L, c)); v1, v2, v3 = bsplit(c * t.astype(np.float64))
        q9.append(np.stack([c1, c1, c2, c2, c3, c3, -v1, -v2, -v3]))
    qaug9 = np.stack(q9).astype(np.float32)
    kaug9 = np.stack([hi, lo, hi, lo, hi, lo, np.ones(L), np.ones(L), np.ones(L)]).astype(np.float32)
    return dict(c_mstrict=mstrict, c_mcausal=mcausal, c_uincl=uincl, c_qaug4=aug(sl4), c_qaug16=qaug9, c_kaug9=kaug9, c_kaug=kaug,
                c_ident=np.eye(128, dtype=np.float32))


NCORES = 8
SEQ = 2048
NTOK = 2 * SEQ
N_EXPERTS = 32
_W_NAMES = ["ev_w_in", "ev_w_out", "ev_lambda_q1", "ev_lambda_k1", "ev_lambda_q2", "ev_lambda_k2", "ev_subln_w",
            "od_w_in", "od_kv_norm_w", "od_w_uv", "od_w_out", "ln_mix_g", "ln_mix_b", "router_w", "router_b",
            "exp_w_gu", "exp_b_gu", "exp_w_down", "exp_b_down", "ln_ffn_g", "ln_ffn_b"]


def build_program(shapes, cshapes):
    nc = bass.Bass("TRN2", target_bir_lowering=False)
    D = {}
    for n in _W_NAMES:
        D[n] = nc.dram_tensor(n, list(shapes[n]), F32, kind="ExternalInput").ap()
    for n, s in cshapes.items():
        D[n] = nc.dram_tensor(n, list(s), F32, kind="ExternalInput").ap()
    x = nc.dram_tensor("x", [NTOK, 1024], F32, kind="ExternalInput").ap()
    out = nc.dram_tensor("out", [NTOK, 1024], F32, kind="ExternalOutput").ap()
    h1 = nc.dram_tensor("h1", [NTOK, 1024], F32, kind="Internal").ap()
    h2 = nc.dram_tensor("h2", [NTOK, 1024], F32, kind="Internal").ap()
    h3 = nc.dram_tensor("h3", [NTOK, 1024], F32, kind="Internal").ap()
    with ExitStack() as st:
        P = Prog(nc, st)
        ident = P.sb("ident", [128, 128]); ident_b = P.sb("ident_b", [128, 128], BF16)
        P.dma("sync", ident[:], D["c_ident"], writes=["ident"])
        P.dma("gpsimd", ident_b[:], D["c_ident"], writes=["ident_b"])
        C = {"dram": D, "ident": ident, "ident_b": ident_b}
        lam_init0 = 0.8 - 0.6 * math.exp(-0.3 * 0)

        def phase(fn):
            with ExitStack() as ts:
                P.stack = ts
                fn()
                P.barrier()
            P.stack = st

        for sq in range(2):
            phase(lambda sq=sq: even_mixer_seq(P, "e%d" % sq, C, 0, x[sq * SEQ:(sq + 1) * SEQ, :], h1[sq * SEQ:(sq + 1) * SEQ, :], SEQ,
                                                lam_init0, D["ln_mix_g"][0], D["ln_mix_b"][0]))
        phase(lambda: moe_phase(P, "m0", C, 0, h1, h2, NTOK, N_EXPERTS))
        for sq in range(2):
            phase(lambda sq=sq: odd_mixer_seq(P, "o%d" % sq, C, 0, h2[sq * SEQ:(sq + 1) * SEQ, :], h3[sq * SEQ:(sq + 1) * SEQ, :], SEQ,
                                               D["ln_mix_g"][1], D["ln_mix_b"][1]))
        phase(lambda: moe_phase(P, "m1", C, 1, h3, out, NTOK, N_EXPERTS))
        P.final_wait()
        P.emit()
    return nc


def kernel(**inputs):
    consts = make_consts(SEQ)
    x = np.ascontiguousarray(np.asarray(inputs["x"], dtype=np.float32)).reshape(NCORES, NTOK, 1024)
    ws = {n: np.ascontiguousarray(np.asarray(inputs[n], dtype=np.float32)) for n in _W_NAMES}
    nc = build_program({n: ws[n].shape for n in _W_NAMES}, {n: v.shape for n, v in consts.items()})
    in_maps = []
    for c in range(NCORES):
        m = dict(ws)
        m.update(consts)
        m["x"] = x[c]
        in_maps.append(m)
    res = run_bass_kernel_spmd(nc, in_maps, core_ids=list(range(NCORES)))
    outs = [np.asarray(r["out"], dtype=np.float32) for r in res.results]
    return np.stack(outs, axis=0).reshape(16, SEQ, 1024)
```

```python
import math
from concourse.bass_utils import run_bass_kernel_spmd
from contextlib import ExitStack
import numpy as np
import concourse.bass as bass
import concourse.mybir as mybir

F32 = mybir.dt.float32
BF16 = mybir.dt.bfloat16
I32 = mybir.dt.int32
ALU = mybir.AluOpType
AF = mybir.ActivationFunctionType
AX = mybir.AxisListType

ENGS = ("sync", "scalar", "vector", "gpsimd", "tensor")


class Prog:
    def __init__(self, nc, stack):
        self.nc = nc
        self.stack = stack
        self.sem_stack = stack
        self.streams = {e: [] for e in ENGS}
        self.esem = {e: stack.enter_context(nc.semaphore("es_" + e)) for e in ENGS}
        self.etick = {e: 0 for e in ENGS}
        self.known = {e: {} for e in ENGS}
        self.res_w = {}
        self.res_r = {}
        self.dsem = {}
        self.semobj = {}
        for e in ENGS:
            self.semobj[id(self.esem[e])] = self.esem[e]
        self.n_ops = 0

    def sb(self, name, shape, dt=F32):
        return self.stack.enter_context(self.nc.sbuf_tensor(name, list(shape), dt))

    def ps(self, name, shape, dt=F32):
        return self.stack.enter_context(self.nc.psum_tensor(name, list(shape), dt))

    def _dma_sem(self, key):
        if key not in self.dsem:
            s = self.sem_stack.enter_context(self.nc.semaphore("ds_%d" % len(self.dsem)))
            self.dsem[key] = [s, 0]
            self.semobj[id(s)] = s
        return self.dsem[key]

    def _deps(self, eng, reads, writes):
        deps = {}

        def add(ev):
            if ev is None:
                return
            s, v = ev
            if deps.get(s, 0) < v:
                deps[s] = v

        for k in reads:
            add(self.res_w.get(k))
        for k in writes:
            add(self.res_w.get(k))
            for s, v in self.res_r.get(k, {}).items():
                add((s, v))
        waits = []
        kn = self.known[eng]
        for s, v in deps.items():
            if kn.get(s, 0) < v:
                kn[s] = v
                waits.append((s, v))
        return waits

    def _commit(self, ev, reads, writes):
        s, v = ev
        for k in reads:
            d = self.res_r.setdefault(k, {})
            if d.get(s, 0) < v:
                d[s] = v
        for k in writes:
            self.res_w[k] = ev
            self.res_r[k] = {}

    def op(self, eng, fn, reads=(), writes=()):
        waits = self._deps(eng, reads, writes)
        self.etick[eng] += 1
        ev = (id(self.esem[eng]), self.etick[eng])
        self.streams[eng].append((waits, fn, ev, 1))
        self._commit(ev, reads, writes)
        self.n_ops += 1

    def dma(self, eng, out, in_, reads=(), writes=(), slot=None, **kw):
        if slot is None:
            k0 = writes[0] if writes else reads[0]
            slot = ("_slot",) + tuple(k0[1:]) if isinstance(k0, tuple) and len(k0) > 1 else ("_slot", k0)
        ds = self._dma_sem(slot)
        waits = self._deps(eng, reads, writes)
        ds[1] += 16
        ev = (id(ds[0]), ds[1])
        fn = lambda e, out=out, in_=in_, kw=kw: e.dma_start(out=out, in_=in_, **kw)
        self.streams[eng].append((waits, fn, ev, 16))
        self._commit(ev, reads, writes)
        self.n_ops += 1

    def barrier(self):
        evs = [(id(self.esem[e]), self.etick[e]) for e in ENGS if self.etick[e] > 0]
        evs += [(id(s), c) for s, c in self.dsem.values() if c > 0]
        for e in ENGS:
            kn = self.known[e]
            waits = []
            for s, v in evs:
                if s == id(self.esem[e]) and False:
                    continue
                if kn.get(s, 0) < v:
                    kn[s] = v
                    waits.append((s, v))
            if waits:
                self.streams[e].append((waits, None, None, 0))

    def final_wait(self, eng="sync"):
        evs = [(id(s), c) for s, c in self.dsem.values() if c > 0]
        evs += [(id(self.esem[e]), self.etick[e]) for e in ENGS if self.etick[e] > 0 and e != eng]
        kn = self.known[eng]
        waits = [(s, v) for s, v in evs if kn.get(s, 0) < v]
        self.streams[eng].append((waits, None, None, 0))

    def emit(self):
        nc = self.nc
        with nc.Block() as block:
            def runner(name):
                def run(e):
                    for waits, fn, ev, inc in self.streams[name]:
                        for s, v in waits:
                            e.wait_ge(self.semobj[s], v)
                        if fn is not None:
                            inst = fn(e)
                            inst.then_inc(self.semobj[ev[0]], inc)
                return run
            block.sync(runner("sync"))
            block.scalar(runner("scalar"))
            block.vector(runner("vector"))
            block.gpsimd(runner("gpsimd"))
            block.tensor(runner("tensor"))


DEPTH = 2
ALPHA = (2 * DEPTH) ** 0.25
LN_EPS = 1e-5
SW_LIMIT = 7.0
SW_ALPHA = 1.702


def layer_norm_tile(P, ph, src_ap, src_keys, dst_ap, dst_key, g_bc, b_bc, st, idx):
    s1, s2, mean, msq, var, rstd, junk = (st[k] for k in ("s1", "s2", "mean", "msq", "var", "rstd", "junk"))
    k = lambda n: (ph, "ln_" + n)
    P.op("vector", lambda e: e.reduce_sum(s1[:, 0:1], src_ap, axis=AX.X), reads=list(src_keys), writes=[k("s1")])
    P.op("vector", lambda e: e.memset(s2[:, 0:1], 0.0), writes=[k("s2")])
    P.op("scalar", lambda e: e.activation(junk[:], src_ap, AF.Square, accum_out=s2[:, 0:1]),
         reads=list(src_keys) + [k("s2")], writes=[k("s2"), k("junk")])
    P.op("vector", lambda e: e.tensor_scalar(mean[:, 0:1], s1[:, 0:1], 1.0 / 1024, None, ALU.mult),
         reads=[k("s1")], writes=[k("mean")])
    P.op("vector", lambda e: e.tensor_tensor(msq[:, 0:1], mean[:, 0:1], mean[:, 0:1], ALU.mult),
         reads=[k("mean")], writes=[k("msq")])
    P.op("vector", lambda e: e.scalar_tensor_tensor(var[:, 0:1], s2[:, 0:1], 1.0 / 1024, msq[:, 0:1], ALU.mult, ALU.subtract),
         reads=[k("s2"), k("msq")], writes=[k("var")])
    P.op("vector", lambda e: e.tensor_scalar(var[:, 0:1], var[:, 0:1], LN_EPS, None, ALU.add),
         reads=[k("var")], writes=[k("var")])
    P.op("scalar", lambda e: e.sqrt(var[:, 0:1], var[:, 0:1]), reads=[k("var")], writes=[k("var")])
    P.op("vector", lambda e: e.reciprocal(rstd[:, 0:1], var[:, 0:1]), reads=[k("var")], writes=[k("rstd")])
    P.op("vector", lambda e: e.tensor_scalar(dst_ap, src_ap, mean[:, 0:1], rstd[:, 0:1], ALU.subtract, ALU.mult),
         reads=list(src_keys) + [k("mean"), k("rstd")], writes=[dst_key])
    P.op("vector", lambda e: e.tensor_tensor(dst_ap, dst_ap, g_bc[:], ALU.mult), reads=[dst_key, (ph, "g_bc")], writes=[dst_key])
    P.op("vector", lambda e: e.tensor_tensor(dst_ap, dst_ap, b_bc[:], ALU.add), reads=[dst_key, (ph, "b_bc")], writes=[dst_key])


def moe_phase(P, ph, C, lay, h_in, h_out, NT, E, stage=99):
    nc = P.nc
    D = C["dram"]
    ident = C["ident"]
    NBLK = NT // 1024
    sb = lambda n, s, d=F32: P.sb(ph + n, s, d)
    K = lambda *a: (ph,) + a

    rw = sb("rw", [128, 8, E]); rb_bc = sb("rb", [128, E])
    bguT = sb("bguT", [128, 16, E]); bd = sb("bd", [E, 1024])
    g_bc = sb("g_bc", [128, 1024]); b_bc = sb("b_bc", [128, 1024])
    xt = [sb("xt%d" % i, [128, 1024]) for i in range(2)]
    hT = sb("hT", [128, 8, 1024], BF16)
    hTf = sb("hTf", [128, 8, 128])
    acc = sb("acc", [128, 8, 1024])
    wgu = [sb("wgu%d" % i, [128, 8, 2048], BF16) for i in range(2)]
    wd = [sb("wd%d" % i, [128, 8, 1024], BF16) for i in range(2)]
    actT = [sb("actT%d" % i, [128, 8, 512], BF16) for i in range(2)]
    gc = [sb("gc%d" % i, [128, 512]) for i in range(2)]
    ua = [sb("ua%d" % i, [128, 512]) for i in range(2)]
    sl = [sb("sl%d" % i, [128, 512]) for i in range(2)]
    cw = sb("cw", [128, 8, E]); cwT = sb("cwT", [E, 128]); cws = sb("cws", [128, 8, E])
    lg = sb("lg", [128, E]); ex = sb("ex", [128, E]); em = sb("em", [128, E])
    m8 = sb("m8", [128, 8]); sm = sb("sm", [128, 8])
    st = {n: sb("st_" + n, [128, 1]) for n in ("s1", "s2", "mean", "msq", "var", "rstd")}
    st["junk"] = sb("junk", [128, 1024], BF16)

    pA = [P.ps(ph + "pA%d" % i, [128, 512]) for i in range(2)]
    pB = [P.ps(ph + "pB%d" % i, [128, 512]) for i in range(2)]
    pY = [P.ps(ph + "pY%d" % i, [128, 512]) for i in range(2)]
    pT = [P.ps(ph + "pT%d" % i, [128, 4, 128]) for i in range(2)]

    import os
    SK = os.environ.get("SKIP", "")
    if "rw" not in SK:
        P.dma("sync", rw[:], D["router_w"][lay].rearrange("(k p) e -> p k e", p=128), writes=[K("rw")])
    if "rb" not in SK:
        P.dma("sync", rb_bc[:], D["router_b"][lay].partition_broadcast(128), writes=[K("rb")])
    bkeys = [K("acc", 0, 0), K("acc", 0, 1), K("acc", 1, 0), K("acc", 1, 1)]
    P.dma("sync", acc[0:E, 0:2, :], D["exp_b_gu"][lay].rearrange("e (a f) -> e a f", a=2), writes=bkeys, slot="bguraw")
    if "bd" not in SK:
        P.dma("sync", bd[:], D["exp_b_down"][lay], writes=[K("bd")])
    if "gb" not in SK:
        P.dma("sync", g_bc[:], D["ln_ffn_g"][lay].partition_broadcast(128), writes=[K("g_bc")])
    if "gb" not in SK:
        P.dma("sync", b_bc[:], D["ln_ffn_b"][lay].partition_broadcast(128), writes=[K("b_bc")])
    for j in range(16):
        h = j % 2
        P.op("tensor", lambda e, j=j, h=h: e.transpose(pT[h][:, 0, 0:E], acc[0:E, j // 8, (j % 8) * 128:(j % 8 + 1) * 128], ident[0:E, 0:E]),
             reads=bkeys + ["ident"], writes=[K("pT", h)])
        P.op("vector", lambda e, j=j, h=h: e.tensor_copy(bguT[:, j, :], pT[h][:, 0, 0:E]),
             reads=[K("pT", h)], writes=[K("bguT")])

    if stage == 0:
        return
    def load_w(b, e, which="both"):
        ws = (b * E + e) % 2
        if "now" in SK:
            return
        if which in ("both", "gu"):
            for kk in range(8):
                P.dma("gpsimd", wgu[ws][:, kk, :], D["exp_w_gu"][lay, e, kk * 128:(kk + 1) * 128, :], writes=[K("wgu", ws, kk)], slot=("wgu", ws))
        if which in ("both", "d"):
            for kk in range(8):
                P.dma("gpsimd", wd[ws][:, kk, :], D["exp_w_down"][lay, e, kk * 128:(kk + 1) * 128, :], writes=[K("wd", ws, kk)], slot=("wd", ws))

    for b in range(NBLK):
        load_w(b, 0)
        for i in range(8 if "noa" not in SK else 0):
            tok0 = b * 1024 + i * 128
            s = i % 2
            P.dma("sync", xt[s][:], h_in[tok0:tok0 + 128, :], writes=[K("xt", s)])
            for h in range(2):
                def tr(e, s=s, h=h):
                    for q in range(4):
                        kk = h * 4 + q
                        r = e.transpose(pT[h][:, q, :], xt[s][:, kk * 128:(kk + 1) * 128], ident[:])
                    return r
                P.op("tensor", tr, reads=[K("xt", s), "ident"], writes=[K("pT", h)])
                P.op(os.environ.get("EVE", "scalar"), lambda e, h=h, i=i: (e.activation(hT[:, 4 * h:4 * h + 4, i * 128:(i + 1) * 128], pT[h][:], AF.Identity) if os.environ.get("EVE", "scalar") == "scalar" else e.tensor_copy(hT[:, 4 * h:4 * h + 4, i * 128:(i + 1) * 128], pT[h][:])),
                     reads=[K("pT", h)], writes=[K("hT", i // 4)])
                P.op("vector", lambda e, h=h: e.tensor_copy(hTf[:, 4 * h:4 * h + 4, :], pT[h][:]),
                     reads=[K("pT", h), K("hT", i // 4)] , writes=[K("hTf", h)])
            CUT = int(os.environ.get("CUT", "99"))
            if CUT < 2:
                continue
            pL = pY[0]
            def rt(e):
                for kk in range(8):
                    r = e.matmul(pL[:, 0:E], hTf[:, kk, :], rw[:, kk, :], start=(kk == 0), stop=(kk == 7))
                return r
            P.op("tensor", rt, reads=[K("hTf", 0), K("hTf", 1), K("rw")], writes=[K("pY", 0)])
            P.op("vector", lambda e: e.tensor_tensor(lg[:], pL[:, 0:E], rb_bc[:], ALU.add), reads=[K("pY", 0), K("rb")], writes=[K("lg")])
            if CUT < 3:
                continue
            P.op("vector", lambda e: e.max(m8[:], lg[:]), reads=[K("lg")], writes=[K("m8")])
            P.op("vector", lambda e: e.tensor_scalar(sm[:, 0:1], m8[:, 0:1], -1.0, None, ALU.mult), reads=[K("m8")], writes=[K("negmx")])
            P.op("scalar", lambda e: e.activation(ex[:], lg[:], AF.Exp, bias=sm[:, 0:1], scale=1.0), reads=[K("lg"), K("negmx")], writes=[K("ex")])
            P.op("vector", lambda e: e.scalar_tensor_tensor(em[:], lg[:], m8[:, 3:4], ex[:], ALU.is_ge, ALU.mult),
                 reads=[K("lg"), K("m8"), K("ex")], writes=[K("em")])
            P.op("vector", lambda e: e.reduce_sum(sm[:, 1:2], em[:], axis=AX.X), reads=[K("em")], writes=[K("Z")])
            P.op("vector", lambda e: e.reciprocal(sm[:, 2:3], sm[:, 1:2]), reads=[K("Z")], writes=[K("rz")])
            P.op("vector", lambda e, i=i: e.tensor_scalar(cw[:, i, :], em[:], sm[:, 2:3], None, ALU.mult), reads=[K("em"), K("rz")], writes=[K("cw", i)])
            P.op("vector", lambda e, i=i: e.tensor_scalar(cws[:, i, :], cw[:, i, :], 1.0 / SW_ALPHA, None, ALU.mult), reads=[K("cw", i)], writes=[K("cws", i)])
            if CUT < 4:
                continue
            pC = pY[1]
            P.op("tensor", lambda e, i=i: e.transpose(pC[0:E, 0:128], cw[:, i, :], ident[:]), reads=[K("cw", i), "ident"], writes=[K("pY", 1)])
            P.op("scalar", lambda e: e.copy(cwT[:], pC[0:E, 0:128]), reads=[K("pY", 1)], writes=[K("cwT")])
            if CUT < 5:
                continue
            for n in range(2):
                P.op("tensor", lambda e, n=n: e.matmul(pA[n][:], cwT[:], bd[:, n * 512:(n + 1) * 512], start=True, stop=True),
                     reads=[K("cwT"), K("bd")], writes=[K("pA", n)])
                P.op("vector", lambda e, n=n, s=s, i=i: e.scalar_tensor_tensor(acc[:, i, n * 512:(n + 1) * 512], xt[s][:, n * 512:(n + 1) * 512],
                                                                      ALPHA, pA[n][:], ALU.mult, ALU.add),
                     reads=[K("xt", s), K("pA", n)], writes=[K("acc", i, n)])
        if stage == 1:
            return
        cntb = [0]
        pend = [None]

        def flush_act():
            if pend[0] is not None:
                p, i, a_s = pend[0]
                P.op("vector", lambda e, p=p, i=i, a_s=a_s: e.scalar_tensor_tensor(actT[a_s][:, i, :], ua[p][:], 1.0 - SW_LIMIT, sl[p][:], ALU.add, ALU.mult),
                     reads=[K("sl", p), K("ua", p)], writes=[K("actT", a_s)])
                pend[0] = None

        def gu_unit(ei, c, i):
            ws = (b * E + ei) % 2
            a_s = c % 2
            p = cntb[0] % 2
            cntb[0] += 1
            def mm_g(e, off, ps, i=i, c=c, ws=ws):
                for kk in range(8):
                    r = e.matmul(ps[:], wgu[ws][:, kk, off + i * 128: off + (i + 1) * 128], hT[:, kk, c * 512:(c + 1) * 512],
                                 start=(kk == 0), stop=(kk == 7))
                return r
            hk = [K("hT", c)]
            P.op("tensor", lambda e, p=p, f=mm_g: f(e, 0, pA[p]), reads=[K("wgu", ws, kk) for kk in range(8)] + hk, writes=[K("pA", p)])
            P.op("tensor", lambda e, p=p, f=mm_g: f(e, 1024, pB[p]), reads=[K("wgu", ws, kk) for kk in range(8)] + hk, writes=[K("pB", p)])
            P.op("vector", lambda e, p=p, i=i, ei=ei: e.tensor_scalar(gc[p][:], pA[p][:], bguT[:, i, ei:ei + 1], SW_LIMIT, ALU.add, ALU.min),
                 reads=[K("pA", p), K("bguT")], writes=[K("gc", p)])
            P.op("vector", lambda e, p=p, i=i, ei=ei: e.tensor_scalar(ua[p][:], pB[p][:], bguT[:, 8 + i, ei:ei + 1], SW_LIMIT, ALU.add, ALU.min),
                 reads=[K("pB", p), K("bguT")], writes=[K("ua", p)])
            P.op("scalar", lambda e, p=p: e.activation(sl[p][:], gc[p][:], AF.Silu, scale=SW_ALPHA),
                 reads=[K("gc", p)], writes=[K("sl", p)])
            P.op("scalar", lambda e, p=p: e.activation(ua[p][:], ua[p][:], AF.Relu, bias=SW_LIMIT),
                 reads=[K("ua", p)], writes=[K("ua", p)])
            flush_act()
            pend[0] = (p, i, a_s)

        def down_unit(ei, c, idx):
            ws = (b * E + ei) % 2
            a_s = c % 2
            j, n = idx // 2, idx % 2
            ti = c * 4 + j
            q = idx % 2
            def mm_d(e, j=j, n=n, q=q, a_s=a_s, ws=ws):
                for f in range(8):
                    r = e.matmul(pY[q][:], actT[a_s][:, f, j * 128:(j + 1) * 128], wd[ws][:, f, n * 512:(n + 1) * 512],
                                 start=(f == 0), stop=(f == 7))
                return r
            P.op("tensor", mm_d, reads=[K("actT", a_s)] + [K("wd", ws, kk) for kk in range(8)], writes=[K("pY", q)])
            P.op("vector", lambda e, q=q, ti=ti, n=n, ei=ei: e.scalar_tensor_tensor(
                acc[:, ti, n * 512:(n + 1) * 512], pY[q][:], cws[:, ti, ei:ei + 1], acc[:, ti, n * 512:(n + 1) * 512], ALU.mult, ALU.add),
                 reads=[K("pY", q), K("cws", ti), K("acc", ti, n)], writes=[K("acc", ti, n)])

        seq = [(ei, c) for ei in range(E) for c in range(2)]
        for k in range(len(seq) + 1):
            if k < len(seq):
                ei, c = seq[k]
                if c == 0 and ei + 1 < E:
                    load_w(b, ei + 1, "gu")
                if c == 1 and ei + 1 < E:
                    load_w(b, ei + 1, "d")
            for idx in range(8):
                if k < len(seq):
                    gu_unit(seq[k][0], seq[k][1], idx)
                else:
                    flush_act()
                if k >= 1:
                    if idx == 0:
                        pass
                    down_unit(seq[k - 1][0], seq[k - 1][1], idx)
            flush_act()
        if stage == 2:
            return
        for i in range(8):
            tok0 = b * 1024 + i * 128
            s = i % 2
            layer_norm_tile(P, ph, acc[:, i, :], [K("acc", i, 0), K("acc", i, 1)], acc[:, i, :], K("acc", i, 0), g_bc, b_bc, st, i)
            P.dma("sync", h_out[tok0:tok0 + 128, :], acc[:, i, :], reads=[K("acc", i, 0)], writes=[K("hout", b, i)], slot=("yo_st", s))


import math

RMS_EPS = 1e-5
NEGM = -30000.0


def load_w_cols(P, ph, wt, wkey, w_dram, c0, ncols, dst0=0):
    for kk in range(8):
        P.dma("gpsimd", wt[:, kk, dst0:dst0 + ncols], w_dram[kk * 128:(kk + 1) * 128, c0:c0 + ncols],
              writes=[(ph, wkey, kk)], slot=(wkey,))


def wkeys(ph, wkey, dsts=(0,)):
    return [(ph, wkey, kk) for kk in range(8)]


def load_xT(P, ph, C, h_rows, L, xT, xt, ps):
    ident = C["ident"]
    for i in range(L // 128):
        s = i % 2
        P.dma("sync", xt[s][:], h_rows[i * 128:(i + 1) * 128, :], writes=[(ph, "xt", s)])
        for h in range(2):
            pt = ps[h]
            def tr(e, s=s, h=h, pt=pt):
                for q in range(4):
                    kk = h * 4 + q
                    r = e.transpose(pt[:, q * 128:(q + 1) * 128], xt[s][:, kk * 128:(kk + 1) * 128], ident[:])
                return r
            P.op("tensor", tr, reads=[(ph, "xt", s), "ident"], writes=[(ph, "ps", h)])
            P.op("vector", lambda e, h=h, i=i, pt=pt: e.tensor_copy(xT[:, 4 * h:4 * h + 4, i * 128:(i + 1) * 128],
                                                                  pt[:].rearrange("p (a b) -> p a b", a=4)),
                 reads=[(ph, "ps", h)], writes=[(ph, "ps", h), (ph, "xT", i // 4)])


def proj_fm(P, ph, wt, wk, c0, M, xT, L, ps, psi, evac):
    for ch in range(L // 512):
        pi = psi[ch % len(psi)]
        def mm(e, ch=ch, pi=pi):
            for kk in range(8):
                r = e.matmul(ps[pi][0:M, :], wt[:, kk, c0:c0 + M], xT[:, kk, ch * 512:(ch + 1) * 512], start=(kk == 0), stop=(kk == 7))
            return r
        P.op("tensor", mm, reads=wk + [(ph, "xT", ch)], writes=[(ph, "ps", pi)])
        evac(ch, ps[pi][0:M, :], (ph, "ps", pi))


def proj_tm(P, ph, wt, wk, c0, N, xT, L, ps, psi, evac):
    for i in range(L // 128):
        pi = psi[i % len(psi)]
        def mm(e, i=i, pi=pi):
            for kk in range(8):
                r = e.matmul(ps[pi][:, 0:N], xT[:, kk, i * 128:(i + 1) * 128], wt[:, kk, c0:c0 + N], start=(kk == 0), stop=(kk == 7))
            return r
        P.op("tensor", mm, reads=wk + [(ph, "xT", i // 4)], writes=[(ph, "ps", pi)])
        evac(i, ps[pi][:, 0:N], (ph, "ps", pi))


def outproj_ln(P, ph, C, mix, L, w_out, h_rows, out_rows, g_vec, b_vec, T, ps):
    ident_b = C["ident_b"]
    wo, mT, xt, g_bc, b_bc, st, yo = T["wo"], T["mT"], T["xt"], T["g_bc"], T["b_bc"], T["st"], T["yo"]
    for half in range(2):
        load_w_cols(P, ph, wo[half], "wb%d" % half, w_out, half * 512, 512)
    P.dma("sync", g_bc, g_vec.partition_broadcast(128), writes=[(ph, "g_bc")] + T["gk"], slot=("g_bc",))
    P.dma("sync", b_bc, b_vec.partition_broadcast(128), writes=[(ph, "b_bc")] + T["bk"], slot=("b_bc",))
    pbf = [ps[6][:].bitcast(BF16), ps[7][:].bitcast(BF16)]
    P.barrier()
    for i in range(L // 128):
        s = i % 2
        P.dma("sync", xt[s][:], h_rows[i * 128:(i + 1) * 128, :], writes=[(ph, "xt", s)])
        def tr(e, i=i, s=s):
            for kk in range(8):
                r = e.transpose(pbf[s][:, kk * 128:(kk + 1) * 128], mix[:, i, kk * 128:(kk + 1) * 128], ident_b[:])
            return r
        P.op("tensor", tr, reads=[(ph, "mix", i), "ident_b"], writes=[(ph, "ps", 6 + s)])
        P.op("vector", lambda e, s=s: e.tensor_copy(mT[s], pbf[s]), reads=[(ph, "ps", 6 + s)], writes=[(ph, "ps", 6 + s), (ph, "mT", s)] + T["mTk"][s])
        for n in range(2):
            pi = 4 + n
            def mm(e, s=s, n=n, pi=pi):
                for kk in range(8):
                    r = e.matmul(ps[pi][:], mT[s][:, kk * 128:(kk + 1) * 128], wo[n][:, kk, :], start=(kk == 0), stop=(kk == 7))
                return r
            P.op("tensor", mm, reads=[(ph, "mT", s)] + T["wok"][n], writes=[(ph, "ps", pi)])
            P.op("vector", lambda e, s=s, n=n, pi=pi: e.scalar_tensor_tensor(yo[s][:, n * 512:(n + 1) * 512], xt[s][:, n * 512:(n + 1) * 512], ALPHA, ps[pi][:], ALU.mult, ALU.add),
                 reads=[(ph, "xt", s), (ph, "ps", pi)], writes=[(ph, "ps", pi), (ph, "yo", s, n)])
        layer_norm_tile(P, ph, yo[s], [(ph, "yo", s, 0), (ph, "yo", s, 1)], yo[s], (ph, "yo", s, 0), g_bc, b_bc, st, i)
        P.dma("sync", out_rows[i * 128:(i + 1) * 128, :], yo[s], reads=[(ph, "yo", s, 0)], writes=[(ph, "hout", i)], slot=("yo_st", s))
        P.res_w[(ph, "yo", s, 1)] = P.res_w[(ph, "yo", s, 0)]
        P.res_r[(ph, "yo", s, 1)] = dict(P.res_r[(ph, "yo", s, 0)])


def even_mixer_seq(P, ph, C, lay_j, h_rows, out_rows, L, lam_init, ln_g, ln_b):
    D = C["dram"]
    ident, ident_b = C["ident"], C["ident_b"]
    NT = L // 128
    NC = L // 512
    sb = lambda n, s, d=F32: P.sb(ph + n, s, d)
    K = lambda *a: (ph,) + a
    w_in = D["ev_w_in"][lay_j]
    ps = [P.ps(ph + "ps%d" % i, [128, 512]) for i in range(8)]

    xt = [sb("xt%d" % i, [128, 1024]) for i in range(2)]
    xT = sb("xT", [128, 8, L], BF16)
    wb = [sb("wb%d" % i, [128, 8, 512], BF16) for i in range(2)]
    va = sb("va", [128, NT, 512], BF16)
    vb = sb("vb", [128, NT, 4, 132], BF16)
    mix = sb("mix", [128, NT, 1024], BF16)
    qa = [sb("qa%d" % i, [128, L], BF16) for i in range(4)]
    ka = [sb("ka%d" % i, [128, L], BF16) for i in range(4)]
    qd = [sb("qd%d" % i, [68, L], BF16) for i in range(2)]
    kd = [sb("kd%d" % i, [68, L], BF16) for i in range(2)]
    mstrict = sb("mstrict", [128, 4, 512], BF16)
    mcausal = sb("mcausal", [128, 4, 512], BF16)
    uincl = sb("uincl", [128, 128], BF16)
    ones1 = sb("ones1", [1, 128], BF16)
    efg = sb("efg", [128, 4, 512])
    atsp = sb("atsp", [128, 4, 512], BF16)
    suf = sb("suf", [1, 512], BF16)
    lamv = sb("lamv", [128, 4, 64]); lam = sb("lam", [128, 4])
    wsub = sb("wsub", [128, 128])
    o14 = sb("o14", [128, 4, 128]); ob4 = sb("ob4", [128, 4, 128]); sm4 = sb("sm4", [128, 4, 8])
    junk = sb("junk", [128, 1024], BF16)

    P.dma("gpsimd", mstrict[:], D["c_mstrict"], writes=[K("mstrict")])
    P.dma("gpsimd", mcausal[:], D["c_mcausal"], writes=[K("mcausal")])
    P.dma("gpsimd", uincl[:], D["c_uincl"], writes=[K("uincl")])
    P.op("vector", lambda e: e.memset(ones1[:], 1.0), writes=[K("ones1")])
    for i, nm in enumerate(["ev_lambda_q1", "ev_lambda_k1", "ev_lambda_q2", "ev_lambda_k2"]):
        P.dma("sync", lamv[:, i, :], D[nm][lay_j].partition_broadcast(128), writes=[K("lamv", i)])
    P.dma("sync", wsub[:], D["ev_subln_w"][lay_j].partition_broadcast(128), writes=[K("wsub")])
    P.op("vector", lambda e: e.tensor_scalar(wsub[:], wsub[:], 1.0 - lam_init, None, ALU.mult), reads=[K("wsub")], writes=[K("wsub")])
    for j in range(2):
        P.op("vector", lambda e, j=j: e.tensor_tensor(lamv[:, 2 * j, :], lamv[:, 2 * j, :], lamv[:, 2 * j + 1, :], ALU.mult),
             reads=[K("lamv", 2 * j), K("lamv", 2 * j + 1)], writes=[K("lamv", 2 * j)])
        P.op("vector", lambda e, j=j: e.reduce_sum(lam[:, j:j + 1], lamv[:, 2 * j, :], axis=AX.X), reads=[K("lamv", 2 * j)], writes=[K("lam", j)])
        P.op("scalar", lambda e, j=j: e.activation(lam[:, j:j + 1], lam[:, j:j + 1], AF.Exp), reads=[K("lam", j)], writes=[K("lam", j)])
    P.op("vector", lambda e: e.tensor_tensor(lam[:, 2:3], lam[:, 1:2], lam[:, 0:1], ALU.subtract), reads=[K("lam", 0), K("lam", 1)], writes=[K("lam", 2)])
    P.op("vector", lambda e: e.tensor_scalar(lam[:, 3:4], lam[:, 2:3], -lam_init, None, ALU.add), reads=[K("lam", 2)], writes=[K("nlam")])

    load_xT(P, ph, C, h_rows, L, xT, xt, ps)

    wslot = [0]
    def next_w(c0, ncols=512):
        s = wslot[0] % 2
        wslot[0] += 1
        load_w_cols(P, ph, wb[s], "wb%d" % s, w_in, c0, ncols)
        return wb[s], wkeys(ph, "wb%d" % s)

    wt, wk = next_w(1024)
    proj_tm(P, ph, wt, wk, 0, 512, xT, L, ps, [2, 3],
            lambda i, pa, pk: P.op("vector", lambda e: e.tensor_copy(va[:, i, :], pa), reads=[pk], writes=[pk, K("va", i)]))
    wt, wk = next_w(2560)
    P.op("vector", lambda e: e.memset(vb[:], 1.0), writes=[K("vb", i) for i in range(NT)])
    proj_tm(P, ph, wt, wk, 0, 512, xT, L, ps, [2, 3],
            lambda i, pa, pk: P.op("vector", lambda e: e.tensor_copy(vb[:, i, :, 0:128], pa.rearrange("p (h d) -> p h d", h=4)), reads=[pk], writes=[pk, K("vb", i)]))

    wt, wk = next_w(0)
    for j in range(4):
        proj_fm(P, ph, wt, wk, j * 128, 128, xT, L, ps, [2, 3],
                lambda ch, pa, pk, j=j: P.op("scalar", lambda e: e.activation(qa[j][:, ch * 512:(ch + 1) * 512], pa, AF.Identity, scale=0.125),
                                            reads=[pk], writes=[pk, K("qa", j)]))
    wt, wk = next_w(512)
    for j in range(4):
        proj_fm(P, ph, wt, wk, j * 128, 128, xT, L, ps, [2, 3],
                lambda ch, pa, pk, j=j: P.op("vector", lambda e: e.tensor_copy(ka[j][:, ch * 512:(ch + 1) * 512], pa),
                                            reads=[pk], writes=[pk, K("ka", j)]))

    it = 0
    suf2 = [suf, sb("suf1", [1, 512], BF16)]
    eg2 = sb("eg2", [128, 2, 512]); at2 = sb("at2", [128, 4, 512], BF16)
    ef4 = [efg[:, i, :] for i in range(4)]
    sp4 = [atsp[:, i, :] for i in range(4)]
    eg = [eg2[:, i, :] for i in range(2)]
    at = [at2[:, i, :] for i in range(4)]

    def sb_head(h):
        s_ = h % 2
        suf = suf2[s_]
        sufk = K("suf", s_)
        qT = qa[h // 2][(h % 2) * 64:(h % 2) * 64 + 64, :]
        kT = ka[h // 2][(h % 2) * 64:(h % 2) * 64 + 64, :]
        qk = [K("qa", h // 2), K("ka", h // 2)]
        tl = [(c, kt) for c in range(NC) for kt in range(4 * c + 3, -1, -1)]
        pg, pgk = ps[4 + s_], K("ps", 4 + s_)
        po, pok = ps[6 + s_], K("ps", 6 + s_)

        def bufs(n):
            i = s_ * 2 + n % 2
            return ps[i], K("ps", i), ef4[i], K("ef", i), sp4[i], K("sp", i)

        def stage_a(n):
            c, kt = tl[n]
            j = kt - 4 * c
            q0 = max(j, 0) * 128
            pz, pzk, efb, efk, spb, spk = bufs(n)
            P.op("tensor", lambda e: e.matmul(pz[:, q0:512], kT[:, kt * 128:(kt + 1) * 128], qT[:, c * 512 + q0:(c + 1) * 512], start=True, stop=True),
                 reads=qk, writes=[pzk])
            P.op("scalar", lambda e: e.activation(efb[:, q0:512], pz[:, q0:512], AF.Exp), reads=[pzk], writes=[pzk, efk])
            P.op("scalar", lambda e: e.activation(spb[:, q0:512], efb[:, q0:512], AF.Ln, bias=1.0), reads=[efk], writes=[spk])
            if j >= 0:
                P.op("vector", lambda e: e.tensor_tensor(spb[:, q0:512], spb[:, q0:512], mstrict[:, j, q0:512], ALU.mult),
                     reads=[spk, K("mstrict")], writes=[spk])

        def stage_b1(n):
            c, kt = tl[n]
            j = kt - 4 * c
            q0 = max(j, 0) * 128
            pz, pzk, efb, efk, spb, spk = bufs(n)
            if kt == 4 * c + 3:
                P.op("vector", lambda e: e.memset(suf[:], 0.0), writes=[sufk])
            def mg(e):
                e.matmul(pg[:, q0:512], uincl[:], spb[:, q0:512], start=True, stop=False)
                return e.matmul(pg[:, q0:512], ones1[:], suf[:, q0:512], start=False, stop=True)
            P.op("tensor", mg, reads=[spk, K("uincl"), K("ones1"), sufk], writes=[pgk])
            P.op("scalar", lambda e: e.activation(eg[s_][:, q0:512], pg[:, q0:512], AF.Exp, scale=-1.0), reads=[pgk], writes=[pgk, K("eg", s_)])
            if kt > 0:
                P.op("vector", lambda e: e.tensor_copy(suf[:, q0:512], pg[0:1, q0:512]), reads=[pgk], writes=[pgk, sufk])
            P.op("vector", lambda e: e.tensor_tensor(at[s_ * 2 + n % 2][:, q0:512], efb[:, q0:512], eg[s_][:, q0:512], ALU.mult),
                 reads=[efk, K("eg", s_)], writes=[K("at", s_ * 2 + n % 2)])
            if j >= 0:
                P.op("vector", lambda e: e.tensor_tensor(at[s_ * 2 + n % 2][:, q0:512], at[s_ * 2 + n % 2][:, q0:512], mstrict[:, j, q0:512], ALU.mult),
                     reads=[K("at", s_ * 2 + n % 2), K("mstrict")], writes=[K("at", s_ * 2 + n % 2)])

        def stage_b2(n):
            c, kt = tl[n]
            j = kt - 4 * c
            def pv(e):
                r = None
                for qs in range(3, max(j, 0) - 1, -1):
                    r = e.matmul(po[:, qs * 64:(qs + 1) * 64], at[s_ * 2 + n % 2][:, qs * 128:(qs + 1) * 128], va[:, kt, h * 64:(h + 1) * 64],
                                 start=(kt == 4 * c + 3 and qs == 3), stop=(kt == 0 and qs == 0))
                return r
            P.op("tensor", pv, reads=[K("at", s_ * 2 + n % 2), K("va", kt)], writes=[pok])
            if kt == 0:
                P.op("vector", lambda e: e.tensor_copy(mix[:, 4 * c:4 * c + 4, h * 64:(h + 1) * 64], po[:, 0:256].rearrange("p (a d) -> p a d", a=4)),
                     reads=[pok], writes=[pok] + [K("mix", 4 * c + qs) for qs in range(4)])

        stage_a(0)
        for n in range(len(tl)):
            if n + 1 < len(tl):
                stage_a(n + 1)
            yield
            stage_b1(n)
            yield
            if n >= 1:
                stage_b2(n - 1)
            yield
        stage_b2(len(tl) - 1)

    for h0 in range(0, 8, 2):
        gens = [sb_head(h0), sb_head(h0 + 1)]
        alive = [True, True]
        while any(alive):
            for gi in range(2):
                if alive[gi]:
                    try:
                        next(gens[gi])
                    except StopIteration:
                        alive[gi] = False

    for h in range(4):
        for m in range(2):
            wt, wk = next_w(1536 + (h * 2 + m) * 64, 64)
            proj_fm(P, ph, wt, wk, 0, 64, xT, L, ps, [2, 3],
                    lambda ch, pa, pk, m=m: P.op("scalar", lambda e: e.activation(qd[m][0:64, ch * 512:(ch + 1) * 512], pa, AF.Identity, scale=0.125),
                                                reads=[pk], writes=[pk, K("qd", m)]))
            wt, wk = next_w(2048 + (h * 2 + m) * 64, 64)
            proj_fm(P, ph, wt, wk, 0, 64, xT, L, ps, [2, 3],
                    lambda ch, pa, pk, m=m: P.op("vector", lambda e: e.tensor_copy(kd[m][0:64, ch * 512:(ch + 1) * 512], pa),
                                                reads=[pk], writes=[pk, K("kd", m)]))
            P.dma("gpsimd", qd[m][64:68, :], D["c_qaug4"][h][:, 0:L], writes=[K("qd", m)], slot=("qaug", m))
            P.dma("gpsimd", kd[m][64:68, :], D["c_kaug"][:, 0:L], writes=[K("kd", m)], slot=("kaug", m))
        for c in range(NC):
            nk = 4 * c + 4
            tiles = [(m, kt) for m in range(2) for kt in range(nk - 1, -1, -1)]
            bsl = []
            for _ in tiles:
                bsl.append(it % 2)
                it += 1

            def emit_s(n, c=c):
                m, kt = tiles[n]
                b = bsl[n]
                q0 = max(kt - 4 * c, 0) * 128
                pz, pzk = ps[b], K("ps", b)
                P.op("tensor", lambda e, pz=pz, kt=kt, c=c, q0=q0, m=m: e.matmul(
                    pz[:, q0:512], kd[m][:, kt * 128:(kt + 1) * 128], qd[m][:, c * 512 + q0:(c + 1) * 512], start=True, stop=True),
                     reads=[K("qd", m), K("kd", m)], writes=[pzk])

            def emit_rest(n, c=c, h=h):
                m, kt = tiles[n]
                b = bsl[n]
                j = kt - 4 * c
                q0 = max(j, 0) * 128
                pz, pzk = ps[b], K("ps", b)
                pos = [ps[4 + 2 * m], ps[5 + 2 * m]]
                P.op("scalar", lambda e, b=b, pz=pz, q0=q0: e.activation(at[b][:, q0:512], pz[:, q0:512], AF.Exp), reads=[pzk], writes=[pzk, K("at", b)])
                if j >= 0:
                    P.op("vector", lambda e, b=b, j=j, q0=q0: e.tensor_tensor(at[b][:, q0:512], at[b][:, q0:512], mcausal[:, j, q0:512], ALU.mult),
                         reads=[K("at", b), K("mcausal")], writes=[K("at", b)])

            def emit_pv(n, c=c, h=h):
                m, kt = tiles[n]
                b = bsl[n]
                j = kt - 4 * c
                pos = [ps[4 + 2 * m], ps[5 + 2 * m]]
                def pv(e, b=b, kt=kt, c=c, j=j, h=h, pos=pos):
                    r = None
                    for qs in range(3, max(j, 0) - 1, -1):
                        qt = 4 * c + qs
                        r = e.matmul(pos[qs // 2][:, (qs % 2) * 256:(qs % 2) * 256 + 129], at[b][:, qs * 128:(qs + 1) * 128], vb[:, kt, h, 0:129],
                                     start=(kt == qt and qs % 2 == 1), stop=(kt == 0 and qs % 2 == 0))
                    return r
                P.op("tensor", pv, reads=[K("at", b), K("vb", kt)], writes=[K("ps", 4 + 2 * m), K("ps", 5 + 2 * m)])

            emit_s(0)
            for n in range(len(tiles)):
                if n + 1 < len(tiles):
                    emit_s(n + 1)
                emit_rest(n)
                if n >= 1:
                    emit_pv(n - 1)
            emit_pv(len(tiles) - 1)
            def P1(qs): return ps[4 + qs // 2][:, (qs % 2) * 256:(qs % 2) * 256 + 129]
            def P2(qs): return ps[6 + qs // 2][:, (qs % 2) * 256:(qs % 2) * 256 + 129]
            def K1(qs): return K("ps", 4 + qs // 2)
            def K2(qs): return K("ps", 6 + qs // 2)
            for qs in range(4):
                P.op("vector", lambda e, qs=qs: e.reciprocal(sm4[:, qs, 0:1], P1(qs)[:, 128:129]), reads=[K1(qs)], writes=[K1(qs), K("sm0", qs)])
            for qs in range(4):
                P.op("vector", lambda e, qs=qs: e.reciprocal(sm4[:, qs, 1:2], P2(qs)[:, 128:129]), reads=[K2(qs)], writes=[K2(qs), K("sm1", qs)])
            for qs in range(4):
                P.op("vector", lambda e, qs=qs: e.tensor_tensor(sm4[:, qs, 1:2], sm4[:, qs, 1:2], lam[:, 3:4], ALU.mult), reads=[K("sm1", qs), K("nlam")], writes=[K("sm1", qs)])
            for qs in range(4):
                P.op("vector", lambda e, qs=qs: e.tensor_scalar(o14[:, qs, :], P1(qs)[:, 0:128], sm4[:, qs, 0:1], None, ALU.mult), reads=[K1(qs), K("sm0", qs)], writes=[K1(qs), K("o1", qs)])
            for qs in range(4):
                P.op("vector", lambda e, qs=qs: e.scalar_tensor_tensor(ob4[:, qs, :], P2(qs)[:, 0:128], sm4[:, qs, 1:2], o14[:, qs, :], ALU.mult, ALU.add),
                     reads=[K2(qs), K("sm1", qs), K("o1", qs)], writes=[K2(qs), K("ob", qs)])
            for qs in range(4):
                P.op("vector", lambda e, qs=qs: e.memset(sm4[:, qs, 2:3], 0.0), writes=[K("sm2", qs)])
            for qs in range(4):
                P.op("scalar", lambda e, qs=qs: e.activation(junk[:, qs * 128:(qs + 1) * 128], ob4[:, qs, :], AF.Square, accum_out=sm4[:, qs, 2:3]),
                     reads=[K("ob", qs), K("sm2", qs)], writes=[K("sm2", qs), K("junk", qs)])
            for qs in range(4):
                P.op("vector", lambda e, qs=qs: e.tensor_scalar(sm4[:, qs, 2:3], sm4[:, qs, 2:3], 1.0 / 128, RMS_EPS, ALU.mult, ALU.add), reads=[K("sm2", qs)], writes=[K("sm2", qs)])
            for qs in range(4):
                P.op("scalar", lambda e, qs=qs: e.sqrt(sm4[:, qs, 2:3], sm4[:, qs, 2:3]), reads=[K("sm2", qs)], writes=[K("sm2", qs)])
            for qs in range(4):
                P.op("vector", lambda e, qs=qs: e.reciprocal(sm4[:, qs, 3:4], sm4[:, qs, 2:3]), reads=[K("sm2", qs)], writes=[K("sm3", qs)])
            for qs in range(4):
                qt = 4 * c + qs
                P.op("vector", lambda e, qs=qs, qt=qt, h=h: e.scalar_tensor_tensor(mix[:, qt, 512 + h * 128:512 + (h + 1) * 128], ob4[:, qs, :], sm4[:, qs, 3:4], wsub[:], ALU.mult, ALU.mult),
                     reads=[K("ob", qs), K("sm3", qs), K("wsub")], writes=[K("mix", qt)])

    if "dbg" in D:
        P.dma("gpsimd", D["dbg"].rearrange("(t p) c -> p t c", p=128), mix[:], reads=[K("mix", i) for i in range(NT)], writes=[K("dbg")])
    xTf = xT[:].rearrange("p a b -> p (a b)").bitcast(F32)
    T = dict(wo=wb, mT=[atsp[:, 0:2, :].rearrange("p a b -> p (a b)"), atsp[:, 2:4, :].rearrange("p a b -> p (a b)")],
             mTk=[[K("sp", 0), K("sp", 1)], [K("sp", 2), K("sp", 3)]],
             xt=xt, g_bc=efg[:, 0:2, :].rearrange("p a b -> p (a b)"), b_bc=efg[:, 2:4, :].rearrange("p a b -> p (a b)"),
             gk=[K("ef", 0), K("ef", 1)], bk=[K("ef", 2), K("ef", 3)],
             st={n: sb("st_" + n, [128, 1]) for n in ("s1", "s2", "mean", "msq", "var", "rstd")},
             yo=[xTf[:, 0:1024], xTf[:, 1024:2048]], wok=[wkeys(ph, "wb0"), wkeys(ph, "wb1")])
    T["st"]["junk"] = junk
    outproj_ln(P, ph, C, mix, L, D["ev_w_out"][lay_j], h_rows, out_rows, ln_g, ln_b, T, ps)


def odd_mixer_seq(P, ph, C, lay_j, h_rows, out_rows, L, ln_g, ln_b):
    D = C["dram"]
    ident, ident_b = C["ident"], C["ident_b"]
    NT = L // 128
    NC = L // 512
    KSEL = min(256, L // 4)
    sb = lambda n, s, d=F32: P.sb(ph + n, s, d)
    K = lambda *a: (ph,) + a
    w_in = D["od_w_in"][lay_j]
    ps = [P.ps(ph + "ps%d" % i, [128, 512]) for i in range(8)]
    pbf = ps[6][:].bitcast(BF16)

    xt = [sb("xt%d" % i, [128, 1024]) for i in range(2)]
    xT = sb("xT", [128, 8, L], BF16)
    wb = [sb("wb%d" % i, [128, 8, 512], BF16) for i in range(2)]
    mix = sb("mix", [128, NT, 1024], BF16)
    qTc = sb("qTc", [128, 16, 512], BF16)
    ckvT = sb("ckvT", [128, L], BF16)
    ckva = sb("ckva", [128, NT, 132], BF16)
    qiT = [sb("qiT%d" % i, [128, L], BF16) for i in range(4)]
    kiT2 = sb("kiT2", [128, L], BF16)
    widx = sb("widx", [128, NT, 8])
    score = sb("score", [128, L]); wkt = sb("wkt", [128, L])
    MB = [sb("MB%d" % i, [128, L], BF16) for i in range(4)]
    MB2 = [xt[0][:].bitcast(BF16), xt[1][:].bitcast(BF16)] + [sb("MBx%d" % i, [128, L], BF16) for i in range(2)]
    MBS = [MB, MB2]
    iti = [0]
    rl = [sb("rl%d" % i, [128, 512]) for i in range(2)]
    atsp = sb("atsp", [128, 4, 512], BF16)
    at = [atsp[:, i, :] for i in range(2)]
    kaug = sb("kaug", [9, L], BF16)
    qaugc = sb("qaugc", [9, 16, 512], BF16)
    wuv = sb("wuv", [128, 16, 64], BF16)
    kvw = sb("kvw", [128, 128])
    oh4 = sb("oh4", [128, 4, 128], BF16); ohT4 = sb("ohT4", [128, 512], BF16); sm4 = sb("sm4", [128, 4])
    m8 = sb("m8", [128, 8]); sm = sb("sm", [128, 8])
    junk = oh4[:].rearrange("p a b -> p (a b)")

    P.dma("gpsimd", kaug[:], D["c_kaug9"][:, 0:L], writes=[K("kaug")])
    P.dma("gpsimd", wuv[:], D["od_w_uv"][lay_j].rearrange("h c d -> c h d"), writes=[K("wuv")])
    P.dma("sync", kvw[:], D["od_kv_norm_w"][lay_j].partition_broadcast(128), writes=[K("kvw")])
    P.op("vector", lambda e: e.memset(ckva[:], 1.0), writes=[K("ckva", i) for i in range(NT)])

    load_xT(P, ph, C, h_rows, L, xT, xt, ps)
    wslot = [0]
    def next_w(c0, ncols=512, dst0=0, new=True):
        if new:
            wslot[0] += 1
        s = wslot[0] % 2
        load_w_cols(P, ph, wb[s], "wb%d" % s, w_in, c0, ncols, dst0=dst0)
        return wb[s], wkeys(ph, "wb%d" % s, (dst0,))

    wt, wk = next_w(2048, 128)
    def ev_ckv(i, pa, pk):
        P.op("vector", lambda e: e.memset(sm[:, 0:1], 0.0), writes=[K("sm0")])
        P.op("scalar", lambda e: e.activation(junk[:, 0:128], pa, AF.Square, accum_out=sm[:, 0:1]), reads=[pk, K("sm0")], writes=[pk, K("sm0"), K("junk")])
        P.op("vector", lambda e: e.tensor_scalar(sm[:, 0:1], sm[:, 0:1], 1.0 / 128, RMS_EPS, ALU.mult, ALU.add), reads=[K("sm0")], writes=[K("sm0")])
        P.op("scalar", lambda e: e.sqrt(sm[:, 0:1], sm[:, 0:1]), reads=[K("sm0")], writes=[K("sm0")])
        P.op("vector", lambda e: e.reciprocal(sm[:, 1:2], sm[:, 0:1]), reads=[K("sm0")], writes=[K("sm1")])
        P.op("vector", lambda e: e.scalar_tensor_tensor(ckva[:, i, 0:128], pa, sm[:, 1:2], kvw[:], ALU.mult, ALU.mult),
             reads=[pk, K("sm1"), K("kvw")], writes=[pk, K("ckva", i)])
        P.op("tensor", lambda e: e.transpose(pbf[:, 0:128], ckva[:, i, 0:128], ident_b[:]), reads=[K("ckva", i), "ident_b"], writes=[K("ps", 6)])
        P.op("vector", lambda e: e.tensor_copy(ckvT[:, i * 128:(i + 1) * 128], pbf[:, 0:128]), reads=[K("ps", 6)], writes=[K("ps", 6), K("ckvT")])
    proj_tm(P, ph, wt, wk, 0, 128, xT, L, ps, [2, 3], ev_ckv)
    wt, wk = next_w(2176, 512)
    for j in range(4):
        proj_fm(P, ph, wt, wk, j * 128, 128, xT, L, ps, [2, 3],
                lambda ch, pa, pk, j=j: P.op("scalar", lambda e: e.activation(qiT[j][:, ch * 512:(ch + 1) * 512], pa, AF.Identity, scale=0.125),
                                            reads=[pk], writes=[pk, K("qiT")]))
    wt, wk0 = next_w(2688, 64, dst0=0)
    _, wk1 = next_w(2688, 64, dst0=64, new=False)
    _, wk2 = next_w(2752, 8, dst0=128, new=False)
    proj_fm(P, ph, wt, wk0 + wk1, 0, 128, xT, L, ps, [2, 3],
            lambda ch, pa, pk: P.op("vector", lambda e: e.tensor_copy(kiT2[:, ch * 512:(ch + 1) * 512], pa), reads=[pk], writes=[pk, K("kiT2")]))
    proj_tm(P, ph, wt, wk2, 128, 8, xT, L, ps, [2, 3],
            lambda i, pa, pk: P.op("vector", lambda e: e.tensor_scalar(widx[:, i, :], pa, 8 ** -0.5, None, ALU.mult), reads=[pk], writes=[pk, K("widx")]))

    if "dbg2" in D and C.get("dumpc", 0) == -1:
        P.dma("gpsimd", D["dbg2"][:, 0:L], kiT2[:, 0:L], reads=[K("kiT2")], writes=[K("dbg2")])
        P.dma("gpsimd", D["dbg3"][:, 0:L], qiT[0][:, 0:L], reads=[K("qiT")], writes=[K("dbg3")])
        P.dma("gpsimd", D["dbg"][0:128, 0:NT * 8], widx[:].rearrange("p a b -> p (a b)"), reads=[K("widx")], writes=[K("dbgw")])
    def dump_w(stage):
        if "dbg2" in D and C.get("dumpat", None) == stage:
            P.dma("gpsimd", D["dbg"][0:128, 0:NT * 8], widx[:].rearrange("p a b -> p (a b)"), reads=[K("widx")], writes=[K("dbgw")])
    it = 0
    for c in range(NC):
        for g in range(4):
            if c == 0:
                dump_w(10 + g)
            wt, wk = next_w(g * 512, 512)
            if c == 0:
                dump_w(20 + g)
            for hh in range(4):
                h = g * 4 + hh
                pi = 2 + h % 2
                def mm(e, hh=hh, pi=pi, wt=wt, c=c):
                    for kk in range(8):
                        r = e.matmul(ps[pi][:], wt[:, kk, hh * 128:(hh + 1) * 128], xT[:, kk, c * 512:(c + 1) * 512], start=(kk == 0), stop=(kk == 7))
                    return r
                P.op("tensor", mm, reads=wk + [K("xT", c)], writes=[K("ps", pi)])
                P.op("scalar", lambda e, h=h, pi=pi: e.activation(qTc[:, h, :], ps[pi][:], AF.Identity, scale=128 ** -0.5),
                     reads=[K("ps", pi)], writes=[K("ps", pi), K("qTc", h)])
        if c == 0:
            dump_w(1)
        P.dma("gpsimd", qaugc[:], D["c_qaug16"][:, :, c * 512:(c + 1) * 512].rearrange("h r l -> r h l"), writes=[K("qaugc")])
        if c == 0:
            P.op("vector", lambda e: e.engine_nop() if False else e.memset(sm[:, 7:8], 0.0), reads=[K("qaugc")], writes=[K("sm7")])
            if C.get("dumpat", None) == 2:
                P.dma("gpsimd", D["dbg"][0:128, 0:NT * 8], widx[:].rearrange("p a b -> p (a b)"), reads=[K("widx"), K("sm7")], writes=[K("dbgw")])
        def idx_chunk(c):
            mset = c % 2
            for qs in range(4):
                qt = 4 * c + qs
                n_s = (qt + 1) * 128
                for sc in range((n_s + 511) // 512):
                    w = min(512, n_s - sc * 512)
                    for ih in range(8):
                        b = iti[0] % 2
                        iti[0] += 1
                        pi = 2 + b
                        P.op("tensor", lambda e, pi=pi, ih=ih, qt=qt, sc=sc, w=w: e.matmul(
                            ps[pi][:, 0:w], qiT[ih // 2][(ih % 2) * 64:(ih % 2) * 64 + 64, qt * 128:(qt + 1) * 128],
                            kiT2[(ih % 2) * 64:(ih % 2) * 64 + 64, sc * 512:sc * 512 + w], start=True, stop=True),
                             reads=[K("qiT"), K("kiT2")], writes=[K("ps", pi)])
                        P.op("scalar", lambda e, pi=pi, b=b, w=w: e.activation(rl[b][:, 0:w], ps[pi][:, 0:w], AF.Relu), reads=[K("ps", pi)], writes=[K("ps", pi), K("rl", b)])
                        if ih == 0:
                            P.op("vector", lambda e, b=b, w=w, sc=sc, qt=qt, ih=ih: e.tensor_scalar(score[:, sc * 512:sc * 512 + w], rl[b][:, 0:w], widx[:, qt, ih:ih + 1], None, ALU.mult),
                                 reads=[K("rl", b), K("widx")], writes=[K("score")])
                        else:
                            P.op("vector", lambda e, b=b, w=w, sc=sc, qt=qt, ih=ih: e.scalar_tensor_tensor(score[:, sc * 512:sc * 512 + w], rl[b][:, 0:w], widx[:, qt, ih:ih + 1],
                                                                                                      score[:, sc * 512:sc * 512 + w], ALU.mult, ALU.add),
                                 reads=[K("rl", b), K("widx"), K("score")], writes=[K("score")])
                P.op("gpsimd", lambda e, qt=qt: e.affine_select(score[:, qt * 128:(qt + 1) * 128], score[:, qt * 128:(qt + 1) * 128], [[-1, 128]], ALU.is_ge, -1e30,
                                                             base=0, channel_multiplier=1), reads=[K("score")], writes=[K("score")])
                if qt * 128 >= KSEL:
                    R = KSEL // 8
                    for r in range(R):
                        src = score if r == 0 else wkt
                        P.op("vector", lambda e, src=src, n_s=n_s: e.max(m8[:], src[:, 0:n_s]), reads=[K("score"), K("wkt")], writes=[K("m8")])
                        if r < R - 1:
                            P.op("vector", lambda e, src=src, n_s=n_s: e.match_replace(wkt[:, 0:n_s], m8[:], src[:, 0:n_s], -1e30),
                                 reads=[K("score"), K("wkt"), K("m8")], writes=[K("wkt")])
                    P.op("vector", lambda e, qs=qs, n_s=n_s: e.tensor_scalar(MBS[mset][qs][:, 0:n_s], score[:, 0:n_s], m8[:, 7:8], NEGM, ALU.is_lt, ALU.mult),
                         reads=[K("score"), K("m8")], writes=[K("MB", mset, qs)])
                else:
                    P.op("vector", lambda e, qs=qs, n_s=n_s: e.tensor_scalar(MBS[mset][qs][:, 0:n_s], score[:, 0:n_s], -1e29, NEGM, ALU.is_lt, ALU.mult),
                         reads=[K("score")], writes=[K("MB", mset, qs)])
            if "dbg2" in D and C.get("dumpc", 0) == -2 and c == 1:
                P.dma("gpsimd", D["dbg2"][:, 0:L], kiT2[:, 0:L], reads=[K("kiT2")], writes=[K("dbg2")])
                P.dma("gpsimd", D["dbg3"][:, 0:L], qiT[0][:, 0:L], reads=[K("qiT")], writes=[K("dbg3")])
                P.dma("gpsimd", D["dbg"][0:128, 0:NT * 8], widx[:].rearrange("p a b -> p (a b)"), reads=[K("widx")], writes=[K("dbgw")])

        def record(fn, *a):
            rec = []
            real = P.op
            P.op = lambda *aa, **kk: rec.append((aa, kk))
            try:
                fn(*a)
            finally:
                P.op = real
            return rec

        if c == 0:
            idx_chunk(0)
        nxt = record(idx_chunk, c + 1) if c + 1 < NC else []
        if "dbg2" in D and c == C.get("dumpc", 0):
            P.dma("gpsimd", D["dbg2"][:, 0:L], MB[3][:, 0:L], reads=[K("MB", 3)], writes=[K("dbg2")])
            P.dma("sync", D["dbg3"][:, 0:L], score[:, 0:L], reads=[K("score")], writes=[K("dbg3")])
        if c == 0:
            dump_w(5)
        nk = 4 * c + 4
        tiles = [(h, kt) for h in range(16) for kt in range(nk - 1, -1, -1)]
        bsl = []
        for _ in tiles:
            bsl.append(it % 2)
            it += 1
        POS = [[ps[4], ps[5]], [ps[4], ps[5]]]
        POSK = [[K("ps", 4), K("ps", 5)], [K("ps", 4), K("ps", 5)]]

        def emit_s(n, c=c):
            h, kt = tiles[n]
            b = bsl[n]
            jm = max(kt - 4 * c, 0)
            q0 = jm * 128
            pz, pzk = ps[b], K("ps", b)
            def sc_mm(e, pz=pz, kt=kt, q0=q0, jm=jm, h=h):
                e.matmul(pz[:, q0:512], ckvT[:, kt * 128:(kt + 1) * 128], qTc[:, h, q0:512], start=True, stop=False)
                r = e.matmul(pz[:, q0:512], kaug[:, kt * 128:(kt + 1) * 128], qaugc[:, h, q0:512], start=False, stop=False)
                for qs in range(jm, 4):
                    r = e.matmul(pz[:, qs * 128:(qs + 1) * 128], MBS[c % 2][qs][:, kt * 128:(kt + 1) * 128], ident_b[:], start=False, stop=(qs == 3))
                return r
            P.op("tensor", sc_mm, reads=[K("ckvT"), K("qTc", h), K("kaug"), K("qaugc"), "ident_b"] + [K("MB", c % 2, q) for q in range(jm, 4)], writes=[pzk])

        def emit_rest(n, c=c):
            h, kt = tiles[n]
            b = bsl[n]
            jm = max(kt - 4 * c, 0)
            q0 = jm * 128
            pz, pzk = ps[b], K("ps", b)
            pos, posk = POS[h % 2], POSK[h % 2]
            P.op("scalar", lambda e, b=b, pz=pz, q0=q0: e.activation(at[b][:, q0:512], pz[:, q0:512], AF.Exp), reads=[pzk], writes=[pzk, K("at", b)])

        def emit_pv(n, c=c):
            h, kt = tiles[n]
            b = bsl[n]
            jm = max(kt - 4 * c, 0)
            pos, posk = POS[h % 2], POSK[h % 2]
            def pv(e, b=b, kt=kt, c=c, jm=jm, pos=pos):
                r = None
                for qs in range(3, jm - 1, -1):
                    qt = 4 * c + qs
                    r = e.matmul(pos[qs // 2][:, (qs % 2) * 256:(qs % 2) * 256 + 129], at[b][:, qs * 128:(qs + 1) * 128], ckva[:, kt, 0:129],
                                 start=(kt == qt and qs % 2 == 1), stop=(kt == 0 and qs % 2 == 0))
                return r
            P.op("tensor", pv, reads=[K("at", b), K("ckva", kt)], writes=list(posk))

        def ep1(h):
            pos, posk = POS[h % 2], POSK[h % 2]
            for qs in range(4):
                p1 = pos[qs // 2][:, (qs % 2) * 256:(qs % 2) * 256 + 129]
                P.op("vector", lambda e, p1=p1, qs=qs: e.reciprocal(sm4[:, qs:qs + 1], p1[:, 128:129]), reads=[posk[qs // 2]], writes=[posk[qs // 2], K("rz", qs)])
            for qs in range(4):
                p1 = pos[qs // 2][:, (qs % 2) * 256:(qs % 2) * 256 + 129]
                P.op("vector", lambda e, p1=p1, qs=qs: e.tensor_scalar(oh4[:, qs, :], p1[:, 0:128], sm4[:, qs:qs + 1], None, ALU.mult),
                     reads=[posk[qs // 2], K("rz", qs)], writes=[posk[qs // 2], K("oh4")])

        def ep2(h):
            def tr(e):
                for qs in range(4):
                    r = e.transpose(pbf[:, qs * 128:(qs + 1) * 128], oh4[:, qs, :], ident_b[:])
                return r
            P.op("tensor", tr, reads=[K("oh4"), "ident_b"], writes=[K("ps", 6)])
            P.op("vector", lambda e: e.tensor_copy(ohT4[:], pbf[:, 0:512]), reads=[K("ps", 6)], writes=[K("ps", 6), K("ohT4")])

        def ep3(h, c=c):
            def mm(e, h=h):
                for qs in range(4):
                    r = e.matmul(ps[7][:, qs * 64:(qs + 1) * 64], ohT4[:, qs * 128:(qs + 1) * 128], wuv[:, h, :], start=(qs == 0), stop=(qs == 3))
                return r
            P.op("tensor", mm, reads=[K("ohT4"), K("wuv")], writes=[K("ps", 7)])
            P.op("scalar", lambda e, h=h, c=c: e.activation(mix[:, 4 * c:4 * c + 4, h * 64:(h + 1) * 64], ps[7][:, 0:256].rearrange("p (a d) -> p a d", a=4), AF.Identity),
                 reads=[K("ps", 7)], writes=[K("ps", 7)] + [K("mix", 4 * c + qs) for qs in range(4)])

        deferred = []
        emit_s(0)
        for n in range(len(tiles) + 1):
            if n + 1 < len(tiles):
                emit_s(n + 1)
            if n < len(tiles):
                emit_rest(n)
            if n >= 1:
                emit_pv(n - 1)
                h, kt = tiles[n - 1]
                if kt == 0:
                    ep1(h)
                    deferred.append((n + 2, ep2, h))
                    deferred.append((n + 4, ep3, h))
            keep = []
            for due, fn, hh in deferred:
                if due <= n:
                    fn(hh)
                else:
                    keep.append((due, fn, hh))
            deferred = keep
            per = -(-len(nxt) // (len(tiles) + 1))
            for aa, kk in nxt[n * per:(n + 1) * per]:
                P.op(*aa, **kk)
        for due, fn, hh in sorted(deferred, key=lambda t: t[0]):
            fn(hh)
        for aa, kk in nxt[(len(tiles) + 1) * per:]:
            P.op(*aa, **kk)

    if "dbg" in D and C.get("dumpc", 0) >= 0 and C.get("dumpat", None) is None:
        P.dma("gpsimd", D["dbg"].rearrange("(t p) c -> p t c", p=128), mix[:], reads=[K("mix", i) for i in range(NT)], writes=[K("dbg")])
    xTf = xT[:].rearrange("p a b -> p (a b)").bitcast(F32)
    T = dict(wo=wb, mT=[atsp[:, 0:2, :].rearrange("p a b -> p (a b)"), atsp[:, 2:4, :].rearrange("p a b -> p (a b)")],
             mTk=[[K("at", 0), K("at", 1)], [K("sp", 0), K("sp", 1)]],
             xt=xt, g_bc=score[:, 0:1024], b_bc=wkt[:, 0:1024],
             gk=[K("score")], bk=[K("wkt")],
             st={n: sb("st_" + n, [128, 1]) for n in ("s1", "s2", "mean", "msq", "var", "rstd")},
             yo=[xTf[:, 0:1024], xTf[:, 1024:2048]], wok=[wkeys(ph, "wb0"), wkeys(ph, "wb1")])
    T["st"]["junk"] = MB[0][:, 0:1024]
    outproj_ln(P, ph, C, mix, L, D["od_w_out"][lay_j], h_rows, out_rows, ln_g, ln_b, T, ps)


def make_consts(L=2048):
    s = np.arange(128)[:, None]; q = np.arange(512)[None, :]
    mstrict = np.stack([((q - j * 128) > s) for j in range(4)], axis=1).astype(np.float32)
    mcausal = np.stack([((q - j * 128) >= s) for j in range(4)], axis=1).astype(np.float32)
    jj = np.arange(128)[:, None]; ss = np.arange(128)[None, :]
    uincl = (jj >= ss).astype(np.float32)
    t = np.arange(L)
    hi = (t // 256) * 256; lo = t % 256
    def aug(slopes):
        qa = np.stack([np.stack([np.full(L, c), np.full(L, c), -c * hi, -c * lo]) for c in slopes]).astype(np.float32)
        return qa
    sl4 = 2.0 ** (-8.0 * np.arange(1, 5) / 4)
    sl16 = 2.0 ** (-8.0 * np.arange(1, 17) / 16)
    kaug = np.stack([hi, lo, np.ones(L), np.ones(L)]).astype(np.float32)
    import ml_dtypes
    def bsplit(v):
        v = np.asarray(v, dtype=np.float64); outp = []
        for _ in range(3):
            p = v.astype(ml_dtypes.bfloat16).astype(np.float64); outp.append(p); v = v - p
        return outp
    q9 = []
    for c in sl16:
        c1, c2, c3 = bsplit(np.full(L, c)); v1, v2, v3 = bsplit(c * t.astype(np.float64))
        q9.append(np.stack([c1, c1, c2, c2, c3, c3, -v1, -v2, -v3]))
    qaug9 = np.stack(q9).astype(np.float32)
    kaug9 = np.stack([hi, lo, hi, lo, hi, lo, np.ones(L), np.ones(L), np.ones(L)]).astype(np.float32)
    return dict(c_mstrict=mstrict, c_mcausal=mcausal, c_uincl=uincl, c_qaug4=aug(sl4), c_qaug16=qaug9, c_kaug9=kaug9, c_kaug=kaug,
                c_ident=np.eye(128, dtype=np.float32))


NCORES = 8
SEQ = 2048
NTOK = 2 * SEQ
N_EXPERTS = 32
_W_NAMES = ["ev_w_in", "ev_w_out", "ev_lambda_q1", "ev_lambda_k1", "ev_lambda_q2", "ev_lambda_k2", "ev_subln_w",
            "od_w_in", "od_kv_norm_w", "od_w_uv", "od_w_out", "ln_mix_g", "ln_mix_b", "router_w", "router_b",
            "exp_w_gu", "exp_b_gu", "exp_w_down", "exp_b_down", "ln_ffn_g", "ln_ffn_b"]


def build_program(shapes, cshapes):
    nc = bass.Bass("TRN2", target_bir_lowering=False)
    D = {}
    for n in _W_NAMES:
        D[n] = nc.dram_tensor(n, list(shapes[n]), F32, kind="ExternalInput").ap()
    for n, s in cshapes.items():
        D[n] = nc.dram_tensor(n, list(s), F32, kind="ExternalInput").ap()
    x = nc.dram_tensor("x", [NTOK, 1024], F32, kind="ExternalInput").ap()
    out = nc.dram_tensor("out", [NTOK, 1024], F32, kind="ExternalOutput").ap()
    h1 = nc.dram_tensor("h1", [NTOK, 1024], F32, kind="Internal").ap()
    h2 = nc.dram_tensor("h2", [NTOK, 1024], F32, kind="Internal").ap()
    h3 = nc.dram_tensor("h3", [NTOK, 1024], F32, kind="Internal").ap()
    with ExitStack() as st:
        P = Prog(nc, st)
        ident = P.sb("ident", [128, 128]); ident_b = P.sb("ident_b", [128, 128], BF16)
        P.dma("sync", ident[:], D["c_ident"], writes=["ident"])
        P.dma("gpsimd", ident_b[:], D["c_ident"], writes=["ident_b"])
        C = {"dram": D, "ident": ident, "ident_b": ident_b}
        lam_init0 = 0.8 - 0.6 * math.exp(-0.3 * 0)

        def phase(fn):
            with ExitStack() as ts:
                P.stack = ts
                fn()
                P.barrier()
            P.stack = st

        for sq in range(2):
            phase(lambda sq=sq: even_mixer_seq(P, "e%d" % sq, C, 0, x[sq * SEQ:(sq + 1) * SEQ, :], h1[sq * SEQ:(sq + 1) * SEQ, :], SEQ,
                                                lam_init0, D["ln_mix_g"][0], D["ln_mix_b"][0]))
        phase(lambda: moe_phase(P, "m0", C, 0, h1, h2, NTOK, N_EXPERTS))
        for sq in range(2):
            phase(lambda sq=sq: odd_mixer_seq(P, "o%d" % sq, C, 0, h2[sq * SEQ:(sq + 1) * SEQ, :], h3[sq * SEQ:(sq + 1) * SEQ, :], SEQ,
                                               D["ln_mix_g"][1], D["ln_mix_b"][1]))
        phase(lambda: moe_phase(P, "m1", C, 1, h3, out, NTOK, N_EXPERTS))
        P.final_wait()
        P.emit()
    return nc


def kernel(**inputs):
    consts = make_consts(SEQ)
    x = np.ascontiguousarray(np.asarray(inputs["x"], dtype=np.float32)).reshape(NCORES, NTOK, 1024)
    ws = {n: np.ascontiguousarray(np.asarray(inputs[n], dtype=np.float32)) for n in _W_NAMES}
    nc = build_program({n: ws[n].shape for n in _W_NAMES}, {n: v.shape for n, v in consts.items()})
    in_maps = []
    for c in range(NCORES):
        m = dict(ws)
        m.update(consts)
        m["x"] = x[c]
        in_maps.append(m)
    res = run_bass_kernel_spmd(nc, in_maps, core_ids=list(range(NCORES)))
    outs = [np.asarray(r["out"], dtype=np.float32) for r in res.results]
    return np.stack(outs, axis=0).reshape(16, SEQ, 1024)
```
